# Optimizing a Trainium2 kernel written in Bass

```python
import jax, jax.numpy as jnp
from jax import lax
import numpy as np

D_MODEL = 1024
BATCH = 4
SEQ = 8192
DEPTH = 2

HEAD_DIM = 64
N_HEADS_FOX = 4
DIL_PATTERNS = ((128, 1), (512, 4), (2048, 16))
N_DIL_GROUPS = 3
HEADS_PER_DIL = 2
N_HEADS_DIL = N_DIL_GROUPS * HEADS_PER_DIL
N_HEADS_MOBA = 6
N_HEADS = N_HEADS_FOX + N_HEADS_DIL + N_HEADS_MOBA
MIX_WIDTH = N_HEADS * HEAD_DIM
MOBA_BLOCK = 256
MOBA_TOPK = 3
MOBA_Q_CHUNK = 32
Q_BLOCK = 128
PLE_DIM = 256
D_FF = -(-8 * D_MODEL // (3 * 256)) * 256
ROPE_THETA = 10000.0
RMS_EPS = 1e-6
NEG_INF = -1e30
N_GATE_COLS = 3 * D_MODEL
W_IN_COLS = 3 * MIX_WIDTH + N_HEADS_FOX + N_GATE_COLS

kernel_name = "hybrid_fox_dilated_moba_block"


def rms_norm(x, g):
    xf = x.astype(jnp.float32)
    var = jnp.mean(xf * xf, axis=-1, keepdims=True)
    return (xf * lax.rsqrt(var + RMS_EPS) * g.astype(jnp.float32)).astype(x.dtype)


def rope_tables(seq):
    inv = 1.0 / (ROPE_THETA ** (jnp.arange(0, HEAD_DIM, 2, dtype=jnp.float32) / HEAD_DIM))
    ang = jnp.arange(seq, dtype=jnp.float32)[:, None] * inv[None, :]
    return jnp.cos(ang), jnp.sin(ang)


def apply_rope(x, cos, sin):
    xf = x.astype(jnp.float32)
    x1, x2 = jnp.split(xf, 2, axis=-1)
    c = cos[None, :, None, :]
    s = sin[None, :, None, :]
    return jnp.concatenate([x1 * c - x2 * s, x2 * c + x1 * s], axis=-1).astype(x.dtype)


def fox_attention(q, k, v, f_logit):
    B, S, H, D = q.shape
    scale = D ** -0.5
    c = jnp.cumsum(jax.nn.log_sigmoid(f_logit.astype(jnp.float32)), axis=1).transpose(0, 2, 1)
    kh = k.transpose(0, 2, 1, 3)
    vh = v.transpose(0, 2, 1, 3)
    nq = S // Q_BLOCK
    qb = q.reshape(B, nq, Q_BLOCK, H, D).transpose(1, 0, 3, 2, 4)
    cb = c.reshape(B, H, nq, Q_BLOCK).transpose(2, 0, 1, 3)
    key_pos = jnp.arange(S)

    def block(args):
        q_blk, c_blk, i = args
        s = jnp.einsum('bhqd,bhkd->bhqk', q_blk, kh).astype(jnp.float32) * scale
        s = s + c_blk[..., None] - c[:, :, None, :]
        qpos = i * Q_BLOCK + jnp.arange(Q_BLOCK)
        s = jnp.where(key_pos[None, :] <= qpos[:, None], s, NEG_INF)
        p = jax.nn.softmax(s, axis=-1).astype(v.dtype)
        return jnp.einsum('bhqk,bhkd->bhqd', p, vh)

    out = lax.map(block, (qb, cb, jnp.arange(nq)))
    return out.transpose(1, 0, 3, 2, 4).reshape(B, S, H * D)


def dilated_attention(q, k, v):
    B, S, _, D = q.shape
    G, Hg = N_DIL_GROUPS, HEADS_PER_DIL
    scale = D ** -0.5
    pad = max(w for w, _ in DIL_PATTERNS)
    qg = q.reshape(B, S, G, Hg, D)
    pad_spec = ((0, 0), (pad, 0), (0, 0), (0, 0), (0, 0))
    kg = jnp.pad(k.reshape(B, S, G, Hg, D), pad_spec)
    vg = jnp.pad(v.reshape(B, S, G, Hg, D), pad_spec)
    k_groups = [kg[:, :, g] for g in range(G)]
    v_groups = [vg[:, :, g] for g in range(G)]
    nq = S // Q_BLOCK

    def block(i):
        t0 = i * Q_BLOCK
        q_blk = lax.dynamic_slice_in_dim(qg, t0, Q_BLOCK, axis=1)
        qpos = t0 + jnp.arange(Q_BLOCK)
        outs, lses = [], []
        for g, (window, dil) in enumerate(DIL_PATTERNS):
            offs = dil * jnp.arange(window // dil + 1)
            kpos = qpos[:, None] - offs[None, :]
            k_sel = jnp.take(k_groups[g], kpos + pad, axis=1)
            v_sel = jnp.take(v_groups[g], kpos + pad, axis=1)
            s = jnp.einsum('bqhd,bqnhd->bqhn', q_blk[:, :, g], k_sel).astype(jnp.float32) * scale
            s = jnp.where((kpos >= 0)[None, :, None, :], s, NEG_INF)
            m = jnp.max(s, axis=-1, keepdims=True)
            e = jnp.exp(s - m)
            den = jnp.sum(e, axis=-1, keepdims=True)
            o = jnp.einsum('bqhn,bqnhd->bqhd', (e / den).astype(v.dtype), v_sel)
            outs.append(o)
            lses.append((m + jnp.log(den))[..., 0])
        wts = jax.nn.softmax(jnp.stack(lses, axis=0), axis=0)
        o = jnp.sum(wts[..., None] * jnp.stack(outs, axis=0).astype(jnp.float32), axis=0)
        return o.astype(v.dtype).reshape(B, Q_BLOCK, Hg * D)

    out = lax.map(block, jnp.arange(nq))
    return out.transpose(1, 0, 2, 3).reshape(B, S, Hg * D)


def moba_attention(q, k, v):
    B, S, H, D = q.shape
    L = MOBA_BLOCK
    scale = D ** -0.5
    nb = -(-S // L)
    s_pad = nb * L
    k_p = jnp.pad(k, ((0, 0), (0, s_pad - S), (0, 0), (0, 0)))
    v_p = jnp.pad(v, ((0, 0), (0, s_pad - S), (0, 0), (0, 0)))
    kb = k_p.reshape(B, nb, L, H, D).transpose(0, 3, 1, 2, 4)
    vb = v_p.reshape(B, nb, L, H, D).transpose(0, 3, 1, 2, 4)
    k_mean = jnp.mean(kb.astype(jnp.float32), axis=3).astype(k.dtype)
    n_sel = min(MOBA_TOPK, nb)
    b_idx = jnp.arange(B)[:, None, None, None]
    h_idx = jnp.arange(H)[None, :, None, None]
    nq = S // MOBA_Q_CHUNK

    def chunk(i):
        t0 = i * MOBA_Q_CHUNK
        q_c = lax.dynamic_slice_in_dim(q, t0, MOBA_Q_CHUNK, axis=1).transpose(0, 2, 1, 3)
        qpos = t0 + jnp.arange(MOBA_Q_CHUNK)
        own = t0 // L
        gate = jnp.einsum('bhqd,bhnd->bhqn', q_c, k_mean).astype(jnp.float32)
        gate = jnp.where(jnp.arange(nb) < own, gate, NEG_INF)
        _, sel = lax.top_k(gate, n_sel)
        sel_valid = sel < own
        k_sel = kb[b_idx, h_idx, sel]
        v_sel = vb[b_idx, h_idx, sel]
        s_sel = jnp.einsum('bhqd,bhqnld->bhqnl', q_c, k_sel).astype(jnp.float32) * scale
        s_sel = jnp.where(sel_valid[..., None], s_sel, NEG_INF)
        k_own = lax.dynamic_slice_in_dim(kb, own, 1, axis=2)[:, :, 0]
        v_own = lax.dynamic_slice_in_dim(vb, own, 1, axis=2)[:, :, 0]
        s_own = jnp.einsum('bhqd,bhld->bhql', q_c, k_own).astype(jnp.float32) * scale
        own_pos = own * L + jnp.arange(L)
        s_own = jnp.where(own_pos[None, :] <= qpos[:, None], s_own, NEG_INF)
        s_all = jnp.concatenate([s_sel.reshape(B, H, MOBA_Q_CHUNK, n_sel * L), s_own], axis=-1)
        p = jax.nn.softmax(s_all, axis=-1).astype(v.dtype)
        p_sel = p[..., : n_sel * L].reshape(B, H, MOBA_Q_CHUNK, n_sel, L)
        p_own = p[..., n_sel * L:]
        o = jnp.einsum('bhqnl,bhqnld->bhqd', p_sel, v_sel) + jnp.einsum('bhql,bhld->bhqd', p_own, v_own)
        return o.transpose(0, 2, 1, 3).reshape(B, MOBA_Q_CHUNK, H * D)

    out = lax.map(chunk, jnp.arange(nq))
    return out.transpose(1, 0, 2, 3).reshape(B, S, H * D)


def setup_inputs(seed: int = 0) -> dict:
    key = jax.random.key(seed)
    ks = jax.random.split(key, 20)
    f32 = jnp.float32

    def w(k, shape, fan_in):
        return jax.random.normal(k, shape, f32) * fan_in ** -0.5

    def gain(k):
        return 1.0 + 0.05 * jax.random.normal(k, (DEPTH, D_MODEL), f32)

    return {
        "x": jax.random.normal(ks[0], (BATCH, SEQ, D_MODEL), f32),
        "p": jax.random.normal(ks[1], (DEPTH, BATCH, SEQ, PLE_DIM), f32),
        "g_mix_pre": gain(ks[2]),
        "w_in": w(ks[3], (DEPTH, D_MODEL, W_IN_COLS), D_MODEL),
        "b_f": 2.0 + 0.5 * jax.random.normal(ks[4], (DEPTH, N_HEADS_FOX), f32),
        "w_br_a": w(ks[5], (DEPTH, N_HEADS_FOX * HEAD_DIM, D_MODEL), N_HEADS_FOX * HEAD_DIM),
        "w_br_b": w(ks[6], (DEPTH, HEADS_PER_DIL * HEAD_DIM, D_MODEL), HEADS_PER_DIL * HEAD_DIM),
        "w_br_c": w(ks[7], (DEPTH, N_HEADS_MOBA * HEAD_DIM, D_MODEL), N_HEADS_MOBA * HEAD_DIM),
        "w_out": w(ks[8], (DEPTH, D_MODEL, D_MODEL), D_MODEL),
        "g_mix_post": gain(ks[9]),
        "g_ffn_pre": gain(ks[10]),
        "w_ffn_gate": w(ks[11], (DEPTH, D_MODEL, D_FF), D_MODEL),
        "w_ffn_up": w(ks[12], (DEPTH, D_MODEL, D_FF), D_MODEL),
        "w_ffn_down": w(ks[13], (DEPTH, D_FF, D_MODEL), D_FF),
        "g_ffn_post": gain(ks[14]),
        "w_ple": w(ks[15], (DEPTH, PLE_DIM, D_MODEL), PLE_DIM),
        "w_ple_gate": w(ks[16], (DEPTH, D_MODEL, D_MODEL), D_MODEL),
        "g_ple_post": gain(ks[17]),
    }


def reference(x, p, g_mix_pre, w_in, b_f, w_br_a, w_br_b, w_br_c, w_out, g_mix_post,
              g_ffn_pre, w_ffn_gate, w_ffn_up, w_ffn_down, g_ffn_post, w_ple, w_ple_gate, g_ple_post):
    B, S, _ = x.shape
    cos, sin = rope_tables(S)
    cos = cos.astype(jnp.float32)
    sin = sin.astype(jnp.float32)
    for i in range(DEPTH):
        h = rms_norm(x, g_mix_pre[i])
        proj = jnp.einsum('bsd,dc->bsc', h, w_in[i])
        q = proj[..., :MIX_WIDTH].reshape(B, S, N_HEADS, HEAD_DIM)
        k = proj[..., MIX_WIDTH:2 * MIX_WIDTH].reshape(B, S, N_HEADS, HEAD_DIM)
        v = proj[..., 2 * MIX_WIDTH:3 * MIX_WIDTH].reshape(B, S, N_HEADS, HEAD_DIM)
        f_logit = proj[..., 3 * MIX_WIDTH:3 * MIX_WIDTH + N_HEADS_FOX] + b_f[i]
        gates = jax.nn.sigmoid(proj[..., 3 * MIX_WIDTH + N_HEADS_FOX:])
        g_a = gates[..., :D_MODEL]
        g_b = gates[..., D_MODEL:2 * D_MODEL]
        g_c = gates[..., 2 * D_MODEL:]
        q_r = apply_rope(q[:, :, N_HEADS_FOX:], cos, sin)
        k_r = apply_rope(k[:, :, N_HEADS_FOX:], cos, sin)
        y_a = fox_attention(q[:, :, :N_HEADS_FOX], k[:, :, :N_HEADS_FOX], v[:, :, :N_HEADS_FOX], f_logit)
        y_b = dilated_attention(q_r[:, :, :N_HEADS_DIL], k_r[:, :, :N_HEADS_DIL],
                                v[:, :, N_HEADS_FOX:N_HEADS_FOX + N_HEADS_DIL])
        y_c = moba_attention(q_r[:, :, N_HEADS_DIL:], k_r[:, :, N_HEADS_DIL:],
                             v[:, :, N_HEADS_FOX + N_HEADS_DIL:])
        merged = (g_a * jnp.einsum('bsc,cd->bsd', y_a, w_br_a[i])
                  + g_b * jnp.einsum('bsc,cd->bsd', y_b, w_br_b[i])
                  + g_c * jnp.einsum('bsc,cd->bsd', y_c, w_br_c[i]))
        x = x + rms_norm(jnp.einsum('bsd,de->bse', merged, w_out[i]), g_mix_post[i])
        h = rms_norm(x, g_ffn_pre[i])
        ff = jax.nn.silu(jnp.einsum('bsd,df->bsf', h, w_ffn_gate[i])) * jnp.einsum('bsd,df->bsf', h, w_ffn_up[i])
        x = x + rms_norm(jnp.einsum('bsf,fd->bsd', ff, w_ffn_down[i]), g_ffn_post[i])
        ple = jnp.einsum('bse,ed->bsd', p[i], w_ple[i]) * jax.nn.sigmoid(jnp.einsum('bsd,de->bse', x, w_ple_gate[i]))
        x = x + rms_norm(ple, g_ple_post[i])
    return x
```

```python
import numpy as np
from contextlib import ExitStack
import concourse.bass as bass
import concourse.mybir as mybir
from concourse.bass_utils import run_bass_kernel_spmd

F32 = mybir.dt.float32
BF = mybir.dt.bfloat16
AF = mybir.ActivationFunctionType
ALU = mybir.AluOpType
AX = mybir.AxisListType

D = 1024
HD = 64
PLE = 256
DFF = 2816
NFF = 22
BIG = 30000.0
DIL = (1, 4, 16)
SAME_ENGINE_SYNC = True


class Buf:
    def __init__(self, name, sem=None):
        self.name = name
        self.w = {}
        self.r = {}
        self.sem = sem
        self.cnt = 0


class Eng:
    def __init__(self, name, sem):
        self.name = name
        self.sem = sem
        self.cnt = 0
        self.seen = {}
        self.prog = []


class Prog:
    def __init__(self, nc, es):
        self.nc = nc
        self.es = es
        self.sems = []
        self.E = {}
        for n in ("pe", "act", "dve", "pool"):
            self.E[n] = Eng(n, self.newsem(n))
        self.E["sp"] = Eng("sp", None)
        self.bufs = []
        self.bynames = {}

    def newsem(self, name):
        h = self.es.enter_context(self.nc.semaphore("s_" + name))
        self.sems.append(h)
        return len(self.sems) - 1

    def buf(self, name, dma=False):
        if name in self.bynames:
            return self.bynames[name]
        b = Buf(name, self.newsem(name) if dma else None)
        self.bufs.append(b)
        self.bynames[name] = b
        return b

    def _waits(self, X, R, W):
        need = {}
        for b in R:
            for k, v in b.w.items():
                need[k] = max(need.get(k, 0), v)
        for b in W:
            for k, v in b.w.items():
                need[k] = max(need.get(k, 0), v)
            for k, v in b.r.items():
                need[k] = max(need.get(k, 0), v)
        out = []
        for k, v in need.items():
            if k == X.sem and (X.name == "pe" or not SAME_ENGINE_SYNC):
                continue
            if X.seen.get(k, 0) >= v:
                continue
            X.seen[k] = v
            out.append((k, v))
        return out

    def op(self, eng, name, R=(), W=(), inc=True, args=(), **kw):
        X = self.E[eng]
        waits = self._waits(X, R, W)
        tok = X.cnt + 1
        if inc:
            X.cnt = tok
        X.prog.append((waits, ("op", name, args, kw), inc))
        for b in R:
            b.r[X.sem] = tok
        for b in W:
            b.w = {X.sem: tok}
            b.r = {}

    def dma(self, q, pairs, sb, R=(), W=()):
        X = self.E[q]
        waits = self._waits(X, R, W)
        first = True
        for o, i in pairs:
            sb.cnt += 16
            X.prog.append((waits if first else [], ("dma", o, i, sb.sem), False))
            first = False
        for b in R:
            b.r[sb.sem] = sb.cnt
        for b in W:
            b.w = {sb.sem: sb.cnt}
            b.r = {}

    def barrier(self):
        toks = {}
        for n in ("pe", "act", "dve", "pool"):
            toks[self.E[n].sem] = self.E[n].cnt
        for b in self.bufs:
            if b.sem is not None and b.cnt > 0:
                toks[b.sem] = b.cnt
        for n, X in self.E.items():
            waits = []
            for k, v in toks.items():
                if v > 0 and X.seen.get(k, 0) < v and not (k == X.sem and n == "pe"):
                    X.seen[k] = v
                    waits.append((k, v))
            X.prog.append((waits, None, False))

    def replay(self, block):
        sems = self.sems

        def run(X, e):
            for waits, fn, inc in X.prog:
                for k, v in waits:
                    e.wait_ge(sems[k], v)
                if fn is None:
                    continue
                if fn[0] == "dma":
                    _, o, i, k = fn
                    e.dma_start(out=o, in_=i).then_inc(sems[k], 16)
                else:
                    _, name, args, kw = fn
                    ins = getattr(e, name)(*args, **kw)
                    if inc:
                        ins.then_inc(sems[X.sem], 1)

        @block.sync
        def _(e):
            run(self.E["sp"], e)

        @block.tensor
        def _(e):
            run(self.E["pe"], e)

        @block.scalar
        def _(e):
            run(self.E["act"], e)

        @block.vector
        def _(e):
            run(self.E["dve"], e)

        @block.gpsimd
        def _(e):
            run(self.E["pool"], e)


class Arena:
    def __init__(self, ap, lo, hi):
        self.ap = ap
        self.lo = lo
        self.hi = hi
        self.p = lo

    def f32(self, n):
        a = self.ap[:, self.p:self.p + n]
        self.p += n
        assert self.p <= self.hi, ("arena overflow", self.p, self.hi)
        return a

    def bf(self, n):
        n2 = (n + 1) // 2
        a = self.ap[:, self.p:self.p + n2].bitcast(BF)
        self.p += n2
        assert self.p <= self.hi, ("arena overflow", self.p, self.hi)
        return a[:, 0:n]

    def reset(self):
        self.p = self.lo


def build(S=8192, L=2, dbg=False):
    NT = S // 512
    NB = S // 128
    NMB = S // 256
    CW = min(2048, S)
    assert NMB <= 32
    nc = bass.Bass("TRN2", target_bir_lowering=False)

    def din(name, shape, dt=F32):
        return nc.dram_tensor(name, shape, dt, kind="ExternalInput").ap()

    def dscr(name, shape, dt=BF):
        return nc.dram_tensor(name, shape, dt, kind=("ExternalOutput" if dbg else "Internal")).ap()

    xT = din("xT", [D, S])
    pT = din("pT", [L * PLE, S])
    wspec = [("wA", 28, 1024), ("wV", 1, 8192), ("wF", 1, 32), ("wG", 24, 1024), ("wBR", 8, 768),
             ("wO", 8, 1024), ("wFG", 22, 1024), ("wFU", 22, 1024), ("wFD", 8, 2816),
             ("wPL", 8, 256), ("wPG", 8, 1024)]
    w32 = {}
    w16 = {}
    for n, ns, nc_ in wspec:
        w32[n] = din(n, [L * ns * 128, nc_])
        w16[n] = nc.dram_tensor(n + "_16", [L * ns * 128, nc_], BF, kind="Internal").ap()
    wns = {n: ns for n, ns, _ in wspec}
    gains = din("gains", [128, L * 5 * 8])
    bfb = din("bfb", [128, L * 4])
    cosT = din("cosT", [128, S])
    sinT = din("sinT", [128, S])
    cmask32 = din("cmask", [128, 256])
    trif = din("trif", [128, 128])
    identf = din("identf", [128, 128])
    blkind32 = din("blkind", [33, S])
    outT = nc.dram_tensor("outT", [D, S], F32, kind="ExternalOutput").ap()

    X32 = dscr("X32", [D, S], F32)
    QT16 = dscr("QT16", [1024, S])
    KT16 = dscr("KT16", [1024, S])
    V16 = dscr("V16", [S, 1024])
    YT16 = dscr("YT16", [768, S])
    KS32 = dscr("KS32", [384, 32], F32)
    BI16 = nc.dram_tensor("BI16", [33, S], BF, kind="Internal").ap()

    es = ExitStack()
    P = Prog(nc, es)
    AW = 47104
    arena_t = es.enter_context(nc.sbuf_tensor("arena", [128, AW], F32))
    PERS = 2048
    pers = Arena(arena_t, 0, PERS)
    ar = Arena(arena_t, PERS, AW)
    psb = [es.enter_context(nc.psum_tensor("psb%d" % i, [128, 512], F32)) for i in range(8)]
    PSB = [P.buf("psb%d" % i) for i in range(8)]

    def ACT(out, in_, func, R, W, **kw):
        P.op("act", "activation", R, W, out=out, in_=in_, func=func, **kw)

    def TT(eng, out, in0, in1, op, R, W):
        P.op(eng, "tensor_tensor", R, W, out=out, in0=in0, in1=in1, op=op)

    def MM(out, lhsT, rhs, start, stop, R, W, inc=True):
        P.op("pe", "matmul", R, W, inc, args=(out,), lhsT=lhsT, rhs=rhs, start=start, stop=stop)

    def mm_group(out_ap, pairs, Rb, Wb):
        n = len(pairs)
        for i, (lt, rh) in enumerate(pairs):
            MM(out_ap, lt, rh, i == 0, i == n - 1, Rb, Wb, inc=(i == n - 1))

    gains_t = pers.f32(L * 40); gains_b = P.buf("gains", True)
    bfb_t = pers.f32(L * 4); bfb_b = P.buf("bfb", True)
    trif_t = pers.f32(128); trif_b = P.buf("trif", True)
    identf_t = pers.f32(128); identf_b = P.buf("identf", True)
    onesf_t = pers.f32(128); onesf_b = P.buf("onesf")
    ones16_t = pers.bf(128); ones16_b = P.buf("ones16")
    cmask_t = pers.bf(256); cmask_b = P.buf("cmask", True)
    eps_t = pers.f32(1); one_t = pers.f32(1); cst_b = P.buf("cst")
    cpos_t = pers.f32(NB * 4).rearrange("p (j h) -> p j h", h=4); cpos_b = P.buf("cpos")
    tall_t = pers.f32((NB + 1) * 4).rearrange("p (j h) -> p j h", h=4); tall_b = P.buf("tall")
    ksum_t = pers.f32(3 * 32).rearrange("p (a n) -> p a n", n=32); ksum_b = P.buf("ksum", True)

    P.dma("sp", [(gains_t, gains)], gains_b, W=[gains_b])
    P.dma("sp", [(bfb_t, bfb)], bfb_b, W=[bfb_b])
    P.dma("sp", [(trif_t, trif)], trif_b, W=[trif_b])
    P.dma("sp", [(identf_t, identf)], identf_b, W=[identf_b])
    P.dma("pool", [(cmask_t, cmask32)], cmask_b, W=[cmask_b])
    P.op("dve", "memset", (), [onesf_b], args=(onesf_t, 1.0))
    P.op("dve", "memset", (), [ones16_b], args=(ones16_t, 1.0))
    P.op("dve", "memset", (), [cst_b], args=(eps_t, 1e-6))
    P.op("dve", "memset", (), [cst_b], args=(one_t, 1.0))
    P.op("dve", "memset", (), [tall_b], args=(tall_t[:, 0, :], 0.0))
    P.op("dve", "memset", (), [ksum_b], args=(ksum_t, 0.0))

    WB = [P.buf("w16_%d" % l, True) for l in range(L)]
    BIb = P.buf("bi16", True)
    P.dma("pool", [(BI16[:, c0:c0 + CW], blkind32[:, c0:c0 + CW]) for c0 in range(0, S, CW)], BIb, W=[BIb])
    for l in range(L):
        pairs = []
        for n, ns, ncol in wspec:
            for s_ in range(ns):
                r0 = (l * ns + s_) * 128
                cw = 2048 if ncol > 2816 else ncol
                for c0 in range(0, ncol, cw):
                    pairs.append((w16[n][r0:r0 + 128, c0:c0 + cw], w32[n][r0:r0 + 128, c0:c0 + cw]))
        P.dma("pool", pairs, WB[l])
        WB[l].w = {WB[l].sem: WB[l].cnt}

    def wslab(n, l, s_):
        r0 = (l * wns[n] + s_) * 128
        return w16[n][r0:r0 + 128, :]

    Xb = P.buf("X32d"); QTb = P.buf("QTd"); KTb = P.buf("KTd"); Vb = P.buf("Vd"); YTb = P.buf("YTd"); KSb = P.buf("KSd")

    def xsrc(l):
        return xT if l == 0 else X32

    def xdst(l):
        return outT if l == L - 1 else X32

    def xtile_ap(dram, t):
        return dram[:, t * 512:(t + 1) * 512].rearrange("(kc p) n -> p kc n", p=128)

    class Ring:
        def __init__(self, n, words, name):
            self.t = [ar.bf(words) for _ in range(n)]
            self.b = [P.buf("%s%d" % (name, i), True) for i in range(n)]
            self.i = 0

        def load(self, dram_ap, ncol, Rb):
            k = self.i % len(self.t)
            self.i += 1
            P.dma("sp", [(self.t[k][:, 0:ncol], dram_ap)], self.b[k], R=Rb, W=[self.b[k]])
            return self.t[k][:, 0:ncol].rearrange("p (k n) -> p k n", n=128), self.b[k]

    def rms_stats(sq_t, sq_b, rstd_t, rstd_b, tmp_t, tmp_b):
        mm_group(psb[7][:, :], [(ones16_t, sq_t[:, kc, :]) for kc in range(8)], [ones16_b, sq_b], [PSB[7]])
        ACT(tmp_t, psb[7][:, :], AF.Sqrt, [PSB[7], cst_b], [tmp_b], bias=eps_t, scale=1.0 / D)
        P.op("dve", "reciprocal", [tmp_b], [rstd_b], out=rstd_t, in_=tmp_t)

    def pre_norm(x_t, x_b, gcol, sq_t, sq_b, rstd_t, rstd_b, tmp_t, tmp_b, h_t, h_b):
        ACT(sq_t, x_t, AF.Square, [x_b], [sq_b])
        rms_stats(sq_t, sq_b, rstd_t, rstd_b, tmp_t, tmp_b)
        for kc in range(8):
            P.op("dve", "scalar_tensor_tensor", [x_b, rstd_b, gains_b], [h_b], out=h_t[:, kc, :], in0=x_t[:, kc, :],
                 scalar=gains_t[:, gcol + kc:gcol + kc + 1], in1=rstd_t, op0=ALU.mult, op1=ALU.mult)

    for l in range(L):
        g0 = l * 40
        P.barrier()
        ar.reset()
        xt = [ar.f32(4096).rearrange("p (k n) -> p k n", n=512) for _ in range(2)]
        xt_b = [P.buf("xt%d" % i, True) for i in range(2)]
        sq_t = ar.bf(4096).rearrange("p (k n) -> p k n", n=512); sq_b = P.buf("sq")
        h_t = ar.bf(4096).rearrange("p (k n) -> p k n", n=512); h_b = P.buf("h")
        rstd_t = ar.f32(512); rstd_b = P.buf("rstd")
        tmp_t = ar.f32(512); tmp_b = P.buf("tmp")
        wv_t = ar.bf(8192).rearrange("p (k n) -> p k n", n=1024); wv_b = P.buf("wvA", True)
        wf_t = ar.bf(32).rearrange("p (k n) -> p k n", n=4); wf_b = P.buf("wfA", True)
        ring = Ring(6, 1024, "ring")
        cs_t = [(ar.f32(512), ar.f32(512)) for _ in range(2)]
        cs_b = [P.buf("csA%d" % i, True) for i in range(2)]
        qst = [ar.bf(4096).rearrange("p (k n) -> p k n", n=512) for _ in range(2)]
        qst_b = [P.buf("qst%d" % i, True) for i in range(2)]
        kst = [ar.bf(4096).rearrange("p (k n) -> p k n", n=512) for _ in range(2)]
        kst_b = [P.buf("kst%d" % i, True) for i in range(2)]
        vst = [ar.bf(4096).rearrange("p (a n) -> p a n", n=1024) for _ in range(2)]
        vst_b = [P.buf("vst%d" % i, True) for i in range(2)]
        r1 = [ar.f32(512) for _ in range(2)]; r1_b = [P.buf("r1_%d" % i) for i in range(2)]
        r2 = [ar.f32(512) for _ in range(2)]; r2_b = [P.buf("r2_%d" % i) for i in range(2)]
        fb_t = ar.f32(4); fb_b = P.buf("fbA")
        fe_t = ar.f32(4); fe_b = P.buf("feA")
        fl_t = ar.f32(4); fl_b = P.buf("flA")

        P.dma("sp", [(wv_t[:, kc, :], wslab("wV", l, 0)[:, kc * 1024:(kc + 1) * 1024]) for kc in range(8)],
              wv_b, R=[WB[l]], W=[wv_b])
        P.dma("sp", [(wf_t, wslab("wF", l, 0).rearrange("p (k n) -> p k n", n=4))], wf_b, R=[WB[l]], W=[wf_b])

        def loadxA(t):
            k = t % 2
            P.dma("sp", [(xt[k][:, 0:4, :], xtile_ap(xsrc(l), t)[:, 0:4, :]),
                         (xt[k][:, 4:8, :], xtile_ap(xsrc(l), t)[:, 4:8, :])], xt_b[k], R=[Xb], W=[xt_b[k]])
            P.dma("sp", [(cs_t[k][0], cosT[:, t * 512:(t + 1) * 512]),
                         (cs_t[k][1], sinT[:, t * 512:(t + 1) * 512])], cs_b[k], W=[cs_b[k]])

        loadxA(0)
        psrot = 0
        for t in range(NT):
            if t + 1 < NT:
                loadxA(t + 1)
            x_t = xt[t % 2]; x_b = xt_b[t % 2]
            cos_t, sin_t = cs_t[t % 2]; c_b = cs_b[t % 2]
            pre_norm(x_t, x_b, g0 + 0, sq_t, sq_b, rstd_t, rstd_b, tmp_t, tmp_b, h_t, h_b)
            qs = qst[t % 2]; qs_b = qst_b[t % 2]; ks = kst[t % 2]; ks_b = kst_b[t % 2]
            si = 0
            for which in range(2):
                stg, stg_b = (qs, qs_b) if which == 0 else (ks, ks_b)
                for pt in range(8):
                    w3, w_b = ring.load(wslab("wA", l, si), 1024, [WB[l]]); si += 1
                    pa = psrot % 6; psrot += 1
                    mm_group(psb[pa][:, :], [(w3[:, kc, :], h_t[:, kc, :]) for kc in range(8)], [w_b, h_b], [PSB[pa]])
                    if pt < 2:
                        ACT(stg[:, pt, :], psb[pa][:, :], AF.Copy, [PSB[pa]], [stg_b])
                    else:
                        w23, w2_b = ring.load(wslab("wA", l, si), 1024, [WB[l]]); si += 1
                        pb = psrot % 6; psrot += 1
                        mm_group(psb[pb][:, :], [(w23[:, kc, :], h_t[:, kc, :]) for kc in range(8)], [w2_b, h_b], [PSB[pb]])
                        ri = (pt + which) % 2
                        TT("dve", r1[ri], psb[pa][:, :], cos_t, ALU.mult, [PSB[pa], c_b], [r1_b[ri]])
                        TT("dve", r2[ri], psb[pb][:, :], sin_t, ALU.mult, [PSB[pb], c_b], [r2_b[ri]])
                        TT("pool", stg[:, pt, :], r1[ri], r2[ri], ALU.add, [r1_b[ri], r2_b[ri]], [stg_b])
                        if which == 1 and pt >= 5:
                            P.op("dve", "tensor_reduce", [stg_b], [ksum_b], out=ksum_t[:, pt - 5, 2 * t:2 * t + 2],
                                 in_=stg[:, pt, :].rearrange("p (a b) -> p a b", b=256), axis=AX.X, op=ALU.add)
            P.dma("pool", [(QT16[:, t * 512:(t + 1) * 512].rearrange("(k p) n -> p k n", p=128), qs)], qs_b, R=[qs_b], W=[QTb])
            P.dma("pool", [(KT16[:, t * 512:(t + 1) * 512].rearrange("(k p) n -> p k n", p=128), ks)], ks_b, R=[ks_b], W=[KTb])
            vs = vst[t % 2]; vs_b = vst_b[t % 2]
            for tb in range(4):
                for hf in range(2):
                    pa = psrot % 6; psrot += 1
                    mm_group(psb[pa][:, :], [(h_t[:, kc, tb * 128:(tb + 1) * 128], wv_t[:, kc, hf * 512:(hf + 1) * 512]) for kc in range(8)],
                             [wv_b, h_b], [PSB[pa]])
                    ACT(vs[:, tb, hf * 512:(hf + 1) * 512], psb[pa][:, :], AF.Copy, [PSB[pa]], [vs_b])
                j = 4 * t + tb
                mm_group(psb[6][:, 0:4], [(h_t[:, kc, tb * 128:(tb + 1) * 128], wf_t[:, kc, :]) for kc in range(8)], [wf_b, h_b], [PSB[6]])
                TT("dve", fb_t, psb[6][:, 0:4], bfb_t[:, l * 4:l * 4 + 4], ALU.add, [PSB[6], bfb_b], [fb_b])
                ACT(fe_t, fb_t, AF.Exp, [fb_b], [fe_b], scale=-1.0)
                ACT(fl_t, fe_t, AF.Ln, [fe_b, cst_b], [fl_b], bias=one_t, scale=1.0)
                mm_group(psb[6][:, 8:12], [(trif_t, fl_t)], [trif_b, fl_b], [PSB[6]])
                mm_group(psb[6][:, 16:20], [(onesf_t, fl_t)], [onesf_b, fl_b], [PSB[6]])
                TT("dve", cpos_t[:, j, :], psb[6][:, 8:12], tall_t[:, j, :], ALU.add, [PSB[6], tall_b], [cpos_b])
                TT("dve", tall_t[:, j + 1, :], psb[6][:, 16:20], tall_t[:, j, :], ALU.add, [PSB[6], tall_b], [tall_b])
            P.dma("pool", [(V16[t * 512:(t + 1) * 512, :].rearrange("(a p) c -> p a c", p=128), vs)], vs_b, R=[vs_b], W=[Vb])
        P.dma("pool", [(KS32.rearrange("(a p) n -> p a n", p=128), ksum_t)], ksum_b, R=[ksum_b], W=[KSb])

        P.barrier()
        ar.reset()
        KP = [ar.bf(S) for _ in range(2)]; KP_b = [P.buf("KP%d" % i, True) for i in range(2)]
        QP = [ar.bf(S) for _ in range(2)]; QP_b = [P.buf("QP%d" % i, True) for i in range(2)]
        QA_b = [[P.buf("QA%d_%d" % (i, t)) for t in range(NT)] for i in range(2)]
        VA = [ar.bf(NB * 128).rearrange("p (j c) -> p j c", c=128) for _ in range(2)]
        VA_b = [P.buf("VA%d" % i, True) for i in range(2)]
        pt_t = [ar.bf(512) for _ in range(4)]; pt_b = [P.buf("pT%d" % i) for i in range(4)]
        rec_t = ar.f32(512); rec_b = P.buf("rec")
        rec0_t = ar.f32(512); rec0_b = P.buf("rec0")
        yst = [ar.bf(512) for _ in range(2)]; yst_b = [P.buf("yst%d" % i, True) for i in range(2)]
        ks16_t = ar.bf(32); ks16_b = P.buf("ks16")
        ks32_t = ar.f32(32); ks32_b = P.buf("ks32", True)
        wk_t = ar.f32(128).rearrange("p (a n) -> p a n", n=32); wk_b = P.buf("wk")
        t8_t = ar.f32(32).rearrange("p (a n) -> p a n", n=8); t8_b = P.buf("t8")
        sb_t = ar.f32(128).rearrange("p (a n) -> p a n", n=32); sb_b = P.buf("selb")
        acc_t = ar.f32(S); acc_b = P.buf("acc")
        for i in range(2):
            P.op("pool", "memset", (), [VA_b[i]], args=(VA[i][:, :, 64:128], 1.0))
        cnt = {"ps": 0, "po": 0, "pt": 0, "y": 0, "kq": 0, "va": 0}

        def load_rows(dst, dst_b, dram, r0, nr, rb, ind=False):
            pairs = [(dst[0:nr, c0:c0 + CW], dram[r0:r0 + nr, c0:c0 + CW]) for c0 in range(0, S, CW)]
            Rb = [rb]
            if ind == 1:
                pairs += [(dst[64:96, c0:c0 + CW], BI16[0:32, c0:c0 + CW]) for c0 in range(0, S, CW)]
                Rb = [rb, BIb]
            if ind == 2:
                pairs += [(dst[64:65, c0:c0 + CW], BI16[32:33, c0:c0 + CW]) for c0 in range(0, S, CW)]
                Rb = [rb, BIb]
            P.dma("sp", pairs, dst_b, R=Rb, W=[dst_b])

        def load_va(k, head, d):
            nbd = NB // d
            pairs = []
            for r in range(d):
                for b0 in range(0, nbd, 8):
                    nb_ = min(8, nbd - b0)
                    src = V16[r + d * 128 * b0: r + d * 128 * b0 + d * (128 * nb_ - 1) + 1: d, head * 64:(head + 1) * 64]
                    pairs.append((VA[k][:, r * nbd + b0: r * nbd + b0 + nb_, 0:64], src.rearrange("(j p) c -> p j c", p=128)))
            P.dma("sp", pairs, VA_b[k], R=[Vb], W=[VA_b[k]])

        def finalize(po, yrow, i):
            yk = cnt["y"] % 2; cnt["y"] += 1
            P.op("dve", "reciprocal", [PSB[po]], [rec_b], out=rec_t[64:128, :], in_=psb[po][64:128, :])
            TT("dve", yst[yk][0:64, :], psb[po][0:64, :], rec_t[64:128, :], ALU.mult, [PSB[po], rec_b], [yst_b[yk]])
            P.dma("pool", [(YT16[yrow:yrow + 64, i * 512:(i + 1) * 512], yst[yk][0:64, :])], yst_b[yk], R=[yst_b[yk]], W=[YTb])

        def attn_tiles(Kt, K_b, Qt, Qbufs, r0, nr, va, va_b, bias_fn, i, yrow):
            po = 4 + cnt["po"] % 2; cnt["po"] += 1
            nj = 4 * i + 4
            for j in range(nj):
                off = max(0, j - 4 * i) * 128
                ps = cnt["ps"] % 3; cnt["ps"] += 1
                pk = cnt["pt"] % 4; cnt["pt"] += 1
                mm_group(psb[ps][:, off:512], [(Kt[r0:r0 + nr, j * 128:(j + 1) * 128], Qt[r0:r0 + nr, i * 512 + off:(i + 1) * 512])],
                         [K_b] + Qbufs, [PSB[ps]])
                if bias_fn is None:
                    ACT(pt_t[pk][:, off:512], psb[ps][:, off:512], AF.Exp, [PSB[ps]], [pt_b[pk]], scale=0.125)
                else:
                    ACT(pt_t[pk][:, off:512], psb[ps][:, off:512], AF.Exp, [PSB[ps], cpos_b], [pt_b[pk]], bias=bias_fn(j, i), scale=0.125)
                if j >= 4 * i:
                    TT("pool", pt_t[pk][:, off:off + 128], pt_t[pk][:, off:off + 128], cmask_t[:, 128:256], ALU.mult,
                       [pt_b[pk], cmask_b], [pt_b[pk]])
                MM(psb[po][:, off:512], va[:, j, :], pt_t[pk][:, off:512], j == 0, j == nj - 1, [va_b, pt_b[pk]], [PSB[po]], inc=True)
            finalize(po, yrow, i)

        for h in range(4):
            kq = cnt["kq"] % 2; cnt["kq"] += 1
            load_rows(KP[kq], KP_b[kq], KT16, h * 64, 64, KTb, ind=2)
            load_rows(QP[kq], QP_b[kq], QT16, h * 64, 64, QTb)
            vk = cnt["va"] % 2; cnt["va"] += 1
            load_va(vk, h, 1)
            for i in range(NT):
                for qb in range(4):
                    P.op("pe", "transpose", [cpos_b, identf_b], [PSB[7]], qb == 3, out=psb[7][0:1, qb * 128:(qb + 1) * 128],
                         in_=cpos_t[:, 4 * i + qb, h:h + 1], identity=identf_t)
                P.op("dve", "tensor_scalar", [PSB[7]], [QA_b[kq][i]], out=QP[kq][64:65, i * 512:(i + 1) * 512], in0=psb[7][0:1, :],
                     scalar1=-8.0, scalar2=None, op0=ALU.mult)
                attn_tiles(KP[kq], KP_b[kq], QP[kq], [QP_b[kq], QA_b[kq][i]], 0, 65, VA[vk], VA_b[vk],
                           (lambda h: lambda j, i: cpos_t[:, j, h:h + 1])(h), i, h * 64)
        for m in range(6):
            hd = 10 + m
            kq = cnt["kq"] % 2; cnt["kq"] += 1
            load_rows(KP[kq], KP_b[kq], KT16, hd * 64, 64, KTb, ind=1)
            load_rows(QP[kq], QP_b[kq], QT16, hd * 64, 64, QTb)
            P.dma("sp", [(ks32_t[0:64, :], KS32[m * 64:(m + 1) * 64, :])], ks32_b, R=[KSb], W=[ks32_b])
            P.op("dve", "tensor_copy", [ks32_b], [ks16_b], out=ks16_t[0:64, :], in_=ks32_t[0:64, :])
            vk = cnt["va"] % 2; cnt["va"] += 1
            load_va(vk, hd, 1)
            Kt = KP[kq]; Qt = QP[kq]
            for i in range(NT):
                for qb in range(4):
                    q0 = i * 512 + qb * 128
                    mm_group(psb[6][:, qb * 32:qb * 32 + NMB], [(Qt[0:64, q0:q0 + 128], ks16_t[0:64, 0:NMB])], [QP_b[kq], ks16_b], [PSB[6]])
                P.op("dve", "memset", (), [wk_b], args=(wk_t, -1e30))
                P.op("dve", "memset", (), [sb_b], args=(sb_t, -1.0))
                for hq in range(2):
                    own = 2 * i + hq
                    if own > 3:
                        P.op("dve", "tensor_copy", [PSB[6]], [wk_b], out=wk_t[:, 2 * hq:2 * hq + 2, 0:own],
                             in_=psb[6][:, 64 * hq:64 * hq + 64].rearrange("p (a n) -> p a n", n=32)[:, :, 0:own])
                    for qq in range(2):
                        qb = 2 * hq + qq
                        if own > 3:
                            P.op("dve", "max", [wk_b], [t8_b], out=t8_t[:, qb, :], in_=wk_t[:, qb, 0:max(own, 8)])
                            P.op("dve", "tensor_scalar", [wk_b, t8_b], [sb_b], out=sb_t[:, qb, 0:own], in0=wk_t[:, qb, 0:own],
                                 scalar1=t8_t[:, qb, 2:3], scalar2=1.0, op0=ALU.is_ge, op1=ALU.subtract)
                            P.op("dve", "memset", (), [sb_b], args=(sb_t[:, qb, own:own + 1], 0.0))
                        else:
                            P.op("dve", "memset", (), [sb_b], args=(sb_t[:, qb, 0:own + 1], 0.0))
                P.op("dve", "tensor_scalar", [sb_b], [sb_b], out=sb_t, in0=sb_t, scalar1=BIG, scalar2=None, op0=ALU.mult)
                for qb in range(4):
                    P.op("pe", "transpose", [sb_b, identf_b], [PSB[7]], qb == 3, out=psb[7][0:32, qb * 128:(qb + 1) * 128],
                         in_=sb_t[:, qb, :], identity=identf_t)
                P.op("dve", "tensor_copy", [PSB[7]], [QA_b[kq][i]], out=Qt[64:96, i * 512:(i + 1) * 512], in_=psb[7][0:32, :])
                attn_tiles(Kt, KP_b[kq], Qt, [QP_b[kq], QA_b[kq][i]], 0, 96, VA[vk], VA_b[vk], None, i, 384 + m * 64)
        for oh in range(2):
            for g in range(3):
                d = DIL[g]
                ptile = 2 + g
                hd = 4 + 2 * g + oh
                kq = cnt["kq"] % 2; cnt["kq"] += 1
                load_rows(KP[kq], KP_b[kq], KT16, ptile * 128, 128, KTb)
                load_rows(QP[kq], QP_b[kq], QT16, ptile * 128, 128, QTb)
                vk = cnt["va"] % 2; cnt["va"] += 1
                load_va(vk, hd, d)
                Kt = KP[kq]; Qt = QP[kq]; r0 = oh * 64
                KQ = [KP_b[kq], QP_b[kq]]
                nbd = NB // d
                for r in range(d):
                    for ub4 in range(0, nbd, 4):
                        po = 4 + cnt["po"] % 2; cnt["po"] += 1
                        nu = min(4, nbd - ub4)
                        for u in range(nu):
                            ub = ub4 + u
                            ps = cnt["ps"] % 3; cnt["ps"] += 1
                            pk = cnt["pt"] % 4; cnt["pt"] += 1
                            qa = r + d * 128 * ub
                            qap = Qt[r0:r0 + 64, qa: qa + d * 127 + 1: d]
                            lo = 0 if ub > 0 else 128
                            if ub > 0:
                                ka = r + d * 128 * (ub - 1)
                                mm_group(psb[ps][:, 0:128], [(Kt[r0:r0 + 64, ka: ka + d * 127 + 1: d], qap)], KQ, [PSB[ps]])
                            mm_group(psb[ps][:, 128:256], [(Kt[r0:r0 + 64, qa: qa + d * 127 + 1: d], qap)], KQ, [PSB[ps]])
                            ACT(pt_t[pk][:, lo:256], psb[ps][:, lo:256], AF.Exp, [PSB[ps]], [pt_b[pk]], scale=0.125)
                            TT("pool", pt_t[pk][:, lo:256], pt_t[pk][:, lo:256], cmask_t[:, lo:256], ALU.mult, [pt_b[pk], cmask_b], [pt_b[pk]])
                            bi = r * nbd + ub
                            if ub > 0:
                                MM(psb[po][:, u * 128:(u + 1) * 128], VA[vk][:, bi - 1, :], pt_t[pk][:, 0:128], True, False,
                                   [VA_b[vk], pt_b[pk]], [PSB[po]], inc=False)
                            MM(psb[po][:, u * 128:(u + 1) * 128], VA[vk][:, bi, :], pt_t[pk][:, 128:256], ub == 0, True,
                               [VA_b[vk], pt_b[pk]], [PSB[po]], inc=True)
                        a0 = r + d * 128 * ub4
                        accv = acc_t[:, a0: a0 + d * (128 * nu - 1) + 1: d]
                        if g == 0:
                            P.op("dve", "tensor_copy", [PSB[po]], [acc_b], out=accv, in_=psb[po][:, 0:128 * nu])
                        else:
                            TT("dve", accv, psb[po][:, 0:128 * nu], accv, ALU.add, [PSB[po], acc_b], [acc_b])
            for i in range(NT):
                yk = cnt["y"] % 2; cnt["y"] += 1
                P.op("dve", "reciprocal", [acc_b], [rec_b], out=rec_t[64:128, :], in_=acc_t[64:128, i * 512:(i + 1) * 512])
                P.op("dve", "tensor_copy", [rec_b], [rec0_b], out=rec0_t[0:64, :], in_=rec_t[64:128, :])
                TT("dve", yst[yk][0:64, :], acc_t[0:64, i * 512:(i + 1) * 512], rec0_t[0:64, :], ALU.mult, [acc_b, rec0_b], [yst_b[yk]])
                P.dma("pool", [(YT16[256 + oh * 64:256 + oh * 64 + 64, i * 512:(i + 1) * 512], yst[yk][0:64, :])], yst_b[yk], R=[yst_b[yk]], W=[YTb])

        P.barrier()
        ar.reset()
        xt = [ar.f32(4096).rearrange("p (k n) -> p k n", n=512) for _ in range(2)]
        xt_b = [P.buf("xt%d" % i, True) for i in range(2)]
        yt = [ar.bf(3072).rearrange("p (k n) -> p k n", n=512) for _ in range(2)]
        yt_b = [P.buf("ytC%d" % i, True) for i in range(2)]
        pp = [ar.f32(1024).rearrange("p (k n) -> p k n", n=512) for _ in range(2)]
        pp_b = [P.buf("ppC%d" % i, True) for i in range(2)]
        p16_t = ar.bf(1024).rearrange("p (k n) -> p k n", n=512); p16_b = P.buf("p16")
        sq_t = ar.bf(4096).rearrange("p (k n) -> p k n", n=512); sq_b = P.buf("sq")
        h_t = ar.bf(4096).rearrange("p (k n) -> p k n", n=512); h_b = P.buf("h")
        mg_t = ar.bf(4096).rearrange("p (k n) -> p k n", n=512); mg_b = P.buf("mgC")
        o_t = ar.f32(4096).rearrange("p (k n) -> p k n", n=512); o_b = P.buf("oC")
        ff_t = ar.bf(NFF * 512).rearrange("p (k n) -> p k n", n=512); ff_b = P.buf("ffC")
        rstd_t = ar.f32(512); rstd_b = P.buf("rstd")
        tmp_t = ar.f32(512); tmp_b = P.buf("tmp")
        gs = [ar.f32(512) for _ in range(3)]; gs_b = [P.buf("gs%d" % i) for i in range(3)]
        ma = [ar.f32(512) for _ in range(3)]; ma_b = [P.buf("ma%d" % i) for i in range(3)]
        tt = [ar.f32(512) for _ in range(2)]; tt_b = [P.buf("tt%d" % i) for i in range(2)]
        ring = Ring(6, 2816, "ring")
        prc = {"i": 0}

        def nps():
            k = prc["i"] % 7; prc["i"] += 1
            return k

        def loadxC(t):
            k = t % 2
            P.dma("sp", [(xt[k][:, 0:4, :], xtile_ap(xsrc(l), t)[:, 0:4, :]),
                         (xt[k][:, 4:8, :], xtile_ap(xsrc(l), t)[:, 4:8, :])], xt_b[k], R=[Xb], W=[xt_b[k]])
            P.dma("sp", [(yt[k], YT16[:, t * 512:(t + 1) * 512].rearrange("(k p) n -> p k n", p=128))], yt_b[k], R=[YTb], W=[yt_b[k]])
            P.dma("sp", [(pp[k], pT[l * PLE:(l + 1) * PLE, t * 512:(t + 1) * 512].rearrange("(k p) n -> p k n", p=128))], pp_b[k], W=[pp_b[k]])

        def post_norm_res(x_t, x_b, gcol):
            rms_stats(sq_t, sq_b, rstd_t, rstd_b, tmp_t, tmp_b)
            for m in range(8):
                k = m % 2
                P.op("dve", "scalar_tensor_tensor", [o_b, rstd_b, gains_b], [tt_b[k]], out=tt[k], in0=o_t[:, m, :],
                     scalar=gains_t[:, gcol + m:gcol + m + 1], in1=rstd_t, op0=ALU.mult, op1=ALU.mult)
                TT("pool", x_t[:, m, :], x_t[:, m, :], tt[k], ALU.add, [x_b, tt_b[k]], [x_b])

        def evac_o(ps, m):
            ACT(o_t[:, m, :], psb[ps][:, :], AF.Copy, [PSB[ps]], [o_b])
            ACT(sq_t[:, m, :], psb[ps][:, :], AF.Square, [PSB[ps]], [sq_b])

        loadxC(0)
        for t in range(NT):
            if t + 1 < NT:
                loadxC(t + 1)
            k = t % 2
            x_t = xt[k]; x_b = xt_b[k]; y_t = yt[k]; y_b = yt_b[k]
            pre_norm(x_t, x_b, g0 + 0, sq_t, sq_b, rstd_t, rstd_b, tmp_t, tmp_b, h_t, h_b)
            ychunks = [(0, 2), (2, 1), (3, 3)]
            for m in range(8):
                wb3, wb_b = ring.load(wslab("wBR", l, m), 768, [WB[l]])
                for b in range(3):
                    wg3, wg_b = ring.load(wslab("wG", l, b * 8 + m), 1024, [WB[l]])
                    pg = nps()
                    mm_group(psb[pg][:, :], [(wg3[:, kc, :], h_t[:, kc, :]) for kc in range(8)], [wg_b, h_b], [PSB[pg]])
                    ACT(gs[b], psb[pg][:, :], AF.Sigmoid, [PSB[pg]], [gs_b[b]])
                    c0, ncc = ychunks[b]
                    pq = nps()
                    mm_group(psb[pq][:, :], [(wb3[:, c0 + c, :], y_t[:, c0 + c, :]) for c in range(ncc)], [wb_b, y_b], [PSB[pq]])
                    TT("dve", ma[b], psb[pq][:, :], gs[b], ALU.mult, [PSB[pq], gs_b[b]], [ma_b[b]])
                TT("pool", ma[0], ma[0], ma[1], ALU.add, [ma_b[0], ma_b[1]], [ma_b[0]])
                TT("pool", mg_t[:, m, :], ma[0], ma[2], ALU.add, [ma_b[0], ma_b[2]], [mg_b])
            for m in range(8):
                w3, w_b = ring.load(wslab("wO", l, m), 1024, [WB[l]])
                ps = nps()
                mm_group(psb[ps][:, :], [(w3[:, kc, :], mg_t[:, kc, :]) for kc in range(8)], [w_b, mg_b], [PSB[ps]])
                evac_o(ps, m)
            post_norm_res(x_t, x_b, g0 + 8)
            pre_norm(x_t, x_b, g0 + 16, sq_t, sq_b, rstd_t, rstd_b, tmp_t, tmp_b, h_t, h_b)
            for j in range(NFF):
                wg3, wg_b = ring.load(wslab("wFG", l, j), 1024, [WB[l]])
                pg = nps()
                mm_group(psb[pg][:, :], [(wg3[:, kc, :], h_t[:, kc, :]) for kc in range(8)], [wg_b, h_b], [PSB[pg]])
                kk = j % 3
                ACT(gs[kk], psb[pg][:, :], AF.Silu, [PSB[pg]], [gs_b[kk]])
                wu3, wu_b = ring.load(wslab("wFU", l, j), 1024, [WB[l]])
                pu = nps()
                mm_group(psb[pu][:, :], [(wu3[:, kc, :], h_t[:, kc, :]) for kc in range(8)], [wu_b, h_b], [PSB[pu]])
                TT("dve", ff_t[:, j, :], psb[pu][:, :], gs[kk], ALU.mult, [PSB[pu], gs_b[kk]], [ff_b])
            for m in range(8):
                w3, w_b = ring.load(wslab("wFD", l, m), 2816, [WB[l]])
                ps = nps()
                mm_group(psb[ps][:, :], [(w3[:, j, :], ff_t[:, j, :]) for j in range(NFF)], [w_b, ff_b], [PSB[ps]])
                evac_o(ps, m)
            post_norm_res(x_t, x_b, g0 + 24)
            ACT(h_t, x_t, AF.Copy, [x_b], [h_b])
            P.op("dve", "tensor_copy", [pp_b[k]], [p16_b], out=p16_t, in_=pp[k])
            for m in range(8):
                wg3, wg_b = ring.load(wslab("wPG", l, m), 1024, [WB[l]])
                pg = nps()
                mm_group(psb[pg][:, :], [(wg3[:, kc, :], h_t[:, kc, :]) for kc in range(8)], [wg_b, h_b], [PSB[pg]])
                kk = m % 3
                ACT(gs[kk], psb[pg][:, :], AF.Sigmoid, [PSB[pg]], [gs_b[kk]])
                wp3, wp_b = ring.load(wslab("wPL", l, m), 256, [WB[l]])
                pu = nps()
                mm_group(psb[pu][:, :], [(wp3[:, kc, :], p16_t[:, kc, :]) for kc in range(2)], [wp_b, p16_b], [PSB[pu]])
                TT("dve", o_t[:, m, :], psb[pu][:, :], gs[kk], ALU.mult, [PSB[pu], gs_b[kk]], [o_b])
                ACT(sq_t[:, m, :], o_t[:, m, :], AF.Square, [o_b], [sq_b])
            post_norm_res(x_t, x_b, g0 + 32)
            P.dma("pool", [(xtile_ap(xdst(l), t)[:, 0:4, :], x_t[:, 0:4, :]), (xtile_ap(xdst(l), t)[:, 4:8, :], x_t[:, 4:8, :])],
                  x_b, R=[x_b], W=[Xb])
    P.barrier()
    block = es.enter_context(nc.Block())
    P.replay(block)
    es.close()
    return nc


def _slabs(w, cols_list, kc):
    out = np.empty((len(cols_list), 128, kc, 128), np.float32)
    wk = w.reshape(kc, 128, w.shape[1])
    for m, cols in enumerate(cols_list):
        out[m] = wk[:, :, cols].transpose(1, 0, 2)
    return out.reshape(len(cols_list) * 128, kc * 128)


def _host_weights(inp, L):
    r = {}
    ar_ = np.arange
    lists = {n: [] for n in ("wA", "wV", "wF", "wG", "wBR", "wO", "wFG", "wFU", "wFD", "wPL", "wPG")}
    for l in range(L):
        w_in = np.asarray(inp["w_in"][l], np.float32)
        colsA = []
        for base in (0, 1024):
            for pt in range(8):
                c = base + pt * 128 + ar_(128)
                colsA.append(c)
                if pt >= 2:
                    sw = base + pt * 128 + (ar_(128) // 64) * 64 + (ar_(128) % 64 + 32) % 64
                    colsA.append(sw)
        lists["wA"].append(_slabs(w_in, colsA, 8))
        wv = w_in[:, 2048:3072].reshape(8, 128, 1024).transpose(1, 0, 2).reshape(128, 8192)
        lists["wV"].append(wv)
        wf = w_in[:, 3072:3076].reshape(8, 128, 4).transpose(1, 0, 2).reshape(128, 32)
        lists["wF"].append(wf)
        lists["wG"].append(_slabs(w_in, [3076 + b * 1024 + m * 128 + ar_(128) for b in range(3) for m in range(8)], 8))
        wbr = np.concatenate([np.asarray(inp["w_br_a"][l]), np.asarray(inp["w_br_b"][l]), np.asarray(inp["w_br_c"][l])], axis=0)
        lists["wBR"].append(_slabs(wbr.astype(np.float32), [m * 128 + ar_(128) for m in range(8)], 6))
        lists["wO"].append(_slabs(np.asarray(inp["w_out"][l], np.float32), [m * 128 + ar_(128) for m in range(8)], 8))
        lists["wFG"].append(_slabs(np.asarray(inp["w_ffn_gate"][l], np.float32), [m * 128 + ar_(128) for m in range(NFF)], 8))
        lists["wFU"].append(_slabs(np.asarray(inp["w_ffn_up"][l], np.float32), [m * 128 + ar_(128) for m in range(NFF)], 8))
        lists["wFD"].append(_slabs(np.asarray(inp["w_ffn_down"][l], np.float32), [m * 128 + ar_(128) for m in range(8)], NFF))
        lists["wPL"].append(_slabs(np.asarray(inp["w_ple"][l], np.float32), [m * 128 + ar_(128) for m in range(8)], 2))
        lists["wPG"].append(_slabs(np.asarray(inp["w_ple_gate"][l], np.float32), [m * 128 + ar_(128) for m in range(8)], 8))
    for n, v in lists.items():
        r[n] = np.ascontiguousarray(np.concatenate(v, axis=0), dtype=np.float32)
    gl = []
    for l in range(L):
        for n in ("g_mix_pre", "g_mix_post", "g_ffn_pre", "g_ffn_post", "g_ple_post"):
            gl.append(np.asarray(inp[n][l], np.float32).reshape(8, 128).T)
    r["gains"] = np.ascontiguousarray(np.concatenate(gl, axis=1), dtype=np.float32)
    r["bfb"] = np.ascontiguousarray(np.broadcast_to(np.asarray(inp["b_f"], np.float32).reshape(1, L * 4), (128, L * 4)))
    return r


def _consts(S):
    c = {}
    inv = (1.0 / (np.float32(10000.0) ** (np.arange(0, 64, 2, dtype=np.float32) / np.float32(64)))).astype(np.float32)
    ang = (np.arange(S, dtype=np.float32)[:, None] * inv[None, :]).astype(np.float32)
    cos = np.cos(ang.astype(np.float64)).astype(np.float32).T
    sin = np.sin(ang.astype(np.float64)).astype(np.float32).T
    c["cosT"] = np.ascontiguousarray(np.concatenate([cos, cos, cos, cos], axis=0))
    c["sinT"] = np.ascontiguousarray(np.concatenate([-sin, sin, -sin, sin], axis=0))
    p = np.arange(128)
    c["cmask"] = np.ascontiguousarray(np.concatenate([(p[:, None] >= p[None, :]), (p[:, None] <= p[None, :])], axis=1).astype(np.float32))
    c["trif"] = np.ascontiguousarray((p[:, None] <= p[None, :]).astype(np.float32))
    c["identf"] = np.eye(128, dtype=np.float32)
    c["blkind"] = np.ascontiguousarray(np.concatenate([(np.arange(S)[None, :] // 256 == np.arange(32)[:, None]), np.ones((1, S), bool)], axis=0).astype(np.float32))
    return c


_NC_CACHE = {}


def kernel(**inputs):
    x = np.asarray(inputs["x"], np.float32)
    p = np.asarray(inputs["p"], np.float32)
    B, S, _ = x.shape
    L = p.shape[0]
    key = (S, L)
    if key not in _NC_CACHE:
        _NC_CACHE[key] = build(S, L)
    nc = _NC_CACHE[key]
    shared = _host_weights(inputs, L)
    shared.update(_consts(S))
    in_maps = []
    for b in range(B):
        m = dict(shared)
        m["xT"] = np.ascontiguousarray(x[b].T)
        m["pT"] = np.ascontiguousarray(p[:, b].transpose(0, 2, 1).reshape(L * PLE, S))
        in_maps.append(m)
    res = run_bass_kernel_spmd(nc, in_maps, core_ids=list(range(B)))
    out = np.stack([np.ascontiguousarray(res.results[b]["outT"].T) for b in range(B)], axis=0)
    return out.astype(np.float32)
```

```python
import numpy as np
from contextlib import ExitStack
import concourse.bass as bass
import concourse.mybir as mybir
from concourse.bass_utils import run_bass_kernel_spmd

F32 = mybir.dt.float32
BF = mybir.dt.bfloat16
AF = mybir.ActivationFunctionType
ALU = mybir.AluOpType
AX = mybir.AxisListType

D = 1024
HD = 64
PLE = 256
DFF = 2816
NFF = 22
BIG = 30000.0
DIL = (1, 4, 16)
SAME_ENGINE_SYNC = True


class Buf:
    def __init__(self, name, sem=None):
        self.name = name
        self.w = {}
        self.r = {}
        self.sem = sem
        self.cnt = 0


class Eng:
    def __init__(self, name, sem):
        self.name = name
        self.sem = sem
        self.cnt = 0
        self.seen = {}
        self.prog = []


class Prog:
    def __init__(self, nc, es):
        self.nc = nc
        self.es = es
        self.sems = []
        self.E = {}
        for n in ("pe", "act", "dve", "pool"):
            self.E[n] = Eng(n, self.newsem(n))
        self.E["sp"] = Eng("sp", None)
        self.bufs = []
        self.bynames = {}

    def newsem(self, name):
        h = self.es.enter_context(self.nc.semaphore("s_" + name))
        self.sems.append(h)
        return len(self.sems) - 1

    def buf(self, name, dma=False):
        if name in self.bynames:
            return self.bynames[name]
        b = Buf(name, self.newsem(name) if dma else None)
        self.bufs.append(b)
        self.bynames[name] = b
        return b

    def _waits(self, X, R, W):
        need = {}
        for b in R:
            for k, v in b.w.items():
                need[k] = max(need.get(k, 0), v)
        for b in W:
            for k, v in b.w.items():
                need[k] = max(need.get(k, 0), v)
            for k, v in b.r.items():
                need[k] = max(need.get(k, 0), v)
        out = []
        for k, v in need.items():
            if k == X.sem and (X.name == "pe" or not SAME_ENGINE_SYNC):
                continue
            if X.seen.get(k, 0) >= v:
                continue
            X.seen[k] = v
            out.append((k, v))
        return out

    def op(self, eng, name, R=(), W=(), inc=True, args=(), **kw):
        X = self.E[eng]
        waits = self._waits(X, R, W)
        tok = X.cnt + 1
        if inc:
            X.cnt = tok
        X.prog.append((waits, ("op", name, args, kw), inc))
        for b in R:
            b.r[X.sem] = tok
        for b in W:
            b.w = {X.sem: tok}
            b.r = {}

    def dma(self, q, pairs, sb, R=(), W=()):
        X = self.E[q]
        waits = self._waits(X, R, W)
        first = True
        for o, i in pairs:
            sb.cnt += 16
            X.prog.append((waits if first else [], ("dma", o, i, sb.sem), False))
            first = False
        for b in R:
            b.r[sb.sem] = sb.cnt
        for b in W:
            b.w = {sb.sem: sb.cnt}
            b.r = {}

    def barrier(self):
        toks = {}
        for n in ("pe", "act", "dve", "pool"):
            toks[self.E[n].sem] = self.E[n].cnt
        for b in self.bufs:
            if b.sem is not None and b.cnt > 0:
                toks[b.sem] = b.cnt
        for n, X in self.E.items():
            waits = []
            for k, v in toks.items():
                if v > 0 and X.seen.get(k, 0) < v and not (k == X.sem and n == "pe"):
                    X.seen[k] = v
                    waits.append((k, v))
            X.prog.append((waits, None, False))

    def replay(self, block):
        sems = self.sems

        def run(X, e):
            for waits, fn, inc in X.prog:
                for k, v in waits:
                    e.wait_ge(sems[k], v)
                if fn is None:
                    continue
                if fn[0] == "dma":
                    _, o, i, k = fn
                    e.dma_start(out=o, in_=i).then_inc(sems[k], 16)
                else:
                    _, name, args, kw = fn
                    ins = getattr(e, name)(*args, **kw)
                    if inc:
                        ins.then_inc(sems[X.sem], 1)

        @block.sync
        def _(e):
            run(self.E["sp"], e)

        @block.tensor
        def _(e):
            run(self.E["pe"], e)

        @block.scalar
        def _(e):
            run(self.E["act"], e)

        @block.vector
        def _(e):
            run(self.E["dve"], e)

        @block.gpsimd
        def _(e):
            run(self.E["pool"], e)


class Arena:
    def __init__(self, ap, lo, hi):
        self.ap = ap
        self.lo = lo
        self.hi = hi
        self.p = lo

    def f32(self, n):
        a = self.ap[:, self.p:self.p + n]
        self.p += n
        assert self.p <= self.hi, ("arena overflow", self.p, self.hi)
        return a

    def bf(self, n):
        n2 = (n + 1) // 2
        a = self.ap[:, self.p:self.p + n2].bitcast(BF)
        self.p += n2
        assert self.p <= self.hi, ("arena overflow", self.p, self.hi)
        return a[:, 0:n]

    def reset(self):
        self.p = self.lo


def build(S=8192, L=2, dbg=False):
    NT = S // 512
    NB = S // 128
    NMB = S // 256
    CW = min(2048, S)
    assert NMB <= 32
    nc = bass.Bass("TRN2", target_bir_lowering=False)

    def din(name, shape, dt=F32):
        return nc.dram_tensor(name, shape, dt, kind="ExternalInput").ap()

    def dscr(name, shape, dt=BF):
        return nc.dram_tensor(name, shape, dt, kind=("ExternalOutput" if dbg else "Internal")).ap()

    xT = din("xT", [D, S])
    pT = din("pT", [L * PLE, S])
    wspec = [("wA", 28, 1024), ("wV", 1, 8192), ("wF", 1, 32), ("wG", 24, 1024), ("wBR", 8, 768),
             ("wO", 8, 1024), ("wFG", 22, 1024), ("wFU", 22, 1024), ("wFD", 8, 2816),
             ("wPL", 8, 256), ("wPG", 8, 1024)]
    w32 = {}
    w16 = {}
    for n, ns, nc_ in wspec:
        w32[n] = din(n, [L * ns * 128, nc_])
        w16[n] = nc.dram_tensor(n + "_16", [L * ns * 128, nc_], BF, kind="Internal").ap()
    wns = {n: ns for n, ns, _ in wspec}
    gains = din("gains", [128, L * 5 * 8])
    bfb = din("bfb", [128, L * 4])
    cosT = din("cosT", [128, S])
    sinT = din("sinT", [128, S])
    cmask32 = din("cmask", [128, 256])
    trif = din("trif", [128, 128])
    identf = din("identf", [128, 128])
    blkind32 = din("blkind", [33, S])
    outT = nc.dram_tensor("outT", [D, S], F32, kind="ExternalOutput").ap()

    X32 = dscr("X32", [D, S], F32)
    QT16 = dscr("QT16", [1024, S])
    KT16 = dscr("KT16", [1024, S])
    V16 = dscr("V16", [S, 1024])
    YT16 = dscr("YT16", [768, S])
    KS32 = dscr("KS32", [384, 32], F32)
    BI16 = nc.dram_tensor("BI16", [33, S], BF, kind="Internal").ap()

    es = ExitStack()
    P = Prog(nc, es)
    AW = 47104
    arena_t = es.enter_context(nc.sbuf_tensor("arena", [128, AW], F32))
    PERS = 2048
    pers = Arena(arena_t, 0, PERS)
    ar = Arena(arena_t, PERS, AW)
    psb = [es.enter_context(nc.psum_tensor("psb%d" % i, [128, 512], F32)) for i in range(8)]
    PSB = [P.buf("psb%d" % i) for i in range(8)]

    def ACT(out, in_, func, R, W, **kw):
        P.op("act", "activation", R, W, out=out, in_=in_, func=func, **kw)

    def TT(eng, out, in0, in1, op, R, W):
        P.op(eng, "tensor_tensor", R, W, out=out, in0=in0, in1=in1, op=op)

    def MM(out, lhsT, rhs, start, stop, R, W, inc=True):
        P.op("pe", "matmul", R, W, inc, args=(out,), lhsT=lhsT, rhs=rhs, start=start, stop=stop)

    def mm_group(out_ap, pairs, Rb, Wb):
        n = len(pairs)
        for i, (lt, rh) in enumerate(pairs):
            MM(out_ap, lt, rh, i == 0, i == n - 1, Rb, Wb, inc=(i == n - 1))

    gains_t = pers.f32(L * 40); gains_b = P.buf("gains", True)
    bfb_t = pers.f32(L * 4); bfb_b = P.buf("bfb", True)
    trif_t = pers.f32(128); trif_b = P.buf("trif", True)
    identf_t = pers.f32(128); identf_b = P.buf("identf", True)
    onesf_t = pers.f32(128); onesf_b = P.buf("onesf")
    ones16_t = pers.bf(128); ones16_b = P.buf("ones16")
    cmask_t = pers.bf(256); cmask_b = P.buf("cmask", True)
    eps_t = pers.f32(1); one_t = pers.f32(1); cst_b = P.buf("cst")
    cpos_t = pers.f32(NB * 4).rearrange("p (j h) -> p j h", h=4); cpos_b = P.buf("cpos")
    tall_t = pers.f32((NB + 1) * 4).rearrange("p (j h) -> p j h", h=4); tall_b = P.buf("tall")
    ksum_t = pers.f32(3 * 32).rearrange("p (a n) -> p a n", n=32); ksum_b = P.buf("ksum", True)

    P.dma("sp", [(gains_t, gains)], gains_b, W=[gains_b])
    P.dma("sp", [(bfb_t, bfb)], bfb_b, W=[bfb_b])
    P.dma("sp", [(trif_t, trif)], trif_b, W=[trif_b])
    P.dma("sp", [(identf_t, identf)], identf_b, W=[identf_b])
    P.dma("pool", [(cmask_t, cmask32)], cmask_b, W=[cmask_b])
    P.op("dve", "memset", (), [onesf_b], args=(onesf_t, 1.0))
    P.op("dve", "memset", (), [ones16_b], args=(ones16_t, 1.0))
    P.op("dve", "memset", (), [cst_b], args=(eps_t, 1e-6))
    P.op("dve", "memset", (), [cst_b], args=(one_t, 1.0))
    P.op("dve", "memset", (), [tall_b], args=(tall_t[:, 0, :], 0.0))
    P.op("dve", "memset", (), [ksum_b], args=(ksum_t, 0.0))

    WB = [P.buf("w16_%d" % l, True) for l in range(L)]
    BIb = P.buf("bi16", True)
    P.dma("pool", [(BI16[:, c0:c0 + CW], blkind32[:, c0:c0 + CW]) for c0 in range(0, S, CW)], BIb, W=[BIb])
    for l in range(L):
        pairs = []
        for n, ns, ncol in wspec:
            for s_ in range(ns):
                r0 = (l * ns + s_) * 128
                cw = 2048 if ncol > 2816 else ncol
                for c0 in range(0, ncol, cw):
                    pairs.append((w16[n][r0:r0 + 128, c0:c0 + cw], w32[n][r0:r0 + 128, c0:c0 + cw]))
        P.dma("pool", pairs, WB[l])
        WB[l].w = {WB[l].sem: WB[l].cnt}

    def wslab(n, l, s_):
        r0 = (l * wns[n] + s_) * 128
        return w16[n][r0:r0 + 128, :]

    Xb = P.buf("X32d"); QTb = P.buf("QTd"); KTb = P.buf("KTd"); Vb = P.buf("Vd"); YTb = P.buf("YTd"); KSb = P.buf("KSd")

    def xsrc(l):
        return xT if l == 0 else X32

    def xdst(l):
        return outT if l == L - 1 else X32

    def xtile_ap(dram, t):
        return dram[:, t * 512:(t + 1) * 512].rearrange("(kc p) n -> p kc n", p=128)

    class Ring:
        def __init__(self, n, words, name):
            self.t = [ar.bf(words) for _ in range(n)]
            self.b = [P.buf("%s%d" % (name, i), True) for i in range(n)]
            self.i = 0

        def load(self, dram_ap, ncol, Rb):
            k = self.i % len(self.t)
            self.i += 1
            P.dma("sp", [(self.t[k][:, 0:ncol], dram_ap)], self.b[k], R=Rb, W=[self.b[k]])
            return self.t[k][:, 0:ncol].rearrange("p (k n) -> p k n", n=128), self.b[k]

    def rms_stats(sq_t, sq_b, rstd_t, rstd_b, tmp_t, tmp_b):
        mm_group(psb[7][:, :], [(ones16_t, sq_t[:, kc, :]) for kc in range(8)], [ones16_b, sq_b], [PSB[7]])
        ACT(tmp_t, psb[7][:, :], AF.Sqrt, [PSB[7], cst_b], [tmp_b], bias=eps_t, scale=1.0 / D)
        P.op("dve", "reciprocal", [tmp_b], [rstd_b], out=rstd_t, in_=tmp_t)

    def pre_norm(x_t, x_b, gcol, sq_t, sq_b, rstd_t, rstd_b, tmp_t, tmp_b, h_t, h_b):
        ACT(sq_t, x_t, AF.Square, [x_b], [sq_b])
        rms_stats(sq_t, sq_b, rstd_t, rstd_b, tmp_t, tmp_b)
        for kc in range(8):
            P.op("dve", "scalar_tensor_tensor", [x_b, rstd_b, gains_b], [h_b], out=h_t[:, kc, :], in0=x_t[:, kc, :],
                 scalar=gains_t[:, gcol + kc:gcol + kc + 1], in1=rstd_t, op0=ALU.mult, op1=ALU.mult)

    for l in range(L):
        g0 = l * 40
        P.barrier()
        ar.reset()
        xt = [ar.f32(4096).rearrange("p (k n) -> p k n", n=512) for _ in range(2)]
        xt_b = [P.buf("xt%d" % i, True) for i in range(2)]
        sq_t = ar.bf(4096).rearrange("p (k n) -> p k n", n=512); sq_b = P.buf("sq")
        h_t = ar.bf(4096).rearrange("p (k n) -> p k n", n=512); h_b = P.buf("h")
        rstd_t = ar.f32(512); rstd_b = P.buf("rstd")
        tmp_t = ar.f32(512); tmp_b = P.buf("tmp")
        wv_t = ar.bf(8192).rearrange("p (k n) -> p k n", n=1024); wv_b = P.buf("wvA", True)
        wf_t = ar.bf(32).rearrange("p (k n) -> p k n", n=4); wf_b = P.buf("wfA", True)
        ring = Ring(6, 1024, "ring")
        cs_t = [(ar.f32(512), ar.f32(512)) for _ in range(2)]
        cs_b = [P.buf("csA%d" % i, True) for i in range(2)]
        qst = [ar.bf(4096).rearrange("p (k n) -> p k n", n=512) for _ in range(2)]
        qst_b = [P.buf("qst%d" % i, True) for i in range(2)]
        kst = [ar.bf(4096).rearrange("p (k n) -> p k n", n=512) for _ in range(2)]
        kst_b = [P.buf("kst%d" % i, True) for i in range(2)]
        vst = [ar.bf(4096).rearrange("p (a n) -> p a n", n=1024) for _ in range(2)]
        vst_b = [P.buf("vst%d" % i, True) for i in range(2)]
        r1 = [ar.f32(512) for _ in range(2)]; r1_b = [P.buf("r1_%d" % i) for i in range(2)]
        r2 = [ar.f32(512) for _ in range(2)]; r2_b = [P.buf("r2_%d" % i) for i in range(2)]
        fb_t = ar.f32(4); fb_b = P.buf("fbA")
        fe_t = ar.f32(4); fe_b = P.buf("feA")
        fl_t = ar.f32(4); fl_b = P.buf("flA")

        P.dma("sp", [(wv_t[:, kc, :], wslab("wV", l, 0)[:, kc * 1024:(kc + 1) * 1024]) for kc in range(8)],
              wv_b, R=[WB[l]], W=[wv_b])
        P.dma("sp", [(wf_t, wslab("wF", l, 0).rearrange("p (k n) -> p k n", n=4))], wf_b, R=[WB[l]], W=[wf_b])

        def loadxA(t):
            k = t % 2
            P.dma("sp", [(xt[k][:, 0:4, :], xtile_ap(xsrc(l), t)[:, 0:4, :]),
                         (xt[k][:, 4:8, :], xtile_ap(xsrc(l), t)[:, 4:8, :])], xt_b[k], R=[Xb], W=[xt_b[k]])
            P.dma("sp", [(cs_t[k][0], cosT[:, t * 512:(t + 1) * 512]),
                         (cs_t[k][1], sinT[:, t * 512:(t + 1) * 512])], cs_b[k], W=[cs_b[k]])

        loadxA(0)
        psrot = 0
        for t in range(NT):
            if t + 1 < NT:
                loadxA(t + 1)
            x_t = xt[t % 2]; x_b = xt_b[t % 2]
            cos_t, sin_t = cs_t[t % 2]; c_b = cs_b[t % 2]
            pre_norm(x_t, x_b, g0 + 0, sq_t, sq_b, rstd_t, rstd_b, tmp_t, tmp_b, h_t, h_b)
            qs = qst[t % 2]; qs_b = qst_b[t % 2]; ks = kst[t % 2]; ks_b = kst_b[t % 2]
            si = 0
            for which in range(2):
                stg, stg_b = (qs, qs_b) if which == 0 else (ks, ks_b)
                for pt in range(8):
                    w3, w_b = ring.load(wslab("wA", l, si), 1024, [WB[l]]); si += 1
                    pa = psrot % 6; psrot += 1
                    mm_group(psb[pa][:, :], [(w3[:, kc, :], h_t[:, kc, :]) for kc in range(8)], [w_b, h_b], [PSB[pa]])
                    if pt < 2:
                        ACT(stg[:, pt, :], psb[pa][:, :], AF.Copy, [PSB[pa]], [stg_b])
                    else:
                        w23, w2_b = ring.load(wslab("wA", l, si), 1024, [WB[l]]); si += 1
                        pb = psrot % 6; psrot += 1
                        mm_group(psb[pb][:, :], [(w23[:, kc, :], h_t[:, kc, :]) for kc in range(8)], [w2_b, h_b], [PSB[pb]])
                        ri = (pt + which) % 2
                        TT("dve", r1[ri], psb[pa][:, :], cos_t, ALU.mult, [PSB[pa], c_b], [r1_b[ri]])
                        TT("dve", r2[ri], psb[pb][:, :], sin_t, ALU.mult, [PSB[pb], c_b], [r2_b[ri]])
                        TT("pool", stg[:, pt, :], r1[ri], r2[ri], ALU.add, [r1_b[ri], r2_b[ri]], [stg_b])
                        if which == 1 and pt >= 5:
                            P.op("dve", "tensor_reduce", [stg_b], [ksum_b], out=ksum_t[:, pt - 5, 2 * t:2 * t + 2],
                                 in_=stg[:, pt, :].rearrange("p (a b) -> p a b", b=256), axis=AX.X, op=ALU.add)
            P.dma("pool", [(QT16[:, t * 512:(t + 1) * 512].rearrange("(k p) n -> p k n", p=128), qs)], qs_b, R=[qs_b], W=[QTb])
            P.dma("pool", [(KT16[:, t * 512:(t + 1) * 512].rearrange("(k p) n -> p k n", p=128), ks)], ks_b, R=[ks_b], W=[KTb])
            vs = vst[t % 2]; vs_b = vst_b[t % 2]
            for tb in range(4):
                for hf in range(2):
                    pa = psrot % 6; psrot += 1
                    mm_group(psb[pa][:, :], [(h_t[:, kc, tb * 128:(tb + 1) * 128], wv_t[:, kc, hf * 512:(hf + 1) * 512]) for kc in range(8)],
                             [wv_b, h_b], [PSB[pa]])
                    ACT(vs[:, tb, hf * 512:(hf + 1) * 512], psb[pa][:, :], AF.Copy, [PSB[pa]], [vs_b])
                j = 4 * t + tb
                mm_group(psb[6][:, 0:4], [(h_t[:, kc, tb * 128:(tb + 1) * 128], wf_t[:, kc, :]) for kc in range(8)], [wf_b, h_b], [PSB[6]])
                TT("dve", fb_t, psb[6][:, 0:4], bfb_t[:, l * 4:l * 4 + 4], ALU.add, [PSB[6], bfb_b], [fb_b])
                ACT(fe_t, fb_t, AF.Exp, [fb_b], [fe_b], scale=-1.0)
                ACT(fl_t, fe_t, AF.Ln, [fe_b, cst_b], [fl_b], bias=one_t, scale=1.0)
                mm_group(psb[6][:, 8:12], [(trif_t, fl_t)], [trif_b, fl_b], [PSB[6]])
                mm_group(psb[6][:, 16:20], [(onesf_t, fl_t)], [onesf_b, fl_b], [PSB[6]])
                TT("dve", cpos_t[:, j, :], psb[6][:, 8:12], tall_t[:, j, :], ALU.add, [PSB[6], tall_b], [cpos_b])
                TT("dve", tall_t[:, j + 1, :], psb[6][:, 16:20], tall_t[:, j, :], ALU.add, [PSB[6], tall_b], [tall_b])
            P.dma("pool", [(V16[t * 512:(t + 1) * 512, :].rearrange("(a p) c -> p a c", p=128), vs)], vs_b, R=[vs_b], W=[Vb])
        P.dma("pool", [(KS32.rearrange("(a p) n -> p a n", p=128), ksum_t)], ksum_b, R=[ksum_b], W=[KSb])

        P.barrier()
        ar.reset()
        KP = [ar.bf(S) for _ in range(2)]; KP_b = [P.buf("KP%d" % i, True) for i in range(2)]
        QP = [ar.bf(S) for _ in range(2)]; QP_b = [P.buf("QP%d" % i, True) for i in range(2)]
        QA_b = [[P.buf("QA%d_%d" % (i, t)) for t in range(NT)] for i in range(2)]
        VA = [ar.bf(NB * 128).rearrange("p (j c) -> p j c", c=128) for _ in range(2)]
        VA_b = [P.buf("VA%d" % i, True) for i in range(2)]
        pt_t = [ar.bf(512) for _ in range(4)]; pt_b = [P.buf("pT%d" % i) for i in range(4)]
        rec_t = ar.f32(512); rec_b = P.buf("rec")
        rec0_t = ar.f32(512); rec0_b = P.buf("rec0")
        yst = [ar.bf(512) for _ in range(2)]; yst_b = [P.buf("yst%d" % i, True) for i in range(2)]
        ks16_t = ar.bf(32); ks16_b = P.buf("ks16")
        ks32_t = ar.f32(32); ks32_b = P.buf("ks32", True)
        wk_t = ar.f32(128).rearrange("p (a n) -> p a n", n=32); wk_b = P.buf("wk")
        t8_t = ar.f32(32).rearrange("p (a n) -> p a n", n=8); t8_b = P.buf("t8")
        sb_t = ar.f32(128).rearrange("p (a n) -> p a n", n=32); sb_b = P.buf("selb")
        acc_t = ar.f32(S); acc_b = P.buf("acc")
        for i in range(2):
            P.op("pool", "memset", (), [VA_b[i]], args=(VA[i][:, :, 64:128], 1.0))
        cnt = {"ps": 0, "po": 0, "pt": 0, "y": 0, "kq": 0, "va": 0}

        def load_rows(dst, dst_b, dram, r0, nr, rb, ind=False):
            pairs = [(dst[0:nr, c0:c0 + CW], dram[r0:r0 + nr, c0:c0 + CW]) for c0 in range(0, S, CW)]
            Rb = [rb]
            if ind == 1:
                pairs += [(dst[64:96, c0:c0 + CW], BI16[0:32, c0:c0 + CW]) for c0 in range(0, S, CW)]
                Rb = [rb, BIb]
            if ind == 2:
                pairs += [(dst[64:65, c0:c0 + CW], BI16[32:33, c0:c0 + CW]) for c0 in range(0, S, CW)]
                Rb = [rb, BIb]
            P.dma("sp", pairs, dst_b, R=Rb, W=[dst_b])

        def load_va(k, head, d):
            nbd = NB // d
            pairs = []
            for r in range(d):
                for b0 in range(0, nbd, 8):
                    nb_ = min(8, nbd - b0)
                    src = V16[r + d * 128 * b0: r + d * 128 * b0 + d * (128 * nb_ - 1) + 1: d, head * 64:(head + 1) * 64]
                    pairs.append((VA[k][:, r * nbd + b0: r * nbd + b0 + nb_, 0:64], src.rearrange("(j p) c -> p j c", p=128)))
            P.dma("sp", pairs, VA_b[k], R=[Vb], W=[VA_b[k]])

        def finalize(po, yrow, i):
            yk = cnt["y"] % 2; cnt["y"] += 1
            P.op("dve", "reciprocal", [PSB[po]], [rec_b], out=rec_t[64:128, :], in_=psb[po][64:128, :])
            TT("dve", yst[yk][0:64, :], psb[po][0:64, :], rec_t[64:128, :], ALU.mult, [PSB[po], rec_b], [yst_b[yk]])
            P.dma("pool", [(YT16[yrow:yrow + 64, i * 512:(i + 1) * 512], yst[yk][0:64, :])], yst_b[yk], R=[yst_b[yk]], W=[YTb])

        LA = 2

        def run_pipe(items):
            n = len(items)
            for idx in range(n + LA):
                if idx < n:
                    items[idx][0]()
                if idx >= LA:
                    items[idx - LA][1]()
                    items[idx - LA][2]()

        def attn_items(Kt, K_b, Qt, Qbufs, r0, nr, va, va_b, bias_fn, i, yrow, hooks):
            items = []
            po = 4 + cnt["po"] % 2; cnt["po"] += 1
            nj = 4 * i + 4
            for j in range(nj):
                off = max(0, j - 4 * i) * 128
                ps = cnt["ps"] % 3; cnt["ps"] += 1
                pk = cnt["pt"] % 4; cnt["pt"] += 1

                def f_score(j=j, off=off, ps=ps):
                    if j in hooks:
                        hooks[j]()
                    mm_group(psb[ps][:, off:512], [(Kt[r0:r0 + nr, j * 128:(j + 1) * 128], Qt[r0:r0 + nr, i * 512 + off:(i + 1) * 512])],
                             [K_b] + Qbufs, [PSB[ps]])

                def f_soft(j=j, off=off, ps=ps, pk=pk):
                    if bias_fn is None:
                        ACT(pt_t[pk][:, off:512], psb[ps][:, off:512], AF.Exp, [PSB[ps]], [pt_b[pk]], scale=0.125)
                    else:
                        ACT(pt_t[pk][:, off:512], psb[ps][:, off:512], AF.Exp, [PSB[ps], cpos_b], [pt_b[pk]], bias=bias_fn(j, i), scale=0.125)
                    if j >= 4 * i:
                        TT("pool", pt_t[pk][:, off:off + 128], pt_t[pk][:, off:off + 128], cmask_t[:, 128:256], ALU.mult,
                           [pt_b[pk], cmask_b], [pt_b[pk]])

                def f_pv(j=j, off=off, pk=pk):
                    MM(psb[po][:, off:512], va[:, j, :], pt_t[pk][:, off:512], j == 0, j == nj - 1, [va_b, pt_b[pk]], [PSB[po]], inc=True)
                    if j == nj - 1:
                        finalize(po, yrow, i)
                items.append((f_score, f_soft, f_pv))
            return items

        for h in range(4):
            kq = cnt["kq"] % 2; cnt["kq"] += 1
            load_rows(KP[kq], KP_b[kq], KT16, h * 64, 64, KTb, ind=2)
            load_rows(QP[kq], QP_b[kq], QT16, h * 64, 64, QTb)
            vk = cnt["va"] % 2; cnt["va"] += 1
            load_va(vk, h, 1)
            items = []

            def mkpre(i, h=h, kq=kq):
                def pre():
                    for qb in range(4):
                        P.op("pe", "transpose", [cpos_b, identf_b], [PSB[7]], qb == 3, out=psb[7][0:1, qb * 128:(qb + 1) * 128],
                             in_=cpos_t[:, 4 * i + qb, h:h + 1], identity=identf_t)
                    P.op("dve", "tensor_scalar", [PSB[7]], [QA_b[kq][i]], out=QP[kq][64:65, i * 512:(i + 1) * 512], in0=psb[7][0:1, :],
                         scalar1=-8.0, scalar2=None, op0=ALU.mult)
                return pre
            mkpre(0)()
            for i in range(NT):
                hooks = {0: mkpre(i + 1)} if i + 1 < NT else {}
                items += attn_items(KP[kq], KP_b[kq], QP[kq], [QP_b[kq], QA_b[kq][i]], 0, 65, VA[vk], VA_b[vk],
                                    (lambda h: lambda j, i: cpos_t[:, j, h:h + 1])(h), i, h * 64, hooks)
            run_pipe(items)
        for m in range(6):
            hd = 10 + m
            kq = cnt["kq"] % 2; cnt["kq"] += 1
            load_rows(KP[kq], KP_b[kq], KT16, hd * 64, 64, KTb, ind=1)
            load_rows(QP[kq], QP_b[kq], QT16, hd * 64, 64, QTb)
            P.dma("sp", [(ks32_t[0:64, :], KS32[m * 64:(m + 1) * 64, :])], ks32_b, R=[KSb], W=[ks32_b])
            P.op("dve", "tensor_copy", [ks32_b], [ks16_b], out=ks16_t[0:64, :], in_=ks32_t[0:64, :])
            vk = cnt["va"] % 2; cnt["va"] += 1
            load_va(vk, hd, 1)
            Kt = KP[kq]; Qt = QP[kq]
            items = []

            def mkpre1(i, kq=kq, Qt=Qt):
                def pre():
                    for qb in range(4):
                        q0 = i * 512 + qb * 128
                        mm_group(psb[6][:, qb * 32:qb * 32 + NMB], [(Qt[0:64, q0:q0 + 128], ks16_t[0:64, 0:NMB])], [QP_b[kq], ks16_b], [PSB[6]])
                    P.op("dve", "memset", (), [wk_b], args=(wk_t, -1e30))
                    P.op("dve", "memset", (), [sb_b], args=(sb_t, -1.0))
                    for hq in range(2):
                        own = 2 * i + hq
                        if own > 3:
                            P.op("dve", "tensor_copy", [PSB[6]], [wk_b], out=wk_t[:, 2 * hq:2 * hq + 2, 0:own],
                                 in_=psb[6][:, 64 * hq:64 * hq + 64].rearrange("p (a n) -> p a n", n=32)[:, :, 0:own])
                        for qq in range(2):
                            qb = 2 * hq + qq
                            if own > 3:
                                P.op("dve", "max", [wk_b], [t8_b], out=t8_t[:, qb, :], in_=wk_t[:, qb, 0:max(own, 8)])
                                P.op("dve", "tensor_scalar", [wk_b, t8_b], [sb_b], out=sb_t[:, qb, 0:own], in0=wk_t[:, qb, 0:own],
                                     scalar1=t8_t[:, qb, 2:3], scalar2=1.0, op0=ALU.is_ge, op1=ALU.subtract)
                                P.op("dve", "memset", (), [sb_b], args=(sb_t[:, qb, own:own + 1], 0.0))
                            else:
                                P.op("dve", "memset", (), [sb_b], args=(sb_t[:, qb, 0:own + 1], 0.0))
                    P.op("dve", "tensor_scalar", [sb_b], [sb_b], out=sb_t, in0=sb_t, scalar1=BIG, scalar2=None, op0=ALU.mult)
                return pre

            def mkpre2(i, kq=kq, Qt=Qt):
                def pre():
                    for qb in range(4):
                        P.op("pe", "transpose", [sb_b, identf_b], [PSB[7]], qb == 3, out=psb[7][0:32, qb * 128:(qb + 1) * 128],
                             in_=sb_t[:, qb, :], identity=identf_t)
                    P.op("dve", "tensor_copy", [PSB[7]], [QA_b[kq][i]], out=Qt[64:96, i * 512:(i + 1) * 512], in_=psb[7][0:32, :])
                return pre
            mkpre1(0)(); mkpre2(0)()
            for i in range(NT):
                hooks = {}
                if i + 1 < NT:
                    hooks[0] = mkpre1(i + 1)
                    hooks[4 * i + 3] = mkpre2(i + 1)
                items += attn_items(Kt, KP_b[kq], Qt, [QP_b[kq], QA_b[kq][i]], 0, 96, VA[vk], VA_b[vk], None, i, 384 + m * 64, hooks)
            run_pipe(items)
        for oh in range(2):
            for g in range(3):
                d = DIL[g]
                ptile = 2 + g
                hd = 4 + 2 * g + oh
                kq = cnt["kq"] % 2; cnt["kq"] += 1
                load_rows(KP[kq], KP_b[kq], KT16, ptile * 128, 128, KTb)
                load_rows(QP[kq], QP_b[kq], QT16, ptile * 128, 128, QTb)
                vk = cnt["va"] % 2; cnt["va"] += 1
                load_va(vk, hd, d)
                Kt = KP[kq]; Qt = QP[kq]; r0 = oh * 64
                KQ = [KP_b[kq], QP_b[kq]]
                nbd = NB // d
                items = []
                for r in range(d):
                    for ub4 in range(0, nbd, 4):
                        po = 4 + cnt["po"] % 2; cnt["po"] += 1
                        nu = min(4, nbd - ub4)
                        for u in range(nu):
                            ub = ub4 + u
                            ps = cnt["ps"] % 3; cnt["ps"] += 1
                            pk = cnt["pt"] % 4; cnt["pt"] += 1
                            qa = r + d * 128 * ub
                            lo = 0 if ub > 0 else 128
                            bi = r * nbd + ub

                            def f_score(ub=ub, ps=ps, qa=qa, d=d, r0=r0, Kt=Kt, Qt=Qt, KQ=KQ):
                                qap = Qt[r0:r0 + 64, qa: qa + d * 127 + 1: d]
                                if ub > 0:
                                    ka = qa - d * 128
                                    mm_group(psb[ps][:, 0:128], [(Kt[r0:r0 + 64, ka: ka + d * 127 + 1: d], qap)], KQ, [PSB[ps]])
                                mm_group(psb[ps][:, 128:256], [(Kt[r0:r0 + 64, qa: qa + d * 127 + 1: d], qap)], KQ, [PSB[ps]])

                            def f_soft(ps=ps, pk=pk, lo=lo):
                                ACT(pt_t[pk][:, lo:256], psb[ps][:, lo:256], AF.Exp, [PSB[ps]], [pt_b[pk]], scale=0.125)
                                TT("pool", pt_t[pk][:, lo:256], pt_t[pk][:, lo:256], cmask_t[:, lo:256], ALU.mult, [pt_b[pk], cmask_b], [pt_b[pk]])

                            def f_pv(ub=ub, u=u, nu=nu, po=po, pk=pk, bi=bi, vk=vk, g=g, r=r, d=d, ub4=ub4):
                                if ub > 0:
                                    MM(psb[po][:, u * 128:(u + 1) * 128], VA[vk][:, bi - 1, :], pt_t[pk][:, 0:128], True, False,
                                       [VA_b[vk], pt_b[pk]], [PSB[po]], inc=False)
                                MM(psb[po][:, u * 128:(u + 1) * 128], VA[vk][:, bi, :], pt_t[pk][:, 128:256], ub == 0, True,
                                   [VA_b[vk], pt_b[pk]], [PSB[po]], inc=True)
                                if u == nu - 1:
                                    a0 = r + d * 128 * ub4
                                    accv = acc_t[:, a0: a0 + d * (128 * nu - 1) + 1: d]
                                    if g == 0:
                                        P.op("dve", "tensor_copy", [PSB[po]], [acc_b], out=accv, in_=psb[po][:, 0:128 * nu])
                                    else:
                                        TT("dve", accv, psb[po][:, 0:128 * nu], accv, ALU.add, [PSB[po], acc_b], [acc_b])
                            items.append((f_score, f_soft, f_pv))
                run_pipe(items)
            for i in range(NT):
                yk = cnt["y"] % 2; cnt["y"] += 1
                P.op("dve", "reciprocal", [acc_b], [rec_b], out=rec_t[64:128, :], in_=acc_t[64:128, i * 512:(i + 1) * 512])
                P.op("dve", "tensor_copy", [rec_b], [rec0_b], out=rec0_t[0:64, :], in_=rec_t[64:128, :])
                TT("dve", yst[yk][0:64, :], acc_t[0:64, i * 512:(i + 1) * 512], rec0_t[0:64, :], ALU.mult, [acc_b, rec0_b], [yst_b[yk]])
                P.dma("pool", [(YT16[256 + oh * 64:256 + oh * 64 + 64, i * 512:(i + 1) * 512], yst[yk][0:64, :])], yst_b[yk], R=[yst_b[yk]], W=[YTb])

        P.barrier()
        ar.reset()
        xt = [ar.f32(4096).rearrange("p (k n) -> p k n", n=512) for _ in range(2)]
        xt_b = [P.buf("xt%d" % i, True) for i in range(2)]
        yt = [ar.bf(3072).rearrange("p (k n) -> p k n", n=512) for _ in range(2)]
        yt_b = [P.buf("ytC%d" % i, True) for i in range(2)]
        pp = [ar.f32(1024).rearrange("p (k n) -> p k n", n=512) for _ in range(2)]
        pp_b = [P.buf("ppC%d" % i, True) for i in range(2)]
        p16_t = ar.bf(1024).rearrange("p (k n) -> p k n", n=512); p16_b = P.buf("p16")
        sq_t = ar.bf(4096).rearrange("p (k n) -> p k n", n=512); sq_b = P.buf("sq")
        h_t = ar.bf(4096).rearrange("p (k n) -> p k n", n=512); h_b = P.buf("h")
        mg_t = ar.bf(4096).rearrange("p (k n) -> p k n", n=512); mg_b = P.buf("mgC")
        o_t = ar.f32(4096).rearrange("p (k n) -> p k n", n=512); o_b = P.buf("oC")
        ff_t = ar.bf(NFF * 512).rearrange("p (k n) -> p k n", n=512); ff_b = P.buf("ffC")
        rstd_t = ar.f32(512); rstd_b = P.buf("rstd")
        tmp_t = ar.f32(512); tmp_b = P.buf("tmp")
        gs = [ar.f32(512) for _ in range(3)]; gs_b = [P.buf("gs%d" % i) for i in range(3)]
        ma = [ar.f32(512) for _ in range(3)]; ma_b = [P.buf("ma%d" % i) for i in range(3)]
        tt = [ar.f32(512) for _ in range(2)]; tt_b = [P.buf("tt%d" % i) for i in range(2)]
        ring = Ring(6, 2816, "ring")
        prc = {"i": 0}

        def nps():
            k = prc["i"] % 7; prc["i"] += 1
            return k

        def loadxC(t):
            k = t % 2
            P.dma("sp", [(xt[k][:, 0:4, :], xtile_ap(xsrc(l), t)[:, 0:4, :]),
                         (xt[k][:, 4:8, :], xtile_ap(xsrc(l), t)[:, 4:8, :])], xt_b[k], R=[Xb], W=[xt_b[k]])
            P.dma("sp", [(yt[k], YT16[:, t * 512:(t + 1) * 512].rearrange("(k p) n -> p k n", p=128))], yt_b[k], R=[YTb], W=[yt_b[k]])
            P.dma("sp", [(pp[k], pT[l * PLE:(l + 1) * PLE, t * 512:(t + 1) * 512].rearrange("(k p) n -> p k n", p=128))], pp_b[k], W=[pp_b[k]])

        def post_norm_res(x_t, x_b, gcol):
            rms_stats(sq_t, sq_b, rstd_t, rstd_b, tmp_t, tmp_b)
            for m in range(8):
                k = m % 2
                P.op("dve", "scalar_tensor_tensor", [o_b, rstd_b, gains_b], [tt_b[k]], out=tt[k], in0=o_t[:, m, :],
                     scalar=gains_t[:, gcol + m:gcol + m + 1], in1=rstd_t, op0=ALU.mult, op1=ALU.mult)
                TT("pool", x_t[:, m, :], x_t[:, m, :], tt[k], ALU.add, [x_b, tt_b[k]], [x_b])

        def evac_o(ps, m):
            ACT(o_t[:, m, :], psb[ps][:, :], AF.Copy, [PSB[ps]], [o_b])
            ACT(sq_t[:, m, :], psb[ps][:, :], AF.Square, [PSB[ps]], [sq_b])

        loadxC(0)
        for t in range(NT):
            if t + 1 < NT:
                loadxC(t + 1)
            k = t % 2
            x_t = xt[k]; x_b = xt_b[k]; y_t = yt[k]; y_b = yt_b[k]
            pre_norm(x_t, x_b, g0 + 0, sq_t, sq_b, rstd_t, rstd_b, tmp_t, tmp_b, h_t, h_b)
            ychunks = [(0, 2), (2, 1), (3, 3)]
            for m in range(8):
                wb3, wb_b = ring.load(wslab("wBR", l, m), 768, [WB[l]])
                for b in range(3):
                    wg3, wg_b = ring.load(wslab("wG", l, b * 8 + m), 1024, [WB[l]])
                    pg = nps()
                    mm_group(psb[pg][:, :], [(wg3[:, kc, :], h_t[:, kc, :]) for kc in range(8)], [wg_b, h_b], [PSB[pg]])
                    ACT(gs[b], psb[pg][:, :], AF.Sigmoid, [PSB[pg]], [gs_b[b]])
                    c0, ncc = ychunks[b]
                    pq = nps()
                    mm_group(psb[pq][:, :], [(wb3[:, c0 + c, :], y_t[:, c0 + c, :]) for c in range(ncc)], [wb_b, y_b], [PSB[pq]])
                    TT("dve", ma[b], psb[pq][:, :], gs[b], ALU.mult, [PSB[pq], gs_b[b]], [ma_b[b]])
                TT("pool", ma[0], ma[0], ma[1], ALU.add, [ma_b[0], ma_b[1]], [ma_b[0]])
                TT("pool", mg_t[:, m, :], ma[0], ma[2], ALU.add, [ma_b[0], ma_b[2]], [mg_b])
            for m in range(8):
                w3, w_b = ring.load(wslab("wO", l, m), 1024, [WB[l]])
                ps = nps()
                mm_group(psb[ps][:, :], [(w3[:, kc, :], mg_t[:, kc, :]) for kc in range(8)], [w_b, mg_b], [PSB[ps]])
                evac_o(ps, m)
            post_norm_res(x_t, x_b, g0 + 8)
            pre_norm(x_t, x_b, g0 + 16, sq_t, sq_b, rstd_t, rstd_b, tmp_t, tmp_b, h_t, h_b)
            for j in range(NFF):
                wg3, wg_b = ring.load(wslab("wFG", l, j), 1024, [WB[l]])
                pg = nps()
                mm_group(psb[pg][:, :], [(wg3[:, kc, :], h_t[:, kc, :]) for kc in range(8)], [wg_b, h_b], [PSB[pg]])
                kk = j % 3
                ACT(gs[kk], psb[pg][:, :], AF.Silu, [PSB[pg]], [gs_b[kk]])
                wu3, wu_b = ring.load(wslab("wFU", l, j), 1024, [WB[l]])
                pu = nps()
                mm_group(psb[pu][:, :], [(wu3[:, kc, :], h_t[:, kc, :]) for kc in range(8)], [wu_b, h_b], [PSB[pu]])
                TT("dve", ff_t[:, j, :], psb[pu][:, :], gs[kk], ALU.mult, [PSB[pu], gs_b[kk]], [ff_b])
            for m in range(8):
                w3, w_b = ring.load(wslab("wFD", l, m), 2816, [WB[l]])
                ps = nps()
                mm_group(psb[ps][:, :], [(w3[:, j, :], ff_t[:, j, :]) for j in range(NFF)], [w_b, ff_b], [PSB[ps]])
                evac_o(ps, m)
            post_norm_res(x_t, x_b, g0 + 24)
            ACT(h_t, x_t, AF.Copy, [x_b], [h_b])
            P.op("dve", "tensor_copy", [pp_b[k]], [p16_b], out=p16_t, in_=pp[k])
            for m in range(8):
                wg3, wg_b = ring.load(wslab("wPG", l, m), 1024, [WB[l]])
                pg = nps()
                mm_group(psb[pg][:, :], [(wg3[:, kc, :], h_t[:, kc, :]) for kc in range(8)], [wg_b, h_b], [PSB[pg]])
                kk = m % 3
                ACT(gs[kk], psb[pg][:, :], AF.Sigmoid, [PSB[pg]], [gs_b[kk]])
                wp3, wp_b = ring.load(wslab("wPL", l, m), 256, [WB[l]])
                pu = nps()
                mm_group(psb[pu][:, :], [(wp3[:, kc, :], p16_t[:, kc, :]) for kc in range(2)], [wp_b, p16_b], [PSB[pu]])
                TT("dve", o_t[:, m, :], psb[pu][:, :], gs[kk], ALU.mult, [PSB[pu], gs_b[kk]], [o_b])
                ACT(sq_t[:, m, :], o_t[:, m, :], AF.Square, [o_b], [sq_b])
            post_norm_res(x_t, x_b, g0 + 32)
            P.dma("pool", [(xtile_ap(xdst(l), t)[:, 0:4, :], x_t[:, 0:4, :]), (xtile_ap(xdst(l), t)[:, 4:8, :], x_t[:, 4:8, :])],
                  x_b, R=[x_b], W=[Xb])
    P.barrier()
    block = es.enter_context(nc.Block())
    P.replay(block)
    es.close()
    return nc


def _slabs(w, cols_list, kc):
    out = np.empty((len(cols_list), 128, kc, 128), np.float32)
    wk = w.reshape(kc, 128, w.shape[1])
    for m, cols in enumerate(cols_list):
        out[m] = wk[:, :, cols].transpose(1, 0, 2)
    return out.reshape(len(cols_list) * 128, kc * 128)


def _host_weights(inp, L):
    r = {}
    ar_ = np.arange
    lists = {n: [] for n in ("wA", "wV", "wF", "wG", "wBR", "wO", "wFG", "wFU", "wFD", "wPL", "wPG")}
    for l in range(L):
        w_in = np.asarray(inp["w_in"][l], np.float32)
        colsA = []
        for base in (0, 1024):
            for pt in range(8):
                c = base + pt * 128 + ar_(128)
                colsA.append(c)
                if pt >= 2:
                    sw = base + pt * 128 + (ar_(128) // 64) * 64 + (ar_(128) % 64 + 32) % 64
                    colsA.append(sw)
        lists["wA"].append(_slabs(w_in, colsA, 8))
        wv = w_in[:, 2048:3072].reshape(8, 128, 1024).transpose(1, 0, 2).reshape(128, 8192)
        lists["wV"].append(wv)
        wf = w_in[:, 3072:3076].reshape(8, 128, 4).transpose(1, 0, 2).reshape(128, 32)
        lists["wF"].append(wf)
        lists["wG"].append(_slabs(w_in, [3076 + b * 1024 + m * 128 + ar_(128) for b in range(3) for m in range(8)], 8))
        wbr = np.concatenate([np.asarray(inp["w_br_a"][l]), np.asarray(inp["w_br_b"][l]), np.asarray(inp["w_br_c"][l])], axis=0)
        lists["wBR"].append(_slabs(wbr.astype(np.float32), [m * 128 + ar_(128) for m in range(8)], 6))
        lists["wO"].append(_slabs(np.asarray(inp["w_out"][l], np.float32), [m * 128 + ar_(128) for m in range(8)], 8))
        lists["wFG"].append(_slabs(np.asarray(inp["w_ffn_gate"][l], np.float32), [m * 128 + ar_(128) for m in range(NFF)], 8))
        lists["wFU"].append(_slabs(np.asarray(inp["w_ffn_up"][l], np.float32), [m * 128 + ar_(128) for m in range(NFF)], 8))
        lists["wFD"].append(_slabs(np.asarray(inp["w_ffn_down"][l], np.float32), [m * 128 + ar_(128) for m in range(8)], NFF))
        lists["wPL"].append(_slabs(np.asarray(inp["w_ple"][l], np.float32), [m * 128 + ar_(128) for m in range(8)], 2))
        lists["wPG"].append(_slabs(np.asarray(inp["w_ple_gate"][l], np.float32), [m * 128 + ar_(128) for m in range(8)], 8))
    for n, v in lists.items():
        r[n] = np.ascontiguousarray(np.concatenate(v, axis=0), dtype=np.float32)
    gl = []
    for l in range(L):
        for n in ("g_mix_pre", "g_mix_post", "g_ffn_pre", "g_ffn_post", "g_ple_post"):
            gl.append(np.asarray(inp[n][l], np.float32).reshape(8, 128).T)
    r["gains"] = np.ascontiguousarray(np.concatenate(gl, axis=1), dtype=np.float32)
    r["bfb"] = np.ascontiguousarray(np.broadcast_to(np.asarray(inp["b_f"], np.float32).reshape(1, L * 4), (128, L * 4)))
    return r


def _consts(S):
    c = {}
    inv = (1.0 / (np.float32(10000.0) ** (np.arange(0, 64, 2, dtype=np.float32) / np.float32(64)))).astype(np.float32)
    ang = (np.arange(S, dtype=np.float32)[:, None] * inv[None, :]).astype(np.float32)
    cos = np.cos(ang.astype(np.float64)).astype(np.float32).T
    sin = np.sin(ang.astype(np.float64)).astype(np.float32).T
    c["cosT"] = np.ascontiguousarray(np.concatenate([cos, cos, cos, cos], axis=0))
    c["sinT"] = np.ascontiguousarray(np.concatenate([-sin, sin, -sin, sin], axis=0))
    p = np.arange(128)
    c["cmask"] = np.ascontiguousarray(np.concatenate([(p[:, None] >= p[None, :]), (p[:, None] <= p[None, :])], axis=1).astype(np.float32))
    c["trif"] = np.ascontiguousarray((p[:, None] <= p[None, :]).astype(np.float32))
    c["identf"] = np.eye(128, dtype=np.float32)
    c["blkind"] = np.ascontiguousarray(np.concatenate([(np.arange(S)[None, :] // 256 == np.arange(32)[:, None]), np.ones((1, S), bool)], axis=0).astype(np.float32))
    return c


_NC_CACHE = {}


def kernel(**inputs):
    x = np.asarray(inputs["x"], np.float32)
    p = np.asarray(inputs["p"], np.float32)
    B, S, _ = x.shape
    L = p.shape[0]
    key = (S, L)
    if key not in _NC_CACHE:
        _NC_CACHE[key] = build(S, L)
    nc = _NC_CACHE[key]
    shared = _host_weights(inputs, L)
    shared.update(_consts(S))
    in_maps = []
    for b in range(B):
        m = dict(shared)
        m["xT"] = np.ascontiguousarray(x[b].T)
        m["pT"] = np.ascontiguousarray(p[:, b].transpose(0, 2, 1).reshape(L * PLE, S))
        in_maps.append(m)
    res = run_bass_kernel_spmd(nc, in_maps, core_ids=list(range(B)))
    out = np.stack([np.ascontiguousarray(res.results[b]["outT"].T) for b in range(B)], axis=0)
    return out.astype(np.float32)
```

```python
import numpy as np
from contextlib import ExitStack
import concourse.bass as bass
import concourse.mybir as mybir
from concourse.bass_utils import run_bass_kernel_spmd

F32 = mybir.dt.float32
BF = mybir.dt.bfloat16
AF = mybir.ActivationFunctionType
ALU = mybir.AluOpType
AX = mybir.AxisListType

D = 1024
HD = 64
PLE = 256
DFF = 2816
NFF = 22
BIG = 30000.0
DIL = (1, 4, 16)
SAME_ENGINE_SYNC = True


class Buf:
    def __init__(self, name, sem=None):
        self.name = name
        self.w = {}
        self.r = {}
        self.sem = sem
        self.cnt = 0


class Eng:
    def __init__(self, name, sem):
        self.name = name
        self.sem = sem
        self.cnt = 0
        self.seen = {}
        self.prog = []


class Prog:
    def __init__(self, nc, es):
        self.nc = nc
        self.es = es
        self.sems = []
        self.E = {}
        for n in ("pe", "act", "dve", "pool"):
            self.E[n] = Eng(n, self.newsem(n))
        self.E["sp"] = Eng("sp", None)
        self.bufs = []
        self.bynames = {}

    def newsem(self, name):
        h = self.es.enter_context(self.nc.semaphore("s_" + name))
        self.sems.append(h)
        return len(self.sems) - 1

    def buf(self, name, dma=False):
        if name in self.bynames:
            return self.bynames[name]
        b = Buf(name, self.newsem(name) if dma else None)
        self.bufs.append(b)
        self.bynames[name] = b
        return b

    def _waits(self, X, R, W):
        need = {}
        for b in R:
            for k, v in b.w.items():
                need[k] = max(need.get(k, 0), v)
        for b in W:
            for k, v in b.w.items():
                need[k] = max(need.get(k, 0), v)
            for k, v in b.r.items():
                need[k] = max(need.get(k, 0), v)
        out = []
        for k, v in need.items():
            if k == X.sem and (X.name == "pe" or not SAME_ENGINE_SYNC):
                continue
            if X.seen.get(k, 0) >= v:
                continue
            X.seen[k] = v
            out.append((k, v))
        return out

    def op(self, eng, name, R=(), W=(), inc=True, args=(), **kw):
        X = self.E[eng]
        waits = self._waits(X, R, W)
        tok = X.cnt + 1
        if inc:
            X.cnt = tok
        X.prog.append((waits, ("op", name, args, kw), inc))
        for b in R:
            b.r[X.sem] = tok
        for b in W:
            b.w = {X.sem: tok}
            b.r = {}

    def dma(self, q, pairs, sb, R=(), W=()):
        X = self.E[q]
        waits = self._waits(X, R, W)
        first = True
        for o, i in pairs:
            sb.cnt += 16
            X.prog.append((waits if first else [], ("dma", o, i, sb.sem), False))
            first = False
        for b in R:
            b.r[sb.sem] = sb.cnt
        for b in W:
            b.w = {sb.sem: sb.cnt}
            b.r = {}

    def barrier(self):
        toks = {}
        for n in ("pe", "act", "dve", "pool"):
            toks[self.E[n].sem] = self.E[n].cnt
        for b in self.bufs:
            if b.sem is not None and b.cnt > 0:
                toks[b.sem] = b.cnt
        for n, X in self.E.items():
            waits = []
            for k, v in toks.items():
                if v > 0 and X.seen.get(k, 0) < v and not (k == X.sem and n == "pe"):
                    X.seen[k] = v
                    waits.append((k, v))
            X.prog.append((waits, None, False))

    def replay(self, block):
        sems = self.sems

        def run(X, e):
            for waits, fn, inc in X.prog:
                for k, v in waits:
                    e.wait_ge(sems[k], v)
                if fn is None:
                    continue
                if fn[0] == "dma":
                    _, o, i, k = fn
                    e.dma_start(out=o, in_=i).then_inc(sems[k], 16)
                else:
                    _, name, args, kw = fn
                    ins = getattr(e, name)(*args, **kw)
                    if inc:
                        ins.then_inc(sems[X.sem], 1)

        @block.sync
        def _(e):
            run(self.E["sp"], e)

        @block.tensor
        def _(e):
            run(self.E["pe"], e)

        @block.scalar
        def _(e):
            run(self.E["act"], e)

        @block.vector
        def _(e):
            run(self.E["dve"], e)

        @block.gpsimd
        def _(e):
            run(self.E["pool"], e)


class Arena:
    def __init__(self, ap, lo, hi):
        self.ap = ap
        self.lo = lo
        self.hi = hi
        self.p = lo

    def f32(self, n):
        a = self.ap[:, self.p:self.p + n]
        self.p += n
        assert self.p <= self.hi, ("arena overflow", self.p, self.hi)
        return a

    def bf(self, n):
        n2 = (n + 1) // 2
        a = self.ap[:, self.p:self.p + n2].bitcast(BF)
        self.p += n2
        assert self.p <= self.hi, ("arena overflow", self.p, self.hi)
        return a[:, 0:n]

    def reset(self):
        self.p = self.lo


def build(S=8192, L=2, dbg=False):
    NT = S // 512
    NB = S // 128
    NMB = S // 256
    CW = min(2048, S)
    assert NMB <= 32
    nc = bass.Bass("TRN2", target_bir_lowering=False)

    def din(name, shape, dt=F32):
        return nc.dram_tensor(name, shape, dt, kind="ExternalInput").ap()

    def dscr(name, shape, dt=BF):
        return nc.dram_tensor(name, shape, dt, kind=("ExternalOutput" if dbg else "Internal")).ap()

    xT = din("xT", [D, S])
    pT = din("pT", [L * PLE, S])
    wspec = [("wA", 28, 1024), ("wV", 1, 8192), ("wF", 1, 32), ("wG", 24, 1024), ("wBR", 8, 768),
             ("wO", 8, 1024), ("wFG", 22, 1024), ("wFU", 22, 1024), ("wFD", 8, 2816),
             ("wPL", 8, 256), ("wPG", 8, 1024)]
    w32 = {}
    w16 = {}
    for n, ns, nc_ in wspec:
        w32[n] = din(n, [L * ns * 128, nc_])
        w16[n] = nc.dram_tensor(n + "_16", [L * ns * 128, nc_], BF, kind="Internal").ap()
    wns = {n: ns for n, ns, _ in wspec}
    gains = din("gains", [128, L * 5 * 8])
    bfb = din("bfb", [128, L * 4])
    cosT = din("cosT", [128, S])
    sinT = din("sinT", [128, S])
    cmask32 = din("cmask", [128, 256])
    trif = din("trif", [128, 128])
    identf = din("identf", [128, 128])
    blkind32 = din("blkind", [33, S])
    outT = nc.dram_tensor("outT", [D, S], F32, kind="ExternalOutput").ap()

    X32 = dscr("X32", [D, S], F32)
    QT16 = dscr("QT16", [1024, S])
    KT16 = dscr("KT16", [1024, S])
    V16 = dscr("V16", [S, 1024])
    YT16 = dscr("YT16", [768, S])
    KS32 = dscr("KS32", [384, 32], F32)
    BI16 = nc.dram_tensor("BI16", [33, S], BF, kind="Internal").ap()

    es = ExitStack()
    P = Prog(nc, es)
    AW = 47104
    arena_t = es.enter_context(nc.sbuf_tensor("arena", [128, AW], F32))
    PERS = 2048
    pers = Arena(arena_t, 0, PERS)
    ar = Arena(arena_t, PERS, AW)
    psb = [es.enter_context(nc.psum_tensor("psb%d" % i, [128, 512], F32)) for i in range(8)]
    PSB = [P.buf("psb%d" % i) for i in range(8)]

    def ACT(out, in_, func, R, W, **kw):
        P.op("act", "activation", R, W, out=out, in_=in_, func=func, **kw)

    def TT(eng, out, in0, in1, op, R, W):
        P.op(eng, "tensor_tensor", R, W, out=out, in0=in0, in1=in1, op=op)

    def MM(out, lhsT, rhs, start, stop, R, W, inc=True):
        P.op("pe", "matmul", R, W, inc, args=(out,), lhsT=lhsT, rhs=rhs, start=start, stop=stop)

    def mm_group(out_ap, pairs, Rb, Wb):
        n = len(pairs)
        for i, (lt, rh) in enumerate(pairs):
            MM(out_ap, lt, rh, i == 0, i == n - 1, Rb, Wb, inc=(i == n - 1))

    gains_t = pers.f32(L * 40); gains_b = P.buf("gains", True)
    bfb_t = pers.f32(L * 4); bfb_b = P.buf("bfb", True)
    trif_t = pers.f32(128); trif_b = P.buf("trif", True)
    identf_t = pers.f32(128); identf_b = P.buf("identf", True)
    onesf_t = pers.f32(128); onesf_b = P.buf("onesf")
    ones16_t = pers.bf(128); ones16_b = P.buf("ones16")
    cmask_t = pers.bf(256); cmask_b = P.buf("cmask", True)
    eps_t = pers.f32(1); one_t = pers.f32(1); cst_b = P.buf("cst")
    cpos_t = pers.f32(NB * 4).rearrange("p (j h) -> p j h", h=4); cpos_b = P.buf("cpos")
    tall_t = pers.f32((NB + 1) * 4).rearrange("p (j h) -> p j h", h=4); tall_b = P.buf("tall")
    ksum_t = pers.f32(3 * 32).rearrange("p (a n) -> p a n", n=32); ksum_b = P.buf("ksum", True)

    P.dma("sp", [(gains_t, gains)], gains_b, W=[gains_b])
    P.dma("sp", [(bfb_t, bfb)], bfb_b, W=[bfb_b])
    P.dma("sp", [(trif_t, trif)], trif_b, W=[trif_b])
    P.dma("sp", [(identf_t, identf)], identf_b, W=[identf_b])
    P.dma("pool", [(cmask_t, cmask32)], cmask_b, W=[cmask_b])
    P.op("dve", "memset", (), [onesf_b], args=(onesf_t, 1.0))
    P.op("dve", "memset", (), [ones16_b], args=(ones16_t, 1.0))
    P.op("dve", "memset", (), [cst_b], args=(eps_t, 1e-6))
    P.op("dve", "memset", (), [cst_b], args=(one_t, 1.0))
    P.op("dve", "memset", (), [tall_b], args=(tall_t[:, 0, :], 0.0))
    P.op("dve", "memset", (), [ksum_b], args=(ksum_t, 0.0))

    WB = [P.buf("w16_%d" % l) for l in range(L)]
    CB = [P.buf("cast%d" % i, True) for i in range(2)]
    BIb = P.buf("bi16", True)
    P.dma("pool", [(BI16[:, c0:c0 + CW], blkind32[:, c0:c0 + CW]) for c0 in range(0, S, CW)], BIb, W=[BIb])
    cg = 0
    for l in range(L):
        pairs = []
        for n, ns, ncol in wspec:
            for s_ in range(ns):
                r0 = (l * ns + s_) * 128
                cw = 2048 if ncol > 2816 else ncol
                for c0 in range(0, ncol, cw):
                    pairs.append((w16[n][r0:r0 + 128, c0:c0 + cw], w32[n][r0:r0 + 128, c0:c0 + cw]))
        for g0_ in range(0, len(pairs), 8):
            cb = CB[cg % 2]; cg += 1
            P.dma("pool", pairs[g0_:g0_ + 8], cb, W=[cb])
        WB[l].w = {CB[0].sem: CB[0].cnt, CB[1].sem: CB[1].cnt}

    def wslab(n, l, s_):
        r0 = (l * wns[n] + s_) * 128
        return w16[n][r0:r0 + 128, :]

    Xb = P.buf("X32d"); QTb = P.buf("QTd"); KTb = P.buf("KTd"); Vb = P.buf("Vd"); YTb = P.buf("YTd"); KSb = P.buf("KSd")

    def xsrc(l):
        return xT if l == 0 else X32

    def xdst(l):
        return outT if l == L - 1 else X32

    def xtile_ap(dram, t):
        return dram[:, t * 512:(t + 1) * 512].rearrange("(kc p) n -> p kc n", p=128)

    class Ring:
        def __init__(self, n, words, name):
            self.t = [ar.bf(words) for _ in range(n)]
            self.b = [P.buf("%s%d" % (name, i), True) for i in range(n)]
            self.i = 0

        def load(self, dram_ap, ncol, Rb):
            k = self.i % len(self.t)
            self.i += 1
            P.dma("sp", [(self.t[k][:, 0:ncol], dram_ap)], self.b[k], R=Rb, W=[self.b[k]])
            return self.t[k][:, 0:ncol].rearrange("p (k n) -> p k n", n=128), self.b[k]

    def rms_stats(sq_t, sq_b, rstd_t, rstd_b, tmp_t, tmp_b):
        mm_group(psb[7][:, :], [(ones16_t, sq_t[:, kc, :]) for kc in range(8)], [ones16_b, sq_b], [PSB[7]])
        ACT(tmp_t, psb[7][:, :], AF.Sqrt, [PSB[7], cst_b], [tmp_b], bias=eps_t, scale=1.0 / D)
        P.op("dve", "reciprocal", [tmp_b], [rstd_b], out=rstd_t, in_=tmp_t)

    def pre_norm(x_t, x_b, gcol, sq_t, sq_b, rstd_t, rstd_b, tmp_t, tmp_b, h_t, h_b):
        ACT(sq_t, x_t, AF.Square, [x_b], [sq_b])
        rms_stats(sq_t, sq_b, rstd_t, rstd_b, tmp_t, tmp_b)
        for kc in range(8):
            P.op("dve", "scalar_tensor_tensor", [x_b, rstd_b, gains_b], [h_b], out=h_t[:, kc, :], in0=x_t[:, kc, :],
                 scalar=gains_t[:, gcol + kc:gcol + kc + 1], in1=rstd_t, op0=ALU.mult, op1=ALU.mult)

    for l in range(L):
        g0 = l * 40
        P.barrier()
        ar.reset()
        xt = [ar.f32(4096).rearrange("p (k n) -> p k n", n=512) for _ in range(2)]
        xt_b = [P.buf("xt%d" % i, True) for i in range(2)]
        sq_t = ar.bf(4096).rearrange("p (k n) -> p k n", n=512); sq_b = P.buf("sq")
        h_t = ar.bf(4096).rearrange("p (k n) -> p k n", n=512); h_b = P.buf("h")
        rstd_t = ar.f32(512); rstd_b = P.buf("rstd")
        tmp_t = ar.f32(512); tmp_b = P.buf("tmp")
        wv_t = ar.bf(8192).rearrange("p (k n) -> p k n", n=1024); wv_b = P.buf("wvA", True)
        wf_t = ar.bf(32).rearrange("p (k n) -> p k n", n=4); wf_b = P.buf("wfA", True)
        ring = Ring(6, 1024, "ring")
        cs_t = [(ar.f32(512), ar.f32(512)) for _ in range(2)]
        cs_b = [P.buf("csA%d" % i, True) for i in range(2)]
        qst = [ar.bf(4096).rearrange("p (k n) -> p k n", n=512) for _ in range(2)]
        qst_b = [P.buf("qst%d" % i, True) for i in range(2)]
        kst = [ar.bf(4096).rearrange("p (k n) -> p k n", n=512) for _ in range(2)]
        kst_b = [P.buf("kst%d" % i, True) for i in range(2)]
        vst = [ar.bf(4096).rearrange("p (a n) -> p a n", n=1024) for _ in range(2)]
        vst_b = [P.buf("vst%d" % i, True) for i in range(2)]
        r1 = [ar.f32(512) for _ in range(2)]; r1_b = [P.buf("r1_%d" % i) for i in range(2)]
        r2 = [ar.f32(512) for _ in range(2)]; r2_b = [P.buf("r2_%d" % i) for i in range(2)]
        fb_t = ar.f32(4); fb_b = P.buf("fbA")
        fe_t = ar.f32(4); fe_b = P.buf("feA")
        fl_t = ar.f32(4); fl_b = P.buf("flA")

        P.dma("sp", [(wv_t[:, kc, :], wslab("wV", l, 0)[:, kc * 1024:(kc + 1) * 1024]) for kc in range(8)],
              wv_b, R=[WB[l]], W=[wv_b])
        P.dma("sp", [(wf_t, wslab("wF", l, 0).rearrange("p (k n) -> p k n", n=4))], wf_b, R=[WB[l]], W=[wf_b])

        def loadxA(t):
            k = t % 2
            P.dma("sp", [(xt[k][:, 0:4, :], xtile_ap(xsrc(l), t)[:, 0:4, :]),
                         (xt[k][:, 4:8, :], xtile_ap(xsrc(l), t)[:, 4:8, :])], xt_b[k], R=[Xb], W=[xt_b[k]])
            P.dma("sp", [(cs_t[k][0], cosT[:, t * 512:(t + 1) * 512]),
                         (cs_t[k][1], sinT[:, t * 512:(t + 1) * 512])], cs_b[k], W=[cs_b[k]])

        loadxA(0)
        psrot = 0
        for t in range(NT):
            if t + 1 < NT:
                loadxA(t + 1)
            x_t = xt[t % 2]; x_b = xt_b[t % 2]
            cos_t, sin_t = cs_t[t % 2]; c_b = cs_b[t % 2]
            pre_norm(x_t, x_b, g0 + 0, sq_t, sq_b, rstd_t, rstd_b, tmp_t, tmp_b, h_t, h_b)
            qs = qst[t % 2]; qs_b = qst_b[t % 2]; ks = kst[t % 2]; ks_b = kst_b[t % 2]
            si = 0
            for which in range(2):
                stg, stg_b = (qs, qs_b) if which == 0 else (ks, ks_b)
                for pt in range(8):
                    w3, w_b = ring.load(wslab("wA", l, si), 1024, [WB[l]]); si += 1
                    pa = psrot % 6; psrot += 1
                    mm_group(psb[pa][:, :], [(w3[:, kc, :], h_t[:, kc, :]) for kc in range(8)], [w_b, h_b], [PSB[pa]])
                    if pt < 2:
                        ACT(stg[:, pt, :], psb[pa][:, :], AF.Copy, [PSB[pa]], [stg_b])
                    else:
                        w23, w2_b = ring.load(wslab("wA", l, si), 1024, [WB[l]]); si += 1
                        pb = psrot % 6; psrot += 1
                        mm_group(psb[pb][:, :], [(w23[:, kc, :], h_t[:, kc, :]) for kc in range(8)], [w2_b, h_b], [PSB[pb]])
                        ri = (pt + which) % 2
                        TT("dve", r1[ri], psb[pa][:, :], cos_t, ALU.mult, [PSB[pa], c_b], [r1_b[ri]])
                        TT("dve", r2[ri], psb[pb][:, :], sin_t, ALU.mult, [PSB[pb], c_b], [r2_b[ri]])
                        TT("pool", stg[:, pt, :], r1[ri], r2[ri], ALU.add, [r1_b[ri], r2_b[ri]], [stg_b])
                        if which == 1 and pt >= 5:
                            P.op("dve", "tensor_reduce", [stg_b], [ksum_b], out=ksum_t[:, pt - 5, 2 * t:2 * t + 2],
                                 in_=stg[:, pt, :].rearrange("p (a b) -> p a b", b=256), axis=AX.X, op=ALU.add)
            P.dma("pool", [(QT16[:, t * 512:(t + 1) * 512].rearrange("(k p) n -> p k n", p=128), qs)], qs_b, R=[qs_b], W=[QTb])
            P.dma("pool", [(KT16[:, t * 512:(t + 1) * 512].rearrange("(k p) n -> p k n", p=128), ks)], ks_b, R=[ks_b], W=[KTb])
            vs = vst[t % 2]; vs_b = vst_b[t % 2]
            for tb in range(4):
                for hf in range(2):
                    pa = psrot % 6; psrot += 1
                    mm_group(psb[pa][:, :], [(h_t[:, kc, tb * 128:(tb + 1) * 128], wv_t[:, kc, hf * 512:(hf + 1) * 512]) for kc in range(8)],
                             [wv_b, h_b], [PSB[pa]])
                    ACT(vs[:, tb, hf * 512:(hf + 1) * 512], psb[pa][:, :], AF.Copy, [PSB[pa]], [vs_b])
                j = 4 * t + tb
                mm_group(psb[6][:, 0:4], [(h_t[:, kc, tb * 128:(tb + 1) * 128], wf_t[:, kc, :]) for kc in range(8)], [wf_b, h_b], [PSB[6]])
                TT("dve", fb_t, psb[6][:, 0:4], bfb_t[:, l * 4:l * 4 + 4], ALU.add, [PSB[6], bfb_b], [fb_b])
                ACT(fe_t, fb_t, AF.Exp, [fb_b], [fe_b], scale=-1.0)
                ACT(fl_t, fe_t, AF.Ln, [fe_b, cst_b], [fl_b], bias=one_t, scale=1.0)
                mm_group(psb[6][:, 8:12], [(trif_t, fl_t)], [trif_b, fl_b], [PSB[6]])
                mm_group(psb[6][:, 16:20], [(onesf_t, fl_t)], [onesf_b, fl_b], [PSB[6]])
                TT("dve", cpos_t[:, j, :], psb[6][:, 8:12], tall_t[:, j, :], ALU.add, [PSB[6], tall_b], [cpos_b])
                TT("dve", tall_t[:, j + 1, :], psb[6][:, 16:20], tall_t[:, j, :], ALU.add, [PSB[6], tall_b], [tall_b])
            P.dma("pool", [(V16[t * 512:(t + 1) * 512, :].rearrange("(a p) c -> p a c", p=128), vs)], vs_b, R=[vs_b], W=[Vb])
        P.dma("pool", [(KS32.rearrange("(a p) n -> p a n", p=128), ksum_t)], ksum_b, R=[ksum_b], W=[KSb])

        P.barrier()
        ar.reset()
        KP = [ar.bf(S) for _ in range(2)]; KP_b = [P.buf("KP%d" % i, True) for i in range(2)]
        QP = [ar.bf(S) for _ in range(2)]; QP_b = [P.buf("QP%d" % i, True) for i in range(2)]
        QA_b = [[P.buf("QA%d_%d" % (i, t)) for t in range(NT)] for i in range(2)]
        VA = [ar.bf(NB * 128).rearrange("p (j c) -> p j c", c=128) for _ in range(2)]
        VA_b = [P.buf("VA%d" % i, True) for i in range(2)]
        pt_t = [ar.bf(512) for _ in range(4)]; pt_b = [P.buf("pT%d" % i) for i in range(4)]
        rec_t = ar.f32(512); rec_b = P.buf("rec")
        rec0_t = ar.f32(512); rec0_b = P.buf("rec0")
        yst = [ar.bf(512) for _ in range(2)]; yst_b = [P.buf("yst%d" % i, True) for i in range(2)]
        ks16_t = ar.bf(32); ks16_b = P.buf("ks16")
        ks32_t = ar.f32(32); ks32_b = P.buf("ks32", True)
        wk_t = ar.f32(128).rearrange("p (a n) -> p a n", n=32); wk_b = P.buf("wk")
        t8_t = ar.f32(32).rearrange("p (a n) -> p a n", n=8); t8_b = P.buf("t8")
        sb_t = ar.f32(128).rearrange("p (a n) -> p a n", n=32); sb_b = P.buf("selb")
        acc_t = ar.f32(S); acc_b = P.buf("acc")
        for i in range(2):
            P.op("pool", "memset", (), [VA_b[i]], args=(VA[i][:, :, 64:128], 1.0))
        cnt = {"ps": 0, "po": 0, "pt": 0, "y": 0, "kq": 0, "va": 0}

        def load_rows(dst, dst_b, dram, r0, nr, rb, ind=False):
            pairs = [(dst[0:nr, c0:c0 + CW], dram[r0:r0 + nr, c0:c0 + CW]) for c0 in range(0, S, CW)]
            Rb = [rb]
            if ind == 1:
                pairs += [(dst[64:96, c0:c0 + CW], BI16[0:32, c0:c0 + CW]) for c0 in range(0, S, CW)]
                Rb = [rb, BIb]
            if ind == 2:
                pairs += [(dst[64:65, c0:c0 + CW], BI16[32:33, c0:c0 + CW]) for c0 in range(0, S, CW)]
                Rb = [rb, BIb]
            P.dma("sp", pairs, dst_b, R=Rb, W=[dst_b])

        def load_va(k, head, d):
            nbd = NB // d
            pairs = []
            for r in range(d):
                for b0 in range(0, nbd, 8):
                    nb_ = min(8, nbd - b0)
                    src = V16[r + d * 128 * b0: r + d * 128 * b0 + d * (128 * nb_ - 1) + 1: d, head * 64:(head + 1) * 64]
                    pairs.append((VA[k][:, r * nbd + b0: r * nbd + b0 + nb_, 0:64], src.rearrange("(j p) c -> p j c", p=128)))
            for g_ in range(0, len(pairs), 4):
                P.dma("sp", pairs[g_:g_ + 4], VA_b[k], R=[Vb], W=[VA_b[k]])

        def finalize(po, yrow, i):
            yk = cnt["y"] % 2; cnt["y"] += 1
            P.op("dve", "reciprocal", [PSB[po]], [rec_b], out=rec_t[64:128, :], in_=psb[po][64:128, :])
            TT("dve", yst[yk][0:64, :], psb[po][0:64, :], rec_t[64:128, :], ALU.mult, [PSB[po], rec_b], [yst_b[yk]])
            P.dma("pool", [(YT16[yrow:yrow + 64, i * 512:(i + 1) * 512], yst[yk][0:64, :])], yst_b[yk], R=[yst_b[yk]], W=[YTb])

        LA = 2

        def run_pipe(items):
            n = len(items)
            for idx in range(n + LA):
                if idx < n:
                    items[idx][0]()
                if idx >= LA:
                    items[idx - LA][1]()
                    items[idx - LA][2]()

        def attn_items(Kt, K_b, Qt, Qbufs, r0, nr, va, va_b, bias_fn, i, yrow, hooks):
            items = []
            po = 4 + cnt["po"] % 2; cnt["po"] += 1
            nj = 4 * i + 4
            for j in range(nj):
                off = max(0, j - 4 * i) * 128
                ps = cnt["ps"] % 3; cnt["ps"] += 1
                pk = cnt["pt"] % 4; cnt["pt"] += 1

                def f_score(j=j, off=off, ps=ps):
                    if j in hooks:
                        hooks[j]()
                    mm_group(psb[ps][:, off:512], [(Kt[r0:r0 + nr, j * 128:(j + 1) * 128], Qt[r0:r0 + nr, i * 512 + off:(i + 1) * 512])],
                             [K_b] + Qbufs, [PSB[ps]])

                def f_soft(j=j, off=off, ps=ps, pk=pk):
                    if bias_fn is None:
                        ACT(pt_t[pk][:, off:512], psb[ps][:, off:512], AF.Exp, [PSB[ps]], [pt_b[pk]], scale=0.125)
                    else:
                        ACT(pt_t[pk][:, off:512], psb[ps][:, off:512], AF.Exp, [PSB[ps], cpos_b], [pt_b[pk]], bias=bias_fn(j, i), scale=0.125)
                    if j >= 4 * i:
                        TT("pool", pt_t[pk][:, off:off + 128], pt_t[pk][:, off:off + 128], cmask_t[:, 128:256], ALU.mult,
                           [pt_b[pk], cmask_b], [pt_b[pk]])

                def f_pv(j=j, off=off, pk=pk):
                    MM(psb[po][:, off:512], va[:, j, :], pt_t[pk][:, off:512], j == 0, j == nj - 1, [va_b, pt_b[pk]], [PSB[po]], inc=True)
                    if j == nj - 1:
                        finalize(po, yrow, i)
                items.append((f_score, f_soft, f_pv))
            return items

        for h in range(4):
            kq = cnt["kq"] % 2; cnt["kq"] += 1
            load_rows(KP[kq], KP_b[kq], KT16, h * 64, 64, KTb, ind=2)
            load_rows(QP[kq], QP_b[kq], QT16, h * 64, 64, QTb)
            vk = cnt["va"] % 2; cnt["va"] += 1
            load_va(vk, h, 1)
            items = []

            def mkpre(i, h=h, kq=kq):
                def pre():
                    for qb in range(4):
                        P.op("pe", "transpose", [cpos_b, identf_b], [PSB[7]], qb == 3, out=psb[7][0:1, qb * 128:(qb + 1) * 128],
                             in_=cpos_t[:, 4 * i + qb, h:h + 1], identity=identf_t)
                    P.op("dve", "tensor_scalar", [PSB[7]], [QA_b[kq][i]], out=QP[kq][64:65, i * 512:(i + 1) * 512], in0=psb[7][0:1, :],
                         scalar1=-8.0, scalar2=None, op0=ALU.mult)
                return pre
            mkpre(0)()
            for i in range(NT):
                hooks = {0: mkpre(i + 1)} if i + 1 < NT else {}
                items += attn_items(KP[kq], KP_b[kq], QP[kq], [QP_b[kq], QA_b[kq][i]], 0, 65, VA[vk], VA_b[vk],
                                    (lambda h: lambda j, i: cpos_t[:, j, h:h + 1])(h), i, h * 64, hooks)
            run_pipe(items)
        for m in range(6):
            hd = 10 + m
            kq = cnt["kq"] % 2; cnt["kq"] += 1
            load_rows(KP[kq], KP_b[kq], KT16, hd * 64, 64, KTb, ind=1)
            load_rows(QP[kq], QP_b[kq], QT16, hd * 64, 64, QTb)
            P.dma("sp", [(ks32_t[0:64, :], KS32[m * 64:(m + 1) * 64, :])], ks32_b, R=[KSb], W=[ks32_b])
            P.op("dve", "tensor_copy", [ks32_b], [ks16_b], out=ks16_t[0:64, :], in_=ks32_t[0:64, :])
            vk = cnt["va"] % 2; cnt["va"] += 1
            load_va(vk, hd, 1)
            Kt = KP[kq]; Qt = QP[kq]
            items = []

            def mkpre1(i, kq=kq, Qt=Qt):
                def pre():
                    for qb in range(4):
                        q0 = i * 512 + qb * 128
                        mm_group(psb[6][:, qb * 32:qb * 32 + NMB], [(Qt[0:64, q0:q0 + 128], ks16_t[0:64, 0:NMB])], [QP_b[kq], ks16_b], [PSB[6]])
                    P.op("dve", "memset", (), [wk_b], args=(wk_t, -1e30))
                    P.op("dve", "memset", (), [sb_b], args=(sb_t, -1.0))
                    for hq in range(2):
                        own = 2 * i + hq
                        if own > 3:
                            P.op("dve", "tensor_copy", [PSB[6]], [wk_b], out=wk_t[:, 2 * hq:2 * hq + 2, 0:own],
                                 in_=psb[6][:, 64 * hq:64 * hq + 64].rearrange("p (a n) -> p a n", n=32)[:, :, 0:own])
                        for qq in range(2):
                            qb = 2 * hq + qq
                            if own > 3:
                                P.op("dve", "max", [wk_b], [t8_b], out=t8_t[:, qb, :], in_=wk_t[:, qb, 0:max(own, 8)])
                                P.op("dve", "tensor_scalar", [wk_b, t8_b], [sb_b], out=sb_t[:, qb, 0:own], in0=wk_t[:, qb, 0:own],
                                     scalar1=t8_t[:, qb, 2:3], scalar2=1.0, op0=ALU.is_ge, op1=ALU.subtract)
                                P.op("dve", "memset", (), [sb_b], args=(sb_t[:, qb, own:own + 1], 0.0))
                            else:
                                P.op("dve", "memset", (), [sb_b], args=(sb_t[:, qb, 0:own + 1], 0.0))
                    P.op("dve", "tensor_scalar", [sb_b], [sb_b], out=sb_t, in0=sb_t, scalar1=BIG, scalar2=None, op0=ALU.mult)
                return pre

            def mkpre2(i, kq=kq, Qt=Qt):
                def pre():
                    for qb in range(4):
                        P.op("pe", "transpose", [sb_b, identf_b], [PSB[7]], qb == 3, out=psb[7][0:32, qb * 128:(qb + 1) * 128],
                             in_=sb_t[:, qb, :], identity=identf_t)
                    P.op("dve", "tensor_copy", [PSB[7]], [QA_b[kq][i]], out=Qt[64:96, i * 512:(i + 1) * 512], in_=psb[7][0:32, :])
                return pre
            mkpre1(0)(); mkpre2(0)()
            for i in range(NT):
                hooks = {}
                if i + 1 < NT:
                    hooks[0] = mkpre1(i + 1)
                    hooks[4 * i + 3] = mkpre2(i + 1)
                items += attn_items(Kt, KP_b[kq], Qt, [QP_b[kq], QA_b[kq][i]], 0, 96, VA[vk], VA_b[vk], None, i, 384 + m * 64, hooks)
            run_pipe(items)
        for oh in range(2):
            for g in range(3):
                d = DIL[g]
                ptile = 2 + g
                hd = 4 + 2 * g + oh
                kq = cnt["kq"] % 2; cnt["kq"] += 1
                load_rows(KP[kq], KP_b[kq], KT16, ptile * 128, 128, KTb)
                load_rows(QP[kq], QP_b[kq], QT16, ptile * 128, 128, QTb)
                vk = cnt["va"] % 2; cnt["va"] += 1
                load_va(vk, hd, d)
                Kt = KP[kq]; Qt = QP[kq]; r0 = oh * 64
                KQ = [KP_b[kq], QP_b[kq]]
                nbd = NB // d
                items = []
                for r in range(d):
                    for ub4 in range(0, nbd, 4):
                        po = 4 + cnt["po"] % 2; cnt["po"] += 1
                        nu = min(4, nbd - ub4)
                        for u in range(nu):
                            ub = ub4 + u
                            ps = cnt["ps"] % 3; cnt["ps"] += 1
                            pk = cnt["pt"] % 4; cnt["pt"] += 1
                            qa = r + d * 128 * ub
                            lo = 0 if ub > 0 else 128
                            bi = r * nbd + ub

                            def f_score(ub=ub, ps=ps, qa=qa, d=d, r0=r0, Kt=Kt, Qt=Qt, KQ=KQ):
                                qap = Qt[r0:r0 + 64, qa: qa + d * 127 + 1: d]
                                if ub > 0:
                                    ka = qa - d * 128
                                    mm_group(psb[ps][:, 0:128], [(Kt[r0:r0 + 64, ka: ka + d * 127 + 1: d], qap)], KQ, [PSB[ps]])
                                mm_group(psb[ps][:, 128:256], [(Kt[r0:r0 + 64, qa: qa + d * 127 + 1: d], qap)], KQ, [PSB[ps]])

                            def f_soft(ps=ps, pk=pk, lo=lo):
                                ACT(pt_t[pk][:, lo:256], psb[ps][:, lo:256], AF.Exp, [PSB[ps]], [pt_b[pk]], scale=0.125)
                                TT("pool", pt_t[pk][:, lo:256], pt_t[pk][:, lo:256], cmask_t[:, lo:256], ALU.mult, [pt_b[pk], cmask_b], [pt_b[pk]])

                            def f_pv(ub=ub, u=u, nu=nu, po=po, pk=pk, bi=bi, vk=vk, g=g, r=r, d=d, ub4=ub4):
                                if ub > 0:
                                    MM(psb[po][:, u * 128:(u + 1) * 128], VA[vk][:, bi - 1, :], pt_t[pk][:, 0:128], True, False,
                                       [VA_b[vk], pt_b[pk]], [PSB[po]], inc=False)
                                MM(psb[po][:, u * 128:(u + 1) * 128], VA[vk][:, bi, :], pt_t[pk][:, 128:256], ub == 0, True,
                                   [VA_b[vk], pt_b[pk]], [PSB[po]], inc=True)
                                if u == nu - 1:
                                    a0 = r + d * 128 * ub4
                                    accv = acc_t[:, a0: a0 + d * (128 * nu - 1) + 1: d]
                                    if g == 0:
                                        P.op("dve", "tensor_copy", [PSB[po]], [acc_b], out=accv, in_=psb[po][:, 0:128 * nu])
                                    else:
                                        TT("dve", accv, psb[po][:, 0:128 * nu], accv, ALU.add, [PSB[po], acc_b], [acc_b])
                            items.append((f_score, f_soft, f_pv))
                run_pipe(items)
            for i in range(NT):
                yk = cnt["y"] % 2; cnt["y"] += 1
                P.op("dve", "reciprocal", [acc_b], [rec_b], out=rec_t[64:128, :], in_=acc_t[64:128, i * 512:(i + 1) * 512])
                P.op("dve", "tensor_copy", [rec_b], [rec0_b], out=rec0_t[0:64, :], in_=rec_t[64:128, :])
                TT("dve", yst[yk][0:64, :], acc_t[0:64, i * 512:(i + 1) * 512], rec0_t[0:64, :], ALU.mult, [acc_b, rec0_b], [yst_b[yk]])
                P.dma("pool", [(YT16[256 + oh * 64:256 + oh * 64 + 64, i * 512:(i + 1) * 512], yst[yk][0:64, :])], yst_b[yk], R=[yst_b[yk]], W=[YTb])

        P.barrier()
        ar.reset()
        xt = [ar.f32(4096).rearrange("p (k n) -> p k n", n=512) for _ in range(2)]
        xt_b = [P.buf("xt%d" % i, True) for i in range(2)]
        yt = [ar.bf(3072).rearrange("p (k n) -> p k n", n=512) for _ in range(2)]
        yt_b = [P.buf("ytC%d" % i, True) for i in range(2)]
        pp = [ar.f32(1024).rearrange("p (k n) -> p k n", n=512) for _ in range(2)]
        pp_b = [P.buf("ppC%d" % i, True) for i in range(2)]
        p16 = [ar.bf(1024).rearrange("p (k n) -> p k n", n=512) for _ in range(2)]
        p16_b = [P.buf("p16_%d" % i) for i in range(2)]
        hh = [ar.bf(4096).rearrange("p (k n) -> p k n", n=512) for _ in range(2)]
        hh_b = [P.buf("hC%d" % i) for i in range(2)]
        sqo_t = ar.bf(4096).rearrange("p (k n) -> p k n", n=512); sqo_b = P.buf("sqo")
        sqx_t = ar.bf(4096).rearrange("p (k n) -> p k n", n=512); sqx_b = P.buf("sqx")
        mg_t = ar.bf(4096).rearrange("p (k n) -> p k n", n=512); mg_b = P.buf("mgC")
        o_t = ar.f32(4096).rearrange("p (k n) -> p k n", n=512); o_b = P.buf("oC")
        ff_t = ar.bf(NFF * 512).rearrange("p (k n) -> p k n", n=512); ff_b = P.buf("ffC")
        rstd_t = ar.f32(512); rstd_b = P.buf("rstd")
        tmp_t = ar.f32(512); tmp_b = P.buf("tmp")
        gs = [ar.f32(512) for _ in range(3)]; gs_b = [P.buf("gs%d" % i) for i in range(3)]
        ma = [ar.f32(512) for _ in range(3)]; ma_b = [P.buf("ma%d" % i) for i in range(3)]
        tt = [ar.f32(512) for _ in range(2)]; tt_b = [P.buf("tt%d" % i) for i in range(2)]
        ring = Ring(8, 1024, "ringC")
        prc = {"i": 0}

        def nps():
            k = prc["i"] % 7; prc["i"] += 1
            return k

        def loadC(t, k):
            P.dma("sp", [(xt[k][:, 0:4, :], xtile_ap(xsrc(l), t)[:, 0:4, :]),
                         (xt[k][:, 4:8, :], xtile_ap(xsrc(l), t)[:, 4:8, :])], xt_b[k], R=[Xb], W=[xt_b[k]])
            P.dma("sp", [(yt[k], YT16[:, t * 512:(t + 1) * 512].rearrange("(k p) n -> p k n", p=128))], yt_b[k], R=[YTb], W=[yt_b[k]])
            P.dma("sp", [(pp[k], pT[l * PLE:(l + 1) * PLE, t * 512:(t + 1) * 512].rearrange("(k p) n -> p k n", p=128))], pp_b[k], W=[pp_b[k]])

        def stats(sq_t, sq_b):
            rms_stats(sq_t, sq_b, rstd_t, rstd_b, tmp_t, tmp_b)

        def mk_h(k, gcol):
            for kc in range(8):
                P.op("dve", "scalar_tensor_tensor", [xt_b[k], rstd_b, gains_b], [hh_b[k]], out=hh[k][:, kc, :], in0=xt[k][:, kc, :],
                     scalar=gains_t[:, gcol + kc:gcol + kc + 1], in1=rstd_t, op0=ALU.mult, op1=ALU.mult)

        def pre1(k):
            ACT(sqx_t, xt[k], AF.Square, [xt_b[k]], [sqx_b])
            stats(sqx_t, sqx_b)
            mk_h(k, g0 + 0)

        def residual(k, gcol, want_sq):
            stats(sqo_t, sqo_b)
            for m in range(8):
                q = m % 2
                P.op("dve", "scalar_tensor_tensor", [o_b, rstd_b, gains_b], [tt_b[q]], out=tt[q], in0=o_t[:, m, :],
                     scalar=gains_t[:, gcol + m:gcol + m + 1], in1=rstd_t, op0=ALU.mult, op1=ALU.mult)
                TT("pool" if m % 2 == 0 else "dve", xt[k][:, m, :], xt[k][:, m, :], tt[q], ALU.add, [xt_b[k], tt_b[q]], [xt_b[k]])
                if want_sq:
                    ACT(sqx_t[:, m, :], xt[k][:, m, :], AF.Square, [xt_b[k]], [sqx_b])

        def evac_o(ps, m):
            ACT(sqo_t[:, m, :], psb[ps][:, :], AF.Square, [PSB[ps]], [sqo_b])
            ACT(o_t[:, m, :], psb[ps][:, :], AF.Copy, [PSB[ps]], [o_b])

        def S1(k, inject):
            ychunks = [(0, 2), (2, 1), (3, 3)]
            for m in range(8):
                if m == 3 and inject is not None:
                    inject()
                wb3, wb_b = ring.load(wslab("wBR", l, m), 768, [WB[l]])
                for b in range(3):
                    wg3, wg_b = ring.load(wslab("wG", l, b * 8 + m), 1024, [WB[l]])
                    pg = nps()
                    mm_group(psb[pg][:, :], [(wg3[:, kc, :], hh[k][:, kc, :]) for kc in range(8)], [wg_b, hh_b[k]], [PSB[pg]])
                    ACT(gs[b], psb[pg][:, :], AF.Sigmoid, [PSB[pg]], [gs_b[b]])
                    c0, ncc = ychunks[b]
                    pq = nps()
                    mm_group(psb[pq][:, :], [(wb3[:, c0 + c, :], yt[k][:, c0 + c, :]) for c in range(ncc)], [wb_b, yt_b[k]], [PSB[pq]])
                    TT("dve", ma[b], psb[pq][:, :], gs[b], ALU.mult, [PSB[pq], gs_b[b]], [ma_b[b]])
                TT("pool", ma[0], ma[0], ma[1], ALU.add, [ma_b[0], ma_b[1]], [ma_b[0]])
                TT("pool", mg_t[:, m, :], ma[0], ma[2], ALU.add, [ma_b[0], ma_b[2]], [mg_b])
            for m in range(8):
                w3, w_b = ring.load(wslab("wO", l, m), 1024, [WB[l]])
                ps = nps()
                mm_group(psb[ps][:, :], [(w3[:, kc, :], mg_t[:, kc, :]) for kc in range(8)], [w_b, mg_b], [PSB[ps]])
                evac_o(ps, m)

        def S2(k, inject):
            for j in range(NFF):
                if j == 8 and inject is not None:
                    inject()
                wg3, wg_b = ring.load(wslab("wFG", l, j), 1024, [WB[l]])
                pg = nps()
                mm_group(psb[pg][:, :], [(wg3[:, kc, :], hh[k][:, kc, :]) for kc in range(8)], [wg_b, hh_b[k]], [PSB[pg]])
                kk = j % 3
                ACT(gs[kk], psb[pg][:, :], AF.Silu, [PSB[pg]], [gs_b[kk]])
                wu3, wu_b = ring.load(wslab("wFU", l, j), 1024, [WB[l]])
                pu = nps()
                mm_group(psb[pu][:, :], [(wu3[:, kc, :], hh[k][:, kc, :]) for kc in range(8)], [wu_b, hh_b[k]], [PSB[pu]])
                TT("dve", ff_t[:, j, :], psb[pu][:, :], gs[kk], ALU.mult, [PSB[pu], gs_b[kk]], [ff_b])
            for m in range(8):
                ps = nps()
                pieces = [(0, 8), (8, 16), (16, NFF)]
                first = True
                for (j0, j1) in pieces:
                    w3, w_b = ring.load(wslab("wFD", l, m)[:, j0 * 128:j1 * 128], (j1 - j0) * 128, [WB[l]])
                    for j in range(j0, j1):
                        MM(psb[ps][:, :], w3[:, j - j0, :], ff_t[:, j, :], first, j == NFF - 1, [w_b, ff_b], [PSB[ps]], inc=(j == j1 - 1))
                        first = False
                evac_o(ps, m)

        def d2(k):
            ACT(hh[k], xt[k], AF.Copy, [xt_b[k]], [hh_b[k]])
            P.op("dve", "tensor_copy", [pp_b[k]], [p16_b[k]], out=p16[k], in_=pp[k])

        def S3(k, inject):
            for m in range(8):
                if m == 3 and inject is not None:
                    inject()
                wg3, wg_b = ring.load(wslab("wPG", l, m), 1024, [WB[l]])
                pg = nps()
                mm_group(psb[pg][:, :], [(wg3[:, kc, :], hh[k][:, kc, :]) for kc in range(8)], [wg_b, hh_b[k]], [PSB[pg]])
                kk = m % 3
                ACT(gs[kk], psb[pg][:, :], AF.Sigmoid, [PSB[pg]], [gs_b[kk]])
                wp3, wp_b = ring.load(wslab("wPL", l, m), 256, [WB[l]])
                pu = nps()
                mm_group(psb[pu][:, :], [(wp3[:, kc, :], p16[k][:, kc, :]) for kc in range(2)], [wp_b, p16_b[k]], [PSB[pu]])
                TT("dve", o_t[:, m, :], psb[pu][:, :], gs[kk], ALU.mult, [PSB[pu], gs_b[kk]], [o_b])
                ACT(sqo_t[:, m, :], o_t[:, m, :], AF.Square, [o_b], [sqo_b])

        def storeC(t, k):
            P.dma("pool", [(xtile_ap(xdst(l), t)[:, 0:4, :], xt[k][:, 0:4, :]), (xtile_ap(xdst(l), t)[:, 4:8, :], xt[k][:, 4:8, :])],
                  xt_b[k], R=[xt_b[k]], W=[Xb])

        def c2(k):
            stats(sqx_t, sqx_b)
            mk_h(k, g0 + 16)

        for tp in range(0, NT, 2):
            tA, tB = tp, tp + 1
            loadC(tA, 0)
            loadC(tB, 1)
            pre1(0)
            S1(0, lambda: pre1(1))
            residual(0, g0 + 8, True)
            S1(1, lambda: c2(0))
            residual(1, g0 + 8, True)
            S2(0, lambda: c2(1))
            residual(0, g0 + 24, False)
            S2(1, lambda: d2(0))
            residual(1, g0 + 24, False)
            S3(0, lambda: d2(1))
            residual(0, g0 + 32, False)
            storeC(tA, 0)
            S3(1, None)
            residual(1, g0 + 32, False)
            storeC(tB, 1)
    P.barrier()
    block = es.enter_context(nc.Block())
    P.replay(block)
    es.close()
    return nc


def _slabs(w, cols_list, kc):
    out = np.empty((len(cols_list), 128, kc, 128), np.float32)
    wk = w.reshape(kc, 128, w.shape[1])
    for m, cols in enumerate(cols_list):
        out[m] = wk[:, :, cols].transpose(1, 0, 2)
    return out.reshape(len(cols_list) * 128, kc * 128)


def _host_weights(inp, L):
    r = {}
    ar_ = np.arange
    lists = {n: [] for n in ("wA", "wV", "wF", "wG", "wBR", "wO", "wFG", "wFU", "wFD", "wPL", "wPG")}
    for l in range(L):
        w_in = np.asarray(inp["w_in"][l], np.float32)
        colsA = []
        for base in (0, 1024):
            for pt in range(8):
                c = base + pt * 128 + ar_(128)
                colsA.append(c)
                if pt >= 2:
                    sw = base + pt * 128 + (ar_(128) // 64) * 64 + (ar_(128) % 64 + 32) % 64
                    colsA.append(sw)
        lists["wA"].append(_slabs(w_in, colsA, 8))
        wv = w_in[:, 2048:3072].reshape(8, 128, 1024).transpose(1, 0, 2).reshape(128, 8192)
        lists["wV"].append(wv)
        wf = w_in[:, 3072:3076].reshape(8, 128, 4).transpose(1, 0, 2).reshape(128, 32)
        lists["wF"].append(wf)
        lists["wG"].append(_slabs(w_in, [3076 + b * 1024 + m * 128 + ar_(128) for b in range(3) for m in range(8)], 8))
        wbr = np.concatenate([np.asarray(inp["w_br_a"][l]), np.asarray(inp["w_br_b"][l]), np.asarray(inp["w_br_c"][l])], axis=0)
        lists["wBR"].append(_slabs(wbr.astype(np.float32), [m * 128 + ar_(128) for m in range(8)], 6))
        lists["wO"].append(_slabs(np.asarray(inp["w_out"][l], np.float32), [m * 128 + ar_(128) for m in range(8)], 8))
        lists["wFG"].append(_slabs(np.asarray(inp["w_ffn_gate"][l], np.float32), [m * 128 + ar_(128) for m in range(NFF)], 8))
        lists["wFU"].append(_slabs(np.asarray(inp["w_ffn_up"][l], np.float32), [m * 128 + ar_(128) for m in range(NFF)], 8))
        lists["wFD"].append(_slabs(np.asarray(inp["w_ffn_down"][l], np.float32), [m * 128 + ar_(128) for m in range(8)], NFF))
        lists["wPL"].append(_slabs(np.asarray(inp["w_ple"][l], np.float32), [m * 128 + ar_(128) for m in range(8)], 2))
        lists["wPG"].append(_slabs(np.asarray(inp["w_ple_gate"][l], np.float32), [m * 128 + ar_(128) for m in range(8)], 8))
    for n, v in lists.items():
        r[n] = np.ascontiguousarray(np.concatenate(v, axis=0), dtype=np.float32)
    gl = []
    for l in range(L):
        for n in ("g_mix_pre", "g_mix_post", "g_ffn_pre", "g_ffn_post", "g_ple_post"):
            gl.append(np.asarray(inp[n][l], np.float32).reshape(8, 128).T)
    r["gains"] = np.ascontiguousarray(np.concatenate(gl, axis=1), dtype=np.float32)
    r["bfb"] = np.ascontiguousarray(np.broadcast_to(np.asarray(inp["b_f"], np.float32).reshape(1, L * 4), (128, L * 4)))
    return r


def _consts(S):
    c = {}
    inv = (1.0 / (np.float32(10000.0) ** (np.arange(0, 64, 2, dtype=np.float32) / np.float32(64)))).astype(np.float32)
    ang = (np.arange(S, dtype=np.float32)[:, None] * inv[None, :]).astype(np.float32)
    cos = np.cos(ang.astype(np.float64)).astype(np.float32).T
    sin = np.sin(ang.astype(np.float64)).astype(np.float32).T
    c["cosT"] = np.ascontiguousarray(np.concatenate([cos, cos, cos, cos], axis=0))
    c["sinT"] = np.ascontiguousarray(np.concatenate([-sin, sin, -sin, sin], axis=0))
    p = np.arange(128)
    c["cmask"] = np.ascontiguousarray(np.concatenate([(p[:, None] >= p[None, :]), (p[:, None] <= p[None, :])], axis=1).astype(np.float32))
    c["trif"] = np.ascontiguousarray((p[:, None] <= p[None, :]).astype(np.float32))
    c["identf"] = np.eye(128, dtype=np.float32)
    c["blkind"] = np.ascontiguousarray(np.concatenate([(np.arange(S)[None, :] // 256 == np.arange(32)[:, None]), np.ones((1, S), bool)], axis=0).astype(np.float32))
    return c


_NC_CACHE = {}


def kernel(**inputs):
    x = np.asarray(inputs["x"], np.float32)
    p = np.asarray(inputs["p"], np.float32)
    B, S, _ = x.shape
    L = p.shape[0]
    key = (S, L)
    if key not in _NC_CACHE:
        _NC_CACHE[key] = build(S, L)
    nc = _NC_CACHE[key]
    shared = _host_weights(inputs, L)
    shared.update(_consts(S))
    in_maps = []
    for b in range(B):
        m = dict(shared)
        m["xT"] = np.ascontiguousarray(x[b].T)
        m["pT"] = np.ascontiguousarray(p[:, b].transpose(0, 2, 1).reshape(L * PLE, S))
        in_maps.append(m)
    res = run_bass_kernel_spmd(nc, in_maps, core_ids=list(range(B)))
    out = np.stack([np.ascontiguousarray(res.results[b]["outT"].T) for b in range(B)], axis=0)
    return out.astype(np.float32)
```

```python
import numpy as np
from contextlib import ExitStack
import concourse.bass as bass
import concourse.mybir as mybir
from concourse.bass_utils import run_bass_kernel_spmd

F32 = mybir.dt.float32
BF = mybir.dt.bfloat16
AF = mybir.ActivationFunctionType
ALU = mybir.AluOpType
AX = mybir.AxisListType

D = 1024
HD = 64
PLE = 256
DFF = 2816
NFF = 22
BIG = 30000.0
DIL = (1, 4, 16)
SAME_ENGINE_SYNC = True


class Buf:
    def __init__(self, name, sem=None):
        self.name = name
        self.w = {}
        self.r = {}
        self.sem = sem
        self.cnt = 0


class Eng:
    def __init__(self, name, sem):
        self.name = name
        self.sem = sem
        self.cnt = 0
        self.seen = {}
        self.prog = []


class Prog:
    def __init__(self, nc, es):
        self.nc = nc
        self.es = es
        self.sems = []
        self.E = {}
        for n in ("pe", "act", "dve", "pool"):
            self.E[n] = Eng(n, self.newsem(n))
        self.E["sp"] = Eng("sp", None)
        self.bufs = []
        self.bynames = {}

    def newsem(self, name):
        h = self.es.enter_context(self.nc.semaphore("s_" + name))
        self.sems.append(h)
        return len(self.sems) - 1

    def buf(self, name, dma=False):
        if name in self.bynames:
            return self.bynames[name]
        b = Buf(name, self.newsem(name) if dma else None)
        self.bufs.append(b)
        self.bynames[name] = b
        return b

    def _waits(self, X, R, W):
        need = {}
        for b in R:
            for k, v in b.w.items():
                need[k] = max(need.get(k, 0), v)
        for b in W:
            for k, v in b.w.items():
                need[k] = max(need.get(k, 0), v)
            for k, v in b.r.items():
                need[k] = max(need.get(k, 0), v)
        out = []
        for k, v in need.items():
            if k == X.sem and (X.name == "pe" or not SAME_ENGINE_SYNC):
                continue
            if X.seen.get(k, 0) >= v:
                continue
            X.seen[k] = v
            out.append((k, v))
        return out

    def op(self, eng, name, R=(), W=(), inc=True, args=(), **kw):
        X = self.E[eng]
        waits = self._waits(X, R, W)
        tok = X.cnt + 1
        if inc:
            X.cnt = tok
        X.prog.append((waits, ("op", name, args, kw), inc))
        for b in R:
            b.r[X.sem] = tok
        for b in W:
            b.w = {X.sem: tok}
            b.r = {}

    def dma(self, q, pairs, sb, R=(), W=()):
        X = self.E[q]
        waits = self._waits(X, R, W)
        first = True
        for o, i in pairs:
            sb.cnt += 16
            X.prog.append((waits if first else [], ("dma", o, i, sb.sem), False))
            first = False
        for b in R:
            b.r[sb.sem] = sb.cnt
        for b in W:
            b.w = {sb.sem: sb.cnt}
            b.r = {}

    def barrier(self):
        toks = {}
        for n in ("pe", "act", "dve", "pool"):
            toks[self.E[n].sem] = self.E[n].cnt
        for b in self.bufs:
            if b.sem is not None and b.cnt > 0:
                toks[b.sem] = b.cnt
        for n, X in self.E.items():
            waits = []
            for k, v in toks.items():
                if v > 0 and X.seen.get(k, 0) < v and not (k == X.sem and n == "pe"):
                    X.seen[k] = v
                    waits.append((k, v))
            X.prog.append((waits, None, False))

    def replay(self, block):
        sems = self.sems

        def run(X, e):
            for waits, fn, inc in X.prog:
                for k, v in waits:
                    e.wait_ge(sems[k], v)
                if fn is None:
                    continue
                if fn[0] == "dma":
                    _, o, i, k = fn
                    e.dma_start(out=o, in_=i).then_inc(sems[k], 16)
                else:
                    _, name, args, kw = fn
                    ins = getattr(e, name)(*args, **kw)
                    if inc:
                        ins.then_inc(sems[X.sem], 1)

        @block.sync
        def _(e):
            run(self.E["sp"], e)

        @block.tensor
        def _(e):
            run(self.E["pe"], e)

        @block.scalar
        def _(e):
            run(self.E["act"], e)

        @block.vector
        def _(e):
            run(self.E["dve"], e)

        @block.gpsimd
        def _(e):
            run(self.E["pool"], e)


class Arena:
    def __init__(self, ap, lo, hi):
        self.ap = ap
        self.lo = lo
        self.hi = hi
        self.p = lo

    def f32(self, n):
        a = self.ap[:, self.p:self.p + n]
        self.p += n
        assert self.p <= self.hi, ("arena overflow", self.p, self.hi)
        return a

    def bf(self, n):
        n2 = (n + 1) // 2
        a = self.ap[:, self.p:self.p + n2].bitcast(BF)
        self.p += n2
        assert self.p <= self.hi, ("arena overflow", self.p, self.hi)
        return a[:, 0:n]

    def reset(self):
        self.p = self.lo


def build(S=8192, L=2, dbg=False):
    NT = S // 512
    NB = S // 128
    NMB = S // 256
    CW = min(2048, S)
    assert NMB <= 32
    nc = bass.Bass("TRN2", target_bir_lowering=False)

    def din(name, shape, dt=F32):
        return nc.dram_tensor(name, shape, dt, kind="ExternalInput").ap()

    def dscr(name, shape, dt=BF):
        return nc.dram_tensor(name, shape, dt, kind=("ExternalOutput" if dbg else "Internal")).ap()

    xT = din("xT", [D, S])
    pT = din("pT", [L * PLE, S])
    wspec = [("wA", 28, 1024), ("wV", 1, 8192), ("wF", 1, 32), ("wG", 24, 1024), ("wBR", 8, 768),
             ("wO", 8, 1024), ("wFG", 22, 1024), ("wFU", 22, 1024), ("wFD", 8, 2816),
             ("wPL", 8, 256), ("wPG", 8, 1024)]
    w32 = {}
    w16 = {}
    for n, ns, nc_ in wspec:
        w32[n] = din(n, [L * ns * 128, nc_])
        w16[n] = nc.dram_tensor(n + "_16", [L * ns * 128, nc_], BF, kind="Internal").ap()
    wns = {n: ns for n, ns, _ in wspec}
    gains = din("gains", [128, L * 5 * 8])
    bfb = din("bfb", [128, L * 4])
    cosT = din("cosT", [128, S])
    sinT = din("sinT", [128, S])
    cmask32 = din("cmask", [128, 256])
    trif = din("trif", [128, 128])
    identf = din("identf", [128, 128])
    blkind32 = din("blkind", [33, S])
    outT = nc.dram_tensor("outT", [D, S], F32, kind="ExternalOutput").ap()

    X32 = dscr("X32", [D, S], F32)
    QT16 = dscr("QT16", [1024, S])
    KT16 = dscr("KT16", [1024, S])
    V16 = dscr("V16", [S, 1024])
    YT16 = dscr("YT16", [768, S])
    KS32 = dscr("KS32", [384, 32], F32)
    BI16 = nc.dram_tensor("BI16", [33, S], BF, kind="Internal").ap()

    es = ExitStack()
    P = Prog(nc, es)
    AW = 52992
    arena_t = es.enter_context(nc.sbuf_tensor("arena", [128, AW], F32))
    PERS = 2048
    pers = Arena(arena_t, 0, PERS)
    ar = Arena(arena_t, PERS, AW)
    psb = [es.enter_context(nc.psum_tensor("psb%d" % i, [128, 512], F32)) for i in range(8)]
    PSB = [P.buf("psb%d" % i) for i in range(8)]

    def ACT(out, in_, func, R, W, **kw):
        P.op("act", "activation", R, W, out=out, in_=in_, func=func, **kw)

    def TT(eng, out, in0, in1, op, R, W):
        P.op(eng, "tensor_tensor", R, W, out=out, in0=in0, in1=in1, op=op)

    def MM(out, lhsT, rhs, start, stop, R, W, inc=True):
        P.op("pe", "matmul", R, W, inc, args=(out,), lhsT=lhsT, rhs=rhs, start=start, stop=stop)

    def mm_group(out_ap, pairs, Rb, Wb):
        n = len(pairs)
        for i, (lt, rh) in enumerate(pairs):
            MM(out_ap, lt, rh, i == 0, i == n - 1, Rb, Wb, inc=(i == n - 1))

    gains_t = pers.f32(L * 40); gains_b = P.buf("gains", True)
    bfb_t = pers.f32(L * 4); bfb_b = P.buf("bfb", True)
    trif_t = pers.f32(128); trif_b = P.buf("trif", True)
    identf_t = pers.f32(128); identf_b = P.buf("identf", True)
    onesf_t = pers.f32(128); onesf_b = P.buf("onesf")
    ones16_t = pers.bf(128); ones16_b = P.buf("ones16")
    cmask_t = pers.bf(256); cmask_b = P.buf("cmask", True)
    eps_t = pers.f32(1); one_t = pers.f32(1); cst_b = P.buf("cst")
    cpos_t = pers.f32(NB * 4).rearrange("p (j h) -> p j h", h=4); cpos_b = P.buf("cpos")
    tall_t = pers.f32((NB + 1) * 4).rearrange("p (j h) -> p j h", h=4); tall_b = P.buf("tall")
    ksum_t = pers.f32(3 * 32).rearrange("p (a n) -> p a n", n=32); ksum_b = P.buf("ksum", True)

    P.dma("sp", [(gains_t, gains)], gains_b, W=[gains_b])
    P.dma("sp", [(bfb_t, bfb)], bfb_b, W=[bfb_b])
    P.dma("sp", [(trif_t, trif)], trif_b, W=[trif_b])
    P.dma("sp", [(identf_t, identf)], identf_b, W=[identf_b])
    P.dma("pool", [(cmask_t, cmask32)], cmask_b, W=[cmask_b])
    P.op("dve", "memset", (), [onesf_b], args=(onesf_t, 1.0))
    P.op("dve", "memset", (), [ones16_b], args=(ones16_t, 1.0))
    P.op("dve", "memset", (), [cst_b], args=(eps_t, 1e-6))
    P.op("dve", "memset", (), [cst_b], args=(one_t, 1.0))
    P.op("dve", "memset", (), [tall_b], args=(tall_t[:, 0, :], 0.0))
    P.op("dve", "memset", (), [ksum_b], args=(ksum_t, 0.0))

    WB = [P.buf("w16_%d" % l) for l in range(L)]
    CB = [P.buf("cast%d" % i, True) for i in range(2)]
    BIb = P.buf("bi16", True)
    P.dma("pool", [(BI16[:, c0:c0 + CW], blkind32[:, c0:c0 + CW]) for c0 in range(0, S, CW)], BIb, W=[BIb])
    cg = 0
    for l in range(L):
        pairs = []
        for n, ns, ncol in wspec:
            for s_ in range(ns):
                r0 = (l * ns + s_) * 128
                cw = 2048 if ncol > 2816 else ncol
                for c0 in range(0, ncol, cw):
                    pairs.append((w16[n][r0:r0 + 128, c0:c0 + cw], w32[n][r0:r0 + 128, c0:c0 + cw]))
        for g0_ in range(0, len(pairs), 8):
            cb = CB[cg % 2]; cg += 1
            P.dma("pool", pairs[g0_:g0_ + 8], cb, W=[cb])
        WB[l].w = {CB[0].sem: CB[0].cnt, CB[1].sem: CB[1].cnt}

    def wslab(n, l, s_):
        r0 = (l * wns[n] + s_) * 128
        return w16[n][r0:r0 + 128, :]

    Xb = P.buf("X32d"); QTb = P.buf("QTd"); KTb = P.buf("KTd"); Vb = P.buf("Vd"); YTb = P.buf("YTd"); KSb = P.buf("KSd")

    def xsrc(l):
        return xT if l == 0 else X32

    def xdst(l):
        return outT if l == L - 1 else X32

    def xtile_ap(dram, t):
        return dram[:, t * 512:(t + 1) * 512].rearrange("(kc p) n -> p kc n", p=128)

    class Ring:
        def __init__(self, n, words, name):
            self.t = [ar.bf(words) for _ in range(n)]
            self.b = [P.buf("%s%d" % (name, i), True) for i in range(n)]
            self.i = 0

        def load(self, dram_ap, ncol, Rb):
            k = self.i % len(self.t)
            self.i += 1
            P.dma("sp", [(self.t[k][:, 0:ncol], dram_ap)], self.b[k], R=Rb, W=[self.b[k]])
            return self.t[k][:, 0:ncol].rearrange("p (k n) -> p k n", n=128), self.b[k]

    def rms_stats(sq_t, sq_b, rstd_t, rstd_b, tmp_t, tmp_b):
        mm_group(psb[7][:, :], [(ones16_t, sq_t[:, kc, :]) for kc in range(8)], [ones16_b, sq_b], [PSB[7]])
        ACT(tmp_t, psb[7][:, :], AF.Sqrt, [PSB[7], cst_b], [tmp_b], bias=eps_t, scale=1.0 / D)
        P.op("dve", "reciprocal", [tmp_b], [rstd_b], out=rstd_t, in_=tmp_t)

    def pre_norm(x_t, x_b, gcol, sq_t, sq_b, rstd_t, rstd_b, tmp_t, tmp_b, h_t, h_b):
        ACT(sq_t, x_t, AF.Square, [x_b], [sq_b])
        rms_stats(sq_t, sq_b, rstd_t, rstd_b, tmp_t, tmp_b)
        for kc in range(8):
            P.op("dve", "scalar_tensor_tensor", [x_b, rstd_b, gains_b], [h_b], out=h_t[:, kc, :], in0=x_t[:, kc, :],
                 scalar=gains_t[:, gcol + kc:gcol + kc + 1], in1=rstd_t, op0=ALU.mult, op1=ALU.mult)

    for l in range(L):
        g0 = l * 40
        P.barrier()
        ar.reset()
        xt = [ar.f32(4096).rearrange("p (k n) -> p k n", n=512) for _ in range(2)]
        xt_b = [P.buf("xt%d" % i, True) for i in range(2)]
        sq_t = ar.bf(4096).rearrange("p (k n) -> p k n", n=512); sq_b = P.buf("sq")
        h_t = ar.bf(4096).rearrange("p (k n) -> p k n", n=512); h_b = P.buf("h")
        rstd_t = ar.f32(512); rstd_b = P.buf("rstd")
        tmp_t = ar.f32(512); tmp_b = P.buf("tmp")
        wv_t = ar.bf(8192).rearrange("p (k n) -> p k n", n=1024); wv_b = P.buf("wvA", True)
        wf_t = ar.bf(32).rearrange("p (k n) -> p k n", n=4); wf_b = P.buf("wfA", True)
        ring = Ring(6, 1024, "ring")
        cs_t = [(ar.f32(512), ar.f32(512)) for _ in range(2)]
        cs_b = [P.buf("csA%d" % i, True) for i in range(2)]
        qst = [ar.bf(4096).rearrange("p (k n) -> p k n", n=512) for _ in range(2)]
        qst_b = [P.buf("qst%d" % i, True) for i in range(2)]
        kst = [ar.bf(4096).rearrange("p (k n) -> p k n", n=512) for _ in range(2)]
        kst_b = [P.buf("kst%d" % i, True) for i in range(2)]
        vst = [ar.bf(4096).rearrange("p (a n) -> p a n", n=1024) for _ in range(2)]
        vst_b = [P.buf("vst%d" % i, True) for i in range(2)]
        r1 = [ar.f32(512) for _ in range(2)]; r1_b = [P.buf("r1_%d" % i) for i in range(2)]
        r2 = [ar.f32(512) for _ in range(2)]; r2_b = [P.buf("r2_%d" % i) for i in range(2)]
        fb_t = ar.f32(4); fb_b = P.buf("fbA")
        fe_t = ar.f32(4); fe_b = P.buf("feA")
        fl_t = ar.f32(4); fl_b = P.buf("flA")

        P.dma("sp", [(wv_t[:, kc, :], wslab("wV", l, 0)[:, kc * 1024:(kc + 1) * 1024]) for kc in range(8)],
              wv_b, R=[WB[l]], W=[wv_b])
        P.dma("sp", [(wf_t, wslab("wF", l, 0).rearrange("p (k n) -> p k n", n=4))], wf_b, R=[WB[l]], W=[wf_b])

        def loadxA(t):
            k = t % 2
            P.dma("sp", [(xt[k][:, 0:4, :], xtile_ap(xsrc(l), t)[:, 0:4, :]),
                         (xt[k][:, 4:8, :], xtile_ap(xsrc(l), t)[:, 4:8, :])], xt_b[k], R=[Xb], W=[xt_b[k]])
            P.dma("sp", [(cs_t[k][0], cosT[:, t * 512:(t + 1) * 512]),
                         (cs_t[k][1], sinT[:, t * 512:(t + 1) * 512])], cs_b[k], W=[cs_b[k]])

        loadxA(0)
        psrot = 0
        for t in range(NT):
            if t + 1 < NT:
                loadxA(t + 1)
            x_t = xt[t % 2]; x_b = xt_b[t % 2]
            cos_t, sin_t = cs_t[t % 2]; c_b = cs_b[t % 2]
            pre_norm(x_t, x_b, g0 + 0, sq_t, sq_b, rstd_t, rstd_b, tmp_t, tmp_b, h_t, h_b)
            qs = qst[t % 2]; qs_b = qst_b[t % 2]; ks = kst[t % 2]; ks_b = kst_b[t % 2]
            si = 0
            for which in range(2):
                stg, stg_b = (qs, qs_b) if which == 0 else (ks, ks_b)
                for pt in range(8):
                    w3, w_b = ring.load(wslab("wA", l, si), 1024, [WB[l]]); si += 1
                    pa = psrot % 6; psrot += 1
                    mm_group(psb[pa][:, :], [(w3[:, kc, :], h_t[:, kc, :]) for kc in range(8)], [w_b, h_b], [PSB[pa]])
                    if pt < 2:
                        ACT(stg[:, pt, :], psb[pa][:, :], AF.Copy, [PSB[pa]], [stg_b])
                    else:
                        w23, w2_b = ring.load(wslab("wA", l, si), 1024, [WB[l]]); si += 1
                        pb = psrot % 6; psrot += 1
                        mm_group(psb[pb][:, :], [(w23[:, kc, :], h_t[:, kc, :]) for kc in range(8)], [w2_b, h_b], [PSB[pb]])
                        ri = (pt + which) % 2
                        TT("dve", r1[ri], psb[pa][:, :], cos_t, ALU.mult, [PSB[pa], c_b], [r1_b[ri]])
                        TT("dve", r2[ri], psb[pb][:, :], sin_t, ALU.mult, [PSB[pb], c_b], [r2_b[ri]])
                        TT("pool", stg[:, pt, :], r1[ri], r2[ri], ALU.add, [r1_b[ri], r2_b[ri]], [stg_b])
                        if which == 1 and pt >= 5:
                            P.op("dve", "tensor_reduce", [stg_b], [ksum_b], out=ksum_t[:, pt - 5, 2 * t:2 * t + 2],
                                 in_=stg[:, pt, :].rearrange("p (a b) -> p a b", b=256), axis=AX.X, op=ALU.add)
            P.dma("pool", [(QT16[:, t * 512:(t + 1) * 512].rearrange("(k p) n -> p k n", p=128), qs)], qs_b, R=[qs_b], W=[QTb])
            P.dma("pool", [(KT16[:, t * 512:(t + 1) * 512].rearrange("(k p) n -> p k n", p=128), ks)], ks_b, R=[ks_b], W=[KTb])
            vs = vst[t % 2]; vs_b = vst_b[t % 2]
            for tb in range(4):
                for hf in range(2):
                    pa = psrot % 6; psrot += 1
                    mm_group(psb[pa][:, :], [(h_t[:, kc, tb * 128:(tb + 1) * 128], wv_t[:, kc, hf * 512:(hf + 1) * 512]) for kc in range(8)],
                             [wv_b, h_b], [PSB[pa]])
                    ACT(vs[:, tb, hf * 512:(hf + 1) * 512], psb[pa][:, :], AF.Copy, [PSB[pa]], [vs_b])
                j = 4 * t + tb
                mm_group(psb[6][:, 0:4], [(h_t[:, kc, tb * 128:(tb + 1) * 128], wf_t[:, kc, :]) for kc in range(8)], [wf_b, h_b], [PSB[6]])
                TT("dve", fb_t, psb[6][:, 0:4], bfb_t[:, l * 4:l * 4 + 4], ALU.add, [PSB[6], bfb_b], [fb_b])
                ACT(fe_t, fb_t, AF.Exp, [fb_b], [fe_b], scale=-1.0)
                ACT(fl_t, fe_t, AF.Ln, [fe_b, cst_b], [fl_b], bias=one_t, scale=1.0)
                mm_group(psb[6][:, 8:12], [(trif_t, fl_t)], [trif_b, fl_b], [PSB[6]])
                mm_group(psb[6][:, 16:20], [(onesf_t, fl_t)], [onesf_b, fl_b], [PSB[6]])
                TT("dve", cpos_t[:, j, :], psb[6][:, 8:12], tall_t[:, j, :], ALU.add, [PSB[6], tall_b], [cpos_b])
                TT("dve", tall_t[:, j + 1, :], psb[6][:, 16:20], tall_t[:, j, :], ALU.add, [PSB[6], tall_b], [tall_b])
            P.dma("pool", [(V16[t * 512:(t + 1) * 512, :].rearrange("(a p) c -> p a c", p=128), vs)], vs_b, R=[vs_b], W=[Vb])
        P.dma("pool", [(KS32.rearrange("(a p) n -> p a n", p=128), ksum_t)], ksum_b, R=[ksum_b], W=[KSb])

        P.barrier()
        ar.reset()
        KP = [ar.bf(S) for _ in range(2)]; KP_b = [P.buf("KP%d" % i, True) for i in range(2)]
        QP = [ar.bf(S) for _ in range(2)]; QP_b = [P.buf("QP%d" % i, True) for i in range(2)]
        QA_b = [[P.buf("QA%d_%d" % (i, t)) for t in range(NT)] for i in range(2)]
        VA = [ar.bf(NB * 128).rearrange("p (j c) -> p j c", c=128) for _ in range(2)]
        VA_b = [P.buf("VA%d" % i, True) for i in range(2)]
        pt_t = [ar.bf(512) for _ in range(4)]; pt_b = [P.buf("pT%d" % i) for i in range(4)]
        rec_t = ar.f32(512); rec_b = P.buf("rec")
        rec0_t = ar.f32(512); rec0_b = P.buf("rec0")
        yst = [ar.bf(512) for _ in range(2)]; yst_b = [P.buf("yst%d" % i, True) for i in range(2)]
        ks16_t = ar.bf(32); ks16_b = P.buf("ks16")
        ks32_t = ar.f32(32); ks32_b = P.buf("ks32", True)
        wk_t = ar.f32(128).rearrange("p (a n) -> p a n", n=32); wk_b = P.buf("wk")
        t8_t = ar.f32(32).rearrange("p (a n) -> p a n", n=8); t8_b = P.buf("t8")
        sb_t = ar.f32(128).rearrange("p (a n) -> p a n", n=32); sb_b = P.buf("selb")
        acc_t = ar.f32(S); acc_b = P.buf("acc")
        for i in range(2):
            P.op("pool", "memset", (), [VA_b[i]], args=(VA[i][:, :, 64:128], 1.0))
        cnt = {"ps": 0, "po": 0, "pt": 0, "y": 0, "kq": 0, "va": 0}

        def load_rows(dst, dst_b, dram, r0, nr, rb, ind=False):
            pairs = [(dst[0:nr, c0:c0 + CW], dram[r0:r0 + nr, c0:c0 + CW]) for c0 in range(0, S, CW)]
            Rb = [rb]
            if ind == 1:
                pairs += [(dst[64:96, c0:c0 + CW], BI16[0:32, c0:c0 + CW]) for c0 in range(0, S, CW)]
                Rb = [rb, BIb]
            if ind == 2:
                pairs += [(dst[64:65, c0:c0 + CW], BI16[32:33, c0:c0 + CW]) for c0 in range(0, S, CW)]
                Rb = [rb, BIb]
            P.dma("sp", pairs, dst_b, R=Rb, W=[dst_b])

        def load_va(k, head, d):
            nbd = NB // d
            pairs = []
            for r in range(d):
                for b0 in range(0, nbd, 8):
                    nb_ = min(8, nbd - b0)
                    src = V16[r + d * 128 * b0: r + d * 128 * b0 + d * (128 * nb_ - 1) + 1: d, head * 64:(head + 1) * 64]
                    pairs.append((VA[k][:, r * nbd + b0: r * nbd + b0 + nb_, 0:64], src.rearrange("(j p) c -> p j c", p=128)))
            for g_ in range(0, len(pairs), 4):
                P.dma("sp", pairs[g_:g_ + 4], VA_b[k], R=[Vb], W=[VA_b[k]])

        def finalize(po, yrow, i):
            yk = cnt["y"] % 2; cnt["y"] += 1
            P.op("dve", "reciprocal", [PSB[po]], [rec_b], out=rec_t[64:128, :], in_=psb[po][64:128, :])
            TT("dve", yst[yk][0:64, :], psb[po][0:64, :], rec_t[64:128, :], ALU.mult, [PSB[po], rec_b], [yst_b[yk]])
            P.dma("pool", [(YT16[yrow:yrow + 64, i * 512:(i + 1) * 512], yst[yk][0:64, :])], yst_b[yk], R=[yst_b[yk]], W=[YTb])

        LA = 2

        def run_pipe(items):
            n = len(items)
            for idx in range(n + LA):
                if idx < n:
                    items[idx][0]()
                if idx >= LA:
                    items[idx - LA][1]()
                    items[idx - LA][2]()

        def attn_items(Kt, K_b, Qt, Qbufs, r0, nr, va, va_b, bias_fn, i, yrow, hooks):
            items = []
            po = 4 + cnt["po"] % 2; cnt["po"] += 1
            nj = 4 * i + 4
            for j in range(nj):
                off = max(0, j - 4 * i) * 128
                ps = cnt["ps"] % 3; cnt["ps"] += 1
                pk = cnt["pt"] % 4; cnt["pt"] += 1

                def f_score(j=j, off=off, ps=ps):
                    if j in hooks:
                        hooks[j]()
                    mm_group(psb[ps][:, off:512], [(Kt[r0:r0 + nr, j * 128:(j + 1) * 128], Qt[r0:r0 + nr, i * 512 + off:(i + 1) * 512])],
                             [K_b] + Qbufs, [PSB[ps]])

                def f_soft(j=j, off=off, ps=ps, pk=pk):
                    if bias_fn is None:
                        ACT(pt_t[pk][:, off:512], psb[ps][:, off:512], AF.Exp, [PSB[ps]], [pt_b[pk]], scale=0.125)
                    else:
                        ACT(pt_t[pk][:, off:512], psb[ps][:, off:512], AF.Exp, [PSB[ps], cpos_b], [pt_b[pk]], bias=bias_fn(j, i), scale=0.125)
                    if j >= 4 * i:
                        TT("pool", pt_t[pk][:, off:off + 128], pt_t[pk][:, off:off + 128], cmask_t[:, 128:256], ALU.mult,
                           [pt_b[pk], cmask_b], [pt_b[pk]])

                def f_pv(j=j, off=off, pk=pk):
                    MM(psb[po][:, off:512], va[:, j, :], pt_t[pk][:, off:512], j == 0, j == nj - 1, [va_b, pt_b[pk]], [PSB[po]], inc=True)
                    if j == nj - 1:
                        finalize(po, yrow, i)
                items.append((f_score, f_soft, f_pv))
            return items

        for h in range(4):
            kq = cnt["kq"] % 2; cnt["kq"] += 1
            load_rows(KP[kq], KP_b[kq], KT16, h * 64, 64, KTb, ind=2)
            load_rows(QP[kq], QP_b[kq], QT16, h * 64, 64, QTb)
            vk = cnt["va"] % 2; cnt["va"] += 1
            load_va(vk, h, 1)
            items = []

            def mkpre(i, h=h, kq=kq):
                def pre():
                    for qb in range(4):
                        P.op("pe", "transpose", [cpos_b, identf_b], [PSB[7]], qb == 3, out=psb[7][0:1, qb * 128:(qb + 1) * 128],
                             in_=cpos_t[:, 4 * i + qb, h:h + 1], identity=identf_t)
                    P.op("dve", "tensor_scalar", [PSB[7]], [QA_b[kq][i]], out=QP[kq][64:65, i * 512:(i + 1) * 512], in0=psb[7][0:1, :],
                         scalar1=-8.0, scalar2=None, op0=ALU.mult)
                return pre
            mkpre(0)()
            for i in range(NT):
                hooks = {0: mkpre(i + 1)} if i + 1 < NT else {}
                items += attn_items(KP[kq], KP_b[kq], QP[kq], [QP_b[kq], QA_b[kq][i]], 0, 65, VA[vk], VA_b[vk],
                                    (lambda h: lambda j, i: cpos_t[:, j, h:h + 1])(h), i, h * 64, hooks)
            run_pipe(items)
        for m in range(6):
            hd = 10 + m
            kq = cnt["kq"] % 2; cnt["kq"] += 1
            load_rows(KP[kq], KP_b[kq], KT16, hd * 64, 64, KTb, ind=1)
            load_rows(QP[kq], QP_b[kq], QT16, hd * 64, 64, QTb)
            P.dma("sp", [(ks32_t[0:64, :], KS32[m * 64:(m + 1) * 64, :])], ks32_b, R=[KSb], W=[ks32_b])
            P.op("dve", "tensor_copy", [ks32_b], [ks16_b], out=ks16_t[0:64, :], in_=ks32_t[0:64, :])
            vk = cnt["va"] % 2; cnt["va"] += 1
            load_va(vk, hd, 1)
            Kt = KP[kq]; Qt = QP[kq]
            items = []

            def mkpre1(i, kq=kq, Qt=Qt):
                def pre():
                    for qb in range(4):
                        q0 = i * 512 + qb * 128
                        mm_group(psb[6][:, qb * 32:qb * 32 + NMB], [(Qt[0:64, q0:q0 + 128], ks16_t[0:64, 0:NMB])], [QP_b[kq], ks16_b], [PSB[6]])
                    P.op("dve", "memset", (), [wk_b], args=(wk_t, -1e30))
                    P.op("dve", "memset", (), [sb_b], args=(sb_t, -1.0))
                    for hq in range(2):
                        own = 2 * i + hq
                        if own > 3:
                            P.op("dve", "tensor_copy", [PSB[6]], [wk_b], out=wk_t[:, 2 * hq:2 * hq + 2, 0:own],
                                 in_=psb[6][:, 64 * hq:64 * hq + 64].rearrange("p (a n) -> p a n", n=32)[:, :, 0:own])
                        for qq in range(2):
                            qb = 2 * hq + qq
                            if own > 3:
                                P.op("dve", "max", [wk_b], [t8_b], out=t8_t[:, qb, :], in_=wk_t[:, qb, 0:max(own, 8)])
                                P.op("dve", "tensor_scalar", [wk_b, t8_b], [sb_b], out=sb_t[:, qb, 0:own], in0=wk_t[:, qb, 0:own],
                                     scalar1=t8_t[:, qb, 2:3], scalar2=1.0, op0=ALU.is_ge, op1=ALU.subtract)
                                P.op("dve", "memset", (), [sb_b], args=(sb_t[:, qb, own:own + 1], 0.0))
                            else:
                                P.op("dve", "memset", (), [sb_b], args=(sb_t[:, qb, 0:own + 1], 0.0))
                    P.op("dve", "tensor_scalar", [sb_b], [sb_b], out=sb_t, in0=sb_t, scalar1=BIG, scalar2=None, op0=ALU.mult)
                return pre

            def mkpre2(i, kq=kq, Qt=Qt):
                def pre():
                    for qb in range(4):
                        P.op("pe", "transpose", [sb_b, identf_b], [PSB[7]], qb == 3, out=psb[7][0:32, qb * 128:(qb + 1) * 128],
                             in_=sb_t[:, qb, :], identity=identf_t)
                    P.op("dve", "tensor_copy", [PSB[7]], [QA_b[kq][i]], out=Qt[64:96, i * 512:(i + 1) * 512], in_=psb[7][0:32, :])
                return pre
            mkpre1(0)(); mkpre2(0)()
            for i in range(NT):
                hooks = {}
                if i + 1 < NT:
                    hooks[0] = mkpre1(i + 1)
                    hooks[4 * i + 3] = mkpre2(i + 1)
                items += attn_items(Kt, KP_b[kq], Qt, [QP_b[kq], QA_b[kq][i]], 0, 96, VA[vk], VA_b[vk], None, i, 384 + m * 64, hooks)
            run_pipe(items)
        for oh in range(2):
            for g in range(3):
                d = DIL[g]
                ptile = 2 + g
                hd = 4 + 2 * g + oh
                kq = cnt["kq"] % 2; cnt["kq"] += 1
                load_rows(KP[kq], KP_b[kq], KT16, ptile * 128, 128, KTb)
                load_rows(QP[kq], QP_b[kq], QT16, ptile * 128, 128, QTb)
                vk = cnt["va"] % 2; cnt["va"] += 1
                load_va(vk, hd, d)
                Kt = KP[kq]; Qt = QP[kq]; r0 = oh * 64
                KQ = [KP_b[kq], QP_b[kq]]
                nbd = NB // d
                items = []
                for r in range(d):
                    for ub4 in range(0, nbd, 4):
                        po = 4 + cnt["po"] % 2; cnt["po"] += 1
                        nu = min(4, nbd - ub4)
                        for u in range(nu):
                            ub = ub4 + u
                            ps = cnt["ps"] % 3; cnt["ps"] += 1
                            pk = cnt["pt"] % 4; cnt["pt"] += 1
                            qa = r + d * 128 * ub
                            lo = 0 if ub > 0 else 128
                            bi = r * nbd + ub

                            def f_score(ub=ub, ps=ps, qa=qa, d=d, r0=r0, Kt=Kt, Qt=Qt, KQ=KQ):
                                qap = Qt[r0:r0 + 64, qa: qa + d * 127 + 1: d]
                                if ub > 0:
                                    ka = qa - d * 128
                                    mm_group(psb[ps][:, 0:128], [(Kt[r0:r0 + 64, ka: ka + d * 127 + 1: d], qap)], KQ, [PSB[ps]])
                                mm_group(psb[ps][:, 128:256], [(Kt[r0:r0 + 64, qa: qa + d * 127 + 1: d], qap)], KQ, [PSB[ps]])

                            def f_soft(ps=ps, pk=pk, lo=lo):
                                ACT(pt_t[pk][:, lo:256], psb[ps][:, lo:256], AF.Exp, [PSB[ps]], [pt_b[pk]], scale=0.125)
                                TT("pool", pt_t[pk][:, lo:256], pt_t[pk][:, lo:256], cmask_t[:, lo:256], ALU.mult, [pt_b[pk], cmask_b], [pt_b[pk]])

                            def f_pv(ub=ub, u=u, nu=nu, po=po, pk=pk, bi=bi, vk=vk, g=g, r=r, d=d, ub4=ub4):
                                if ub > 0:
                                    MM(psb[po][:, u * 128:(u + 1) * 128], VA[vk][:, bi - 1, :], pt_t[pk][:, 0:128], True, False,
                                       [VA_b[vk], pt_b[pk]], [PSB[po]], inc=False)
                                MM(psb[po][:, u * 128:(u + 1) * 128], VA[vk][:, bi, :], pt_t[pk][:, 128:256], ub == 0, True,
                                   [VA_b[vk], pt_b[pk]], [PSB[po]], inc=True)
                                if u == nu - 1:
                                    a0 = r + d * 128 * ub4
                                    accv = acc_t[:, a0: a0 + d * (128 * nu - 1) + 1: d]
                                    if g == 0:
                                        P.op("dve", "tensor_copy", [PSB[po]], [acc_b], out=accv, in_=psb[po][:, 0:128 * nu])
                                    else:
                                        TT("dve", accv, psb[po][:, 0:128 * nu], accv, ALU.add, [PSB[po], acc_b], [acc_b])
                            items.append((f_score, f_soft, f_pv))
                run_pipe(items)
            for i in range(NT):
                yk = cnt["y"] % 2; cnt["y"] += 1
                P.op("dve", "reciprocal", [acc_b], [rec_b], out=rec_t[64:128, :], in_=acc_t[64:128, i * 512:(i + 1) * 512])
                P.op("dve", "tensor_copy", [rec_b], [rec0_b], out=rec0_t[0:64, :], in_=rec_t[64:128, :])
                TT("dve", yst[yk][0:64, :], acc_t[0:64, i * 512:(i + 1) * 512], rec0_t[0:64, :], ALU.mult, [acc_b, rec0_b], [yst_b[yk]])
                P.dma("pool", [(YT16[256 + oh * 64:256 + oh * 64 + 64, i * 512:(i + 1) * 512], yst[yk][0:64, :])], yst_b[yk], R=[yst_b[yk]], W=[YTb])

        P.barrier()
        ar.reset()
        xt = [ar.f32(4096).rearrange("p (k n) -> p k n", n=512) for _ in range(4)]
        xt_b = [P.buf("xt%d" % i, True) for i in range(4)]
        yt = [ar.bf(3072).rearrange("p (k n) -> p k n", n=512) for _ in range(2)]
        yt_b = [P.buf("ytC%d" % i, True) for i in range(2)]
        pp = [ar.f32(1024).rearrange("p (k n) -> p k n", n=512) for _ in range(2)]
        pp_b = [P.buf("ppC%d" % i, True) for i in range(2)]
        p16 = [ar.bf(1024).rearrange("p (k n) -> p k n", n=512) for _ in range(2)]
        p16_b = [P.buf("p16_%d" % i) for i in range(2)]
        hh = [ar.bf(4096).rearrange("p (k n) -> p k n", n=512) for _ in range(2)]
        hh_b = [P.buf("hC%d" % i) for i in range(2)]
        sqo_t = ar.bf(4096).rearrange("p (k n) -> p k n", n=512); sqo_b = P.buf("sqo")
        sqx_t = ar.bf(4096).rearrange("p (k n) -> p k n", n=512); sqx_b = P.buf("sqx")
        mg_t = ar.bf(4096).rearrange("p (k n) -> p k n", n=512); mg_b = P.buf("mgC")
        o_t = ar.f32(4096).rearrange("p (k n) -> p k n", n=512); o_b = P.buf("oC")
        ff_t = ar.bf(NFF * 512).rearrange("p (k n) -> p k n", n=512); ff_b = P.buf("ffC")
        rstd_t = ar.f32(512); rstd_b = P.buf("rstd")
        tmp_t = ar.f32(512); tmp_b = P.buf("tmp")
        gs = [ar.f32(512) for _ in range(3)]; gs_b = [P.buf("gs%d" % i) for i in range(3)]
        ma = [ar.f32(512) for _ in range(3)]; ma_b = [P.buf("ma%d" % i) for i in range(3)]
        tt = [ar.f32(512) for _ in range(2)]; tt_b = [P.buf("tt%d" % i) for i in range(2)]
        ring = Ring(6, 1024, "ringC")
        prc = {"i": 0}

        def nps():
            k = prc["i"] % 7; prc["i"] += 1
            return k

        def loadXY(t):
            k = t % 2; xk = t % 4
            P.dma("sp", [(xt[xk][:, 0:4, :], xtile_ap(xsrc(l), t)[:, 0:4, :]),
                         (xt[xk][:, 4:8, :], xtile_ap(xsrc(l), t)[:, 4:8, :])], xt_b[xk], R=[Xb], W=[xt_b[xk]])
            P.dma("sp", [(yt[k], YT16[:, t * 512:(t + 1) * 512].rearrange("(k p) n -> p k n", p=128))], yt_b[k], R=[YTb], W=[yt_b[k]])

        def loadP(t):
            k = t % 2
            P.dma("sp", [(pp[k], pT[l * PLE:(l + 1) * PLE, t * 512:(t + 1) * 512].rearrange("(k p) n -> p k n", p=128))], pp_b[k], W=[pp_b[k]])

        def stats(sq_t, sq_b):
            rms_stats(sq_t, sq_b, rstd_t, rstd_b, tmp_t, tmp_b)

        XK = {0: 0, 1: 1}

        def mk_h(k, gcol):
            for kc in range(8):
                P.op("dve", "scalar_tensor_tensor", [xt_b[XK[k]], rstd_b, gains_b], [hh_b[k]], out=hh[k][:, kc, :], in0=xt[XK[k]][:, kc, :],
                     scalar=gains_t[:, gcol + kc:gcol + kc + 1], in1=rstd_t, op0=ALU.mult, op1=ALU.mult)

        def pre1(k):
            ACT(sqx_t, xt[XK[k]], AF.Square, [xt_b[XK[k]]], [sqx_b])
            stats(sqx_t, sqx_b)
            mk_h(k, g0 + 0)

        def residual(k, gcol, want_sq):
            stats(sqo_t, sqo_b)
            for m in range(8):
                q = m % 2
                P.op("dve", "scalar_tensor_tensor", [o_b, rstd_b, gains_b], [tt_b[q]], out=tt[q], in0=o_t[:, m, :],
                     scalar=gains_t[:, gcol + m:gcol + m + 1], in1=rstd_t, op0=ALU.mult, op1=ALU.mult)
                xk = XK[k]
                TT("pool" if m % 2 == 0 else "dve", xt[xk][:, m, :], xt[xk][:, m, :], tt[q], ALU.add, [xt_b[xk], tt_b[q]], [xt_b[xk]])
                if want_sq:
                    ACT(sqx_t[:, m, :], xt[xk][:, m, :], AF.Square, [xt_b[xk]], [sqx_b])

        def evac_o(ps, m):
            ACT(sqo_t[:, m, :], psb[ps][:, :], AF.Square, [PSB[ps]], [sqo_b])
            ACT(o_t[:, m, :], psb[ps][:, :], AF.Copy, [PSB[ps]], [o_b])

        def S1(k, inject):
            ychunks = [(0, 2), (2, 1), (3, 3)]
            for m in range(8):
                if m == 3 and inject is not None:
                    inject()
                wb3, wb_b = ring.load(wslab("wBR", l, m), 768, [WB[l]])
                for b in range(3):
                    wg3, wg_b = ring.load(wslab("wG", l, b * 8 + m), 1024, [WB[l]])
                    pg = nps()
                    mm_group(psb[pg][:, :], [(wg3[:, kc, :], hh[k][:, kc, :]) for kc in range(8)], [wg_b, hh_b[k]], [PSB[pg]])
                    ACT(gs[b], psb[pg][:, :], AF.Sigmoid, [PSB[pg]], [gs_b[b]])
                    c0, ncc = ychunks[b]
                    pq = nps()
                    mm_group(psb[pq][:, :], [(wb3[:, c0 + c, :], yt[k][:, c0 + c, :]) for c in range(ncc)], [wb_b, yt_b[k]], [PSB[pq]])
                    TT("dve", ma[b], psb[pq][:, :], gs[b], ALU.mult, [PSB[pq], gs_b[b]], [ma_b[b]])
                TT("pool", ma[0], ma[0], ma[1], ALU.add, [ma_b[0], ma_b[1]], [ma_b[0]])
                TT("pool", mg_t[:, m, :], ma[0], ma[2], ALU.add, [ma_b[0], ma_b[2]], [mg_b])
            for m in range(8):
                w3, w_b = ring.load(wslab("wO", l, m), 1024, [WB[l]])
                ps = nps()
                mm_group(psb[ps][:, :], [(w3[:, kc, :], mg_t[:, kc, :]) for kc in range(8)], [w_b, mg_b], [PSB[ps]])
                evac_o(ps, m)

        def S2(k, inject):
            for j in range(NFF):
                if j == 8 and inject is not None:
                    inject()
                wg3, wg_b = ring.load(wslab("wFG", l, j), 1024, [WB[l]])
                pg = nps()
                mm_group(psb[pg][:, :], [(wg3[:, kc, :], hh[k][:, kc, :]) for kc in range(8)], [wg_b, hh_b[k]], [PSB[pg]])
                kk = j % 3
                ACT(gs[kk], psb[pg][:, :], AF.Silu, [PSB[pg]], [gs_b[kk]])
                wu3, wu_b = ring.load(wslab("wFU", l, j), 1024, [WB[l]])
                pu = nps()
                mm_group(psb[pu][:, :], [(wu3[:, kc, :], hh[k][:, kc, :]) for kc in range(8)], [wu_b, hh_b[k]], [PSB[pu]])
                TT("dve", ff_t[:, j, :], psb[pu][:, :], gs[kk], ALU.mult, [PSB[pu], gs_b[kk]], [ff_b])
            for m in range(8):
                ps = nps()
                pieces = [(0, 8), (8, 16), (16, NFF)]
                first = True
                for (j0, j1) in pieces:
                    w3, w_b = ring.load(wslab("wFD", l, m)[:, j0 * 128:j1 * 128], (j1 - j0) * 128, [WB[l]])
                    for j in range(j0, j1):
                        MM(psb[ps][:, :], w3[:, j - j0, :], ff_t[:, j, :], first, j == NFF - 1, [w_b, ff_b], [PSB[ps]], inc=(j == j1 - 1))
                        first = False
                evac_o(ps, m)

        def d2(k):
            ACT(hh[k], xt[XK[k]], AF.Copy, [xt_b[XK[k]]], [hh_b[k]])
            P.op("dve", "tensor_copy", [pp_b[k]], [p16_b[k]], out=p16[k], in_=pp[k])

        def S3(k, inject):
            for m in range(8):
                if m == 3 and inject is not None:
                    inject()
                wg3, wg_b = ring.load(wslab("wPG", l, m), 1024, [WB[l]])
                pg = nps()
                mm_group(psb[pg][:, :], [(wg3[:, kc, :], hh[k][:, kc, :]) for kc in range(8)], [wg_b, hh_b[k]], [PSB[pg]])
                kk = m % 3
                ACT(gs[kk], psb[pg][:, :], AF.Sigmoid, [PSB[pg]], [gs_b[kk]])
                wp3, wp_b = ring.load(wslab("wPL", l, m), 256, [WB[l]])
                pu = nps()
                mm_group(psb[pu][:, :], [(wp3[:, kc, :], p16[k][:, kc, :]) for kc in range(2)], [wp_b, p16_b[k]], [PSB[pu]])
                TT("dve", o_t[:, m, :], psb[pu][:, :], gs[kk], ALU.mult, [PSB[pu], gs_b[kk]], [o_b])
                ACT(sqo_t[:, m, :], o_t[:, m, :], AF.Square, [o_b], [sqo_b])

        def storeC(t):
            xk = t % 4
            P.dma("pool", [(xtile_ap(xdst(l), t)[:, 0:4, :], xt[xk][:, 0:4, :]), (xtile_ap(xdst(l), t)[:, 4:8, :], xt[xk][:, 4:8, :])],
                  xt_b[xk], R=[xt_b[xk]], W=[Xb])

        def c2(k):
            stats(sqx_t, sqx_b)
            mk_h(k, g0 + 16)

        loadXY(0); loadP(0); loadXY(1); loadP(1)
        XK[0] = 0
        pre1(0)
        for tp in range(0, NT, 2):
            tA, tB = tp, tp + 1
            nxt = tp + 2 < NT
            XK[0] = tA % 4; XK[1] = tB % 4

            def inj_pre1B():
                pre1(1)
            S1(0, inj_pre1B)
            if nxt:
                loadXY(tA + 2)
            residual(0, g0 + 8, True)
            S1(1, lambda: c2(0))
            if nxt:
                loadXY(tB + 2)
            residual(1, g0 + 8, True)
            S2(0, lambda: c2(1))
            residual(0, g0 + 24, False)
            S2(1, lambda: d2(0))
            if nxt:
                loadP(tA + 2)
            residual(1, g0 + 24, False)
            S3(0, lambda: d2(1))
            if nxt:
                loadP(tB + 2)
            residual(0, g0 + 32, False)
            storeC(tA)

            def inj_next():
                XK[0] = (tA + 2) % 4
                pre1(0)
                XK[0] = tA % 4
            S3(1, inj_next if nxt else None)
            residual(1, g0 + 32, False)
            storeC(tB)
    P.barrier()
    block = es.enter_context(nc.Block())
    P.replay(block)
    es.close()
    return nc


def _slabs(w, cols_list, kc):
    out = np.empty((len(cols_list), 128, kc, 128), np.float32)
    wk = w.reshape(kc, 128, w.shape[1])
    for m, cols in enumerate(cols_list):
        out[m] = wk[:, :, cols].transpose(1, 0, 2)
    return out.reshape(len(cols_list) * 128, kc * 128)


def _host_weights(inp, L):
    r = {}
    ar_ = np.arange
    lists = {n: [] for n in ("wA", "wV", "wF", "wG", "wBR", "wO", "wFG", "wFU", "wFD", "wPL", "wPG")}
    for l in range(L):
        w_in = np.asarray(inp["w_in"][l], np.float32)
        colsA = []
        for base in (0, 1024):
            for pt in range(8):
                c = base + pt * 128 + ar_(128)
                colsA.append(c)
                if pt >= 2:
                    sw = base + pt * 128 + (ar_(128) // 64) * 64 + (ar_(128) % 64 + 32) % 64
                    colsA.append(sw)
        lists["wA"].append(_slabs(w_in, colsA, 8))
        wv = w_in[:, 2048:3072].reshape(8, 128, 1024).transpose(1, 0, 2).reshape(128, 8192)
        lists["wV"].append(wv)
        wf = w_in[:, 3072:3076].reshape(8, 128, 4).transpose(1, 0, 2).reshape(128, 32)
        lists["wF"].append(wf)
        lists["wG"].append(_slabs(w_in, [3076 + b * 1024 + m * 128 + ar_(128) for b in range(3) for m in range(8)], 8))
        wbr = np.concatenate([np.asarray(inp["w_br_a"][l]), np.asarray(inp["w_br_b"][l]), np.asarray(inp["w_br_c"][l])], axis=0)
        lists["wBR"].append(_slabs(wbr.astype(np.float32), [m * 128 + ar_(128) for m in range(8)], 6))
        lists["wO"].append(_slabs(np.asarray(inp["w_out"][l], np.float32), [m * 128 + ar_(128) for m in range(8)], 8))
        lists["wFG"].append(_slabs(np.asarray(inp["w_ffn_gate"][l], np.float32), [m * 128 + ar_(128) for m in range(NFF)], 8))
        lists["wFU"].append(_slabs(np.asarray(inp["w_ffn_up"][l], np.float32), [m * 128 + ar_(128) for m in range(NFF)], 8))
        lists["wFD"].append(_slabs(np.asarray(inp["w_ffn_down"][l], np.float32), [m * 128 + ar_(128) for m in range(8)], NFF))
        lists["wPL"].append(_slabs(np.asarray(inp["w_ple"][l], np.float32), [m * 128 + ar_(128) for m in range(8)], 2))
        lists["wPG"].append(_slabs(np.asarray(inp["w_ple_gate"][l], np.float32), [m * 128 + ar_(128) for m in range(8)], 8))
    for n, v in lists.items():
        r[n] = np.ascontiguousarray(np.concatenate(v, axis=0), dtype=np.float32)
    gl = []
    for l in range(L):
        for n in ("g_mix_pre", "g_mix_post", "g_ffn_pre", "g_ffn_post", "g_ple_post"):
            gl.append(np.asarray(inp[n][l], np.float32).reshape(8, 128).T)
    r["gains"] = np.ascontiguousarray(np.concatenate(gl, axis=1), dtype=np.float32)
    r["bfb"] = np.ascontiguousarray(np.broadcast_to(np.asarray(inp["b_f"], np.float32).reshape(1, L * 4), (128, L * 4)))
    return r


def _consts(S):
    c = {}
    inv = (1.0 / (np.float32(10000.0) ** (np.arange(0, 64, 2, dtype=np.float32) / np.float32(64)))).astype(np.float32)
    ang = (np.arange(S, dtype=np.float32)[:, None] * inv[None, :]).astype(np.float32)
    cos = np.cos(ang.astype(np.float64)).astype(np.float32).T
    sin = np.sin(ang.astype(np.float64)).astype(np.float32).T
    c["cosT"] = np.ascontiguousarray(np.concatenate([cos, cos, cos, cos], axis=0))
    c["sinT"] = np.ascontiguousarray(np.concatenate([-sin, sin, -sin, sin], axis=0))
    p = np.arange(128)
    c["cmask"] = np.ascontiguousarray(np.concatenate([(p[:, None] >= p[None, :]), (p[:, None] <= p[None, :])], axis=1).astype(np.float32))
    c["trif"] = np.ascontiguousarray((p[:, None] <= p[None, :]).astype(np.float32))
    c["identf"] = np.eye(128, dtype=np.float32)
    c["blkind"] = np.ascontiguousarray(np.concatenate([(np.arange(S)[None, :] // 256 == np.arange(32)[:, None]), np.ones((1, S), bool)], axis=0).astype(np.float32))
    return c


_NC_CACHE = {}


def kernel(**inputs):
    x = np.asarray(inputs["x"], np.float32)
    p = np.asarray(inputs["p"], np.float32)
    B, S, _ = x.shape
    L = p.shape[0]
    key = (S, L)
    if key not in _NC_CACHE:
        _NC_CACHE[key] = build(S, L)
    nc = _NC_CACHE[key]
    shared = _host_weights(inputs, L)
    shared.update(_consts(S))
    in_maps = []
    for b in range(B):
        m = dict(shared)
        m["xT"] = np.ascontiguousarray(x[b].T)
        m["pT"] = np.ascontiguousarray(p[:, b].transpose(0, 2, 1).reshape(L * PLE, S))
        in_maps.append(m)
    res = run_bass_kernel_spmd(nc, in_maps, core_ids=list(range(B)))
    out = np.stack([np.ascontiguousarray(res.results[b]["outT"].T) for b in range(B)], axis=0)
    return out.astype(np.float32)
```

```python
import numpy as np
from contextlib import ExitStack
import concourse.bass as bass
import concourse.mybir as mybir
from concourse.bass_utils import run_bass_kernel_spmd

F32 = mybir.dt.float32
BF = mybir.dt.bfloat16
AF = mybir.ActivationFunctionType
ALU = mybir.AluOpType
AX = mybir.AxisListType

D = 1024
HD = 64
PLE = 256
DFF = 2816
NFF = 22
BIG = 30000.0
DIL = (1, 4, 16)
SAME_ENGINE_SYNC = True


class Buf:
    def __init__(self, name, sem=None):
        self.name = name
        self.w = {}
        self.r = {}
        self.sem = sem
        self.cnt = 0


class Eng:
    def __init__(self, name, sem):
        self.name = name
        self.sem = sem
        self.cnt = 0
        self.seen = {}
        self.prog = []


class Prog:
    def __init__(self, nc, es):
        self.nc = nc
        self.es = es
        self.sems = []
        self.E = {}
        for n in ("pe", "act", "dve", "pool"):
            self.E[n] = Eng(n, self.newsem(n))
        self.E["sp"] = Eng("sp", None)
        self.bufs = []
        self.bynames = {}

    def newsem(self, name):
        h = self.es.enter_context(self.nc.semaphore("s_" + name))
        self.sems.append(h)
        return len(self.sems) - 1

    def buf(self, name, dma=False):
        if name in self.bynames:
            return self.bynames[name]
        b = Buf(name, self.newsem(name) if dma else None)
        self.bufs.append(b)
        self.bynames[name] = b
        return b

    def _waits(self, X, R, W):
        need = {}
        for b in R:
            for k, v in b.w.items():
                need[k] = max(need.get(k, 0), v)
        for b in W:
            for k, v in b.w.items():
                need[k] = max(need.get(k, 0), v)
            for k, v in b.r.items():
                need[k] = max(need.get(k, 0), v)
        out = []
        for k, v in need.items():
            if k == X.sem and (X.name == "pe" or not SAME_ENGINE_SYNC):
                continue
            if X.seen.get(k, 0) >= v:
                continue
            X.seen[k] = v
            out.append((k, v))
        return out

    def op(self, eng, name, R=(), W=(), inc=True, args=(), **kw):
        X = self.E[eng]
        waits = self._waits(X, R, W)
        tok = X.cnt + 1
        if inc:
            X.cnt = tok
        X.prog.append((waits, ("op", name, args, kw), inc))
        for b in R:
            b.r[X.sem] = tok
        for b in W:
            b.w = {X.sem: tok}
            b.r = {}

    def dma(self, q, pairs, sb, R=(), W=()):
        X = self.E[q]
        waits = self._waits(X, R, W)
        first = True
        for o, i in pairs:
            sb.cnt += 16
            X.prog.append((waits if first else [], ("dma", o, i, sb.sem), False))
            first = False
        for b in R:
            b.r[sb.sem] = sb.cnt
        for b in W:
            b.w = {sb.sem: sb.cnt}
            b.r = {}

    def barrier(self):
        toks = {}
        for n in ("pe", "act", "dve", "pool"):
            toks[self.E[n].sem] = self.E[n].cnt
        for b in self.bufs:
            if b.sem is not None and b.cnt > 0:
                toks[b.sem] = b.cnt
        for n, X in self.E.items():
            waits = []
            for k, v in toks.items():
                if v > 0 and X.seen.get(k, 0) < v and not (k == X.sem and n == "pe"):
                    X.seen[k] = v
                    waits.append((k, v))
            X.prog.append((waits, None, False))

    def replay(self, block):
        sems = self.sems

        def run(X, e):
            for waits, fn, inc in X.prog:
                for k, v in waits:
                    e.wait_ge(sems[k], v)
                if fn is None:
                    continue
                if fn[0] == "dma":
                    _, o, i, k = fn
                    e.dma_start(out=o, in_=i).then_inc(sems[k], 16)
                else:
                    _, name, args, kw = fn
                    ins = getattr(e, name)(*args, **kw)
                    if inc:
                        ins.then_inc(sems[X.sem], 1)

        @block.sync
        def _(e):
            run(self.E["sp"], e)

        @block.tensor
        def _(e):
            run(self.E["pe"], e)

        @block.scalar
        def _(e):
            run(self.E["act"], e)

        @block.vector
        def _(e):
            run(self.E["dve"], e)

        @block.gpsimd
        def _(e):
            run(self.E["pool"], e)


class Arena:
    def __init__(self, ap, lo, hi):
        self.ap = ap
        self.lo = lo
        self.hi = hi
        self.p = lo

    def f32(self, n):
        a = self.ap[:, self.p:self.p + n]
        self.p += n
        assert self.p <= self.hi, ("arena overflow", self.p, self.hi)
        return a

    def bf(self, n):
        n2 = (n + 1) // 2
        a = self.ap[:, self.p:self.p + n2].bitcast(BF)
        self.p += n2
        assert self.p <= self.hi, ("arena overflow", self.p, self.hi)
        return a[:, 0:n]

    def reset(self):
        self.p = self.lo


def build(S=8192, L=2, dbg=False):
    NT = S // 512
    NB = S // 128
    NMB = S // 256
    CW = min(2048, S)
    assert NMB <= 32
    nc = bass.Bass("TRN2", target_bir_lowering=False)

    def din(name, shape, dt=F32):
        return nc.dram_tensor(name, shape, dt, kind="ExternalInput").ap()

    def dscr(name, shape, dt=BF):
        return nc.dram_tensor(name, shape, dt, kind=("ExternalOutput" if dbg else "Internal")).ap()

    xT = din("xT", [D, S])
    pT = din("pT", [L * PLE, S])
    wspec = [("wA", 28, 1024), ("wV", 1, 8192), ("wF", 1, 32), ("wG", 24, 1024), ("wBR", 8, 768),
             ("wO", 8, 1024), ("wFG", 22, 1024), ("wFU", 22, 1024), ("wFD", 8, 2816),
             ("wPL", 8, 256), ("wPG", 8, 1024)]
    w32 = {}
    w16 = {}
    for n, ns, nc_ in wspec:
        w32[n] = din(n, [L * ns * 128, nc_])
        w16[n] = nc.dram_tensor(n + "_16", [L * ns * 128, nc_], BF, kind="Internal").ap()
    wns = {n: ns for n, ns, _ in wspec}
    gains = din("gains", [128, L * 5 * 8])
    bfb = din("bfb", [128, L * 4])
    cosT = din("cosT", [128, S])
    sinT = din("sinT", [128, S])
    cmask32 = din("cmask", [128, 256])
    trif = din("trif", [128, 128])
    identf = din("identf", [128, 128])
    blkind32 = din("blkind", [33, S])
    outT = nc.dram_tensor("outT", [D, S], F32, kind="ExternalOutput").ap()

    X32 = dscr("X32", [D, S], F32)
    QT16 = dscr("QT16", [1024, S])
    KT16 = dscr("KT16", [1024, S])
    V16 = dscr("V16", [S, 1024])
    YT16 = dscr("YT16", [768, S])
    KS32 = dscr("KS32", [384, 32], F32)
    BI16 = nc.dram_tensor("BI16", [33, S], BF, kind="Internal").ap()

    es = ExitStack()
    P = Prog(nc, es)
    AW = 52992
    arena_t = es.enter_context(nc.sbuf_tensor("arena", [128, AW], F32))
    PERS = 2048
    pers = Arena(arena_t, 0, PERS)
    ar = Arena(arena_t, PERS, AW)
    psb = [es.enter_context(nc.psum_tensor("psb%d" % i, [128, 512], F32)) for i in range(8)]
    PSB = [P.buf("psb%d" % i) for i in range(8)]

    def ACT(out, in_, func, R, W, **kw):
        P.op("act", "activation", R, W, out=out, in_=in_, func=func, **kw)

    def TT(eng, out, in0, in1, op, R, W):
        P.op(eng, "tensor_tensor", R, W, out=out, in0=in0, in1=in1, op=op)

    def MM(out, lhsT, rhs, start, stop, R, W, inc=True):
        P.op("pe", "matmul", R, W, inc, args=(out,), lhsT=lhsT, rhs=rhs, start=start, stop=stop)

    def mm_group(out_ap, pairs, Rb, Wb):
        n = len(pairs)
        for i, (lt, rh) in enumerate(pairs):
            MM(out_ap, lt, rh, i == 0, i == n - 1, Rb, Wb, inc=(i == n - 1))

    gains_t = pers.f32(L * 40); gains_b = P.buf("gains", True)
    bfb_t = pers.f32(L * 4); bfb_b = P.buf("bfb", True)
    trif_t = pers.f32(128); trif_b = P.buf("trif", True)
    identf_t = pers.f32(128); identf_b = P.buf("identf", True)
    onesf_t = pers.f32(128); onesf_b = P.buf("onesf")
    ones16_t = pers.bf(128); ones16_b = P.buf("ones16")
    cmask_t = pers.bf(256); cmask_b = P.buf("cmask", True)
    eps_t = pers.f32(1); one_t = pers.f32(1); cst_b = P.buf("cst")
    cpos_t = pers.f32(NB * 4).rearrange("p (j h) -> p j h", h=4); cpos_b = P.buf("cpos")
    tall_t = pers.f32((NB + 1) * 4).rearrange("p (j h) -> p j h", h=4); tall_b = P.buf("tall")
    ksum_t = pers.f32(3 * 32).rearrange("p (a n) -> p a n", n=32); ksum_b = P.buf("ksum", True)

    P.dma("sp", [(gains_t, gains)], gains_b, W=[gains_b])
    P.dma("sp", [(bfb_t, bfb)], bfb_b, W=[bfb_b])
    P.dma("sp", [(trif_t, trif)], trif_b, W=[trif_b])
    P.dma("sp", [(identf_t, identf)], identf_b, W=[identf_b])
    P.dma("pool", [(cmask_t, cmask32)], cmask_b, W=[cmask_b])
    P.op("dve", "memset", (), [onesf_b], args=(onesf_t, 1.0))
    P.op("dve", "memset", (), [ones16_b], args=(ones16_t, 1.0))
    P.op("dve", "memset", (), [cst_b], args=(eps_t, 1e-6))
    P.op("dve", "memset", (), [cst_b], args=(one_t, 1.0))
    P.op("dve", "memset", (), [tall_b], args=(tall_t[:, 0, :], 0.0))
    P.op("dve", "memset", (), [ksum_b], args=(ksum_t, 0.0))

    WB = [P.buf("w16_%d" % l) for l in range(L)]
    CB = [P.buf("cast%d" % i, True) for i in range(2)]
    BIb = P.buf("bi16", True)
    P.dma("pool", [(BI16[:, c0:c0 + CW], blkind32[:, c0:c0 + CW]) for c0 in range(0, S, CW)], BIb, W=[BIb])
    cg = 0
    for l in range(L):
        pairs = []
        for n, ns, ncol in wspec:
            for s_ in range(ns):
                r0 = (l * ns + s_) * 128
                cw = 2048 if ncol > 2816 else ncol
                for c0 in range(0, ncol, cw):
                    pairs.append((w16[n][r0:r0 + 128, c0:c0 + cw], w32[n][r0:r0 + 128, c0:c0 + cw]))
        for g0_ in range(0, len(pairs), 8):
            cb = CB[cg % 2]; cg += 1
            P.dma("pool", pairs[g0_:g0_ + 8], cb, W=[cb])
        WB[l].w = {CB[0].sem: CB[0].cnt, CB[1].sem: CB[1].cnt}

    def wslab(n, l, s_):
        r0 = (l * wns[n] + s_) * 128
        return w16[n][r0:r0 + 128, :]

    Xb = P.buf("X32d"); QTb = P.buf("QTd"); KTb = P.buf("KTd"); Vb = P.buf("Vd"); YTb = P.buf("YTd"); KSb = P.buf("KSd")

    def xsrc(l):
        return xT if l == 0 else X32

    def xdst(l):
        return outT if l == L - 1 else X32

    def xtile_ap(dram, t):
        return dram[:, t * 512:(t + 1) * 512].rearrange("(kc p) n -> p kc n", p=128)

    class Ring:
        def __init__(self, n, words, name):
            self.t = [ar.bf(words) for _ in range(n)]
            self.b = [P.buf("%s%d" % (name, i), True) for i in range(n)]
            self.i = 0

        def load(self, dram_ap, ncol, Rb):
            k = self.i % len(self.t)
            self.i += 1
            P.dma("sp", [(self.t[k][:, 0:ncol], dram_ap)], self.b[k], R=Rb, W=[self.b[k]])
            return self.t[k][:, 0:ncol].rearrange("p (k n) -> p k n", n=128), self.b[k]

    def rms_stats(sq_t, sq_b, rstd_t, rstd_b, tmp_t, tmp_b):
        mm_group(psb[7][:, :], [(ones16_t, sq_t[:, kc, :]) for kc in range(8)], [ones16_b, sq_b], [PSB[7]])
        ACT(tmp_t, psb[7][:, :], AF.Sqrt, [PSB[7], cst_b], [tmp_b], bias=eps_t, scale=1.0 / D)
        P.op("dve", "reciprocal", [tmp_b], [rstd_b], out=rstd_t, in_=tmp_t)

    def pre_norm(x_t, x_b, gcol, sq_t, sq_b, rstd_t, rstd_b, tmp_t, tmp_b, h_t, h_b):
        ACT(sq_t, x_t, AF.Square, [x_b], [sq_b])
        rms_stats(sq_t, sq_b, rstd_t, rstd_b, tmp_t, tmp_b)
        for kc in range(8):
            P.op("dve", "scalar_tensor_tensor", [x_b, rstd_b, gains_b], [h_b], out=h_t[:, kc, :], in0=x_t[:, kc, :],
                 scalar=gains_t[:, gcol + kc:gcol + kc + 1], in1=rstd_t, op0=ALU.mult, op1=ALU.mult)

    for l in range(L):
        g0 = l * 40
        P.barrier()
        ar.reset()
        xt = [ar.f32(4096).rearrange("p (k n) -> p k n", n=512) for _ in range(2)]
        xt_b = [P.buf("xt%d" % i, True) for i in range(2)]
        sq_t = ar.bf(4096).rearrange("p (k n) -> p k n", n=512); sq_b = P.buf("sq")
        h_t = ar.bf(4096).rearrange("p (k n) -> p k n", n=512); h_b = P.buf("h")
        rstd_t = ar.f32(512); rstd_b = P.buf("rstd")
        tmp_t = ar.f32(512); tmp_b = P.buf("tmp")
        wv_t = ar.bf(8192).rearrange("p (k n) -> p k n", n=1024); wv_b = P.buf("wvA", True)
        wf_t = ar.bf(32).rearrange("p (k n) -> p k n", n=4); wf_b = P.buf("wfA", True)
        ring = Ring(6, 1024, "ring")
        cs_t = [(ar.f32(512), ar.f32(512)) for _ in range(2)]
        cs_b = [P.buf("csA%d" % i, True) for i in range(2)]
        qst = [ar.bf(4096).rearrange("p (k n) -> p k n", n=512) for _ in range(2)]
        qst_b = [P.buf("qst%d" % i, True) for i in range(2)]
        kst = [ar.bf(4096).rearrange("p (k n) -> p k n", n=512) for _ in range(2)]
        kst_b = [P.buf("kst%d" % i, True) for i in range(2)]
        vst = [ar.bf(4096).rearrange("p (a n) -> p a n", n=1024) for _ in range(2)]
        vst_b = [P.buf("vst%d" % i, True) for i in range(2)]
        r1 = [ar.f32(512) for _ in range(2)]; r1_b = [P.buf("r1_%d" % i) for i in range(2)]
        r2 = [ar.f32(512) for _ in range(2)]; r2_b = [P.buf("r2_%d" % i) for i in range(2)]
        fb_t = ar.f32(4); fb_b = P.buf("fbA")
        fe_t = ar.f32(4); fe_b = P.buf("feA")
        fl_t = ar.f32(4); fl_b = P.buf("flA")

        P.dma("sp", [(wv_t[:, kc, :], wslab("wV", l, 0)[:, kc * 1024:(kc + 1) * 1024]) for kc in range(8)],
              wv_b, R=[WB[l]], W=[wv_b])
        P.dma("sp", [(wf_t, wslab("wF", l, 0).rearrange("p (k n) -> p k n", n=4))], wf_b, R=[WB[l]], W=[wf_b])

        def loadxA(t):
            k = t % 2
            P.dma("sp", [(xt[k][:, 0:4, :], xtile_ap(xsrc(l), t)[:, 0:4, :]),
                         (xt[k][:, 4:8, :], xtile_ap(xsrc(l), t)[:, 4:8, :])], xt_b[k], R=[Xb], W=[xt_b[k]])
            P.dma("sp", [(cs_t[k][0], cosT[:, t * 512:(t + 1) * 512]),
                         (cs_t[k][1], sinT[:, t * 512:(t + 1) * 512])], cs_b[k], W=[cs_b[k]])

        loadxA(0)
        psrot = 0
        for t in range(NT):
            if t + 1 < NT:
                loadxA(t + 1)
            x_t = xt[t % 2]; x_b = xt_b[t % 2]
            cos_t, sin_t = cs_t[t % 2]; c_b = cs_b[t % 2]
            pre_norm(x_t, x_b, g0 + 0, sq_t, sq_b, rstd_t, rstd_b, tmp_t, tmp_b, h_t, h_b)
            qs = qst[t % 2]; qs_b = qst_b[t % 2]; ks = kst[t % 2]; ks_b = kst_b[t % 2]
            si = 0
            for which in range(2):
                stg, stg_b = (qs, qs_b) if which == 0 else (ks, ks_b)
                for pt in range(8):
                    w3, w_b = ring.load(wslab("wA", l, si), 1024, [WB[l]]); si += 1
                    pa = psrot % 6; psrot += 1
                    mm_group(psb[pa][:, :], [(w3[:, kc, :], h_t[:, kc, :]) for kc in range(8)], [w_b, h_b], [PSB[pa]])
                    if pt < 2:
                        ACT(stg[:, pt, :], psb[pa][:, :], AF.Copy, [PSB[pa]], [stg_b])
                    else:
                        w23, w2_b = ring.load(wslab("wA", l, si), 1024, [WB[l]]); si += 1
                        pb = psrot % 6; psrot += 1
                        mm_group(psb[pb][:, :], [(w23[:, kc, :], h_t[:, kc, :]) for kc in range(8)], [w2_b, h_b], [PSB[pb]])
                        ri = (pt + which) % 2
                        TT("dve", r1[ri], psb[pa][:, :], cos_t, ALU.mult, [PSB[pa], c_b], [r1_b[ri]])
                        TT("dve", r2[ri], psb[pb][:, :], sin_t, ALU.mult, [PSB[pb], c_b], [r2_b[ri]])
                        TT("pool", stg[:, pt, :], r1[ri], r2[ri], ALU.add, [r1_b[ri], r2_b[ri]], [stg_b])
                        if which == 1 and pt >= 5:
                            P.op("dve", "tensor_reduce", [stg_b], [ksum_b], out=ksum_t[:, pt - 5, 2 * t:2 * t + 2],
                                 in_=stg[:, pt, :].rearrange("p (a b) -> p a b", b=256), axis=AX.X, op=ALU.add)
            P.dma("pool", [(QT16[:, t * 512:(t + 1) * 512].rearrange("(k p) n -> p k n", p=128), qs)], qs_b, R=[qs_b], W=[QTb])
            P.dma("pool", [(KT16[:, t * 512:(t + 1) * 512].rearrange("(k p) n -> p k n", p=128), ks)], ks_b, R=[ks_b], W=[KTb])
            vs = vst[t % 2]; vs_b = vst_b[t % 2]
            for tb in range(4):
                for hf in range(2):
                    pa = psrot % 6; psrot += 1
                    mm_group(psb[pa][:, :], [(h_t[:, kc, tb * 128:(tb + 1) * 128], wv_t[:, kc, hf * 512:(hf + 1) * 512]) for kc in range(8)],
                             [wv_b, h_b], [PSB[pa]])
                    ACT(vs[:, tb, hf * 512:(hf + 1) * 512], psb[pa][:, :], AF.Copy, [PSB[pa]], [vs_b])
                j = 4 * t + tb
                mm_group(psb[6][:, 0:4], [(h_t[:, kc, tb * 128:(tb + 1) * 128], wf_t[:, kc, :]) for kc in range(8)], [wf_b, h_b], [PSB[6]])
                TT("dve", fb_t, psb[6][:, 0:4], bfb_t[:, l * 4:l * 4 + 4], ALU.add, [PSB[6], bfb_b], [fb_b])
                ACT(fe_t, fb_t, AF.Exp, [fb_b], [fe_b], scale=-1.0)
                ACT(fl_t, fe_t, AF.Ln, [fe_b, cst_b], [fl_b], bias=one_t, scale=1.0)
                mm_group(psb[6][:, 8:12], [(trif_t, fl_t)], [trif_b, fl_b], [PSB[6]])
                mm_group(psb[6][:, 16:20], [(onesf_t, fl_t)], [onesf_b, fl_b], [PSB[6]])
                TT("dve", cpos_t[:, j, :], psb[6][:, 8:12], tall_t[:, j, :], ALU.add, [PSB[6], tall_b], [cpos_b])
                TT("dve", tall_t[:, j + 1, :], psb[6][:, 16:20], tall_t[:, j, :], ALU.add, [PSB[6], tall_b], [tall_b])
            P.dma("pool", [(V16[t * 512:(t + 1) * 512, :].rearrange("(a p) c -> p a c", p=128), vs)], vs_b, R=[vs_b], W=[Vb])
        P.dma("pool", [(KS32.rearrange("(a p) n -> p a n", p=128), ksum_t)], ksum_b, R=[ksum_b], W=[KSb])

        P.barrier()
        ar.reset()
        KP = [ar.bf(S) for _ in range(2)]; KP_b = [P.buf("KP%d" % i, True) for i in range(2)]
        QP = [ar.bf(S) for _ in range(2)]; QP_b = [P.buf("QP%d" % i, True) for i in range(2)]
        QA_b = [[P.buf("QA%d_%d" % (i, t)) for t in range(NT)] for i in range(2)]
        VA = [ar.bf(NB * 128).rearrange("p (j c) -> p j c", c=128) for _ in range(2)]
        VA_b = [P.buf("VA%d" % i, True) for i in range(2)]
        pt_t = [ar.bf(512) for _ in range(4)]; pt_b = [P.buf("pT%d" % i) for i in range(4)]
        rec_t = ar.f32(512); rec_b = P.buf("rec")
        rec0_t = ar.f32(512); rec0_b = P.buf("rec0")
        yst = [ar.bf(512) for _ in range(2)]; yst_b = [P.buf("yst%d" % i, True) for i in range(2)]
        ks16_t = ar.bf(32); ks16_b = P.buf("ks16")
        ks32_t = ar.f32(32); ks32_b = P.buf("ks32", True)
        wk_t = ar.f32(128).rearrange("p (a n) -> p a n", n=32); wk_b = P.buf("wk")
        t8_t = ar.f32(32).rearrange("p (a n) -> p a n", n=8); t8_b = P.buf("t8")
        sb_t = ar.f32(128).rearrange("p (a n) -> p a n", n=32); sb_b = P.buf("selb")
        acc_t = ar.f32(S); acc_b = P.buf("acc")
        for i in range(2):
            P.op("pool", "memset", (), [VA_b[i]], args=(VA[i][:, :, 64:128], 1.0))
        cnt = {"ps": 0, "po": 0, "pt": 0, "y": 0, "kq": 0, "va": 0}

        def load_rows(dst, dst_b, dram, r0, nr, rb, ind=False):
            pairs = [(dst[0:nr, c0:c0 + CW], dram[r0:r0 + nr, c0:c0 + CW]) for c0 in range(0, S, CW)]
            Rb = [rb]
            if ind == 1:
                pairs += [(dst[64:96, c0:c0 + CW], BI16[0:32, c0:c0 + CW]) for c0 in range(0, S, CW)]
                Rb = [rb, BIb]
            if ind == 2:
                pairs += [(dst[64:65, c0:c0 + CW], BI16[32:33, c0:c0 + CW]) for c0 in range(0, S, CW)]
                Rb = [rb, BIb]
            P.dma("sp", pairs, dst_b, R=Rb, W=[dst_b])

        def load_va(k, head, d):
            nbd = NB // d
            pairs = []
            for r in range(d):
                for b0 in range(0, nbd, 8):
                    nb_ = min(8, nbd - b0)
                    src = V16[r + d * 128 * b0: r + d * 128 * b0 + d * (128 * nb_ - 1) + 1: d, head * 64:(head + 1) * 64]
                    pairs.append((VA[k][:, r * nbd + b0: r * nbd + b0 + nb_, 0:64], src.rearrange("(j p) c -> p j c", p=128)))
            for g_ in range(0, len(pairs), 4):
                P.dma("sp", pairs[g_:g_ + 4], VA_b[k], R=[Vb], W=[VA_b[k]])

        def finalize(po, yrow, i):
            yk = cnt["y"] % 2; cnt["y"] += 1
            P.op("dve", "reciprocal", [PSB[po]], [rec_b], out=rec_t[64:128, :], in_=psb[po][64:128, :])
            TT("dve", yst[yk][0:64, :], psb[po][0:64, :], rec_t[64:128, :], ALU.mult, [PSB[po], rec_b], [yst_b[yk]])
            P.dma("pool", [(YT16[yrow:yrow + 64, i * 512:(i + 1) * 512], yst[yk][0:64, :])], yst_b[yk], R=[yst_b[yk]], W=[YTb])

        LA = 2

        def run_pipe(items):
            n = len(items)
            for idx in range(n + LA):
                if idx < n:
                    items[idx][0]()
                if idx >= LA:
                    items[idx - LA][1]()
                    items[idx - LA][2]()

        def attn_items(Kt, K_b, Qt, Qbufs, r0, nr, va, va_b, bias_fn, i, yrow, hooks):
            items = []
            po = 4 + cnt["po"] % 2; cnt["po"] += 1
            nj = 4 * i + 4
            for j in range(nj):
                off = max(0, j - 4 * i) * 128
                ps = cnt["ps"] % 3; cnt["ps"] += 1
                pk = cnt["pt"] % 4; cnt["pt"] += 1

                def f_score(j=j, off=off, ps=ps):
                    if j in hooks:
                        hooks[j]()
                    mm_group(psb[ps][:, off:512], [(Kt[r0:r0 + nr, j * 128:(j + 1) * 128], Qt[r0:r0 + nr, i * 512 + off:(i + 1) * 512])],
                             [K_b] + Qbufs, [PSB[ps]])

                def f_soft(j=j, off=off, ps=ps, pk=pk):
                    if bias_fn is None:
                        ACT(pt_t[pk][:, off:512], psb[ps][:, off:512], AF.Exp, [PSB[ps]], [pt_b[pk]], scale=0.125)
                    else:
                        ACT(pt_t[pk][:, off:512], psb[ps][:, off:512], AF.Exp, [PSB[ps], cpos_b], [pt_b[pk]], bias=bias_fn(j, i), scale=0.125)
                    if j >= 4 * i:
                        TT("pool", pt_t[pk][:, off:off + 128], pt_t[pk][:, off:off + 128], cmask_t[:, 128:256], ALU.mult,
                           [pt_b[pk], cmask_b], [pt_b[pk]])

                def f_pv(j=j, off=off, pk=pk):
                    MM(psb[po][:, off:512], va[:, j, :], pt_t[pk][:, off:512], j == 0, j == nj - 1, [va_b, pt_b[pk]], [PSB[po]], inc=True)
                    if j == nj - 1:
                        finalize(po, yrow, i)
                items.append((f_score, f_soft, f_pv))
            return items

        for h in range(4):
            kq = cnt["kq"] % 2; cnt["kq"] += 1
            load_rows(KP[kq], KP_b[kq], KT16, h * 64, 64, KTb, ind=2)
            load_rows(QP[kq], QP_b[kq], QT16, h * 64, 64, QTb)
            vk = cnt["va"] % 2; cnt["va"] += 1
            load_va(vk, h, 1)
            items = []

            def mkpre(i, h=h, kq=kq):
                def pre():
                    for qb in range(4):
                        P.op("pe", "transpose", [cpos_b, identf_b], [PSB[7]], qb == 3, out=psb[7][0:1, qb * 128:(qb + 1) * 128],
                             in_=cpos_t[:, 4 * i + qb, h:h + 1], identity=identf_t)
                    P.op("dve", "tensor_scalar", [PSB[7]], [QA_b[kq][i]], out=QP[kq][64:65, i * 512:(i + 1) * 512], in0=psb[7][0:1, :],
                         scalar1=-8.0, scalar2=None, op0=ALU.mult)
                return pre
            mkpre(0)()
            for i in range(NT):
                hooks = {0: mkpre(i + 1)} if i + 1 < NT else {}
                items += attn_items(KP[kq], KP_b[kq], QP[kq], [QP_b[kq], QA_b[kq][i]], 0, 65, VA[vk], VA_b[vk],
                                    (lambda h: lambda j, i: cpos_t[:, j, h:h + 1])(h), i, h * 64, hooks)
            run_pipe(items)
        for m in range(6):
            hd = 10 + m
            kq = cnt["kq"] % 2; cnt["kq"] += 1
            load_rows(KP[kq], KP_b[kq], KT16, hd * 64, 64, KTb, ind=1)
            load_rows(QP[kq], QP_b[kq], QT16, hd * 64, 64, QTb)
            P.dma("sp", [(ks32_t[0:64, :], KS32[m * 64:(m + 1) * 64, :])], ks32_b, R=[KSb], W=[ks32_b])
            P.op("dve", "tensor_copy", [ks32_b], [ks16_b], out=ks16_t[0:64, :], in_=ks32_t[0:64, :])
            vk = cnt["va"] % 2; cnt["va"] += 1
            load_va(vk, hd, 1)
            Kt = KP[kq]; Qt = QP[kq]
            items = []

            def mkpre1(i, kq=kq, Qt=Qt):
                def pre():
                    for qb in range(4):
                        q0 = i * 512 + qb * 128
                        mm_group(psb[6][:, qb * 32:qb * 32 + NMB], [(Qt[0:64, q0:q0 + 128], ks16_t[0:64, 0:NMB])], [QP_b[kq], ks16_b], [PSB[6]])
                    P.op("dve", "memset", (), [wk_b], args=(wk_t, -1e30))
                    P.op("dve", "memset", (), [sb_b], args=(sb_t, -1.0))
                    for hq in range(2):
                        own = 2 * i + hq
                        if own > 3:
                            P.op("dve", "tensor_copy", [PSB[6]], [wk_b], out=wk_t[:, 2 * hq:2 * hq + 2, 0:own],
                                 in_=psb[6][:, 64 * hq:64 * hq + 64].rearrange("p (a n) -> p a n", n=32)[:, :, 0:own])
                        for qq in range(2):
                            qb = 2 * hq + qq
                            if own > 3:
                                P.op("dve", "max", [wk_b], [t8_b], out=t8_t[:, qb, :], in_=wk_t[:, qb, 0:max(own, 8)])
                                P.op("dve", "tensor_scalar", [wk_b, t8_b], [sb_b], out=sb_t[:, qb, 0:own], in0=wk_t[:, qb, 0:own],
                                     scalar1=t8_t[:, qb, 2:3], scalar2=1.0, op0=ALU.is_ge, op1=ALU.subtract)
                                P.op("dve", "memset", (), [sb_b], args=(sb_t[:, qb, own:own + 1], 0.0))
                            else:
                                P.op("dve", "memset", (), [sb_b], args=(sb_t[:, qb, 0:own + 1], 0.0))
                    P.op("dve", "tensor_scalar", [sb_b], [sb_b], out=sb_t, in0=sb_t, scalar1=BIG, scalar2=None, op0=ALU.mult)
                return pre

            def mkpre2(i, kq=kq, Qt=Qt):
                def pre():
                    for qb in range(4):
                        P.op("pe", "transpose", [sb_b, identf_b], [PSB[7]], qb == 3, out=psb[7][0:32, qb * 128:(qb + 1) * 128],
                             in_=sb_t[:, qb, :], identity=identf_t)
                    P.op("dve", "tensor_copy", [PSB[7]], [QA_b[kq][i]], out=Qt[64:96, i * 512:(i + 1) * 512], in_=psb[7][0:32, :])
                return pre
            mkpre1(0)(); mkpre2(0)()
            for i in range(NT):
                hooks = {}
                if i + 1 < NT:
                    hooks[0] = mkpre1(i + 1)
                    hooks[4 * i + 3] = mkpre2(i + 1)
                items += attn_items(Kt, KP_b[kq], Qt, [QP_b[kq], QA_b[kq][i]], 0, 96, VA[vk], VA_b[vk], None, i, 384 + m * 64, hooks)
            run_pipe(items)
        for oh in range(2):
            for g in range(3):
                d = DIL[g]
                ptile = 2 + g
                hd = 4 + 2 * g + oh
                kq = cnt["kq"] % 2; cnt["kq"] += 1
                load_rows(KP[kq], KP_b[kq], KT16, ptile * 128, 128, KTb)
                load_rows(QP[kq], QP_b[kq], QT16, ptile * 128, 128, QTb)
                vk = cnt["va"] % 2; cnt["va"] += 1
                load_va(vk, hd, d)
                Kt = KP[kq]; Qt = QP[kq]; r0 = oh * 64
                KQ = [KP_b[kq], QP_b[kq]]
                nbd = NB // d
                items = []
                for r in range(d):
                    for ub4 in range(0, nbd, 4):
                        po = 4 + cnt["po"] % 2; cnt["po"] += 1
                        nu = min(4, nbd - ub4)
                        for u in range(nu):
                            ub = ub4 + u
                            ps = cnt["ps"] % 3; cnt["ps"] += 1
                            pk = cnt["pt"] % 4; cnt["pt"] += 1
                            qa = r + d * 128 * ub
                            lo = 0 if ub > 0 else 128
                            bi = r * nbd + ub

                            def f_score(ub=ub, ps=ps, qa=qa, d=d, r0=r0, Kt=Kt, Qt=Qt, KQ=KQ):
                                qap = Qt[r0:r0 + 64, qa: qa + d * 127 + 1: d]
                                if ub > 0:
                                    ka = qa - d * 128
                                    mm_group(psb[ps][:, 0:128], [(Kt[r0:r0 + 64, ka: ka + d * 127 + 1: d], qap)], KQ, [PSB[ps]])
                                mm_group(psb[ps][:, 128:256], [(Kt[r0:r0 + 64, qa: qa + d * 127 + 1: d], qap)], KQ, [PSB[ps]])

                            def f_soft(ps=ps, pk=pk, lo=lo):
                                ACT(pt_t[pk][:, lo:256], psb[ps][:, lo:256], AF.Exp, [PSB[ps]], [pt_b[pk]], scale=0.125)
                                TT("pool", pt_t[pk][:, lo:256], pt_t[pk][:, lo:256], cmask_t[:, lo:256], ALU.mult, [pt_b[pk], cmask_b], [pt_b[pk]])

                            def f_pv(ub=ub, u=u, nu=nu, po=po, pk=pk, bi=bi, vk=vk, g=g, r=r, d=d, ub4=ub4):
                                if ub > 0:
                                    MM(psb[po][:, u * 128:(u + 1) * 128], VA[vk][:, bi - 1, :], pt_t[pk][:, 0:128], True, False,
                                       [VA_b[vk], pt_b[pk]], [PSB[po]], inc=False)
                                MM(psb[po][:, u * 128:(u + 1) * 128], VA[vk][:, bi, :], pt_t[pk][:, 128:256], ub == 0, True,
                                   [VA_b[vk], pt_b[pk]], [PSB[po]], inc=True)
                                if u == nu - 1:
                                    a0 = r + d * 128 * ub4
                                    accv = acc_t[:, a0: a0 + d * (128 * nu - 1) + 1: d]
                                    if g == 0:
                                        P.op("dve", "tensor_copy", [PSB[po]], [acc_b], out=accv, in_=psb[po][:, 0:128 * nu])
                                    else:
                                        TT("dve", accv, psb[po][:, 0:128 * nu], accv, ALU.add, [PSB[po], acc_b], [acc_b])
                            items.append((f_score, f_soft, f_pv))
                run_pipe(items)
            for i in range(NT):
                yk = cnt["y"] % 2; cnt["y"] += 1
                P.op("dve", "reciprocal", [acc_b], [rec_b], out=rec_t[64:128, :], in_=acc_t[64:128, i * 512:(i + 1) * 512])
                P.op("dve", "tensor_copy", [rec_b], [rec0_b], out=rec0_t[0:64, :], in_=rec_t[64:128, :])
                TT("dve", yst[yk][0:64, :], acc_t[0:64, i * 512:(i + 1) * 512], rec0_t[0:64, :], ALU.mult, [acc_b, rec0_b], [yst_b[yk]])
                P.dma("pool", [(YT16[256 + oh * 64:256 + oh * 64 + 64, i * 512:(i + 1) * 512], yst[yk][0:64, :])], yst_b[yk], R=[yst_b[yk]], W=[YTb])

        P.barrier()
        ar.reset()
        xt = [ar.f32(4096).rearrange("p (k n) -> p k n", n=512) for _ in range(4)]
        xt_b = [P.buf("xt%d" % i, True) for i in range(4)]
        yt = [ar.bf(3072).rearrange("p (k n) -> p k n", n=512) for _ in range(2)]
        yt_b = [P.buf("ytC%d" % i, True) for i in range(2)]
        pp = [ar.f32(1024).rearrange("p (k n) -> p k n", n=512) for _ in range(2)]
        pp_b = [P.buf("ppC%d" % i, True) for i in range(2)]
        p16 = [ar.bf(1024).rearrange("p (k n) -> p k n", n=512) for _ in range(2)]
        p16_b = [P.buf("p16_%d" % i) for i in range(2)]
        hh = [ar.bf(4096).rearrange("p (k n) -> p k n", n=512) for _ in range(2)]
        hh_b = [P.buf("hC%d" % i) for i in range(2)]
        sqo_t = ar.bf(4096).rearrange("p (k n) -> p k n", n=512); sqo_b = P.buf("sqo")
        sqx_t = ar.bf(4096).rearrange("p (k n) -> p k n", n=512); sqx_b = P.buf("sqx")
        mg_t = ar.bf(4096).rearrange("p (k n) -> p k n", n=512); mg_b = P.buf("mgC")
        o_t = ar.f32(4096).rearrange("p (k n) -> p k n", n=512); o_b = P.buf("oC")
        ff_t = ar.bf(NFF * 512).rearrange("p (k n) -> p k n", n=512); ff_b = P.buf("ffC")
        rstd_t = ar.f32(512); rstd_b = P.buf("rstd")
        tmp_t = ar.f32(512); tmp_b = P.buf("tmp")
        gs = [ar.f32(512) for _ in range(3)]; gs_b = [P.buf("gs%d" % i) for i in range(3)]
        ma = [ar.f32(512) for _ in range(3)]; ma_b = [P.buf("ma%d" % i) for i in range(3)]
        tt = [ar.f32(512) for _ in range(2)]; tt_b = [P.buf("tt%d" % i) for i in range(2)]
        ring = Ring(6, 1024, "ringC")
        prc = {"i": 0}

        def nps():
            k = prc["i"] % 7; prc["i"] += 1
            return k

        def loadXY(t):
            k = t % 2; xk = t % 4
            P.dma("sp", [(xt[xk][:, 0:4, :], xtile_ap(xsrc(l), t)[:, 0:4, :]),
                         (xt[xk][:, 4:8, :], xtile_ap(xsrc(l), t)[:, 4:8, :])], xt_b[xk], R=[Xb], W=[xt_b[xk]])
            P.dma("sp", [(yt[k], YT16[:, t * 512:(t + 1) * 512].rearrange("(k p) n -> p k n", p=128))], yt_b[k], R=[YTb], W=[yt_b[k]])

        def loadP(t):
            k = t % 2
            P.dma("sp", [(pp[k], pT[l * PLE:(l + 1) * PLE, t * 512:(t + 1) * 512].rearrange("(k p) n -> p k n", p=128))], pp_b[k], W=[pp_b[k]])

        def stats(sq_t, sq_b):
            rms_stats(sq_t, sq_b, rstd_t, rstd_b, tmp_t, tmp_b)

        XK = {0: 0, 1: 1}

        def mk_h(k, gcol):
            for kc in range(8):
                P.op("dve", "scalar_tensor_tensor", [xt_b[XK[k]], rstd_b, gains_b], [hh_b[k]], out=hh[k][:, kc, :], in0=xt[XK[k]][:, kc, :],
                     scalar=gains_t[:, gcol + kc:gcol + kc + 1], in1=rstd_t, op0=ALU.mult, op1=ALU.mult)

        def pre1(k):
            for kc in range(8):
                ACT(sqx_t[:, kc, :], xt[XK[k]][:, kc, :], AF.Square, [xt_b[XK[k]]], [sqx_b])
            stats(sqx_t, sqx_b)
            mk_h(k, g0 + 0)

        def residual_steps(k, gcol, want_sq):
            xk = XK[k]
            steps = [lambda: stats(sqo_t, sqo_b)]

            def mk(m):
                def step():
                    q = m % 2
                    eng = "pool" if m % 2 == 0 else "dve"
                    P.op("dve", "scalar_tensor_tensor", [o_b, rstd_b, gains_b], [tt_b[q]], out=tt[q], in0=o_t[:, m, :],
                         scalar=gains_t[:, gcol + m:gcol + m + 1], in1=rstd_t, op0=ALU.mult, op1=ALU.mult)
                    TT(eng, xt[xk][:, m, :], xt[xk][:, m, :], tt[q], ALU.add, [xt_b[xk], tt_b[q]], [xt_b[xk]])
                    if want_sq:
                        TT(eng, sqx_t[:, m, :], xt[xk][:, m, :], xt[xk][:, m, :], ALU.mult, [xt_b[xk]], [sqx_b])
                return step
            return steps + [mk(m) for m in range(8)]

        def run_bg(bg, n=1):
            for _ in range(n):
                if bg:
                    bg.pop(0)()

        def flush_bg(bg):
            while bg:
                bg.pop(0)()

        def evac_o(ps, m):
            ACT(sqo_t[:, m, :], psb[ps][:, :], AF.Square, [PSB[ps]], [sqo_b])
            ACT(o_t[:, m, :], psb[ps][:, :], AF.Copy, [PSB[ps]], [o_b])

        def S1(k, inject, bg):
            ychunks = [(0, 2), (2, 1), (3, 3)]
            for m in range(8):
                if m == 3:
                    flush_bg(bg)
                    if inject is not None:
                        inject()
                wb3, wb_b = ring.load(wslab("wBR", l, m), 768, [WB[l]])
                for b in range(3):
                    run_bg(bg)
                    wg3, wg_b = ring.load(wslab("wG", l, b * 8 + m), 1024, [WB[l]])
                    pg = nps()
                    mm_group(psb[pg][:, :], [(wg3[:, kc, :], hh[k][:, kc, :]) for kc in range(8)], [wg_b, hh_b[k]], [PSB[pg]])
                    ACT(gs[b], psb[pg][:, :], AF.Sigmoid, [PSB[pg]], [gs_b[b]])
                    c0, ncc = ychunks[b]
                    pq = nps()
                    mm_group(psb[pq][:, :], [(wb3[:, c0 + c, :], yt[k][:, c0 + c, :]) for c in range(ncc)], [wb_b, yt_b[k]], [PSB[pq]])
                    TT("dve", ma[b], psb[pq][:, :], gs[b], ALU.mult, [PSB[pq], gs_b[b]], [ma_b[b]])
                TT("pool", ma[0], ma[0], ma[1], ALU.add, [ma_b[0], ma_b[1]], [ma_b[0]])
                TT("pool", mg_t[:, m, :], ma[0], ma[2], ALU.add, [ma_b[0], ma_b[2]], [mg_b])
            for m in range(8):
                w3, w_b = ring.load(wslab("wO", l, m), 1024, [WB[l]])
                ps = nps()
                mm_group(psb[ps][:, :], [(w3[:, kc, :], mg_t[:, kc, :]) for kc in range(8)], [w_b, mg_b], [PSB[ps]])
                evac_o(ps, m)

        def S2(k, inject, bg):
            for j in range(NFF):
                run_bg(bg)
                if j == 8:
                    flush_bg(bg)
                    if inject is not None:
                        inject()
                wg3, wg_b = ring.load(wslab("wFG", l, j), 1024, [WB[l]])
                pg = nps()
                mm_group(psb[pg][:, :], [(wg3[:, kc, :], hh[k][:, kc, :]) for kc in range(8)], [wg_b, hh_b[k]], [PSB[pg]])
                kk = j % 3
                ACT(gs[kk], psb[pg][:, :], AF.Silu, [PSB[pg]], [gs_b[kk]])
                wu3, wu_b = ring.load(wslab("wFU", l, j), 1024, [WB[l]])
                pu = nps()
                mm_group(psb[pu][:, :], [(wu3[:, kc, :], hh[k][:, kc, :]) for kc in range(8)], [wu_b, hh_b[k]], [PSB[pu]])
                TT("dve", ff_t[:, j, :], psb[pu][:, :], gs[kk], ALU.mult, [PSB[pu], gs_b[kk]], [ff_b])
            for m in range(8):
                ps = nps()
                pieces = [(0, 8), (8, 16), (16, NFF)]
                first = True
                for (j0, j1) in pieces:
                    w3, w_b = ring.load(wslab("wFD", l, m)[:, j0 * 128:j1 * 128], (j1 - j0) * 128, [WB[l]])
                    for j in range(j0, j1):
                        MM(psb[ps][:, :], w3[:, j - j0, :], ff_t[:, j, :], first, j == NFF - 1, [w_b, ff_b], [PSB[ps]], inc=(j == j1 - 1))
                        first = False
                evac_o(ps, m)

        def d2(k):
            ACT(hh[k], xt[XK[k]], AF.Copy, [xt_b[XK[k]]], [hh_b[k]])
            P.op("dve", "tensor_copy", [pp_b[k]], [p16_b[k]], out=p16[k], in_=pp[k])

        def S3(k, inject, bg):
            for m in range(8):
                run_bg(bg, 3)
                if m == 3:
                    flush_bg(bg)
                    if inject is not None:
                        inject()
                wg3, wg_b = ring.load(wslab("wPG", l, m), 1024, [WB[l]])
                pg = nps()
                mm_group(psb[pg][:, :], [(wg3[:, kc, :], hh[k][:, kc, :]) for kc in range(8)], [wg_b, hh_b[k]], [PSB[pg]])
                kk = m % 3
                ACT(gs[kk], psb[pg][:, :], AF.Sigmoid, [PSB[pg]], [gs_b[kk]])
                wp3, wp_b = ring.load(wslab("wPL", l, m), 256, [WB[l]])
                pu = nps()
                mm_group(psb[pu][:, :], [(wp3[:, kc, :], p16[k][:, kc, :]) for kc in range(2)], [wp_b, p16_b[k]], [PSB[pu]])
                TT("dve", o_t[:, m, :], psb[pu][:, :], gs[kk], ALU.mult, [PSB[pu], gs_b[kk]], [o_b])
                ACT(sqo_t[:, m, :], o_t[:, m, :], AF.Square, [o_b], [sqo_b])

        def storeC(t):
            xk = t % 4
            P.dma("pool", [(xtile_ap(xdst(l), t)[:, 0:4, :], xt[xk][:, 0:4, :]), (xtile_ap(xdst(l), t)[:, 4:8, :], xt[xk][:, 4:8, :])],
                  xt_b[xk], R=[xt_b[xk]], W=[Xb])

        def c2(k):
            stats(sqx_t, sqx_b)
            mk_h(k, g0 + 16)

        loadXY(0); loadP(0); loadXY(1); loadP(1)
        XK[0] = 0
        pre1(0)
        bgq = []
        for tp in range(0, NT, 2):
            tA, tB = tp, tp + 1
            nxt = tp + 2 < NT
            XK[0] = tA % 4; XK[1] = tB % 4

            def inj_pre1B():
                pre1(1)
            S1(0, inj_pre1B, bgq)
            if nxt:
                loadXY(tA + 2)
            bgq = residual_steps(0, g0 + 8, True)
            S1(1, lambda: c2(0), bgq)
            if nxt:
                loadXY(tB + 2)
            bgq = residual_steps(1, g0 + 8, True)
            S2(0, lambda: c2(1), bgq)
            bgq = residual_steps(0, g0 + 24, False)
            S2(1, lambda: d2(0), bgq)
            if nxt:
                loadP(tA + 2)
            bgq = residual_steps(1, g0 + 24, False)
            S3(0, lambda: d2(1), bgq)
            if nxt:
                loadP(tB + 2)
            bgq = residual_steps(0, g0 + 32, False) + [(lambda t=tA: storeC(t))]

            def inj_next(tA=tA):
                XK[0] = (tA + 2) % 4
                pre1(0)
                XK[0] = tA % 4
            S3(1, inj_next if nxt else None, bgq)
            bgq = residual_steps(1, g0 + 32, False) + [(lambda t=tB: storeC(t))]
        flush_bg(bgq)
    P.barrier()
    block = es.enter_context(nc.Block())
    P.replay(block)
    es.close()
    return nc


def _slabs(w, cols_list, kc):
    out = np.empty((len(cols_list), 128, kc, 128), np.float32)
    wk = w.reshape(kc, 128, w.shape[1])
    for m, cols in enumerate(cols_list):
        out[m] = wk[:, :, cols].transpose(1, 0, 2)
    return out.reshape(len(cols_list) * 128, kc * 128)


def _host_weights(inp, L):
    r = {}
    ar_ = np.arange
    lists = {n: [] for n in ("wA", "wV", "wF", "wG", "wBR", "wO", "wFG", "wFU", "wFD", "wPL", "wPG")}
    for l in range(L):
        w_in = np.asarray(inp["w_in"][l], np.float32)
        colsA = []
        for base in (0, 1024):
            for pt in range(8):
                c = base + pt * 128 + ar_(128)
                colsA.append(c)
                if pt >= 2:
                    sw = base + pt * 128 + (ar_(128) // 64) * 64 + (ar_(128) % 64 + 32) % 64
                    colsA.append(sw)
        lists["wA"].append(_slabs(w_in, colsA, 8))
        wv = w_in[:, 2048:3072].reshape(8, 128, 1024).transpose(1, 0, 2).reshape(128, 8192)
        lists["wV"].append(wv)
        wf = w_in[:, 3072:3076].reshape(8, 128, 4).transpose(1, 0, 2).reshape(128, 32)
        lists["wF"].append(wf)
        lists["wG"].append(_slabs(w_in, [3076 + b * 1024 + m * 128 + ar_(128) for b in range(3) for m in range(8)], 8))
        wbr = np.concatenate([np.asarray(inp["w_br_a"][l]), np.asarray(inp["w_br_b"][l]), np.asarray(inp["w_br_c"][l])], axis=0)
        lists["wBR"].append(_slabs(wbr.astype(np.float32), [m * 128 + ar_(128) for m in range(8)], 6))
        lists["wO"].append(_slabs(np.asarray(inp["w_out"][l], np.float32), [m * 128 + ar_(128) for m in range(8)], 8))
        lists["wFG"].append(_slabs(np.asarray(inp["w_ffn_gate"][l], np.float32), [m * 128 + ar_(128) for m in range(NFF)], 8))
        lists["wFU"].append(_slabs(np.asarray(inp["w_ffn_up"][l], np.float32), [m * 128 + ar_(128) for m in range(NFF)], 8))
        lists["wFD"].append(_slabs(np.asarray(inp["w_ffn_down"][l], np.float32), [m * 128 + ar_(128) for m in range(8)], NFF))
        lists["wPL"].append(_slabs(np.asarray(inp["w_ple"][l], np.float32), [m * 128 + ar_(128) for m in range(8)], 2))
        lists["wPG"].append(_slabs(np.asarray(inp["w_ple_gate"][l], np.float32), [m * 128 + ar_(128) for m in range(8)], 8))
    for n, v in lists.items():
        r[n] = np.ascontiguousarray(np.concatenate(v, axis=0), dtype=np.float32)
    gl = []
    for l in range(L):
        for n in ("g_mix_pre", "g_mix_post", "g_ffn_pre", "g_ffn_post", "g_ple_post"):
            gl.append(np.asarray(inp[n][l], np.float32).reshape(8, 128).T)
    r["gains"] = np.ascontiguousarray(np.concatenate(gl, axis=1), dtype=np.float32)
    r["bfb"] = np.ascontiguousarray(np.broadcast_to(np.asarray(inp["b_f"], np.float32).reshape(1, L * 4), (128, L * 4)))
    return r


def _consts(S):
    c = {}
    inv = (1.0 / (np.float32(10000.0) ** (np.arange(0, 64, 2, dtype=np.float32) / np.float32(64)))).astype(np.float32)
    ang = (np.arange(S, dtype=np.float32)[:, None] * inv[None, :]).astype(np.float32)
    cos = np.cos(ang.astype(np.float64)).astype(np.float32).T
    sin = np.sin(ang.astype(np.float64)).astype(np.float32).T
    c["cosT"] = np.ascontiguousarray(np.concatenate([cos, cos, cos, cos], axis=0))
    c["sinT"] = np.ascontiguousarray(np.concatenate([-sin, sin, -sin, sin], axis=0))
    p = np.arange(128)
    c["cmask"] = np.ascontiguousarray(np.concatenate([(p[:, None] >= p[None, :]), (p[:, None] <= p[None, :])], axis=1).astype(np.float32))
    c["trif"] = np.ascontiguousarray((p[:, None] <= p[None, :]).astype(np.float32))
    c["identf"] = np.eye(128, dtype=np.float32)
    c["blkind"] = np.ascontiguousarray(np.concatenate([(np.arange(S)[None, :] // 256 == np.arange(32)[:, None]), np.ones((1, S), bool)], axis=0).astype(np.float32))
    return c


_NC_CACHE = {}


def kernel(**inputs):
    x = np.asarray(inputs["x"], np.float32)
    p = np.asarray(inputs["p"], np.float32)
    B, S, _ = x.shape
    L = p.shape[0]
    key = (S, L)
    if key not in _NC_CACHE:
        _NC_CACHE[key] = build(S, L)
    nc = _NC_CACHE[key]
    shared = _host_weights(inputs, L)
    shared.update(_consts(S))
    in_maps = []
    for b in range(B):
        m = dict(shared)
        m["xT"] = np.ascontiguousarray(x[b].T)
        m["pT"] = np.ascontiguousarray(p[:, b].transpose(0, 2, 1).reshape(L * PLE, S))
        in_maps.append(m)
    res = run_bass_kernel_spmd(nc, in_maps, core_ids=list(range(B)))
    out = np.stack([np.ascontiguousarray(res.results[b]["outT"].T) for b in range(B)], axis=0)
    return out.astype(np.float32)
```

```python
import numpy as np
from contextlib import ExitStack
import concourse.bass as bass
import concourse.mybir as mybir
from concourse.bass_utils import run_bass_kernel_spmd

F32 = mybir.dt.float32
BF = mybir.dt.bfloat16
AF = mybir.ActivationFunctionType
ALU = mybir.AluOpType
AX = mybir.AxisListType

D = 1024
HD = 64
PLE = 256
DFF = 2816
NFF = 22
BIG = 30000.0
DIL = (1, 4, 16)
SAME_ENGINE_SYNC = True


class Buf:
    def __init__(self, name, sem=None):
        self.name = name
        self.w = {}
        self.r = {}
        self.sem = sem
        self.cnt = 0


class Eng:
    def __init__(self, name, sem):
        self.name = name
        self.sem = sem
        self.cnt = 0
        self.seen = {}
        self.prog = []


class Prog:
    def __init__(self, nc, es):
        self.nc = nc
        self.es = es
        self.sems = []
        self.E = {}
        for n in ("pe", "act", "dve", "pool"):
            self.E[n] = Eng(n, self.newsem(n))
        self.E["sp"] = Eng("sp", None)
        self.bufs = []
        self.bynames = {}

    def newsem(self, name):
        h = self.es.enter_context(self.nc.semaphore("s_" + name))
        self.sems.append(h)
        return len(self.sems) - 1

    def buf(self, name, dma=False):
        if name in self.bynames:
            return self.bynames[name]
        b = Buf(name, self.newsem(name) if dma else None)
        self.bufs.append(b)
        self.bynames[name] = b
        return b

    def _waits(self, X, R, W):
        need = {}
        for b in R:
            for k, v in b.w.items():
                need[k] = max(need.get(k, 0), v)
        for b in W:
            for k, v in b.w.items():
                need[k] = max(need.get(k, 0), v)
            for k, v in b.r.items():
                need[k] = max(need.get(k, 0), v)
        out = []
        for k, v in need.items():
            if k == X.sem and (X.name == "pe" or not SAME_ENGINE_SYNC):
                continue
            if X.seen.get(k, 0) >= v:
                continue
            X.seen[k] = v
            out.append((k, v))
        return out

    def op(self, eng, name, R=(), W=(), inc=True, args=(), **kw):
        X = self.E[eng]
        waits = self._waits(X, R, W)
        tok = X.cnt + 1
        if inc:
            X.cnt = tok
        X.prog.append((waits, ("op", name, args, kw), inc))
        for b in R:
            b.r[X.sem] = tok
        for b in W:
            b.w = {X.sem: tok}
            b.r = {}

    def dma(self, q, pairs, sb, R=(), W=()):
        X = self.E[q]
        waits = self._waits(X, R, W)
        first = True
        for o, i in pairs:
            sb.cnt += 16
            X.prog.append((waits if first else [], ("dma", o, i, sb.sem), False))
            first = False
        for b in R:
            b.r[sb.sem] = sb.cnt
        for b in W:
            b.w = {sb.sem: sb.cnt}
            b.r = {}

    def barrier(self):
        toks = {}
        for n in ("pe", "act", "dve", "pool"):
            toks[self.E[n].sem] = self.E[n].cnt
        for b in self.bufs:
            if b.sem is not None and b.cnt > 0:
                toks[b.sem] = b.cnt
        for n, X in self.E.items():
            waits = []
            for k, v in toks.items():
                if v > 0 and X.seen.get(k, 0) < v and not (k == X.sem and n == "pe"):
                    X.seen[k] = v
                    waits.append((k, v))
            X.prog.append((waits, None, False))

    def replay(self, block):
        sems = self.sems

        def run(X, e):
            for waits, fn, inc in X.prog:
                for k, v in waits:
                    e.wait_ge(sems[k], v)
                if fn is None:
                    continue
                if fn[0] == "dma":
                    _, o, i, k = fn
                    e.dma_start(out=o, in_=i).then_inc(sems[k], 16)
                else:
                    _, name, args, kw = fn
                    ins = getattr(e, name)(*args, **kw)
                    if inc:
                        ins.then_inc(sems[X.sem], 1)

        @block.sync
        def _(e):
            run(self.E["sp"], e)

        @block.tensor
        def _(e):
            run(self.E["pe"], e)

        @block.scalar
        def _(e):
            run(self.E["act"], e)

        @block.vector
        def _(e):
            run(self.E["dve"], e)

        @block.gpsimd
        def _(e):
            run(self.E["pool"], e)


class Arena:
    def __init__(self, ap, lo, hi):
        self.ap = ap
        self.lo = lo
        self.hi = hi
        self.p = lo

    def f32(self, n):
        a = self.ap[:, self.p:self.p + n]
        self.p += n
        assert self.p <= self.hi, ("arena overflow", self.p, self.hi)
        return a

    def bf(self, n):
        n2 = (n + 1) // 2
        a = self.ap[:, self.p:self.p + n2].bitcast(BF)
        self.p += n2
        assert self.p <= self.hi, ("arena overflow", self.p, self.hi)
        return a[:, 0:n]

    def reset(self):
        self.p = self.lo


def build(S=8192, L=2, dbg=False):
    NT = S // 512
    NB = S // 128
    NMB = S // 256
    CW = min(2048, S)
    assert NMB <= 32
    nc = bass.Bass("TRN2", target_bir_lowering=False)

    def din(name, shape, dt=F32):
        return nc.dram_tensor(name, shape, dt, kind="ExternalInput").ap()

    def dscr(name, shape, dt=BF):
        return nc.dram_tensor(name, shape, dt, kind=("ExternalOutput" if dbg else "Internal")).ap()

    xT = din("xT", [D, S])
    pT = din("pT", [L * PLE, S])
    wspec = [("wA", 28, 1024), ("wV", 1, 8192), ("wF", 1, 32), ("wG", 24, 1024), ("wBR", 8, 768),
             ("wO", 8, 1024), ("wFG", 22, 1024), ("wFU", 22, 1024), ("wFD", 8, 2816),
             ("wPL", 8, 256), ("wPG", 8, 1024)]
    w32 = {}
    w16 = {}
    for n, ns, nc_ in wspec:
        w32[n] = din(n, [L * ns * 128, nc_])
        w16[n] = nc.dram_tensor(n + "_16", [L * ns * 128, nc_], BF, kind="Internal").ap()
    wns = {n: ns for n, ns, _ in wspec}
    gains = din("gains", [128, L * 5 * 8])
    bfb = din("bfb", [128, L * 16])
    cosT = din("cosT", [128, S])
    sinT = din("sinT", [128, S])
    cmask32 = din("cmask", [128, 256])
    trif = din("trif", [128, 128])
    identf = din("identf", [128, 128])
    blkind32 = din("blkind", [33, S])
    outT = nc.dram_tensor("outT", [D, S], F32, kind="ExternalOutput").ap()

    X32 = dscr("X32", [D, S], F32)
    QT16 = dscr("QT16", [1024, S])
    KT16 = dscr("KT16", [1024, S])
    V16 = dscr("V16", [S, 1024])
    YT16 = dscr("YT16", [768, S])
    KS32 = dscr("KS32", [384, 32], F32)
    BI16 = nc.dram_tensor("BI16", [33, S], BF, kind="Internal").ap()

    es = ExitStack()
    P = Prog(nc, es)
    AW = 52992
    arena_t = es.enter_context(nc.sbuf_tensor("arena", [128, AW], F32))
    PERS = 2048
    pers = Arena(arena_t, 0, PERS)
    ar = Arena(arena_t, PERS, AW)
    psb = [es.enter_context(nc.psum_tensor("psb%d" % i, [128, 512], F32)) for i in range(8)]
    PSB = [P.buf("psb%d" % i) for i in range(8)]

    def ACT(out, in_, func, R, W, **kw):
        P.op("act", "activation", R, W, out=out, in_=in_, func=func, **kw)

    def TT(eng, out, in0, in1, op, R, W):
        P.op(eng, "tensor_tensor", R, W, out=out, in0=in0, in1=in1, op=op)

    def MM(out, lhsT, rhs, start, stop, R, W, inc=True):
        P.op("pe", "matmul", R, W, inc, args=(out,), lhsT=lhsT, rhs=rhs, start=start, stop=stop)

    def mm_group(out_ap, pairs, Rb, Wb):
        n = len(pairs)
        for i, (lt, rh) in enumerate(pairs):
            MM(out_ap, lt, rh, i == 0, i == n - 1, Rb, Wb, inc=(i == n - 1))

    gains_t = pers.f32(L * 40); gains_b = P.buf("gains", True)
    bfb_t = pers.f32(L * 16); bfb_b = P.buf("bfb", True)
    trif_t = pers.f32(128); trif_b = P.buf("trif", True)
    identf_t = pers.f32(128); identf_b = P.buf("identf", True)
    onesf_t = pers.f32(128); onesf_b = P.buf("onesf")
    ones16_t = pers.bf(128); ones16_b = P.buf("ones16")
    cmask_t = pers.bf(256); cmask_b = P.buf("cmask", True)
    eps_t = pers.f32(1); one_t = pers.f32(1); cst_b = P.buf("cst")
    cpos_t = pers.f32(NB * 4).rearrange("p (j h) -> p j h", h=4); cpos_b = P.buf("cpos")
    tall_t = pers.f32((NB + 1) * 4).rearrange("p (j h) -> p j h", h=4); tall_b = P.buf("tall")
    ksum_t = pers.f32(3 * 32).rearrange("p (a n) -> p a n", n=32); ksum_b = P.buf("ksum", True)

    P.dma("sp", [(gains_t, gains)], gains_b, W=[gains_b])
    P.dma("sp", [(bfb_t, bfb)], bfb_b, W=[bfb_b])
    P.dma("sp", [(trif_t, trif)], trif_b, W=[trif_b])
    P.dma("sp", [(identf_t, identf)], identf_b, W=[identf_b])
    P.dma("pool", [(cmask_t, cmask32)], cmask_b, W=[cmask_b])
    P.op("dve", "memset", (), [onesf_b], args=(onesf_t, 1.0))
    P.op("dve", "memset", (), [ones16_b], args=(ones16_t, 1.0))
    P.op("dve", "memset", (), [cst_b], args=(eps_t, 1e-6))
    P.op("dve", "memset", (), [cst_b], args=(one_t, 1.0))
    P.op("dve", "memset", (), [tall_b], args=(tall_t[:, 0, :], 0.0))
    P.op("dve", "memset", (), [ksum_b], args=(ksum_t, 0.0))

    WB = [P.buf("w16_%d" % l) for l in range(L)]
    CB = [P.buf("cast%d" % i, True) for i in range(2)]
    BIb = P.buf("bi16", True)
    P.dma("pool", [(BI16[:, c0:c0 + CW], blkind32[:, c0:c0 + CW]) for c0 in range(0, S, CW)], BIb, W=[BIb])
    cg = 0
    for l in range(L):
        pairs = []
        for n, ns, ncol in wspec:
            for s_ in range(ns):
                r0 = (l * ns + s_) * 128
                cw = 2048 if ncol > 2816 else ncol
                for c0 in range(0, ncol, cw):
                    pairs.append((w16[n][r0:r0 + 128, c0:c0 + cw], w32[n][r0:r0 + 128, c0:c0 + cw]))
        for g0_ in range(0, len(pairs), 8):
            cb = CB[cg % 2]; cg += 1
            P.dma("pool", pairs[g0_:g0_ + 8], cb, W=[cb])
        WB[l].w = {CB[0].sem: CB[0].cnt, CB[1].sem: CB[1].cnt}

    def wslab(n, l, s_):
        r0 = (l * wns[n] + s_) * 128
        return w16[n][r0:r0 + 128, :]

    Xb = P.buf("X32d"); QTb = P.buf("QTd"); KTb = P.buf("KTd"); Vb = P.buf("Vd"); YTb = P.buf("YTd"); KSb = P.buf("KSd")

    def xsrc(l):
        return xT if l == 0 else X32

    def xdst(l):
        return outT if l == L - 1 else X32

    def xtile_ap(dram, t):
        return dram[:, t * 512:(t + 1) * 512].rearrange("(kc p) n -> p kc n", p=128)

    class Ring:
        def __init__(self, n, words, name):
            self.t = [ar.bf(words) for _ in range(n)]
            self.b = [P.buf("%s%d" % (name, i), True) for i in range(n)]
            self.i = 0

        def load(self, dram_ap, ncol, Rb):
            k = self.i % len(self.t)
            self.i += 1
            P.dma("sp", [(self.t[k][:, 0:ncol], dram_ap)], self.b[k], R=Rb, W=[self.b[k]])
            return self.t[k][:, 0:ncol].rearrange("p (k n) -> p k n", n=128), self.b[k]

    def rms_stats(sq_t, sq_b, rstd_t, rstd_b, tmp_t, tmp_b):
        mm_group(psb[7][:, :], [(ones16_t, sq_t[:, kc, :]) for kc in range(8)], [ones16_b, sq_b], [PSB[7]])
        ACT(tmp_t, psb[7][:, :], AF.Sqrt, [PSB[7], cst_b], [tmp_b], bias=eps_t, scale=1.0 / D)
        P.op("dve", "reciprocal", [tmp_b], [rstd_b], out=rstd_t, in_=tmp_t)

    def pre_norm(x_t, x_b, gcol, sq_t, sq_b, rstd_t, rstd_b, tmp_t, tmp_b, h_t, h_b):
        ACT(sq_t, x_t, AF.Square, [x_b], [sq_b])
        rms_stats(sq_t, sq_b, rstd_t, rstd_b, tmp_t, tmp_b)
        for kc in range(8):
            P.op("dve", "scalar_tensor_tensor", [x_b, rstd_b, gains_b], [h_b], out=h_t[:, kc, :], in0=x_t[:, kc, :],
                 scalar=gains_t[:, gcol + kc:gcol + kc + 1], in1=rstd_t, op0=ALU.mult, op1=ALU.mult)

    for l in range(L):
        g0 = l * 40
        P.barrier()
        ar.reset()
        xt = [ar.f32(4096).rearrange("p (k n) -> p k n", n=512) for _ in range(2)]
        xt_b = [P.buf("xt%d" % i, True) for i in range(2)]
        sq_t = ar.bf(4096).rearrange("p (k n) -> p k n", n=512); sq_b = P.buf("sq")
        hA = [ar.bf(4096).rearrange("p (k n) -> p k n", n=512) for _ in range(2)]
        hA_b = [P.buf("hA%d" % i) for i in range(2)]
        rstd_t = ar.f32(512); rstd_b = P.buf("rstd")
        tmp_t = ar.f32(512); tmp_b = P.buf("tmp")
        wv_t = ar.bf(8192).rearrange("p (k n) -> p k n", n=1024); wv_b = P.buf("wvA", True)
        wf_t = ar.bf(32).rearrange("p (k n) -> p k n", n=4); wf_b = P.buf("wfA", True)
        ring = Ring(6, 1024, "ring")
        cs_t = [(ar.f32(512), ar.f32(512)) for _ in range(2)]
        cs_b = [P.buf("csA%d" % i, True) for i in range(2)]
        qst = [ar.bf(4096).rearrange("p (k n) -> p k n", n=512) for _ in range(2)]
        qst_b = [P.buf("qst%d" % i, True) for i in range(2)]
        kst = [ar.bf(4096).rearrange("p (k n) -> p k n", n=512) for _ in range(2)]
        kst_b = [P.buf("kst%d" % i, True) for i in range(2)]
        vst = [ar.bf(4096).rearrange("p (a n) -> p a n", n=1024) for _ in range(2)]
        vst_b = [P.buf("vst%d" % i, True) for i in range(2)]
        r1 = [ar.f32(512) for _ in range(2)]; r1_b = [P.buf("r1_%d" % i) for i in range(2)]
        r2 = [ar.f32(512) for _ in range(2)]; r2_b = [P.buf("r2_%d" % i) for i in range(2)]
        fb_t = ar.f32(16); fb_b = P.buf("fbA")
        fe_t = ar.f32(16); fe_b = P.buf("feA")
        fl_t = ar.f32(16); fl_b = P.buf("flA")

        P.dma("sp", [(wv_t[:, kc, :], wslab("wV", l, 0)[:, kc * 1024:(kc + 1) * 1024]) for kc in range(8)],
              wv_b, R=[WB[l]], W=[wv_b])
        P.dma("sp", [(wf_t, wslab("wF", l, 0).rearrange("p (k n) -> p k n", n=4))], wf_b, R=[WB[l]], W=[wf_b])

        def loadxA(t):
            k = t % 2
            P.dma("sp", [(xt[k][:, 0:4, :], xtile_ap(xsrc(l), t)[:, 0:4, :]),
                         (xt[k][:, 4:8, :], xtile_ap(xsrc(l), t)[:, 4:8, :])], xt_b[k], R=[Xb], W=[xt_b[k]])
            P.dma("sp", [(cs_t[k][0], cosT[:, t * 512:(t + 1) * 512]),
                         (cs_t[k][1], sinT[:, t * 512:(t + 1) * 512])], cs_b[k], W=[cs_b[k]])

        loadxA(0)
        psrot = 0
        pre_norm(xt[0], xt_b[0], g0 + 0, sq_t, sq_b, rstd_t, rstd_b, tmp_t, tmp_b, hA[0], hA_b[0])
        for t in range(NT):
            if t + 1 < NT:
                loadxA(t + 1)
            h_t = hA[t % 2]; h_b = hA_b[t % 2]
            cos_t, sin_t = cs_t[t % 2]; c_b = cs_b[t % 2]
            for tb in range(4):
                mm_group(psb[6][:, tb * 4:tb * 4 + 4], [(h_t[:, kc, tb * 128:(tb + 1) * 128], wf_t[:, kc, :]) for kc in range(8)], [wf_b, h_b], [PSB[6]])
            TT("dve", fb_t, psb[6][:, 0:16], bfb_t[:, l * 16:l * 16 + 16], ALU.add, [PSB[6], bfb_b], [fb_b])
            ACT(fe_t, fb_t, AF.Exp, [fb_b], [fe_b], scale=-1.0)
            ACT(fl_t, fe_t, AF.Ln, [fe_b, cst_b], [fl_b], bias=one_t, scale=1.0)
            qs = qst[t % 2]; qs_b = qst_b[t % 2]; ks = kst[t % 2]; ks_b = kst_b[t % 2]
            si = 0
            for which in range(2):
                stg, stg_b = (qs, qs_b) if which == 0 else (ks, ks_b)
                for pt in range(8):
                    w3, w_b = ring.load(wslab("wA", l, si), 1024, [WB[l]]); si += 1
                    pa = psrot % 6; psrot += 1
                    mm_group(psb[pa][:, :], [(w3[:, kc, :], h_t[:, kc, :]) for kc in range(8)], [w_b, h_b], [PSB[pa]])
                    if pt < 2:
                        ACT(stg[:, pt, :], psb[pa][:, :], AF.Copy, [PSB[pa]], [stg_b])
                    else:
                        w23, w2_b = ring.load(wslab("wA", l, si), 1024, [WB[l]]); si += 1
                        pb = psrot % 6; psrot += 1
                        mm_group(psb[pb][:, :], [(w23[:, kc, :], h_t[:, kc, :]) for kc in range(8)], [w2_b, h_b], [PSB[pb]])
                        ri = (pt + which) % 2
                        TT("dve", r1[ri], psb[pa][:, :], cos_t, ALU.mult, [PSB[pa], c_b], [r1_b[ri]])
                        TT("dve", r2[ri], psb[pb][:, :], sin_t, ALU.mult, [PSB[pb], c_b], [r2_b[ri]])
                        TT("pool", stg[:, pt, :], r1[ri], r2[ri], ALU.add, [r1_b[ri], r2_b[ri]], [stg_b])
                        if which == 1 and pt >= 5:
                            P.op("dve", "tensor_reduce", [stg_b], [ksum_b], out=ksum_t[:, pt - 5, 2 * t:2 * t + 2],
                                 in_=stg[:, pt, :].rearrange("p (a b) -> p a b", b=256), axis=AX.X, op=ALU.add)
                if which == 0 and t + 1 < NT:
                    pre_norm(xt[(t + 1) % 2], xt_b[(t + 1) % 2], g0 + 0, sq_t, sq_b, rstd_t, rstd_b, tmp_t, tmp_b,
                             hA[(t + 1) % 2], hA_b[(t + 1) % 2])
            P.dma("pool", [(QT16[:, t * 512:(t + 1) * 512].rearrange("(k p) n -> p k n", p=128), qs)], qs_b, R=[qs_b], W=[QTb])
            P.dma("pool", [(KT16[:, t * 512:(t + 1) * 512].rearrange("(k p) n -> p k n", p=128), ks)], ks_b, R=[ks_b], W=[KTb])
            for tb in range(4):
                mm_group(psb[6][:, 32 + tb * 4:36 + tb * 4], [(trif_t, fl_t[:, tb * 4:tb * 4 + 4])], [trif_b, fl_b], [PSB[6]])
                mm_group(psb[6][:, 64 + tb * 4:68 + tb * 4], [(onesf_t, fl_t[:, tb * 4:tb * 4 + 4])], [onesf_b, fl_b], [PSB[6]])
            for tb in range(4):
                j = 4 * t + tb
                TT("dve", cpos_t[:, j, :], psb[6][:, 32 + tb * 4:36 + tb * 4], tall_t[:, j, :], ALU.add, [PSB[6], tall_b], [cpos_b])
                TT("dve", tall_t[:, j + 1, :], psb[6][:, 64 + tb * 4:68 + tb * 4], tall_t[:, j, :], ALU.add, [PSB[6], tall_b], [tall_b])
            vs = vst[t % 2]; vs_b = vst_b[t % 2]
            for tb in range(4):
                for hf in range(2):
                    pa = psrot % 6; psrot += 1
                    mm_group(psb[pa][:, :], [(h_t[:, kc, tb * 128:(tb + 1) * 128], wv_t[:, kc, hf * 512:(hf + 1) * 512]) for kc in range(8)],
                             [wv_b, h_b], [PSB[pa]])
                    ACT(vs[:, tb, hf * 512:(hf + 1) * 512], psb[pa][:, :], AF.Copy, [PSB[pa]], [vs_b])
            P.dma("pool", [(V16[t * 512:(t + 1) * 512, :].rearrange("(a p) c -> p a c", p=128), vs)], vs_b, R=[vs_b], W=[Vb])
        P.dma("pool", [(KS32.rearrange("(a p) n -> p a n", p=128), ksum_t)], ksum_b, R=[ksum_b], W=[KSb])

        P.barrier()
        ar.reset()
        KP = [ar.bf(S) for _ in range(2)]; KP_b = [P.buf("KP%d" % i, True) for i in range(2)]
        QP = [ar.bf(S) for _ in range(2)]; QP_b = [P.buf("QP%d" % i, True) for i in range(2)]
        QA_b = [[P.buf("QA%d_%d" % (i, t)) for t in range(NT)] for i in range(2)]
        VA = [ar.bf(NB * 128).rearrange("p (j c) -> p j c", c=128) for _ in range(2)]
        VA_b = [P.buf("VA%d" % i, True) for i in range(2)]
        pt_t = [ar.bf(512) for _ in range(4)]; pt_b = [P.buf("pT%d" % i) for i in range(4)]
        rec_t = ar.f32(512); rec_b = P.buf("rec")
        rec0_t = ar.f32(512); rec0_b = P.buf("rec0")
        yst = [ar.bf(512) for _ in range(2)]; yst_b = [P.buf("yst%d" % i, True) for i in range(2)]
        ks16_t = ar.bf(32); ks16_b = P.buf("ks16")
        ks32_t = ar.f32(32); ks32_b = P.buf("ks32", True)
        wk_t = ar.f32(128).rearrange("p (a n) -> p a n", n=32); wk_b = P.buf("wk")
        t8_t = ar.f32(32).rearrange("p (a n) -> p a n", n=8); t8_b = P.buf("t8")
        sb_t = ar.f32(128).rearrange("p (a n) -> p a n", n=32); sb_b = P.buf("selb")
        acc_t = ar.f32(S); acc_b = P.buf("acc")
        for i in range(2):
            P.op("pool", "memset", (), [VA_b[i]], args=(VA[i][:, :, 64:128], 1.0))
        cnt = {"ps": 0, "po": 0, "pt": 0, "y": 0, "kq": 0, "va": 0}

        def load_rows(dst, dst_b, dram, r0, nr, rb, ind=False):
            pairs = [(dst[0:nr, c0:c0 + CW], dram[r0:r0 + nr, c0:c0 + CW]) for c0 in range(0, S, CW)]
            Rb = [rb]
            if ind == 1:
                pairs += [(dst[64:96, c0:c0 + CW], BI16[0:32, c0:c0 + CW]) for c0 in range(0, S, CW)]
                Rb = [rb, BIb]
            if ind == 2:
                pairs += [(dst[64:65, c0:c0 + CW], BI16[32:33, c0:c0 + CW]) for c0 in range(0, S, CW)]
                Rb = [rb, BIb]
            P.dma("sp", pairs, dst_b, R=Rb, W=[dst_b])

        def load_va(k, head, d):
            nbd = NB // d
            pairs = []
            for r in range(d):
                for b0 in range(0, nbd, 8):
                    nb_ = min(8, nbd - b0)
                    src = V16[r + d * 128 * b0: r + d * 128 * b0 + d * (128 * nb_ - 1) + 1: d, head * 64:(head + 1) * 64]
                    pairs.append((VA[k][:, r * nbd + b0: r * nbd + b0 + nb_, 0:64], src.rearrange("(j p) c -> p j c", p=128)))
            for g_ in range(0, len(pairs), 4):
                P.dma("sp", pairs[g_:g_ + 4], VA_b[k], R=[Vb], W=[VA_b[k]])

        def finalize(po, yrow, i):
            yk = cnt["y"] % 2; cnt["y"] += 1
            P.op("dve", "reciprocal", [PSB[po]], [rec_b], out=rec_t[64:128, :], in_=psb[po][64:128, :])
            TT("dve", yst[yk][0:64, :], psb[po][0:64, :], rec_t[64:128, :], ALU.mult, [PSB[po], rec_b], [yst_b[yk]])
            P.dma("pool", [(YT16[yrow:yrow + 64, i * 512:(i + 1) * 512], yst[yk][0:64, :])], yst_b[yk], R=[yst_b[yk]], W=[YTb])

        LA = 2

        def run_pipe(items):
            n = len(items)
            for idx in range(n + LA):
                if idx < n:
                    items[idx][0]()
                if idx >= LA:
                    items[idx - LA][1]()
                    items[idx - LA][2]()

        def attn_items(Kt, K_b, Qt, Qbufs, r0, nr, va, va_b, bias_fn, i, yrow, hooks):
            items = []
            po = 4 + cnt["po"] % 2; cnt["po"] += 1
            nj = 4 * i + 4
            for j in range(nj):
                off = max(0, j - 4 * i) * 128
                ps = cnt["ps"] % 3; cnt["ps"] += 1
                pk = cnt["pt"] % 4; cnt["pt"] += 1

                def f_score(j=j, off=off, ps=ps):
                    if j in hooks:
                        hooks[j]()
                    mm_group(psb[ps][:, off:512], [(Kt[r0:r0 + nr, j * 128:(j + 1) * 128], Qt[r0:r0 + nr, i * 512 + off:(i + 1) * 512])],
                             [K_b] + Qbufs, [PSB[ps]])

                def f_soft(j=j, off=off, ps=ps, pk=pk):
                    if bias_fn is None:
                        ACT(pt_t[pk][:, off:512], psb[ps][:, off:512], AF.Exp, [PSB[ps]], [pt_b[pk]], scale=0.125)
                    else:
                        ACT(pt_t[pk][:, off:512], psb[ps][:, off:512], AF.Exp, [PSB[ps], cpos_b], [pt_b[pk]], bias=bias_fn(j, i), scale=0.125)
                    if j >= 4 * i:
                        TT("pool", pt_t[pk][:, off:off + 128], pt_t[pk][:, off:off + 128], cmask_t[:, 128:256], ALU.mult,
                           [pt_b[pk], cmask_b], [pt_b[pk]])

                def f_pv(j=j, off=off, pk=pk):
                    MM(psb[po][:, off:512], va[:, j, :], pt_t[pk][:, off:512], j == 0, j == nj - 1, [va_b, pt_b[pk]], [PSB[po]], inc=True)
                    if j == nj - 1:
                        finalize(po, yrow, i)
                items.append((f_score, f_soft, f_pv))
            return items

        for h in range(4):
            kq = cnt["kq"] % 2; cnt["kq"] += 1
            load_rows(KP[kq], KP_b[kq], KT16, h * 64, 64, KTb, ind=2)
            load_rows(QP[kq], QP_b[kq], QT16, h * 64, 64, QTb)
            vk = cnt["va"] % 2; cnt["va"] += 1
            load_va(vk, h, 1)
            items = []

            def mkpre(i, h=h, kq=kq):
                def pre():
                    for qb in range(4):
                        P.op("pe", "transpose", [cpos_b, identf_b], [PSB[7]], qb == 3, out=psb[7][0:1, qb * 128:(qb + 1) * 128],
                             in_=cpos_t[:, 4 * i + qb, h:h + 1], identity=identf_t)
                    P.op("dve", "tensor_scalar", [PSB[7]], [QA_b[kq][i]], out=QP[kq][64:65, i * 512:(i + 1) * 512], in0=psb[7][0:1, :],
                         scalar1=-8.0, scalar2=None, op0=ALU.mult)
                return pre
            mkpre(0)()
            for i in range(NT):
                hooks = {0: mkpre(i + 1)} if i + 1 < NT else {}
                items += attn_items(KP[kq], KP_b[kq], QP[kq], [QP_b[kq], QA_b[kq][i]], 0, 65, VA[vk], VA_b[vk],
                                    (lambda h: lambda j, i: cpos_t[:, j, h:h + 1])(h), i, h * 64, hooks)
            run_pipe(items)
        for m in range(6):
            hd = 10 + m
            kq = cnt["kq"] % 2; cnt["kq"] += 1
            load_rows(KP[kq], KP_b[kq], KT16, hd * 64, 64, KTb, ind=1)
            load_rows(QP[kq], QP_b[kq], QT16, hd * 64, 64, QTb)
            P.dma("sp", [(ks32_t[0:64, :], KS32[m * 64:(m + 1) * 64, :])], ks32_b, R=[KSb], W=[ks32_b])
            P.op("dve", "tensor_copy", [ks32_b], [ks16_b], out=ks16_t[0:64, :], in_=ks32_t[0:64, :])
            vk = cnt["va"] % 2; cnt["va"] += 1
            load_va(vk, hd, 1)
            Kt = KP[kq]; Qt = QP[kq]
            items = []

            def mkpre1(i, kq=kq, Qt=Qt):
                def pre():
                    for qb in range(4):
                        q0 = i * 512 + qb * 128
                        mm_group(psb[6][:, qb * 32:qb * 32 + NMB], [(Qt[0:64, q0:q0 + 128], ks16_t[0:64, 0:NMB])], [QP_b[kq], ks16_b], [PSB[6]])
                    P.op("dve", "memset", (), [wk_b], args=(wk_t, -1e30))
                    P.op("dve", "memset", (), [sb_b], args=(sb_t, -1.0))
                    for hq in range(2):
                        own = 2 * i + hq
                        if own > 3:
                            P.op("dve", "tensor_copy", [PSB[6]], [wk_b], out=wk_t[:, 2 * hq:2 * hq + 2, 0:own],
                                 in_=psb[6][:, 64 * hq:64 * hq + 64].rearrange("p (a n) -> p a n", n=32)[:, :, 0:own])
                        for qq in range(2):
                            qb = 2 * hq + qq
                            if own > 3:
                                P.op("dve", "max", [wk_b], [t8_b], out=t8_t[:, qb, :], in_=wk_t[:, qb, 0:max(own, 8)])
                                P.op("dve", "tensor_scalar", [wk_b, t8_b], [sb_b], out=sb_t[:, qb, 0:own], in0=wk_t[:, qb, 0:own],
                                     scalar1=t8_t[:, qb, 2:3], scalar2=1.0, op0=ALU.is_ge, op1=ALU.subtract)
                                P.op("dve", "memset", (), [sb_b], args=(sb_t[:, qb, own:own + 1], 0.0))
                            else:
                                P.op("dve", "memset", (), [sb_b], args=(sb_t[:, qb, 0:own + 1], 0.0))
                    P.op("dve", "tensor_scalar", [sb_b], [sb_b], out=sb_t, in0=sb_t, scalar1=BIG, scalar2=None, op0=ALU.mult)
                return pre

            def mkpre2(i, kq=kq, Qt=Qt):
                def pre():
                    for qb in range(4):
                        P.op("pe", "transpose", [sb_b, identf_b], [PSB[7]], qb == 3, out=psb[7][0:32, qb * 128:(qb + 1) * 128],
                             in_=sb_t[:, qb, :], identity=identf_t)
                    P.op("dve", "tensor_copy", [PSB[7]], [QA_b[kq][i]], out=Qt[64:96, i * 512:(i + 1) * 512], in_=psb[7][0:32, :])
                return pre
            mkpre1(0)(); mkpre2(0)()
            for i in range(NT):
                hooks = {}
                if i + 1 < NT:
                    hooks[0] = mkpre1(i + 1)
                    hooks[4 * i + 3] = mkpre2(i + 1)
                items += attn_items(Kt, KP_b[kq], Qt, [QP_b[kq], QA_b[kq][i]], 0, 96, VA[vk], VA_b[vk], None, i, 384 + m * 64, hooks)
            run_pipe(items)
        for oh in range(2):
            for g in range(3):
                d = DIL[g]
                ptile = 2 + g
                hd = 4 + 2 * g + oh
                kq = cnt["kq"] % 2; cnt["kq"] += 1
                load_rows(KP[kq], KP_b[kq], KT16, ptile * 128, 128, KTb)
                load_rows(QP[kq], QP_b[kq], QT16, ptile * 128, 128, QTb)
                vk = cnt["va"] % 2; cnt["va"] += 1
                load_va(vk, hd, d)
                Kt = KP[kq]; Qt = QP[kq]; r0 = oh * 64
                KQ = [KP_b[kq], QP_b[kq]]
                nbd = NB // d
                items = []
                for r in range(d):
                    for ub4 in range(0, nbd, 4):
                        po = 4 + cnt["po"] % 2; cnt["po"] += 1
                        nu = min(4, nbd - ub4)
                        for u in range(nu):
                            ub = ub4 + u
                            ps = cnt["ps"] % 3; cnt["ps"] += 1
                            pk = cnt["pt"] % 4; cnt["pt"] += 1
                            qa = r + d * 128 * ub
                            lo = 0 if ub > 0 else 128
                            bi = r * nbd + ub

                            def f_score(ub=ub, ps=ps, qa=qa, d=d, r0=r0, Kt=Kt, Qt=Qt, KQ=KQ):
                                qap = Qt[r0:r0 + 64, qa: qa + d * 127 + 1: d]
                                if ub > 0:
                                    ka = qa - d * 128
                                    mm_group(psb[ps][:, 0:128], [(Kt[r0:r0 + 64, ka: ka + d * 127 + 1: d], qap)], KQ, [PSB[ps]])
                                mm_group(psb[ps][:, 128:256], [(Kt[r0:r0 + 64, qa: qa + d * 127 + 1: d], qap)], KQ, [PSB[ps]])

                            def f_soft(ps=ps, pk=pk, lo=lo):
                                ACT(pt_t[pk][:, lo:256], psb[ps][:, lo:256], AF.Exp, [PSB[ps]], [pt_b[pk]], scale=0.125)
                                TT("pool", pt_t[pk][:, lo:256], pt_t[pk][:, lo:256], cmask_t[:, lo:256], ALU.mult, [pt_b[pk], cmask_b], [pt_b[pk]])

                            def f_pv(ub=ub, u=u, nu=nu, po=po, pk=pk, bi=bi, vk=vk, g=g, r=r, d=d, ub4=ub4):
                                if ub > 0:
                                    MM(psb[po][:, u * 128:(u + 1) * 128], VA[vk][:, bi - 1, :], pt_t[pk][:, 0:128], True, False,
                                       [VA_b[vk], pt_b[pk]], [PSB[po]], inc=False)
                                MM(psb[po][:, u * 128:(u + 1) * 128], VA[vk][:, bi, :], pt_t[pk][:, 128:256], ub == 0, True,
                                   [VA_b[vk], pt_b[pk]], [PSB[po]], inc=True)
                                if u == nu - 1:
                                    a0 = r + d * 128 * ub4
                                    accv = acc_t[:, a0: a0 + d * (128 * nu - 1) + 1: d]
                                    if g == 0:
                                        P.op("dve", "tensor_copy", [PSB[po]], [acc_b], out=accv, in_=psb[po][:, 0:128 * nu])
                                    else:
                                        TT("dve", accv, psb[po][:, 0:128 * nu], accv, ALU.add, [PSB[po], acc_b], [acc_b])
                            items.append((f_score, f_soft, f_pv))
                run_pipe(items)
            for i in range(NT):
                yk = cnt["y"] % 2; cnt["y"] += 1
                P.op("dve", "reciprocal", [acc_b], [rec_b], out=rec_t[64:128, :], in_=acc_t[64:128, i * 512:(i + 1) * 512])
                P.op("dve", "tensor_copy", [rec_b], [rec0_b], out=rec0_t[0:64, :], in_=rec_t[64:128, :])
                TT("dve", yst[yk][0:64, :], acc_t[0:64, i * 512:(i + 1) * 512], rec0_t[0:64, :], ALU.mult, [acc_b, rec0_b], [yst_b[yk]])
                P.dma("pool", [(YT16[256 + oh * 64:256 + oh * 64 + 64, i * 512:(i + 1) * 512], yst[yk][0:64, :])], yst_b[yk], R=[yst_b[yk]], W=[YTb])

        P.barrier()
        ar.reset()
        xt = [ar.f32(4096).rearrange("p (k n) -> p k n", n=512) for _ in range(4)]
        xt_b = [P.buf("xt%d" % i, True) for i in range(4)]
        yt = [ar.bf(3072).rearrange("p (k n) -> p k n", n=512) for _ in range(2)]
        yt_b = [P.buf("ytC%d" % i, True) for i in range(2)]
        pp = [ar.f32(1024).rearrange("p (k n) -> p k n", n=512) for _ in range(2)]
        pp_b = [P.buf("ppC%d" % i, True) for i in range(2)]
        p16 = [ar.bf(1024).rearrange("p (k n) -> p k n", n=512) for _ in range(2)]
        p16_b = [P.buf("p16_%d" % i) for i in range(2)]
        hh = [ar.bf(4096).rearrange("p (k n) -> p k n", n=512) for _ in range(2)]
        hh_b = [P.buf("hC%d" % i) for i in range(2)]
        sqo_t = ar.bf(4096).rearrange("p (k n) -> p k n", n=512); sqo_b = P.buf("sqo")
        sqx_t = ar.bf(4096).rearrange("p (k n) -> p k n", n=512); sqx_b = P.buf("sqx")
        mg_t = ar.bf(4096).rearrange("p (k n) -> p k n", n=512); mg_b = P.buf("mgC")
        o_t = ar.f32(4096).rearrange("p (k n) -> p k n", n=512); o_b = P.buf("oC")
        ff_t = ar.bf(NFF * 512).rearrange("p (k n) -> p k n", n=512); ff_b = P.buf("ffC")
        rstd_t = ar.f32(512); rstd_b = P.buf("rstd")
        tmp_t = ar.f32(512); tmp_b = P.buf("tmp")
        gs = [ar.f32(512) for _ in range(3)]; gs_b = [P.buf("gs%d" % i) for i in range(3)]
        ma = [ar.f32(512) for _ in range(3)]; ma_b = [P.buf("ma%d" % i) for i in range(3)]
        tt = [ar.f32(512) for _ in range(2)]; tt_b = [P.buf("tt%d" % i) for i in range(2)]
        ring = Ring(6, 1024, "ringC")
        prc = {"i": 0}

        def nps():
            k = prc["i"] % 7; prc["i"] += 1
            return k

        def loadXY(t):
            k = t % 2; xk = t % 4
            P.dma("sp", [(xt[xk][:, 0:4, :], xtile_ap(xsrc(l), t)[:, 0:4, :]),
                         (xt[xk][:, 4:8, :], xtile_ap(xsrc(l), t)[:, 4:8, :])], xt_b[xk], R=[Xb], W=[xt_b[xk]])
            P.dma("sp", [(yt[k], YT16[:, t * 512:(t + 1) * 512].rearrange("(k p) n -> p k n", p=128))], yt_b[k], R=[YTb], W=[yt_b[k]])

        def loadP(t):
            k = t % 2
            P.dma("sp", [(pp[k], pT[l * PLE:(l + 1) * PLE, t * 512:(t + 1) * 512].rearrange("(k p) n -> p k n", p=128))], pp_b[k], W=[pp_b[k]])

        def stats(sq_t, sq_b):
            rms_stats(sq_t, sq_b, rstd_t, rstd_b, tmp_t, tmp_b)

        XK = {0: 0, 1: 1}

        def mk_h(k, gcol):
            for kc in range(8):
                P.op("dve", "scalar_tensor_tensor", [xt_b[XK[k]], rstd_b, gains_b], [hh_b[k]], out=hh[k][:, kc, :], in0=xt[XK[k]][:, kc, :],
                     scalar=gains_t[:, gcol + kc:gcol + kc + 1], in1=rstd_t, op0=ALU.mult, op1=ALU.mult)

        def pre1(k):
            for kc in range(8):
                ACT(sqx_t[:, kc, :], xt[XK[k]][:, kc, :], AF.Square, [xt_b[XK[k]]], [sqx_b])
            stats(sqx_t, sqx_b)
            mk_h(k, g0 + 0)

        def residual_steps(k, gcol, want_sq):
            xk = XK[k]
            steps = [lambda: stats(sqo_t, sqo_b)]

            def mk(m):
                def step():
                    q = m % 2
                    eng = "pool" if m % 2 == 0 else "dve"
                    P.op("dve", "scalar_tensor_tensor", [o_b, rstd_b, gains_b], [tt_b[q]], out=tt[q], in0=o_t[:, m, :],
                         scalar=gains_t[:, gcol + m:gcol + m + 1], in1=rstd_t, op0=ALU.mult, op1=ALU.mult)
                    TT(eng, xt[xk][:, m, :], xt[xk][:, m, :], tt[q], ALU.add, [xt_b[xk], tt_b[q]], [xt_b[xk]])
                    if want_sq:
                        TT(eng, sqx_t[:, m, :], xt[xk][:, m, :], xt[xk][:, m, :], ALU.mult, [xt_b[xk]], [sqx_b])
                return step
            return steps + [mk(m) for m in range(8)]

        def run_bg(bg, n=1):
            for _ in range(n):
                if bg:
                    bg.pop(0)()

        def flush_bg(bg):
            while bg:
                bg.pop(0)()

        def evac_o(ps, m):
            ACT(sqo_t[:, m, :], psb[ps][:, :], AF.Square, [PSB[ps]], [sqo_b])
            ACT(o_t[:, m, :], psb[ps][:, :], AF.Copy, [PSB[ps]], [o_b])

        def S1(k, inject, bg):
            ychunks = [(0, 2), (2, 1), (3, 3)]
            for m in range(8):
                if m == 3:
                    flush_bg(bg)
                    if inject is not None:
                        inject()
                wb3, wb_b = ring.load(wslab("wBR", l, m), 768, [WB[l]])
                for b in range(3):
                    run_bg(bg)
                    wg3, wg_b = ring.load(wslab("wG", l, b * 8 + m), 1024, [WB[l]])
                    pg = nps()
                    mm_group(psb[pg][:, :], [(wg3[:, kc, :], hh[k][:, kc, :]) for kc in range(8)], [wg_b, hh_b[k]], [PSB[pg]])
                    ACT(gs[b], psb[pg][:, :], AF.Sigmoid, [PSB[pg]], [gs_b[b]])
                    c0, ncc = ychunks[b]
                    pq = nps()
                    mm_group(psb[pq][:, :], [(wb3[:, c0 + c, :], yt[k][:, c0 + c, :]) for c in range(ncc)], [wb_b, yt_b[k]], [PSB[pq]])
                    TT("dve", ma[b], psb[pq][:, :], gs[b], ALU.mult, [PSB[pq], gs_b[b]], [ma_b[b]])
                TT("pool", ma[0], ma[0], ma[1], ALU.add, [ma_b[0], ma_b[1]], [ma_b[0]])
                TT("pool", mg_t[:, m, :], ma[0], ma[2], ALU.add, [ma_b[0], ma_b[2]], [mg_b])
            for m in range(8):
                w3, w_b = ring.load(wslab("wO", l, m), 1024, [WB[l]])
                ps = nps()
                mm_group(psb[ps][:, :], [(w3[:, kc, :], mg_t[:, kc, :]) for kc in range(8)], [w_b, mg_b], [PSB[ps]])
                evac_o(ps, m)

        def S2(k, inject, bg):
            for j in range(NFF):
                run_bg(bg)
                if j == 8:
                    flush_bg(bg)
                    if inject is not None:
                        inject()
                wg3, wg_b = ring.load(wslab("wFG", l, j), 1024, [WB[l]])
                pg = nps()
                mm_group(psb[pg][:, :], [(wg3[:, kc, :], hh[k][:, kc, :]) for kc in range(8)], [wg_b, hh_b[k]], [PSB[pg]])
                kk = j % 3
                ACT(gs[kk], psb[pg][:, :], AF.Silu, [PSB[pg]], [gs_b[kk]])
                wu3, wu_b = ring.load(wslab("wFU", l, j), 1024, [WB[l]])
                pu = nps()
                mm_group(psb[pu][:, :], [(wu3[:, kc, :], hh[k][:, kc, :]) for kc in range(8)], [wu_b, hh_b[k]], [PSB[pu]])
                TT("dve", ff_t[:, j, :], psb[pu][:, :], gs[kk], ALU.mult, [PSB[pu], gs_b[kk]], [ff_b])
            for m in range(8):
                ps = nps()
                pieces = [(0, 8), (8, 16), (16, NFF)]
                first = True
                for (j0, j1) in pieces:
                    w3, w_b = ring.load(wslab("wFD", l, m)[:, j0 * 128:j1 * 128], (j1 - j0) * 128, [WB[l]])
                    for j in range(j0, j1):
                        MM(psb[ps][:, :], w3[:, j - j0, :], ff_t[:, j, :], first, j == NFF - 1, [w_b, ff_b], [PSB[ps]], inc=(j == j1 - 1))
                        first = False
                evac_o(ps, m)

        def d2(k):
            ACT(hh[k], xt[XK[k]], AF.Copy, [xt_b[XK[k]]], [hh_b[k]])
            P.op("dve", "tensor_copy", [pp_b[k]], [p16_b[k]], out=p16[k], in_=pp[k])

        def S3(k, inject, bg):
            for m in range(8):
                run_bg(bg, 3)
                if m == 3:
                    flush_bg(bg)
                    if inject is not None:
                        inject()
                wg3, wg_b = ring.load(wslab("wPG", l, m), 1024, [WB[l]])
                pg = nps()
                mm_group(psb[pg][:, :], [(wg3[:, kc, :], hh[k][:, kc, :]) for kc in range(8)], [wg_b, hh_b[k]], [PSB[pg]])
                kk = m % 3
                ACT(gs[kk], psb[pg][:, :], AF.Sigmoid, [PSB[pg]], [gs_b[kk]])
                wp3, wp_b = ring.load(wslab("wPL", l, m), 256, [WB[l]])
                pu = nps()
                mm_group(psb[pu][:, :], [(wp3[:, kc, :], p16[k][:, kc, :]) for kc in range(2)], [wp_b, p16_b[k]], [PSB[pu]])
                TT("dve", o_t[:, m, :], psb[pu][:, :], gs[kk], ALU.mult, [PSB[pu], gs_b[kk]], [o_b])
                ACT(sqo_t[:, m, :], o_t[:, m, :], AF.Square, [o_b], [sqo_b])

        def storeC(t):
            xk = t % 4
            P.dma("pool", [(xtile_ap(xdst(l), t)[:, 0:4, :], xt[xk][:, 0:4, :]), (xtile_ap(xdst(l), t)[:, 4:8, :], xt[xk][:, 4:8, :])],
                  xt_b[xk], R=[xt_b[xk]], W=[Xb])

        def c2(k):
            stats(sqx_t, sqx_b)
            mk_h(k, g0 + 16)

        loadXY(0); loadP(0); loadXY(1); loadP(1)
        XK[0] = 0
        pre1(0)
        bgq = []
        for tp in range(0, NT, 2):
            tA, tB = tp, tp + 1
            nxt = tp + 2 < NT
            XK[0] = tA % 4; XK[1] = tB % 4

            def inj_pre1B():
                pre1(1)
            S1(0, inj_pre1B, bgq)
            if nxt:
                loadXY(tA + 2)
            bgq = residual_steps(0, g0 + 8, True)
            S1(1, lambda: c2(0), bgq)
            if nxt:
                loadXY(tB + 2)
            bgq = residual_steps(1, g0 + 8, True)
            S2(0, lambda: c2(1), bgq)
            bgq = residual_steps(0, g0 + 24, False)
            S2(1, lambda: d2(0), bgq)
            if nxt:
                loadP(tA + 2)
            bgq = residual_steps(1, g0 + 24, False)
            S3(0, lambda: d2(1), bgq)
            if nxt:
                loadP(tB + 2)
            bgq = residual_steps(0, g0 + 32, False) + [(lambda t=tA: storeC(t))]

            def inj_next(tA=tA):
                XK[0] = (tA + 2) % 4
                pre1(0)
                XK[0] = tA % 4
            S3(1, inj_next if nxt else None, bgq)
            bgq = residual_steps(1, g0 + 32, False) + [(lambda t=tB: storeC(t))]
        flush_bg(bgq)
    P.barrier()
    block = es.enter_context(nc.Block())
    P.replay(block)
    es.close()
    return nc


def _slabs(w, cols_list, kc):
    out = np.empty((len(cols_list), 128, kc, 128), np.float32)
    wk = w.reshape(kc, 128, w.shape[1])
    for m, cols in enumerate(cols_list):
        out[m] = wk[:, :, cols].transpose(1, 0, 2)
    return out.reshape(len(cols_list) * 128, kc * 128)


def _host_weights(inp, L):
    r = {}
    ar_ = np.arange
    lists = {n: [] for n in ("wA", "wV", "wF", "wG", "wBR", "wO", "wFG", "wFU", "wFD", "wPL", "wPG")}
    for l in range(L):
        w_in = np.asarray(inp["w_in"][l], np.float32)
        colsA = []
        for base in (0, 1024):
            for pt in range(8):
                c = base + pt * 128 + ar_(128)
                colsA.append(c)
                if pt >= 2:
                    sw = base + pt * 128 + (ar_(128) // 64) * 64 + (ar_(128) % 64 + 32) % 64
                    colsA.append(sw)
        lists["wA"].append(_slabs(w_in, colsA, 8))
        wv = w_in[:, 2048:3072].reshape(8, 128, 1024).transpose(1, 0, 2).reshape(128, 8192)
        lists["wV"].append(wv)
        wf = w_in[:, 3072:3076].reshape(8, 128, 4).transpose(1, 0, 2).reshape(128, 32)
        lists["wF"].append(wf)
        lists["wG"].append(_slabs(w_in, [3076 + b * 1024 + m * 128 + ar_(128) for b in range(3) for m in range(8)], 8))
        wbr = np.concatenate([np.asarray(inp["w_br_a"][l]), np.asarray(inp["w_br_b"][l]), np.asarray(inp["w_br_c"][l])], axis=0)
        lists["wBR"].append(_slabs(wbr.astype(np.float32), [m * 128 + ar_(128) for m in range(8)], 6))
        lists["wO"].append(_slabs(np.asarray(inp["w_out"][l], np.float32), [m * 128 + ar_(128) for m in range(8)], 8))
        lists["wFG"].append(_slabs(np.asarray(inp["w_ffn_gate"][l], np.float32), [m * 128 + ar_(128) for m in range(NFF)], 8))
        lists["wFU"].append(_slabs(np.asarray(inp["w_ffn_up"][l], np.float32), [m * 128 + ar_(128) for m in range(NFF)], 8))
        lists["wFD"].append(_slabs(np.asarray(inp["w_ffn_down"][l], np.float32), [m * 128 + ar_(128) for m in range(8)], NFF))
        lists["wPL"].append(_slabs(np.asarray(inp["w_ple"][l], np.float32), [m * 128 + ar_(128) for m in range(8)], 2))
        lists["wPG"].append(_slabs(np.asarray(inp["w_ple_gate"][l], np.float32), [m * 128 + ar_(128) for m in range(8)], 8))
    for n, v in lists.items():
        r[n] = np.ascontiguousarray(np.concatenate(v, axis=0), dtype=np.float32)
    gl = []
    for l in range(L):
        for n in ("g_mix_pre", "g_mix_post", "g_ffn_pre", "g_ffn_post", "g_ple_post"):
            gl.append(np.asarray(inp[n][l], np.float32).reshape(8, 128).T)
    r["gains"] = np.ascontiguousarray(np.concatenate(gl, axis=1), dtype=np.float32)
    r["bfb"] = np.ascontiguousarray(np.broadcast_to(np.tile(np.asarray(inp["b_f"], np.float32).reshape(L, 1, 4), (1, 4, 1)).reshape(1, L * 16), (128, L * 16)))
    return r


def _consts(S):
    c = {}
    inv = (1.0 / (np.float32(10000.0) ** (np.arange(0, 64, 2, dtype=np.float32) / np.float32(64)))).astype(np.float32)
    ang = (np.arange(S, dtype=np.float32)[:, None] * inv[None, :]).astype(np.float32)
    cos = np.cos(ang.astype(np.float64)).astype(np.float32).T
    sin = np.sin(ang.astype(np.float64)).astype(np.float32).T
    c["cosT"] = np.ascontiguousarray(np.concatenate([cos, cos, cos, cos], axis=0))
    c["sinT"] = np.ascontiguousarray(np.concatenate([-sin, sin, -sin, sin], axis=0))
    p = np.arange(128)
    c["cmask"] = np.ascontiguousarray(np.concatenate([(p[:, None] >= p[None, :]), (p[:, None] <= p[None, :])], axis=1).astype(np.float32))
    c["trif"] = np.ascontiguousarray((p[:, None] <= p[None, :]).astype(np.float32))
    c["identf"] = np.eye(128, dtype=np.float32)
    c["blkind"] = np.ascontiguousarray(np.concatenate([(np.arange(S)[None, :] // 256 == np.arange(32)[:, None]), np.ones((1, S), bool)], axis=0).astype(np.float32))
    return c


_NC_CACHE = {}


def kernel(**inputs):
    x = np.asarray(inputs["x"], np.float32)
    p = np.asarray(inputs["p"], np.float32)
    B, S, _ = x.shape
    L = p.shape[0]
    key = (S, L)
    if key not in _NC_CACHE:
        _NC_CACHE[key] = build(S, L)
    nc = _NC_CACHE[key]
    shared = _host_weights(inputs, L)
    shared.update(_consts(S))
    in_maps = []
    for b in range(B):
        m = dict(shared)
        m["xT"] = np.ascontiguousarray(x[b].T)
        m["pT"] = np.ascontiguousarray(p[:, b].transpose(0, 2, 1).reshape(L * PLE, S))
        in_maps.append(m)
    res = run_bass_kernel_spmd(nc, in_maps, core_ids=list(range(B)))
    out = np.stack([np.ascontiguousarray(res.results[b]["outT"].T) for b in range(B)], axis=0)
    return out.astype(np.float32)
```

```python
import numpy as np
from contextlib import ExitStack
import concourse.bass as bass
import concourse.mybir as mybir
from concourse.bass_utils import run_bass_kernel_spmd

F32 = mybir.dt.float32
BF = mybir.dt.bfloat16
AF = mybir.ActivationFunctionType
ALU = mybir.AluOpType
AX = mybir.AxisListType

D = 1024
HD = 64
PLE = 256
DFF = 2816
NFF = 22
BIG = 30000.0
DIL = (1, 4, 16)
SAME_ENGINE_SYNC = True


class Buf:
    def __init__(self, name, sem=None):
        self.name = name
        self.w = {}
        self.r = {}
        self.sem = sem
        self.cnt = 0


class Eng:
    def __init__(self, name, sem):
        self.name = name
        self.sem = sem
        self.cnt = 0
        self.seen = {}
        self.prog = []


class Prog:
    def __init__(self, nc, es):
        self.nc = nc
        self.es = es
        self.sems = []
        self.E = {}
        for n in ("pe", "act", "dve", "pool"):
            self.E[n] = Eng(n, self.newsem(n))
        self.E["sp"] = Eng("sp", None)
        self.bufs = []
        self.bynames = {}

    def newsem(self, name):
        h = self.es.enter_context(self.nc.semaphore("s_" + name))
        self.sems.append(h)
        return len(self.sems) - 1

    def buf(self, name, dma=False):
        if name in self.bynames:
            return self.bynames[name]
        b = Buf(name, self.newsem(name) if dma else None)
        self.bufs.append(b)
        self.bynames[name] = b
        return b

    def _waits(self, X, R, W):
        need = {}
        for b in R:
            for k, v in b.w.items():
                need[k] = max(need.get(k, 0), v)
        for b in W:
            for k, v in b.w.items():
                need[k] = max(need.get(k, 0), v)
            for k, v in b.r.items():
                need[k] = max(need.get(k, 0), v)
        out = []
        for k, v in need.items():
            if k == X.sem and (X.name == "pe" or not SAME_ENGINE_SYNC):
                continue
            if X.seen.get(k, 0) >= v:
                continue
            X.seen[k] = v
            out.append((k, v))
        return out

    def op(self, eng, name, R=(), W=(), inc=True, args=(), **kw):
        X = self.E[eng]
        waits = self._waits(X, R, W)
        tok = X.cnt + 1
        if inc:
            X.cnt = tok
        X.prog.append((waits, ("op", name, args, kw), inc))
        for b in R:
            b.r[X.sem] = tok
        for b in W:
            b.w = {X.sem: tok}
            b.r = {}

    def dma(self, q, pairs, sb, R=(), W=()):
        X = self.E[q]
        waits = self._waits(X, R, W)
        first = True
        for o, i in pairs:
            sb.cnt += 16
            X.prog.append((waits if first else [], ("dma", o, i, sb.sem), False))
            first = False
        for b in R:
            b.r[sb.sem] = sb.cnt
        for b in W:
            b.w = {sb.sem: sb.cnt}
            b.r = {}

    def barrier(self):
        toks = {}
        for n in ("pe", "act", "dve", "pool"):
            toks[self.E[n].sem] = self.E[n].cnt
        for b in self.bufs:
            if b.sem is not None and b.cnt > 0:
                toks[b.sem] = b.cnt
        for n, X in self.E.items():
            waits = []
            for k, v in toks.items():
                if v > 0 and X.seen.get(k, 0) < v and not (k == X.sem and n == "pe"):
                    X.seen[k] = v
                    waits.append((k, v))
            X.prog.append((waits, None, False))

    def replay(self, block):
        sems = self.sems

        def run(X, e):
            for waits, fn, inc in X.prog:
                for k, v in waits:
                    e.wait_ge(sems[k], v)
                if fn is None:
                    continue
                if fn[0] == "dma":
                    _, o, i, k = fn
                    e.dma_start(out=o, in_=i).then_inc(sems[k], 16)
                else:
                    _, name, args, kw = fn
                    ins = getattr(e, name)(*args, **kw)
                    if inc:
                        ins.then_inc(sems[X.sem], 1)

        @block.sync
        def _(e):
            run(self.E["sp"], e)

        @block.tensor
        def _(e):
            run(self.E["pe"], e)

        @block.scalar
        def _(e):
            run(self.E["act"], e)

        @block.vector
        def _(e):
            run(self.E["dve"], e)

        @block.gpsimd
        def _(e):
            run(self.E["pool"], e)


class Arena:
    def __init__(self, ap, lo, hi):
        self.ap = ap
        self.lo = lo
        self.hi = hi
        self.p = lo

    def f32(self, n):
        a = self.ap[:, self.p:self.p + n]
        self.p += n
        assert self.p <= self.hi, ("arena overflow", self.p, self.hi)
        return a

    def bf(self, n):
        n2 = (n + 1) // 2
        a = self.ap[:, self.p:self.p + n2].bitcast(BF)
        self.p += n2
        assert self.p <= self.hi, ("arena overflow", self.p, self.hi)
        return a[:, 0:n]

    def reset(self):
        self.p = self.lo


def build(S=8192, L=2, dbg=False):
    NT = S // 512
    NB = S // 128
    NMB = S // 256
    CW = min(2048, S)
    assert NMB <= 32
    nc = bass.Bass("TRN2", target_bir_lowering=False)

    def din(name, shape, dt=F32):
        return nc.dram_tensor(name, shape, dt, kind="ExternalInput").ap()

    def dscr(name, shape, dt=BF):
        return nc.dram_tensor(name, shape, dt, kind=("ExternalOutput" if dbg else "Internal")).ap()

    xT = din("xT", [D, S])
    pT = din("pT", [L * PLE, S])
    wspec = [("wA", 28, 1024), ("wV", 1, 8192), ("wF", 1, 32), ("wG", 24, 1024), ("wBR", 8, 768),
             ("wO", 8, 1024), ("wFG", 22, 1024), ("wFU", 22, 1024), ("wFD", 8, 2816),
             ("wPL", 8, 256), ("wPG", 8, 1024)]
    w32 = {}
    w16 = {}
    for n, ns, nc_ in wspec:
        w32[n] = din(n, [L * ns * 128, nc_])
        w16[n] = nc.dram_tensor(n + "_16", [L * ns * 128, nc_], BF, kind="Internal").ap()
    wns = {n: ns for n, ns, _ in wspec}
    gains = din("gains", [128, L * 5 * 8])
    bfb = din("bfb", [128, L * 16])
    cosT = din("cosT", [128, S])
    sinT = din("sinT", [128, S])
    cmask32 = din("cmask", [128, 256])
    trif = din("trif", [128, 128])
    identf = din("identf", [128, 128])
    blkind32 = din("blkind", [33, S])
    outT = nc.dram_tensor("outT", [D, S], F32, kind="ExternalOutput").ap()

    X32 = dscr("X32", [D, S], F32)
    QT16 = dscr("QT16", [1024, S])
    KT16 = dscr("KT16", [1024, S])
    V16 = dscr("V16", [S, 1024])
    YT16 = dscr("YT16", [768, S])
    KS32 = dscr("KS32", [384, 32], F32)
    BI16 = nc.dram_tensor("BI16", [33, S], BF, kind="Internal").ap()

    es = ExitStack()
    P = Prog(nc, es)
    AW = 52992
    arena_t = es.enter_context(nc.sbuf_tensor("arena", [128, AW], F32))
    PERS = 2048
    pers = Arena(arena_t, 0, PERS)
    ar = Arena(arena_t, PERS, AW)
    psb = [es.enter_context(nc.psum_tensor("psb%d" % i, [128, 512], F32)) for i in range(8)]
    PSB = [P.buf("psb%d" % i) for i in range(8)]

    def ACT(out, in_, func, R, W, **kw):
        P.op("act", "activation", R, W, out=out, in_=in_, func=func, **kw)

    def TT(eng, out, in0, in1, op, R, W):
        P.op(eng, "tensor_tensor", R, W, out=out, in0=in0, in1=in1, op=op)

    def MM(out, lhsT, rhs, start, stop, R, W, inc=True):
        P.op("pe", "matmul", R, W, inc, args=(out,), lhsT=lhsT, rhs=rhs, start=start, stop=stop)

    def mm_group(out_ap, pairs, Rb, Wb):
        n = len(pairs)
        for i, (lt, rh) in enumerate(pairs):
            MM(out_ap, lt, rh, i == 0, i == n - 1, Rb, Wb, inc=(i == n - 1))

    gains_t = pers.f32(L * 40); gains_b = P.buf("gains", True)
    bfb_t = pers.f32(L * 16); bfb_b = P.buf("bfb", True)
    trif_t = pers.f32(128); trif_b = P.buf("trif", True)
    identf_t = pers.f32(128); identf_b = P.buf("identf", True)
    onesf_t = pers.f32(128); onesf_b = P.buf("onesf")
    ones16_t = pers.bf(128); ones16_b = P.buf("ones16")
    cmask_t = pers.bf(256); cmask_b = P.buf("cmask", True)
    eps_t = pers.f32(1); one_t = pers.f32(1); cst_b = P.buf("cst")
    cpos_t = pers.f32(NB * 4).rearrange("p (j h) -> p j h", h=4); cpos_b = P.buf("cpos")
    tall_t = pers.f32((NB + 1) * 4).rearrange("p (j h) -> p j h", h=4); tall_b = P.buf("tall")
    ksum_t = pers.f32(3 * 32).rearrange("p (a n) -> p a n", n=32); ksum_b = P.buf("ksum", True)

    P.dma("sp", [(gains_t, gains)], gains_b, W=[gains_b])
    P.dma("sp", [(bfb_t, bfb)], bfb_b, W=[bfb_b])
    P.dma("sp", [(trif_t, trif)], trif_b, W=[trif_b])
    P.dma("sp", [(identf_t, identf)], identf_b, W=[identf_b])
    P.dma("pool", [(cmask_t, cmask32)], cmask_b, W=[cmask_b])
    P.op("dve", "memset", (), [onesf_b], args=(onesf_t, 1.0))
    P.op("dve", "memset", (), [ones16_b], args=(ones16_t, 1.0))
    P.op("dve", "memset", (), [cst_b], args=(eps_t, 1e-6))
    P.op("dve", "memset", (), [cst_b], args=(one_t, 1.0))
    P.op("dve", "memset", (), [tall_b], args=(tall_t[:, 0, :], 0.0))
    P.op("dve", "memset", (), [ksum_b], args=(ksum_t, 0.0))

    WB = [P.buf("w16_%d" % l) for l in range(L)]
    CB = [P.buf("cast%d" % i, True) for i in range(2)]
    BIb = P.buf("bi16", True)
    P.dma("pool", [(BI16[:, c0:c0 + CW], blkind32[:, c0:c0 + CW]) for c0 in range(0, S, CW)], BIb, W=[BIb])
    cg = 0
    for l in range(L):
        pairs = []
        for n, ns, ncol in wspec:
            for s_ in range(ns):
                r0 = (l * ns + s_) * 128
                cw = 2048 if ncol > 2816 else ncol
                for c0 in range(0, ncol, cw):
                    pairs.append((w16[n][r0:r0 + 128, c0:c0 + cw], w32[n][r0:r0 + 128, c0:c0 + cw]))
        for g0_ in range(0, len(pairs), 8):
            cb = CB[cg % 2]; cg += 1
            P.dma("pool", pairs[g0_:g0_ + 8], cb, W=[cb])
        WB[l].w = {CB[0].sem: CB[0].cnt, CB[1].sem: CB[1].cnt}

    def wslab(n, l, s_):
        r0 = (l * wns[n] + s_) * 128
        return w16[n][r0:r0 + 128, :]

    Xb = P.buf("X32d"); QTb = P.buf("QTd"); KTb = P.buf("KTd"); Vb = P.buf("Vd"); YTb = P.buf("YTd"); KSb = P.buf("KSd")

    def xsrc(l):
        return xT if l == 0 else X32

    def xdst(l):
        return outT if l == L - 1 else X32

    def xtile_ap(dram, t):
        return dram[:, t * 512:(t + 1) * 512].rearrange("(kc p) n -> p kc n", p=128)

    class Ring:
        def __init__(self, n, words, name):
            self.t = [ar.bf(words) for _ in range(n)]
            self.b = [P.buf("%s%d" % (name, i), True) for i in range(n)]
            self.i = 0

        def load(self, dram_ap, ncol, Rb):
            k = self.i % len(self.t)
            self.i += 1
            P.dma("sp", [(self.t[k][:, 0:ncol], dram_ap)], self.b[k], R=Rb, W=[self.b[k]])
            return self.t[k][:, 0:ncol].rearrange("p (k n) -> p k n", n=128), self.b[k]

    def rms_stats(sq_t, sq_b, rstd_t, rstd_b, tmp_t, tmp_b):
        mm_group(psb[7][:, :], [(ones16_t, sq_t[:, kc, :]) for kc in range(8)], [ones16_b, sq_b], [PSB[7]])
        ACT(tmp_t, psb[7][:, :], AF.Sqrt, [PSB[7], cst_b], [tmp_b], bias=eps_t, scale=1.0 / D)
        P.op("dve", "reciprocal", [tmp_b], [rstd_b], out=rstd_t, in_=tmp_t)

    def pre_norm(x_t, x_b, gcol, sq_t, sq_b, rstd_t, rstd_b, tmp_t, tmp_b, h_t, h_b):
        ACT(sq_t, x_t, AF.Square, [x_b], [sq_b])
        rms_stats(sq_t, sq_b, rstd_t, rstd_b, tmp_t, tmp_b)
        for kc in range(8):
            P.op("dve", "scalar_tensor_tensor", [x_b, rstd_b, gains_b], [h_b], out=h_t[:, kc, :], in0=x_t[:, kc, :],
                 scalar=gains_t[:, gcol + kc:gcol + kc + 1], in1=rstd_t, op0=ALU.mult, op1=ALU.mult)

    for l in range(L):
        g0 = l * 40
        P.barrier()
        ar.reset()
        xt = [ar.f32(4096).rearrange("p (k n) -> p k n", n=512) for _ in range(2)]
        xt_b = [P.buf("xt%d" % i, True) for i in range(2)]
        sq_t = ar.bf(4096).rearrange("p (k n) -> p k n", n=512); sq_b = P.buf("sq")
        hA = [ar.bf(4096).rearrange("p (k n) -> p k n", n=512) for _ in range(2)]
        hA_b = [P.buf("hA%d" % i) for i in range(2)]
        rstd_t = ar.f32(512); rstd_b = P.buf("rstd")
        tmp_t = ar.f32(512); tmp_b = P.buf("tmp")
        wv_t = ar.bf(8192).rearrange("p (k n) -> p k n", n=1024); wv_b = P.buf("wvA", True)
        wf_t = ar.bf(32).rearrange("p (k n) -> p k n", n=4); wf_b = P.buf("wfA", True)
        ring = Ring(6, 1024, "ring")
        cs_t = [(ar.f32(512), ar.f32(512)) for _ in range(2)]
        cs_b = [P.buf("csA%d" % i, True) for i in range(2)]
        qst = [ar.bf(4096).rearrange("p (k n) -> p k n", n=512) for _ in range(2)]
        qst_b = [P.buf("qst%d" % i, True) for i in range(2)]
        kst = [ar.bf(4096).rearrange("p (k n) -> p k n", n=512) for _ in range(2)]
        kst_b = [P.buf("kst%d" % i, True) for i in range(2)]
        vst = [ar.bf(4096).rearrange("p (a n) -> p a n", n=1024) for _ in range(2)]
        vst_b = [P.buf("vst%d" % i, True) for i in range(2)]
        r1 = [ar.f32(512) for _ in range(2)]; r1_b = [P.buf("r1_%d" % i) for i in range(2)]
        r2 = [ar.f32(512) for _ in range(2)]; r2_b = [P.buf("r2_%d" % i) for i in range(2)]
        fb_t = ar.f32(16); fb_b = P.buf("fbA")
        fe_t = ar.f32(16); fe_b = P.buf("feA")
        fl_t = ar.f32(16); fl_b = P.buf("flA")

        P.dma("sp", [(wv_t[:, kc, :], wslab("wV", l, 0)[:, kc * 1024:(kc + 1) * 1024]) for kc in range(8)],
              wv_b, R=[WB[l]], W=[wv_b])
        P.dma("sp", [(wf_t, wslab("wF", l, 0).rearrange("p (k n) -> p k n", n=4))], wf_b, R=[WB[l]], W=[wf_b])

        def loadxA(t):
            k = t % 2
            P.dma("sp", [(xt[k][:, 0:4, :], xtile_ap(xsrc(l), t)[:, 0:4, :]),
                         (xt[k][:, 4:8, :], xtile_ap(xsrc(l), t)[:, 4:8, :])], xt_b[k], R=[Xb], W=[xt_b[k]])
            P.dma("sp", [(cs_t[k][0], cosT[:, t * 512:(t + 1) * 512]),
                         (cs_t[k][1], sinT[:, t * 512:(t + 1) * 512])], cs_b[k], W=[cs_b[k]])

        loadxA(0)
        psrot = 0
        pre_norm(xt[0], xt_b[0], g0 + 0, sq_t, sq_b, rstd_t, rstd_b, tmp_t, tmp_b, hA[0], hA_b[0])
        for t in range(NT):
            if t + 1 < NT:
                loadxA(t + 1)
            h_t = hA[t % 2]; h_b = hA_b[t % 2]
            cos_t, sin_t = cs_t[t % 2]; c_b = cs_b[t % 2]
            for tb in range(4):
                mm_group(psb[6][:, tb * 4:tb * 4 + 4], [(h_t[:, kc, tb * 128:(tb + 1) * 128], wf_t[:, kc, :]) for kc in range(8)], [wf_b, h_b], [PSB[6]])
            TT("dve", fb_t, psb[6][:, 0:16], bfb_t[:, l * 16:l * 16 + 16], ALU.add, [PSB[6], bfb_b], [fb_b])
            ACT(fe_t, fb_t, AF.Exp, [fb_b], [fe_b], scale=-1.0)
            ACT(fl_t, fe_t, AF.Ln, [fe_b, cst_b], [fl_b], bias=one_t, scale=1.0)
            qs = qst[t % 2]; qs_b = qst_b[t % 2]; ks = kst[t % 2]; ks_b = kst_b[t % 2]
            si = 0
            for which in range(2):
                stg, stg_b = (qs, qs_b) if which == 0 else (ks, ks_b)
                for pt in range(8):
                    w3, w_b = ring.load(wslab("wA", l, si), 1024, [WB[l]]); si += 1
                    pa = psrot % 6; psrot += 1
                    mm_group(psb[pa][:, :], [(w3[:, kc, :], h_t[:, kc, :]) for kc in range(8)], [w_b, h_b], [PSB[pa]])
                    if pt < 2:
                        ACT(stg[:, pt, :], psb[pa][:, :], AF.Copy, [PSB[pa]], [stg_b])
                    else:
                        w23, w2_b = ring.load(wslab("wA", l, si), 1024, [WB[l]]); si += 1
                        pb = psrot % 6; psrot += 1
                        mm_group(psb[pb][:, :], [(w23[:, kc, :], h_t[:, kc, :]) for kc in range(8)], [w2_b, h_b], [PSB[pb]])
                        ri = (pt + which) % 2
                        TT("dve", r1[ri], psb[pa][:, :], cos_t, ALU.mult, [PSB[pa], c_b], [r1_b[ri]])
                        TT("dve", r2[ri], psb[pb][:, :], sin_t, ALU.mult, [PSB[pb], c_b], [r2_b[ri]])
                        TT("pool", stg[:, pt, :], r1[ri], r2[ri], ALU.add, [r1_b[ri], r2_b[ri]], [stg_b])
                        if which == 1 and pt >= 5:
                            P.op("dve", "tensor_reduce", [stg_b], [ksum_b], out=ksum_t[:, pt - 5, 2 * t:2 * t + 2],
                                 in_=stg[:, pt, :].rearrange("p (a b) -> p a b", b=256), axis=AX.X, op=ALU.add)
                if which == 0 and t + 1 < NT:
                    pre_norm(xt[(t + 1) % 2], xt_b[(t + 1) % 2], g0 + 0, sq_t, sq_b, rstd_t, rstd_b, tmp_t, tmp_b,
                             hA[(t + 1) % 2], hA_b[(t + 1) % 2])
            P.dma("pool", [(QT16[:, t * 512:(t + 1) * 512].rearrange("(k p) n -> p k n", p=128), qs)], qs_b, R=[qs_b], W=[QTb])
            P.dma("pool", [(KT16[:, t * 512:(t + 1) * 512].rearrange("(k p) n -> p k n", p=128), ks)], ks_b, R=[ks_b], W=[KTb])
            for tb in range(4):
                mm_group(psb[6][:, 32 + tb * 4:36 + tb * 4], [(trif_t, fl_t[:, tb * 4:tb * 4 + 4])], [trif_b, fl_b], [PSB[6]])
                mm_group(psb[6][:, 64 + tb * 4:68 + tb * 4], [(onesf_t, fl_t[:, tb * 4:tb * 4 + 4])], [onesf_b, fl_b], [PSB[6]])
            for tb in range(4):
                j = 4 * t + tb
                TT("dve", cpos_t[:, j, :], psb[6][:, 32 + tb * 4:36 + tb * 4], tall_t[:, j, :], ALU.add, [PSB[6], tall_b], [cpos_b])
                TT("dve", tall_t[:, j + 1, :], psb[6][:, 64 + tb * 4:68 + tb * 4], tall_t[:, j, :], ALU.add, [PSB[6], tall_b], [tall_b])
            vs = vst[t % 2]; vs_b = vst_b[t % 2]
            for tb in range(4):
                for hf in range(2):
                    pa = psrot % 6; psrot += 1
                    mm_group(psb[pa][:, :], [(h_t[:, kc, tb * 128:(tb + 1) * 128], wv_t[:, kc, hf * 512:(hf + 1) * 512]) for kc in range(8)],
                             [wv_b, h_b], [PSB[pa]])
                    ACT(vs[:, tb, hf * 512:(hf + 1) * 512], psb[pa][:, :], AF.Copy, [PSB[pa]], [vs_b])
            P.dma("pool", [(V16[t * 512:(t + 1) * 512, :].rearrange("(a p) c -> p a c", p=128), vs)], vs_b, R=[vs_b], W=[Vb])
        P.dma("pool", [(KS32.rearrange("(a p) n -> p a n", p=128), ksum_t)], ksum_b, R=[ksum_b], W=[KSb])

        P.barrier()
        ar.reset()
        KP = [ar.bf(S) for _ in range(2)]; KP_b = [P.buf("KP%d" % i, True) for i in range(2)]
        QP = [ar.bf(S) for _ in range(2)]; QP_b = [P.buf("QP%d" % i, True) for i in range(2)]
        QA_b = [[P.buf("QA%d_%d" % (i, t)) for t in range(NT)] for i in range(2)]
        VA = [ar.bf(NB * 128).rearrange("p (j c) -> p j c", c=128) for _ in range(2)]
        VA_b = [P.buf("VA%d" % i, True) for i in range(2)]
        pt_t = [ar.bf(512) for _ in range(6)]; pt_b = [P.buf("pT%d" % i) for i in range(6)]
        rec_t = ar.f32(512); rec_b = P.buf("rec")
        rec0_t = ar.f32(512); rec0_b = P.buf("rec0")
        yst = [ar.bf(512) for _ in range(2)]; yst_b = [P.buf("yst%d" % i, True) for i in range(2)]
        ks16_t = ar.bf(32); ks16_b = P.buf("ks16")
        ks32_t = ar.f32(32); ks32_b = P.buf("ks32", True)
        wk_t = ar.f32(128).rearrange("p (a n) -> p a n", n=32); wk_b = P.buf("wk")
        t8_t = ar.f32(32).rearrange("p (a n) -> p a n", n=8); t8_b = P.buf("t8")
        sb_t = ar.f32(128).rearrange("p (a n) -> p a n", n=32); sb_b = P.buf("selb")
        acc_t = ar.f32(S); acc_b = P.buf("acc")
        for i in range(2):
            P.op("pool", "memset", (), [VA_b[i]], args=(VA[i][:, :, 64:128], 1.0))
        cnt = {"ps": 0, "po": 0, "pt": 0, "y": 0, "kq": 0, "va": 0}

        def load_rows(dst, dst_b, dram, r0, nr, rb, ind=False):
            pairs = [(dst[0:nr, c0:c0 + CW], dram[r0:r0 + nr, c0:c0 + CW]) for c0 in range(0, S, CW)]
            Rb = [rb]
            if ind == 1:
                pairs += [(dst[64:96, c0:c0 + CW], BI16[0:32, c0:c0 + CW]) for c0 in range(0, S, CW)]
                Rb = [rb, BIb]
            if ind == 2:
                pairs += [(dst[64:65, c0:c0 + CW], BI16[32:33, c0:c0 + CW]) for c0 in range(0, S, CW)]
                Rb = [rb, BIb]
            P.dma("sp", pairs, dst_b, R=Rb, W=[dst_b])

        def load_va(k, head, d):
            nbd = NB // d
            pairs = []
            for r in range(d):
                for b0 in range(0, nbd, 8):
                    nb_ = min(8, nbd - b0)
                    src = V16[r + d * 128 * b0: r + d * 128 * b0 + d * (128 * nb_ - 1) + 1: d, head * 64:(head + 1) * 64]
                    pairs.append((VA[k][:, r * nbd + b0: r * nbd + b0 + nb_, 0:64], src.rearrange("(j p) c -> p j c", p=128)))
            for g_ in range(0, len(pairs), 4):
                P.dma("sp", pairs[g_:g_ + 4], VA_b[k], R=[Vb], W=[VA_b[k]])

        def finalize(po, yrow, i):
            yk = cnt["y"] % 2; cnt["y"] += 1
            P.op("dve", "reciprocal", [PSB[po]], [rec_b], out=rec_t[64:128, :], in_=psb[po][64:128, :])
            TT("dve", yst[yk][0:64, :], psb[po][0:64, :], rec_t[64:128, :], ALU.mult, [PSB[po], rec_b], [yst_b[yk]])
            P.dma("pool", [(YT16[yrow:yrow + 64, i * 512:(i + 1) * 512], yst[yk][0:64, :])], yst_b[yk], R=[yst_b[yk]], W=[YTb])

        LA = 3

        def run_pipe(items):
            n = len(items)
            for idx in range(n + LA):
                if idx < n:
                    items[idx][0]()
                if idx >= LA:
                    items[idx - LA][1]()
                    items[idx - LA][2]()

        def attn_items(Kt, K_b, Qt, Qbufs, r0, nr, va, va_b, bias_fn, i, yrow, hooks):
            items = []
            po = 4 + cnt["po"] % 2; cnt["po"] += 1
            nj = 4 * i + 4
            for j in range(nj):
                off = max(0, j - 4 * i) * 128
                ps = cnt["ps"] % 4; cnt["ps"] += 1
                pk = cnt["pt"] % 6; cnt["pt"] += 1

                def f_score(j=j, off=off, ps=ps):
                    if j in hooks:
                        hooks[j]()
                    mm_group(psb[ps][:, off:512], [(Kt[r0:r0 + nr, j * 128:(j + 1) * 128], Qt[r0:r0 + nr, i * 512 + off:(i + 1) * 512])],
                             [K_b] + Qbufs, [PSB[ps]])

                def f_soft(j=j, off=off, ps=ps, pk=pk):
                    if bias_fn is None:
                        ACT(pt_t[pk][:, off:512], psb[ps][:, off:512], AF.Exp, [PSB[ps]], [pt_b[pk]], scale=0.125)
                    else:
                        ACT(pt_t[pk][:, off:512], psb[ps][:, off:512], AF.Exp, [PSB[ps], cpos_b], [pt_b[pk]], bias=bias_fn(j, i), scale=0.125)
                    if j >= 4 * i:
                        TT("pool", pt_t[pk][:, off:off + 128], pt_t[pk][:, off:off + 128], cmask_t[:, 128:256], ALU.mult,
                           [pt_b[pk], cmask_b], [pt_b[pk]])

                def f_pv(j=j, off=off, pk=pk):
                    MM(psb[po][:, off:512], va[:, j, :], pt_t[pk][:, off:512], j == 0, j == nj - 1, [va_b, pt_b[pk]], [PSB[po]], inc=True)
                    if j == nj - 1:
                        finalize(po, yrow, i)
                items.append((f_score, f_soft, f_pv))
            return items

        for h in range(4):
            kq = cnt["kq"] % 2; cnt["kq"] += 1
            load_rows(KP[kq], KP_b[kq], KT16, h * 64, 64, KTb, ind=2)
            load_rows(QP[kq], QP_b[kq], QT16, h * 64, 64, QTb)
            vk = cnt["va"] % 2; cnt["va"] += 1
            load_va(vk, h, 1)
            items = []

            def mkpre(i, h=h, kq=kq):
                def pre():
                    for qb in range(4):
                        P.op("pe", "transpose", [cpos_b, identf_b], [PSB[7]], qb == 3, out=psb[7][0:1, qb * 128:(qb + 1) * 128],
                             in_=cpos_t[:, 4 * i + qb, h:h + 1], identity=identf_t)
                    P.op("dve", "tensor_scalar", [PSB[7]], [QA_b[kq][i]], out=QP[kq][64:65, i * 512:(i + 1) * 512], in0=psb[7][0:1, :],
                         scalar1=-8.0, scalar2=None, op0=ALU.mult)
                return pre
            mkpre(0)()
            for i in range(NT):
                hooks = {0: mkpre(i + 1)} if i + 1 < NT else {}
                items += attn_items(KP[kq], KP_b[kq], QP[kq], [QP_b[kq], QA_b[kq][i]], 0, 65, VA[vk], VA_b[vk],
                                    (lambda h: lambda j, i: cpos_t[:, j, h:h + 1])(h), i, h * 64, hooks)
            run_pipe(items)
        for m in range(6):
            hd = 10 + m
            kq = cnt["kq"] % 2; cnt["kq"] += 1
            load_rows(KP[kq], KP_b[kq], KT16, hd * 64, 64, KTb, ind=1)
            load_rows(QP[kq], QP_b[kq], QT16, hd * 64, 64, QTb)
            P.dma("sp", [(ks32_t[0:64, :], KS32[m * 64:(m + 1) * 64, :])], ks32_b, R=[KSb], W=[ks32_b])
            P.op("dve", "tensor_copy", [ks32_b], [ks16_b], out=ks16_t[0:64, :], in_=ks32_t[0:64, :])
            vk = cnt["va"] % 2; cnt["va"] += 1
            load_va(vk, hd, 1)
            Kt = KP[kq]; Qt = QP[kq]
            items = []

            def mkpre1(i, kq=kq, Qt=Qt):
                def pre():
                    for qb in range(4):
                        q0 = i * 512 + qb * 128
                        mm_group(psb[6][:, qb * 32:qb * 32 + NMB], [(Qt[0:64, q0:q0 + 128], ks16_t[0:64, 0:NMB])], [QP_b[kq], ks16_b], [PSB[6]])
                    P.op("dve", "memset", (), [wk_b], args=(wk_t, -1e30))
                    P.op("dve", "memset", (), [sb_b], args=(sb_t, -1.0))
                    for hq in range(2):
                        own = 2 * i + hq
                        if own > 3:
                            P.op("dve", "tensor_copy", [PSB[6]], [wk_b], out=wk_t[:, 2 * hq:2 * hq + 2, 0:own],
                                 in_=psb[6][:, 64 * hq:64 * hq + 64].rearrange("p (a n) -> p a n", n=32)[:, :, 0:own])
                        for qq in range(2):
                            qb = 2 * hq + qq
                            if own > 3:
                                P.op("dve", "max", [wk_b], [t8_b], out=t8_t[:, qb, :], in_=wk_t[:, qb, 0:max(own, 8)])
                                P.op("dve", "tensor_scalar", [wk_b, t8_b], [sb_b], out=sb_t[:, qb, 0:own], in0=wk_t[:, qb, 0:own],
                                     scalar1=t8_t[:, qb, 2:3], scalar2=1.0, op0=ALU.is_ge, op1=ALU.subtract)
                                P.op("dve", "memset", (), [sb_b], args=(sb_t[:, qb, own:own + 1], 0.0))
                            else:
                                P.op("dve", "memset", (), [sb_b], args=(sb_t[:, qb, 0:own + 1], 0.0))
                    P.op("dve", "tensor_scalar", [sb_b], [sb_b], out=sb_t, in0=sb_t, scalar1=BIG, scalar2=None, op0=ALU.mult)
                return pre

            def mkpre2(i, kq=kq, Qt=Qt):
                def pre():
                    for qb in range(4):
                        P.op("pe", "transpose", [sb_b, identf_b], [PSB[7]], qb == 3, out=psb[7][0:32, qb * 128:(qb + 1) * 128],
                             in_=sb_t[:, qb, :], identity=identf_t)
                    P.op("dve", "tensor_copy", [PSB[7]], [QA_b[kq][i]], out=Qt[64:96, i * 512:(i + 1) * 512], in_=psb[7][0:32, :])
                return pre
            mkpre1(0)(); mkpre2(0)()
            for i in range(NT):
                hooks = {}
                if i + 1 < NT:
                    hooks[0] = mkpre1(i + 1)
                    hooks[4 * i + 3] = mkpre2(i + 1)
                items += attn_items(Kt, KP_b[kq], Qt, [QP_b[kq], QA_b[kq][i]], 0, 96, VA[vk], VA_b[vk], None, i, 384 + m * 64, hooks)
            run_pipe(items)
        for oh in range(2):
            for g in range(3):
                d = DIL[g]
                ptile = 2 + g
                hd = 4 + 2 * g + oh
                kq = cnt["kq"] % 2; cnt["kq"] += 1
                load_rows(KP[kq], KP_b[kq], KT16, ptile * 128, 128, KTb)
                load_rows(QP[kq], QP_b[kq], QT16, ptile * 128, 128, QTb)
                vk = cnt["va"] % 2; cnt["va"] += 1
                load_va(vk, hd, d)
                Kt = KP[kq]; Qt = QP[kq]; r0 = oh * 64
                KQ = [KP_b[kq], QP_b[kq]]
                nbd = NB // d
                items = []
                for r in range(d):
                    for ub4 in range(0, nbd, 4):
                        po = 4 + cnt["po"] % 2; cnt["po"] += 1
                        nu = min(4, nbd - ub4)
                        for u in range(nu):
                            ub = ub4 + u
                            ps = cnt["ps"] % 4; cnt["ps"] += 1
                            pk = cnt["pt"] % 6; cnt["pt"] += 1
                            qa = r + d * 128 * ub
                            lo = 0 if ub > 0 else 128
                            bi = r * nbd + ub

                            def f_score(ub=ub, ps=ps, qa=qa, d=d, r0=r0, Kt=Kt, Qt=Qt, KQ=KQ):
                                qap = Qt[r0:r0 + 64, qa: qa + d * 127 + 1: d]
                                if ub > 0:
                                    ka = qa - d * 128
                                    mm_group(psb[ps][:, 0:128], [(Kt[r0:r0 + 64, ka: ka + d * 127 + 1: d], qap)], KQ, [PSB[ps]])
                                mm_group(psb[ps][:, 128:256], [(Kt[r0:r0 + 64, qa: qa + d * 127 + 1: d], qap)], KQ, [PSB[ps]])

                            def f_soft(ps=ps, pk=pk, lo=lo):
                                ACT(pt_t[pk][:, lo:256], psb[ps][:, lo:256], AF.Exp, [PSB[ps]], [pt_b[pk]], scale=0.125)
                                TT("pool", pt_t[pk][:, lo:256], pt_t[pk][:, lo:256], cmask_t[:, lo:256], ALU.mult, [pt_b[pk], cmask_b], [pt_b[pk]])

                            def f_pv(ub=ub, u=u, nu=nu, po=po, pk=pk, bi=bi, vk=vk, g=g, r=r, d=d, ub4=ub4):
                                if ub > 0:
                                    MM(psb[po][:, u * 128:(u + 1) * 128], VA[vk][:, bi - 1, :], pt_t[pk][:, 0:128], True, False,
                                       [VA_b[vk], pt_b[pk]], [PSB[po]], inc=False)
                                MM(psb[po][:, u * 128:(u + 1) * 128], VA[vk][:, bi, :], pt_t[pk][:, 128:256], ub == 0, True,
                                   [VA_b[vk], pt_b[pk]], [PSB[po]], inc=True)
                                if u == nu - 1:
                                    a0 = r + d * 128 * ub4
                                    accv = acc_t[:, a0: a0 + d * (128 * nu - 1) + 1: d]
                                    if g == 0:
                                        P.op("dve", "tensor_copy", [PSB[po]], [acc_b], out=accv, in_=psb[po][:, 0:128 * nu])
                                    else:
                                        TT("dve", accv, psb[po][:, 0:128 * nu], accv, ALU.add, [PSB[po], acc_b], [acc_b])
                            items.append((f_score, f_soft, f_pv))
                run_pipe(items)
            for i in range(NT):
                yk = cnt["y"] % 2; cnt["y"] += 1
                P.op("dve", "reciprocal", [acc_b], [rec_b], out=rec_t[64:128, :], in_=acc_t[64:128, i * 512:(i + 1) * 512])
                P.op("dve", "tensor_copy", [rec_b], [rec0_b], out=rec0_t[0:64, :], in_=rec_t[64:128, :])
                TT("dve", yst[yk][0:64, :], acc_t[0:64, i * 512:(i + 1) * 512], rec0_t[0:64, :], ALU.mult, [acc_b, rec0_b], [yst_b[yk]])
                P.dma("pool", [(YT16[256 + oh * 64:256 + oh * 64 + 64, i * 512:(i + 1) * 512], yst[yk][0:64, :])], yst_b[yk], R=[yst_b[yk]], W=[YTb])

        P.barrier()
        ar.reset()
        xt = [ar.f32(4096).rearrange("p (k n) -> p k n", n=512) for _ in range(4)]
        xt_b = [P.buf("xt%d" % i, True) for i in range(4)]
        yt = [ar.bf(3072).rearrange("p (k n) -> p k n", n=512) for _ in range(2)]
        yt_b = [P.buf("ytC%d" % i, True) for i in range(2)]
        pp = [ar.f32(1024).rearrange("p (k n) -> p k n", n=512) for _ in range(2)]
        pp_b = [P.buf("ppC%d" % i, True) for i in range(2)]
        p16 = [ar.bf(1024).rearrange("p (k n) -> p k n", n=512) for _ in range(2)]
        p16_b = [P.buf("p16_%d" % i) for i in range(2)]
        hh = [ar.bf(4096).rearrange("p (k n) -> p k n", n=512) for _ in range(2)]
        hh_b = [P.buf("hC%d" % i) for i in range(2)]
        sqo_t = ar.bf(4096).rearrange("p (k n) -> p k n", n=512); sqo_b = P.buf("sqo")
        sqx_t = ar.bf(4096).rearrange("p (k n) -> p k n", n=512); sqx_b = P.buf("sqx")
        mg_t = ar.bf(4096).rearrange("p (k n) -> p k n", n=512); mg_b = P.buf("mgC")
        o_t = ar.f32(4096).rearrange("p (k n) -> p k n", n=512); o_b = P.buf("oC")
        ff_t = ar.bf(NFF * 512).rearrange("p (k n) -> p k n", n=512); ff_b = P.buf("ffC")
        rstd_t = ar.f32(512); rstd_b = P.buf("rstd")
        tmp_t = ar.f32(512); tmp_b = P.buf("tmp")
        gs = [ar.f32(512) for _ in range(3)]; gs_b = [P.buf("gs%d" % i) for i in range(3)]
        ma = [ar.f32(512) for _ in range(3)]; ma_b = [P.buf("ma%d" % i) for i in range(3)]
        tt = [ar.f32(512) for _ in range(2)]; tt_b = [P.buf("tt%d" % i) for i in range(2)]
        ring = Ring(6, 1024, "ringC")
        prc = {"i": 0}

        def nps():
            k = prc["i"] % 7; prc["i"] += 1
            return k

        def loadXY(t):
            k = t % 2; xk = t % 4
            P.dma("sp", [(xt[xk][:, 0:4, :], xtile_ap(xsrc(l), t)[:, 0:4, :]),
                         (xt[xk][:, 4:8, :], xtile_ap(xsrc(l), t)[:, 4:8, :])], xt_b[xk], R=[Xb], W=[xt_b[xk]])
            P.dma("sp", [(yt[k], YT16[:, t * 512:(t + 1) * 512].rearrange("(k p) n -> p k n", p=128))], yt_b[k], R=[YTb], W=[yt_b[k]])

        def loadP(t):
            k = t % 2
            P.dma("sp", [(pp[k], pT[l * PLE:(l + 1) * PLE, t * 512:(t + 1) * 512].rearrange("(k p) n -> p k n", p=128))], pp_b[k], W=[pp_b[k]])

        def stats(sq_t, sq_b):
            rms_stats(sq_t, sq_b, rstd_t, rstd_b, tmp_t, tmp_b)

        XK = {0: 0, 1: 1}

        def mk_h(k, gcol):
            for kc in range(8):
                P.op("dve", "scalar_tensor_tensor", [xt_b[XK[k]], rstd_b, gains_b], [hh_b[k]], out=hh[k][:, kc, :], in0=xt[XK[k]][:, kc, :],
                     scalar=gains_t[:, gcol + kc:gcol + kc + 1], in1=rstd_t, op0=ALU.mult, op1=ALU.mult)

        def pre1(k):
            for kc in range(8):
                ACT(sqx_t[:, kc, :], xt[XK[k]][:, kc, :], AF.Square, [xt_b[XK[k]]], [sqx_b])
            stats(sqx_t, sqx_b)
            mk_h(k, g0 + 0)

        def residual_steps(k, gcol, want_sq):
            xk = XK[k]
            steps = [lambda: stats(sqo_t, sqo_b)]

            def mk(m):
                def step():
                    q = m % 2
                    eng = "pool" if m % 2 == 0 else "dve"
                    P.op("dve", "scalar_tensor_tensor", [o_b, rstd_b, gains_b], [tt_b[q]], out=tt[q], in0=o_t[:, m, :],
                         scalar=gains_t[:, gcol + m:gcol + m + 1], in1=rstd_t, op0=ALU.mult, op1=ALU.mult)
                    TT(eng, xt[xk][:, m, :], xt[xk][:, m, :], tt[q], ALU.add, [xt_b[xk], tt_b[q]], [xt_b[xk]])
                    if want_sq:
                        TT(eng, sqx_t[:, m, :], xt[xk][:, m, :], xt[xk][:, m, :], ALU.mult, [xt_b[xk]], [sqx_b])
                return step
            return steps + [mk(m) for m in range(8)]

        def run_bg(bg, n=1):
            for _ in range(n):
                if bg:
                    bg.pop(0)()

        def flush_bg(bg):
            while bg:
                bg.pop(0)()

        def evac_o(ps, m):
            ACT(sqo_t[:, m, :], psb[ps][:, :], AF.Square, [PSB[ps]], [sqo_b])
            ACT(o_t[:, m, :], psb[ps][:, :], AF.Copy, [PSB[ps]], [o_b])

        def S1(k, inject, bg):
            ychunks = [(0, 2), (2, 1), (3, 3)]
            for m in range(8):
                if m == 3:
                    flush_bg(bg)
                    if inject is not None:
                        inject()
                wb3, wb_b = ring.load(wslab("wBR", l, m), 768, [WB[l]])
                for b in range(3):
                    run_bg(bg)
                    wg3, wg_b = ring.load(wslab("wG", l, b * 8 + m), 1024, [WB[l]])
                    pg = nps()
                    mm_group(psb[pg][:, :], [(wg3[:, kc, :], hh[k][:, kc, :]) for kc in range(8)], [wg_b, hh_b[k]], [PSB[pg]])
                    ACT(gs[b], psb[pg][:, :], AF.Sigmoid, [PSB[pg]], [gs_b[b]])
                    c0, ncc = ychunks[b]
                    pq = nps()
                    mm_group(psb[pq][:, :], [(wb3[:, c0 + c, :], yt[k][:, c0 + c, :]) for c in range(ncc)], [wb_b, yt_b[k]], [PSB[pq]])
                    TT("dve", ma[b], psb[pq][:, :], gs[b], ALU.mult, [PSB[pq], gs_b[b]], [ma_b[b]])
                TT("pool", ma[0], ma[0], ma[1], ALU.add, [ma_b[0], ma_b[1]], [ma_b[0]])
                TT("pool", mg_t[:, m, :], ma[0], ma[2], ALU.add, [ma_b[0], ma_b[2]], [mg_b])
            for m in range(8):
                w3, w_b = ring.load(wslab("wO", l, m), 1024, [WB[l]])
                ps = nps()
                mm_group(psb[ps][:, :], [(w3[:, kc, :], mg_t[:, kc, :]) for kc in range(8)], [w_b, mg_b], [PSB[ps]])
                evac_o(ps, m)

        def S2(k, inject, bg):
            for j in range(NFF):
                run_bg(bg)
                if j == 8:
                    flush_bg(bg)
                    if inject is not None:
                        inject()
                wg3, wg_b = ring.load(wslab("wFG", l, j), 1024, [WB[l]])
                pg = nps()
                mm_group(psb[pg][:, :], [(wg3[:, kc, :], hh[k][:, kc, :]) for kc in range(8)], [wg_b, hh_b[k]], [PSB[pg]])
                kk = j % 3
                ACT(gs[kk], psb[pg][:, :], AF.Silu, [PSB[pg]], [gs_b[kk]])
                wu3, wu_b = ring.load(wslab("wFU", l, j), 1024, [WB[l]])
                pu = nps()
                mm_group(psb[pu][:, :], [(wu3[:, kc, :], hh[k][:, kc, :]) for kc in range(8)], [wu_b, hh_b[k]], [PSB[pu]])
                TT("dve", ff_t[:, j, :], psb[pu][:, :], gs[kk], ALU.mult, [PSB[pu], gs_b[kk]], [ff_b])
            for m in range(8):
                ps = nps()
                pieces = [(0, 8), (8, 16), (16, NFF)]
                first = True
                for (j0, j1) in pieces:
                    w3, w_b = ring.load(wslab("wFD", l, m)[:, j0 * 128:j1 * 128], (j1 - j0) * 128, [WB[l]])
                    for j in range(j0, j1):
                        MM(psb[ps][:, :], w3[:, j - j0, :], ff_t[:, j, :], first, j == NFF - 1, [w_b, ff_b], [PSB[ps]], inc=(j == j1 - 1))
                        first = False
                evac_o(ps, m)

        def d2(k):
            ACT(hh[k], xt[XK[k]], AF.Copy, [xt_b[XK[k]]], [hh_b[k]])
            P.op("dve", "tensor_copy", [pp_b[k]], [p16_b[k]], out=p16[k], in_=pp[k])

        def S3(k, inject, bg):
            for m in range(8):
                run_bg(bg, 3)
                if m == 3:
                    flush_bg(bg)
                    if inject is not None:
                        inject()
                wg3, wg_b = ring.load(wslab("wPG", l, m), 1024, [WB[l]])
                pg = nps()
                mm_group(psb[pg][:, :], [(wg3[:, kc, :], hh[k][:, kc, :]) for kc in range(8)], [wg_b, hh_b[k]], [PSB[pg]])
                kk = m % 3
                ACT(gs[kk], psb[pg][:, :], AF.Sigmoid, [PSB[pg]], [gs_b[kk]])
                wp3, wp_b = ring.load(wslab("wPL", l, m), 256, [WB[l]])
                pu = nps()
                mm_group(psb[pu][:, :], [(wp3[:, kc, :], p16[k][:, kc, :]) for kc in range(2)], [wp_b, p16_b[k]], [PSB[pu]])
                TT("dve", o_t[:, m, :], psb[pu][:, :], gs[kk], ALU.mult, [PSB[pu], gs_b[kk]], [o_b])
                ACT(sqo_t[:, m, :], o_t[:, m, :], AF.Square, [o_b], [sqo_b])

        def storeC(t):
            xk = t % 4
            P.dma("pool", [(xtile_ap(xdst(l), t)[:, 0:4, :], xt[xk][:, 0:4, :]), (xtile_ap(xdst(l), t)[:, 4:8, :], xt[xk][:, 4:8, :])],
                  xt_b[xk], R=[xt_b[xk]], W=[Xb])

        def c2(k):
            stats(sqx_t, sqx_b)
            mk_h(k, g0 + 16)

        loadXY(0); loadP(0); loadXY(1); loadP(1)
        XK[0] = 0
        pre1(0)
        bgq = []
        for tp in range(0, NT, 2):
            tA, tB = tp, tp + 1
            nxt = tp + 2 < NT
            XK[0] = tA % 4; XK[1] = tB % 4

            def inj_pre1B():
                pre1(1)
            S1(0, inj_pre1B, bgq)
            if nxt:
                loadXY(tA + 2)
            bgq = residual_steps(0, g0 + 8, True)
            S1(1, lambda: c2(0), bgq)
            if nxt:
                loadXY(tB + 2)
            bgq = residual_steps(1, g0 + 8, True)
            S2(0, lambda: c2(1), bgq)
            bgq = residual_steps(0, g0 + 24, False)
            S2(1, lambda: d2(0), bgq)
            if nxt:
                loadP(tA + 2)
            bgq = residual_steps(1, g0 + 24, False)
            S3(0, lambda: d2(1), bgq)
            if nxt:
                loadP(tB + 2)
            bgq = residual_steps(0, g0 + 32, False) + [(lambda t=tA: storeC(t))]

            def inj_next(tA=tA):
                XK[0] = (tA + 2) % 4
                pre1(0)
                XK[0] = tA % 4
            S3(1, inj_next if nxt else None, bgq)
            bgq = residual_steps(1, g0 + 32, False) + [(lambda t=tB: storeC(t))]
        flush_bg(bgq)
    P.barrier()
    block = es.enter_context(nc.Block())
    P.replay(block)
    es.close()
    return nc


def _slabs(w, cols_list, kc):
    out = np.empty((len(cols_list), 128, kc, 128), np.float32)
    wk = w.reshape(kc, 128, w.shape[1])
    for m, cols in enumerate(cols_list):
        out[m] = wk[:, :, cols].transpose(1, 0, 2)
    return out.reshape(len(cols_list) * 128, kc * 128)


def _host_weights(inp, L):
    r = {}
    ar_ = np.arange
    lists = {n: [] for n in ("wA", "wV", "wF", "wG", "wBR", "wO", "wFG", "wFU", "wFD", "wPL", "wPG")}
    for l in range(L):
        w_in = np.asarray(inp["w_in"][l], np.float32)
        colsA = []
        for base in (0, 1024):
            for pt in range(8):
                c = base + pt * 128 + ar_(128)
                colsA.append(c)
                if pt >= 2:
                    sw = base + pt * 128 + (ar_(128) // 64) * 64 + (ar_(128) % 64 + 32) % 64
                    colsA.append(sw)
        lists["wA"].append(_slabs(w_in, colsA, 8))
        wv = w_in[:, 2048:3072].reshape(8, 128, 1024).transpose(1, 0, 2).reshape(128, 8192)
        lists["wV"].append(wv)
        wf = w_in[:, 3072:3076].reshape(8, 128, 4).transpose(1, 0, 2).reshape(128, 32)
        lists["wF"].append(wf)
        lists["wG"].append(_slabs(w_in, [3076 + b * 1024 + m * 128 + ar_(128) for b in range(3) for m in range(8)], 8))
        wbr = np.concatenate([np.asarray(inp["w_br_a"][l]), np.asarray(inp["w_br_b"][l]), np.asarray(inp["w_br_c"][l])], axis=0)
        lists["wBR"].append(_slabs(wbr.astype(np.float32), [m * 128 + ar_(128) for m in range(8)], 6))
        lists["wO"].append(_slabs(np.asarray(inp["w_out"][l], np.float32), [m * 128 + ar_(128) for m in range(8)], 8))
        lists["wFG"].append(_slabs(np.asarray(inp["w_ffn_gate"][l], np.float32), [m * 128 + ar_(128) for m in range(NFF)], 8))
        lists["wFU"].append(_slabs(np.asarray(inp["w_ffn_up"][l], np.float32), [m * 128 + ar_(128) for m in range(NFF)], 8))
        lists["wFD"].append(_slabs(np.asarray(inp["w_ffn_down"][l], np.float32), [m * 128 + ar_(128) for m in range(8)], NFF))
        lists["wPL"].append(_slabs(np.asarray(inp["w_ple"][l], np.float32), [m * 128 + ar_(128) for m in range(8)], 2))
        lists["wPG"].append(_slabs(np.asarray(inp["w_ple_gate"][l], np.float32), [m * 128 + ar_(128) for m in range(8)], 8))
    for n, v in lists.items():
        r[n] = np.ascontiguousarray(np.concatenate(v, axis=0), dtype=np.float32)
    gl = []
    for l in range(L):
        for n in ("g_mix_pre", "g_mix_post", "g_ffn_pre", "g_ffn_post", "g_ple_post"):
            gl.append(np.asarray(inp[n][l], np.float32).reshape(8, 128).T)
    r["gains"] = np.ascontiguousarray(np.concatenate(gl, axis=1), dtype=np.float32)
    r["bfb"] = np.ascontiguousarray(np.broadcast_to(np.tile(np.asarray(inp["b_f"], np.float32).reshape(L, 1, 4), (1, 4, 1)).reshape(1, L * 16), (128, L * 16)))
    return r


def _consts(S):
    c = {}
    inv = (1.0 / (np.float32(10000.0) ** (np.arange(0, 64, 2, dtype=np.float32) / np.float32(64)))).astype(np.float32)
    ang = (np.arange(S, dtype=np.float32)[:, None] * inv[None, :]).astype(np.float32)
    cos = np.cos(ang.astype(np.float64)).astype(np.float32).T
    sin = np.sin(ang.astype(np.float64)).astype(np.float32).T
    c["cosT"] = np.ascontiguousarray(np.concatenate([cos, cos, cos, cos], axis=0))
    c["sinT"] = np.ascontiguousarray(np.concatenate([-sin, sin, -sin, sin], axis=0))
    p = np.arange(128)
    c["cmask"] = np.ascontiguousarray(np.concatenate([(p[:, None] >= p[None, :]), (p[:, None] <= p[None, :])], axis=1).astype(np.float32))
    c["trif"] = np.ascontiguousarray((p[:, None] <= p[None, :]).astype(np.float32))
    c["identf"] = np.eye(128, dtype=np.float32)
    c["blkind"] = np.ascontiguousarray(np.concatenate([(np.arange(S)[None, :] // 256 == np.arange(32)[:, None]), np.ones((1, S), bool)], axis=0).astype(np.float32))
    return c


_NC_CACHE = {}


def kernel(**inputs):
    x = np.asarray(inputs["x"], np.float32)
    p = np.asarray(inputs["p"], np.float32)
    B, S, _ = x.shape
    L = p.shape[0]
    key = (S, L)
    if key not in _NC_CACHE:
        _NC_CACHE[key] = build(S, L)
    nc = _NC_CACHE[key]
    shared = _host_weights(inputs, L)
    shared.update(_consts(S))
    in_maps = []
    for b in range(B):
        m = dict(shared)
        m["xT"] = np.ascontiguousarray(x[b].T)
        m["pT"] = np.ascontiguousarray(p[:, b].transpose(0, 2, 1).reshape(L * PLE, S))
        in_maps.append(m)
    res = run_bass_kernel_spmd(nc, in_maps, core_ids=list(range(B)))
    out = np.stack([np.ascontiguousarray(res.results[b]["outT"].T) for b in range(B)], axis=0)
    return out.astype(np.float32)
```

```python
import numpy as np
from contextlib import ExitStack
import concourse.bass as bass
import concourse.mybir as mybir
from concourse.bass_utils import run_bass_kernel_spmd

F32 = mybir.dt.float32
BF = mybir.dt.bfloat16
AF = mybir.ActivationFunctionType
ALU = mybir.AluOpType
AX = mybir.AxisListType

D = 1024
HD = 64
PLE = 256
DFF = 2816
NFF = 22
BIG = 30000.0
DIL = (1, 4, 16)
SAME_ENGINE_SYNC = True


class Buf:
    def __init__(self, name, sem=None):
        self.name = name
        self.w = {}
        self.r = {}
        self.sem = sem
        self.cnt = 0


class Eng:
    def __init__(self, name, sem):
        self.name = name
        self.sem = sem
        self.cnt = 0
        self.seen = {}
        self.prog = []


class Prog:
    def __init__(self, nc, es):
        self.nc = nc
        self.es = es
        self.sems = []
        self.E = {}
        for n in ("pe", "act", "dve", "pool"):
            self.E[n] = Eng(n, self.newsem(n))
        self.E["sp"] = Eng("sp", None)
        self.bufs = []
        self.bynames = {}

    def newsem(self, name):
        h = self.es.enter_context(self.nc.semaphore("s_" + name))
        self.sems.append(h)
        return len(self.sems) - 1

    def buf(self, name, dma=False):
        if name in self.bynames:
            return self.bynames[name]
        b = Buf(name, self.newsem(name) if dma else None)
        self.bufs.append(b)
        self.bynames[name] = b
        return b

    def _waits(self, X, R, W):
        need = {}
        for b in R:
            for k, v in b.w.items():
                need[k] = max(need.get(k, 0), v)
        for b in W:
            for k, v in b.w.items():
                need[k] = max(need.get(k, 0), v)
            for k, v in b.r.items():
                need[k] = max(need.get(k, 0), v)
        out = []
        for k, v in need.items():
            if k == X.sem and (X.name == "pe" or not SAME_ENGINE_SYNC):
                continue
            if X.seen.get(k, 0) >= v:
                continue
            X.seen[k] = v
            out.append((k, v))
        return out

    def op(self, eng, name, R=(), W=(), inc=True, args=(), **kw):
        X = self.E[eng]
        waits = self._waits(X, R, W)
        tok = X.cnt + 1
        if inc:
            X.cnt = tok
        X.prog.append((waits, ("op", name, args, kw), inc))
        for b in R:
            b.r[X.sem] = tok
        for b in W:
            b.w = {X.sem: tok}
            b.r = {}

    def dma(self, q, pairs, sb, R=(), W=()):
        X = self.E[q]
        waits = self._waits(X, R, W)
        first = True
        for o, i in pairs:
            sb.cnt += 16
            X.prog.append((waits if first else [], ("dma", o, i, sb.sem), False))
            first = False
        for b in R:
            b.r[sb.sem] = sb.cnt
        for b in W:
            b.w = {sb.sem: sb.cnt}
            b.r = {}

    def barrier(self):
        toks = {}
        for n in ("pe", "act", "dve", "pool"):
            toks[self.E[n].sem] = self.E[n].cnt
        for b in self.bufs:
            if b.sem is not None and b.cnt > 0:
                toks[b.sem] = b.cnt
        for n, X in self.E.items():
            waits = []
            for k, v in toks.items():
                if v > 0 and X.seen.get(k, 0) < v and not (k == X.sem and n == "pe"):
                    X.seen[k] = v
                    waits.append((k, v))
            X.prog.append((waits, None, False))

    def replay(self, block):
        sems = self.sems

        def run(X, e):
            for waits, fn, inc in X.prog:
                for k, v in waits:
                    e.wait_ge(sems[k], v)
                if fn is None:
                    continue
                if fn[0] == "dma":
                    _, o, i, k = fn
                    e.dma_start(out=o, in_=i).then_inc(sems[k], 16)
                else:
                    _, name, args, kw = fn
                    ins = getattr(e, name)(*args, **kw)
                    if inc:
                        ins.then_inc(sems[X.sem], 1)

        @block.sync
        def _(e):
            run(self.E["sp"], e)

        @block.tensor
        def _(e):
            run(self.E["pe"], e)

        @block.scalar
        def _(e):
            run(self.E["act"], e)

        @block.vector
        def _(e):
            run(self.E["dve"], e)

        @block.gpsimd
        def _(e):
            run(self.E["pool"], e)


class Arena:
    def __init__(self, ap, lo, hi):
        self.ap = ap
        self.lo = lo
        self.hi = hi
        self.p = lo

    def f32(self, n):
        a = self.ap[:, self.p:self.p + n]
        self.p += n
        assert self.p <= self.hi, ("arena overflow", self.p, self.hi)
        return a

    def bf(self, n):
        n2 = (n + 1) // 2
        a = self.ap[:, self.p:self.p + n2].bitcast(BF)
        self.p += n2
        assert self.p <= self.hi, ("arena overflow", self.p, self.hi)
        return a[:, 0:n]

    def reset(self):
        self.p = self.lo


def build(S=8192, L=2, dbg=False):
    NT = S // 512
    NB = S // 128
    NMB = S // 256
    CW = min(2048, S)
    assert NMB <= 32
    nc = bass.Bass("TRN2", target_bir_lowering=False)

    def din(name, shape, dt=F32):
        return nc.dram_tensor(name, shape, dt, kind="ExternalInput").ap()

    def dscr(name, shape, dt=BF):
        return nc.dram_tensor(name, shape, dt, kind=("ExternalOutput" if dbg else "Internal")).ap()

    xT = din("xT", [D, S])
    pT = din("pT", [L * PLE, S])
    wspec = [("wA", 28, 1024), ("wV", 1, 8192), ("wF", 1, 32), ("wG", 24, 1024), ("wBR", 8, 768),
             ("wO", 8, 1024), ("wFG", 22, 1024), ("wFU", 22, 1024), ("wFD", 8, 2816),
             ("wPL", 8, 256), ("wPG", 8, 1024)]
    w32 = {}
    w16 = {}
    for n, ns, nc_ in wspec:
        w32[n] = din(n, [L * ns * 128, nc_])
        w16[n] = nc.dram_tensor(n + "_16", [L * ns * 128, nc_], BF, kind="Internal").ap()
    wns = {n: ns for n, ns, _ in wspec}
    gains = din("gains", [128, L * 5 * 8])
    bfb = din("bfb", [128, L * 16])
    cosT = din("cosT", [128, S])
    sinT = din("sinT", [128, S])
    cmask32 = din("cmask", [128, 256])
    trif = din("trif", [128, 128])
    identf = din("identf", [128, 128])
    blkind32 = din("blkind", [33, S])
    outT = nc.dram_tensor("outT", [D, S], F32, kind="ExternalOutput").ap()

    X32 = dscr("X32", [D, S], F32)
    QT16 = dscr("QT16", [1024, S])
    KT16 = dscr("KT16", [1024, S])
    V16 = dscr("V16", [S, 1024])
    YT16 = dscr("YT16", [768, S])
    KS32 = dscr("KS32", [384, 32], F32)
    BI16 = nc.dram_tensor("BI16", [33, S], BF, kind="Internal").ap()

    es = ExitStack()
    P = Prog(nc, es)
    AW = 52992
    arena_t = es.enter_context(nc.sbuf_tensor("arena", [128, AW], F32))
    PERS = 2048
    pers = Arena(arena_t, 0, PERS)
    ar = Arena(arena_t, PERS, AW)
    psb = [es.enter_context(nc.psum_tensor("psb%d" % i, [128, 512], F32)) for i in range(8)]
    PSB = [P.buf("psb%d" % i) for i in range(8)]

    def ACT(out, in_, func, R, W, **kw):
        P.op("act", "activation", R, W, out=out, in_=in_, func=func, **kw)

    def TT(eng, out, in0, in1, op, R, W):
        P.op(eng, "tensor_tensor", R, W, out=out, in0=in0, in1=in1, op=op)

    def MM(out, lhsT, rhs, start, stop, R, W, inc=True):
        P.op("pe", "matmul", R, W, inc, args=(out,), lhsT=lhsT, rhs=rhs, start=start, stop=stop)

    def mm_group(out_ap, pairs, Rb, Wb):
        n = len(pairs)
        for i, (lt, rh) in enumerate(pairs):
            MM(out_ap, lt, rh, i == 0, i == n - 1, Rb, Wb, inc=(i == n - 1))

    gains_t = pers.f32(L * 40); gains_b = P.buf("gains", True)
    bfb_t = pers.f32(L * 16); bfb_b = P.buf("bfb", True)
    trif_t = pers.f32(128); trif_b = P.buf("trif", True)
    identf_t = pers.f32(128); identf_b = P.buf("identf", True)
    onesf_t = pers.f32(128); onesf_b = P.buf("onesf")
    ones16_t = pers.bf(128); ones16_b = P.buf("ones16")
    cmask_t = pers.bf(256); cmask_b = P.buf("cmask", True)
    eps_t = pers.f32(1); one_t = pers.f32(1); cst_b = P.buf("cst")
    cpos_t = pers.f32(NB * 4).rearrange("p (j h) -> p j h", h=4); cpos_b = P.buf("cpos")
    tall_t = pers.f32((NB + 1) * 4).rearrange("p (j h) -> p j h", h=4); tall_b = P.buf("tall")
    ksum_t = pers.f32(3 * 32).rearrange("p (a n) -> p a n", n=32); ksum_b = P.buf("ksum", True)

    P.dma("sp", [(gains_t, gains)], gains_b, W=[gains_b])
    P.dma("sp", [(bfb_t, bfb)], bfb_b, W=[bfb_b])
    P.dma("sp", [(trif_t, trif)], trif_b, W=[trif_b])
    P.dma("sp", [(identf_t, identf)], identf_b, W=[identf_b])
    P.dma("pool", [(cmask_t, cmask32)], cmask_b, W=[cmask_b])
    P.op("dve", "memset", (), [onesf_b], args=(onesf_t, 1.0))
    P.op("dve", "memset", (), [ones16_b], args=(ones16_t, 1.0))
    P.op("dve", "memset", (), [cst_b], args=(eps_t, 1e-6))
    P.op("dve", "memset", (), [cst_b], args=(one_t, 1.0))
    P.op("dve", "memset", (), [tall_b], args=(tall_t[:, 0, :], 0.0))
    P.op("dve", "memset", (), [ksum_b], args=(ksum_t, 0.0))

    WB = [P.buf("w16_%d" % l) for l in range(L)]
    CB = [P.buf("cast%d" % i, True) for i in range(2)]
    BIb = P.buf("bi16", True)
    P.dma("pool", [(BI16[:, c0:c0 + CW], blkind32[:, c0:c0 + CW]) for c0 in range(0, S, CW)], BIb, W=[BIb])
    cg = 0
    for l in range(L):
        pairs = []
        for n, ns, ncol in wspec:
            for s_ in range(ns):
                r0 = (l * ns + s_) * 128
                cw = 2048 if ncol > 2816 else ncol
                for c0 in range(0, ncol, cw):
                    pairs.append((w16[n][r0:r0 + 128, c0:c0 + cw], w32[n][r0:r0 + 128, c0:c0 + cw]))
        for g0_ in range(0, len(pairs), 8):
            cb = CB[cg % 2]; cg += 1
            P.dma("pool", pairs[g0_:g0_ + 8], cb, W=[cb])
        WB[l].w = {CB[0].sem: CB[0].cnt, CB[1].sem: CB[1].cnt}

    def wslab(n, l, s_):
        r0 = (l * wns[n] + s_) * 128
        return w16[n][r0:r0 + 128, :]

    Xb = P.buf("X32d"); QTb = P.buf("QTd"); KTb = P.buf("KTd"); Vb = P.buf("Vd"); YTb = P.buf("YTd"); KSb = P.buf("KSd")

    def xsrc(l):
        return xT if l == 0 else X32

    def xdst(l):
        return outT if l == L - 1 else X32

    def xtile_ap(dram, t):
        return dram[:, t * 512:(t + 1) * 512].rearrange("(kc p) n -> p kc n", p=128)

    class Ring:
        def __init__(self, n, words, name):
            self.t = [ar.bf(words) for _ in range(n)]
            self.b = [P.buf("%s%d" % (name, i), True) for i in range(n)]
            self.i = 0

        def load(self, dram_ap, ncol, Rb):
            k = self.i % len(self.t)
            self.i += 1
            P.dma("sp", [(self.t[k][:, 0:ncol], dram_ap)], self.b[k], R=Rb, W=[self.b[k]])
            return self.t[k][:, 0:ncol].rearrange("p (k n) -> p k n", n=128), self.b[k]

    def rms_stats(sq_t, sq_b, rstd_t, rstd_b, tmp_t, tmp_b):
        mm_group(psb[7][:, :], [(ones16_t, sq_t[:, kc, :]) for kc in range(8)], [ones16_b, sq_b], [PSB[7]])
        ACT(tmp_t, psb[7][:, :], AF.Sqrt, [PSB[7], cst_b], [tmp_b], bias=eps_t, scale=1.0 / D)
        P.op("dve", "reciprocal", [tmp_b], [rstd_b], out=rstd_t, in_=tmp_t)

    def pre_norm(x_t, x_b, gcol, sq_t, sq_b, rstd_t, rstd_b, tmp_t, tmp_b, h_t, h_b):
        ACT(sq_t, x_t, AF.Square, [x_b], [sq_b])
        rms_stats(sq_t, sq_b, rstd_t, rstd_b, tmp_t, tmp_b)
        for kc in range(8):
            P.op("dve", "scalar_tensor_tensor", [x_b, rstd_b, gains_b], [h_b], out=h_t[:, kc, :], in0=x_t[:, kc, :],
                 scalar=gains_t[:, gcol + kc:gcol + kc + 1], in1=rstd_t, op0=ALU.mult, op1=ALU.mult)

    for l in range(L):
        g0 = l * 40
        P.barrier()
        ar.reset()
        xt = [ar.f32(4096).rearrange("p (k n) -> p k n", n=512) for _ in range(2)]
        xt_b = [P.buf("xt%d" % i, True) for i in range(2)]
        sq_t = ar.bf(4096).rearrange("p (k n) -> p k n", n=512); sq_b = P.buf("sq")
        hA = [ar.bf(4096).rearrange("p (k n) -> p k n", n=512) for _ in range(2)]
        hA_b = [P.buf("hA%d" % i) for i in range(2)]
        rstd_t = ar.f32(512); rstd_b = P.buf("rstd")
        tmp_t = ar.f32(512); tmp_b = P.buf("tmp")
        wv_t = ar.bf(8192).rearrange("p (k n) -> p k n", n=1024); wv_b = P.buf("wvA", True)
        wf_t = ar.bf(32).rearrange("p (k n) -> p k n", n=4); wf_b = P.buf("wfA", True)
        ring = Ring(6, 1024, "ring")
        cs_t = [(ar.f32(512), ar.f32(512)) for _ in range(2)]
        cs_b = [P.buf("csA%d" % i, True) for i in range(2)]
        qst = [ar.bf(4096).rearrange("p (k n) -> p k n", n=512) for _ in range(2)]
        qst_b = [P.buf("qst%d" % i, True) for i in range(2)]
        kst = [ar.bf(4096).rearrange("p (k n) -> p k n", n=512) for _ in range(2)]
        kst_b = [P.buf("kst%d" % i, True) for i in range(2)]
        vst = [ar.bf(4096).rearrange("p (a n) -> p a n", n=1024) for _ in range(2)]
        vst_b = [P.buf("vst%d" % i, True) for i in range(2)]
        r1 = [ar.f32(512) for _ in range(2)]; r1_b = [P.buf("r1_%d" % i) for i in range(2)]
        r2 = [ar.f32(512) for _ in range(2)]; r2_b = [P.buf("r2_%d" % i) for i in range(2)]
        fb_t = ar.f32(16); fb_b = P.buf("fbA")
        fe_t = ar.f32(16); fe_b = P.buf("feA")
        fl_t = ar.f32(16); fl_b = P.buf("flA")

        P.dma("sp", [(wv_t[:, kc, :], wslab("wV", l, 0)[:, kc * 1024:(kc + 1) * 1024]) for kc in range(8)],
              wv_b, R=[WB[l]], W=[wv_b])
        P.dma("sp", [(wf_t, wslab("wF", l, 0).rearrange("p (k n) -> p k n", n=4))], wf_b, R=[WB[l]], W=[wf_b])

        def loadxA(t):
            k = t % 2
            P.dma("sp", [(xt[k][:, 0:4, :], xtile_ap(xsrc(l), t)[:, 0:4, :]),
                         (xt[k][:, 4:8, :], xtile_ap(xsrc(l), t)[:, 4:8, :])], xt_b[k], R=[Xb], W=[xt_b[k]])
            P.dma("sp", [(cs_t[k][0], cosT[:, t * 512:(t + 1) * 512]),
                         (cs_t[k][1], sinT[:, t * 512:(t + 1) * 512])], cs_b[k], W=[cs_b[k]])

        loadxA(0)
        psrot = 0
        pre_norm(xt[0], xt_b[0], g0 + 0, sq_t, sq_b, rstd_t, rstd_b, tmp_t, tmp_b, hA[0], hA_b[0])
        for t in range(NT):
            if t + 1 < NT:
                loadxA(t + 1)
            h_t = hA[t % 2]; h_b = hA_b[t % 2]
            cos_t, sin_t = cs_t[t % 2]; c_b = cs_b[t % 2]
            for tb in range(4):
                mm_group(psb[6][:, tb * 4:tb * 4 + 4], [(h_t[:, kc, tb * 128:(tb + 1) * 128], wf_t[:, kc, :]) for kc in range(8)], [wf_b, h_b], [PSB[6]])
            TT("dve", fb_t, psb[6][:, 0:16], bfb_t[:, l * 16:l * 16 + 16], ALU.add, [PSB[6], bfb_b], [fb_b])
            ACT(fe_t, fb_t, AF.Exp, [fb_b], [fe_b], scale=-1.0)
            ACT(fl_t, fe_t, AF.Ln, [fe_b, cst_b], [fl_b], bias=one_t, scale=1.0)
            qs = qst[t % 2]; qs_b = qst_b[t % 2]; ks = kst[t % 2]; ks_b = kst_b[t % 2]
            si = 0
            for which in range(2):
                stg, stg_b = (qs, qs_b) if which == 0 else (ks, ks_b)
                for pt in range(8):
                    w3, w_b = ring.load(wslab("wA", l, si), 1024, [WB[l]]); si += 1
                    pa = psrot % 6; psrot += 1
                    mm_group(psb[pa][:, :], [(w3[:, kc, :], h_t[:, kc, :]) for kc in range(8)], [w_b, h_b], [PSB[pa]])
                    if pt < 2:
                        ACT(stg[:, pt, :], psb[pa][:, :], AF.Copy, [PSB[pa]], [stg_b])
                    else:
                        w23, w2_b = ring.load(wslab("wA", l, si), 1024, [WB[l]]); si += 1
                        pb = psrot % 6; psrot += 1
                        mm_group(psb[pb][:, :], [(w23[:, kc, :], h_t[:, kc, :]) for kc in range(8)], [w2_b, h_b], [PSB[pb]])
                        ri = (pt + which) % 2
                        TT("dve", r1[ri], psb[pa][:, :], cos_t, ALU.mult, [PSB[pa], c_b], [r1_b[ri]])
                        TT("dve", r2[ri], psb[pb][:, :], sin_t, ALU.mult, [PSB[pb], c_b], [r2_b[ri]])
                        TT("pool", stg[:, pt, :], r1[ri], r2[ri], ALU.add, [r1_b[ri], r2_b[ri]], [stg_b])
                        if which == 1 and pt >= 5:
                            P.op("dve", "tensor_reduce", [stg_b], [ksum_b], out=ksum_t[:, pt - 5, 2 * t:2 * t + 2],
                                 in_=stg[:, pt, :].rearrange("p (a b) -> p a b", b=256), axis=AX.X, op=ALU.add)
                if which == 0 and t + 1 < NT:
                    pre_norm(xt[(t + 1) % 2], xt_b[(t + 1) % 2], g0 + 0, sq_t, sq_b, rstd_t, rstd_b, tmp_t, tmp_b,
                             hA[(t + 1) % 2], hA_b[(t + 1) % 2])
            P.dma("pool", [(QT16[:, t * 512:(t + 1) * 512].rearrange("(k p) n -> p k n", p=128), qs)], qs_b, R=[qs_b], W=[QTb])
            P.dma("pool", [(KT16[:, t * 512:(t + 1) * 512].rearrange("(k p) n -> p k n", p=128), ks)], ks_b, R=[ks_b], W=[KTb])
            for tb in range(4):
                mm_group(psb[6][:, 32 + tb * 4:36 + tb * 4], [(trif_t, fl_t[:, tb * 4:tb * 4 + 4])], [trif_b, fl_b], [PSB[6]])
                mm_group(psb[6][:, 64 + tb * 4:68 + tb * 4], [(onesf_t, fl_t[:, tb * 4:tb * 4 + 4])], [onesf_b, fl_b], [PSB[6]])
            for tb in range(4):
                j = 4 * t + tb
                TT("dve", cpos_t[:, j, :], psb[6][:, 32 + tb * 4:36 + tb * 4], tall_t[:, j, :], ALU.add, [PSB[6], tall_b], [cpos_b])
                TT("dve", tall_t[:, j + 1, :], psb[6][:, 64 + tb * 4:68 + tb * 4], tall_t[:, j, :], ALU.add, [PSB[6], tall_b], [tall_b])
            vs = vst[t % 2]; vs_b = vst_b[t % 2]
            for tb in range(4):
                for hf in range(2):
                    pa = psrot % 6; psrot += 1
                    mm_group(psb[pa][:, :], [(h_t[:, kc, tb * 128:(tb + 1) * 128], wv_t[:, kc, hf * 512:(hf + 1) * 512]) for kc in range(8)],
                             [wv_b, h_b], [PSB[pa]])
                    ACT(vs[:, tb, hf * 512:(hf + 1) * 512], psb[pa][:, :], AF.Copy, [PSB[pa]], [vs_b])
            P.dma("pool", [(V16[t * 512:(t + 1) * 512, :].rearrange("(a p) c -> p a c", p=128), vs)], vs_b, R=[vs_b], W=[Vb])
        P.dma("pool", [(KS32.rearrange("(a p) n -> p a n", p=128), ksum_t)], ksum_b, R=[ksum_b], W=[KSb])

        P.barrier()
        ar.reset()
        KP = [ar.bf(S) for _ in range(2)]; KP_b = [P.buf("KP%d" % i, True) for i in range(2)]
        QP = [ar.bf(S) for _ in range(2)]; QP_b = [P.buf("QP%d" % i, True) for i in range(2)]
        QA_b = [[P.buf("QA%d_%d" % (i, t)) for t in range(NT)] for i in range(2)]
        VA = [ar.bf(NB * 128).rearrange("p (j c) -> p j c", c=128) for _ in range(2)]
        VA_b = [P.buf("VA%d" % i, True) for i in range(2)]
        pt_t = [ar.bf(512) for _ in range(6)]; pt_b = [P.buf("pT%d" % i) for i in range(6)]
        rec_t = ar.f32(512); rec_b = P.buf("rec")
        rec0_t = ar.f32(512); rec0_b = P.buf("rec0")
        yst = [ar.bf(512) for _ in range(2)]; yst_b = [P.buf("yst%d" % i, True) for i in range(2)]
        ks16_t = ar.bf(32); ks16_b = P.buf("ks16")
        ks32_t = ar.f32(32); ks32_b = P.buf("ks32", True)
        wk_t = ar.f32(128).rearrange("p (a n) -> p a n", n=32); wk_b = P.buf("wk")
        t8_t = ar.f32(32).rearrange("p (a n) -> p a n", n=8); t8_b = P.buf("t8")
        sb_t = ar.f32(128).rearrange("p (a n) -> p a n", n=32); sb_b = P.buf("selb")
        acc_t = ar.f32(S); acc_b = P.buf("acc")
        for i in range(2):
            P.op("pool", "memset", (), [VA_b[i]], args=(VA[i][:, :, 64:128], 1.0))
        cnt = {"ps": 0, "po": 0, "pt": 0, "y": 0, "kq": 0, "va": 0}

        def load_rows(dst, dst_b, dram, r0, nr, rb, ind=False):
            pairs = [(dst[0:nr, c0:c0 + CW], dram[r0:r0 + nr, c0:c0 + CW]) for c0 in range(0, S, CW)]
            Rb = [rb]
            if ind == 1:
                pairs += [(dst[64:96, c0:c0 + CW], BI16[0:32, c0:c0 + CW]) for c0 in range(0, S, CW)]
                Rb = [rb, BIb]
            if ind == 2:
                pairs += [(dst[64:65, c0:c0 + CW], BI16[32:33, c0:c0 + CW]) for c0 in range(0, S, CW)]
                Rb = [rb, BIb]
            P.dma("sp", pairs, dst_b, R=Rb, W=[dst_b])

        def load_va(k, head, d):
            nbd = NB // d
            pairs = []
            for r in range(d):
                for b0 in range(0, nbd, 8):
                    nb_ = min(8, nbd - b0)
                    src = V16[r + d * 128 * b0: r + d * 128 * b0 + d * (128 * nb_ - 1) + 1: d, head * 64:(head + 1) * 64]
                    pairs.append((VA[k][:, r * nbd + b0: r * nbd + b0 + nb_, 0:64], src.rearrange("(j p) c -> p j c", p=128)))
            for g_ in range(0, len(pairs), 4):
                P.dma("sp", pairs[g_:g_ + 4], VA_b[k], R=[Vb], W=[VA_b[k]])

        def finalize(po, yrow, i):
            yk = cnt["y"] % 2; cnt["y"] += 1
            P.op("dve", "reciprocal", [PSB[po]], [rec_b], out=rec_t[64:128, :], in_=psb[po][64:128, :])
            TT("dve", yst[yk][0:64, :], psb[po][0:64, :], rec_t[64:128, :], ALU.mult, [PSB[po], rec_b], [yst_b[yk]])
            P.dma("pool", [(YT16[yrow:yrow + 64, i * 512:(i + 1) * 512], yst[yk][0:64, :])], yst_b[yk], R=[yst_b[yk]], W=[YTb])

        LA = 3

        def run_pipe(items):
            n = len(items)
            for idx in range(n + LA):
                if idx < n:
                    items[idx][0]()
                if idx >= LA:
                    items[idx - LA][1]()
                    items[idx - LA][2]()

        def attn_items(Kt, K_b, Qt, Qbufs, r0, nr, va, va_b, bias_fn, i, yrow, hooks):
            items = []
            po = 4 + cnt["po"] % 2; cnt["po"] += 1
            nj = 4 * i + 4
            for j in range(nj):
                off = max(0, j - 4 * i) * 128
                ps = cnt["ps"] % 4; cnt["ps"] += 1
                pk = cnt["pt"] % 6; cnt["pt"] += 1

                def f_score(j=j, off=off, ps=ps):
                    if j in hooks:
                        hooks[j]()
                    mm_group(psb[ps][:, off:512], [(Kt[r0:r0 + nr, j * 128:(j + 1) * 128], Qt[r0:r0 + nr, i * 512 + off:(i + 1) * 512])],
                             [K_b] + Qbufs, [PSB[ps]])

                def f_soft(j=j, off=off, ps=ps, pk=pk):
                    if bias_fn is None:
                        ACT(pt_t[pk][:, off:512], psb[ps][:, off:512], AF.Exp, [PSB[ps]], [pt_b[pk]], scale=0.125)
                    else:
                        ACT(pt_t[pk][:, off:512], psb[ps][:, off:512], AF.Exp, [PSB[ps], cpos_b], [pt_b[pk]], bias=bias_fn(j, i), scale=0.125)
                    if j >= 4 * i:
                        TT("pool", pt_t[pk][:, off:off + 128], pt_t[pk][:, off:off + 128], cmask_t[:, 128:256], ALU.mult,
                           [pt_b[pk], cmask_b], [pt_b[pk]])

                def f_pv(j=j, off=off, pk=pk):
                    MM(psb[po][:, off:512], va[:, j, :], pt_t[pk][:, off:512], j == 0, j == nj - 1, [va_b, pt_b[pk]], [PSB[po]], inc=True)
                    if j == nj - 1:
                        finalize(po, yrow, i)
                items.append((f_score, f_soft, f_pv))
            return items

        for h in range(4):
            kq = cnt["kq"] % 2; cnt["kq"] += 1
            load_rows(KP[kq], KP_b[kq], KT16, h * 64, 64, KTb, ind=2)
            load_rows(QP[kq], QP_b[kq], QT16, h * 64, 64, QTb)
            vk = cnt["va"] % 2; cnt["va"] += 1
            load_va(vk, h, 1)
            items = []

            def mkpre(i, h=h, kq=kq):
                def pre():
                    for qb in range(4):
                        P.op("pe", "transpose", [cpos_b, identf_b], [PSB[7]], qb == 3, out=psb[7][0:1, qb * 128:(qb + 1) * 128],
                             in_=cpos_t[:, 4 * i + qb, h:h + 1], identity=identf_t)
                    P.op("dve", "tensor_scalar", [PSB[7]], [QA_b[kq][i]], out=QP[kq][64:65, i * 512:(i + 1) * 512], in0=psb[7][0:1, :],
                         scalar1=-8.0, scalar2=None, op0=ALU.mult)
                return pre
            mkpre(0)()
            for i in range(NT):
                hooks = {0: mkpre(i + 1)} if i + 1 < NT else {}
                items += attn_items(KP[kq], KP_b[kq], QP[kq], [QP_b[kq], QA_b[kq][i]], 0, 65, VA[vk], VA_b[vk],
                                    (lambda h: lambda j, i: cpos_t[:, j, h:h + 1])(h), i, h * 64, hooks)
            run_pipe(items)
        for m in range(6):
            hd = 10 + m
            kq = cnt["kq"] % 2; cnt["kq"] += 1
            load_rows(KP[kq], KP_b[kq], KT16, hd * 64, 64, KTb, ind=1)
            load_rows(QP[kq], QP_b[kq], QT16, hd * 64, 64, QTb)
            P.dma("sp", [(ks32_t[0:64, :], KS32[m * 64:(m + 1) * 64, :])], ks32_b, R=[KSb], W=[ks32_b])
            P.op("dve", "tensor_copy", [ks32_b], [ks16_b], out=ks16_t[0:64, :], in_=ks32_t[0:64, :])
            vk = cnt["va"] % 2; cnt["va"] += 1
            load_va(vk, hd, 1)
            Kt = KP[kq]; Qt = QP[kq]
            items = []

            def mkpre1(i, kq=kq, Qt=Qt):
                def pre():
                    for qb in range(4):
                        q0 = i * 512 + qb * 128
                        mm_group(psb[6][:, qb * 32:qb * 32 + NMB], [(Qt[0:64, q0:q0 + 128], ks16_t[0:64, 0:NMB])], [QP_b[kq], ks16_b], [PSB[6]])
                    P.op("dve", "memset", (), [wk_b], args=(wk_t, -1e30))
                    P.op("dve", "memset", (), [sb_b], args=(sb_t, -1.0))
                    for hq in range(2):
                        own = 2 * i + hq
                        if own > 3:
                            P.op("dve", "tensor_copy", [PSB[6]], [wk_b], out=wk_t[:, 2 * hq:2 * hq + 2, 0:own],
                                 in_=psb[6][:, 64 * hq:64 * hq + 64].rearrange("p (a n) -> p a n", n=32)[:, :, 0:own])
                        for qq in range(2):
                            qb = 2 * hq + qq
                            if own > 3:
                                P.op("dve", "max", [wk_b], [t8_b], out=t8_t[:, qb, :], in_=wk_t[:, qb, 0:max(own, 8)])
                                P.op("dve", "tensor_scalar", [wk_b, t8_b], [sb_b], out=sb_t[:, qb, 0:own], in0=wk_t[:, qb, 0:own],
                                     scalar1=t8_t[:, qb, 2:3], scalar2=1.0, op0=ALU.is_ge, op1=ALU.subtract)
                                P.op("dve", "memset", (), [sb_b], args=(sb_t[:, qb, own:own + 1], 0.0))
                            else:
                                P.op("dve", "memset", (), [sb_b], args=(sb_t[:, qb, 0:own + 1], 0.0))
                    P.op("dve", "tensor_scalar", [sb_b], [sb_b], out=sb_t, in0=sb_t, scalar1=BIG, scalar2=None, op0=ALU.mult)
                return pre

            def mkpre2(i, kq=kq, Qt=Qt):
                def pre():
                    for qb in range(4):
                        P.op("pe", "transpose", [sb_b, identf_b], [PSB[7]], qb == 3, out=psb[7][0:32, qb * 128:(qb + 1) * 128],
                             in_=sb_t[:, qb, :], identity=identf_t)
                    P.op("dve", "tensor_copy", [PSB[7]], [QA_b[kq][i]], out=Qt[64:96, i * 512:(i + 1) * 512], in_=psb[7][0:32, :])
                return pre
            mkpre1(0)(); mkpre2(0)()
            for i in range(NT):
                hooks = {}
                if i + 1 < NT:
                    hooks[0] = mkpre1(i + 1)
                    hooks[4 * i + 3] = mkpre2(i + 1)
                items += attn_items(Kt, KP_b[kq], Qt, [QP_b[kq], QA_b[kq][i]], 0, 96, VA[vk], VA_b[vk], None, i, 384 + m * 64, hooks)
            run_pipe(items)
        for oh in range(2):
            for g in range(3):
                d = DIL[g]
                ptile = 2 + g
                hd = 4 + 2 * g + oh
                kq = cnt["kq"] % 2; cnt["kq"] += 1
                load_rows(KP[kq], KP_b[kq], KT16, ptile * 128, 128, KTb)
                load_rows(QP[kq], QP_b[kq], QT16, ptile * 128, 128, QTb)
                vk = cnt["va"] % 2; cnt["va"] += 1
                load_va(vk, hd, d)
                Kt = KP[kq]; Qt = QP[kq]; r0 = oh * 64
                KQ = [KP_b[kq], QP_b[kq]]
                nbd = NB // d
                items = []
                for r in range(d):
                    for ub4 in range(0, nbd, 4):
                        po = 4 + cnt["po"] % 2; cnt["po"] += 1
                        nu = min(4, nbd - ub4)
                        for u in range(nu):
                            ub = ub4 + u
                            ps = cnt["ps"] % 4; cnt["ps"] += 1
                            pk = cnt["pt"] % 6; cnt["pt"] += 1
                            qa = r + d * 128 * ub
                            lo = 0 if ub > 0 else 128
                            bi = r * nbd + ub

                            def f_score(ub=ub, ps=ps, qa=qa, d=d, r0=r0, Kt=Kt, Qt=Qt, KQ=KQ):
                                qap = Qt[r0:r0 + 64, qa: qa + d * 127 + 1: d]
                                if ub > 0:
                                    ka = qa - d * 128
                                    mm_group(psb[ps][:, 0:128], [(Kt[r0:r0 + 64, ka: ka + d * 127 + 1: d], qap)], KQ, [PSB[ps]])
                                mm_group(psb[ps][:, 128:256], [(Kt[r0:r0 + 64, qa: qa + d * 127 + 1: d], qap)], KQ, [PSB[ps]])

                            def f_soft(ps=ps, pk=pk, lo=lo):
                                ACT(pt_t[pk][:, lo:256], psb[ps][:, lo:256], AF.Exp, [PSB[ps]], [pt_b[pk]], scale=0.125)
                                TT("pool", pt_t[pk][:, lo:256], pt_t[pk][:, lo:256], cmask_t[:, lo:256], ALU.mult, [pt_b[pk], cmask_b], [pt_b[pk]])

                            def f_pv(ub=ub, u=u, nu=nu, po=po, pk=pk, bi=bi, vk=vk, g=g, r=r, d=d, ub4=ub4):
                                if ub > 0:
                                    MM(psb[po][:, u * 128:(u + 1) * 128], VA[vk][:, bi - 1, :], pt_t[pk][:, 0:128], True, False,
                                       [VA_b[vk], pt_b[pk]], [PSB[po]], inc=False)
                                MM(psb[po][:, u * 128:(u + 1) * 128], VA[vk][:, bi, :], pt_t[pk][:, 128:256], ub == 0, True,
                                   [VA_b[vk], pt_b[pk]], [PSB[po]], inc=True)
                                if u == nu - 1:
                                    a0 = r + d * 128 * ub4
                                    accv = acc_t[:, a0: a0 + d * (128 * nu - 1) + 1: d]
                                    if g == 0:
                                        P.op("dve", "tensor_copy", [PSB[po]], [acc_b], out=accv, in_=psb[po][:, 0:128 * nu])
                                    else:
                                        TT("dve", accv, psb[po][:, 0:128 * nu], accv, ALU.add, [PSB[po], acc_b], [acc_b])
                            items.append((f_score, f_soft, f_pv))
                run_pipe(items)
            for i in range(NT):
                yk = cnt["y"] % 2; cnt["y"] += 1
                P.op("dve", "reciprocal", [acc_b], [rec_b], out=rec_t[64:128, :], in_=acc_t[64:128, i * 512:(i + 1) * 512])
                P.op("dve", "tensor_copy", [rec_b], [rec0_b], out=rec0_t[0:64, :], in_=rec_t[64:128, :])
                TT("dve", yst[yk][0:64, :], acc_t[0:64, i * 512:(i + 1) * 512], rec0_t[0:64, :], ALU.mult, [acc_b, rec0_b], [yst_b[yk]])
                P.dma("pool", [(YT16[256 + oh * 64:256 + oh * 64 + 64, i * 512:(i + 1) * 512], yst[yk][0:64, :])], yst_b[yk], R=[yst_b[yk]], W=[YTb])

        P.barrier()
        ar.reset()
        xt = [ar.f32(4096).rearrange("p (k n) -> p k n", n=512) for _ in range(4)]
        xt_b = [P.buf("xt%d" % i, True) for i in range(4)]
        yt = [ar.bf(3072).rearrange("p (k n) -> p k n", n=512) for _ in range(2)]
        yt_b = [P.buf("ytC%d" % i, True) for i in range(2)]
        pp = [ar.f32(1024).rearrange("p (k n) -> p k n", n=512) for _ in range(2)]
        pp_b = [P.buf("ppC%d" % i, True) for i in range(2)]
        p16 = [ar.bf(1024).rearrange("p (k n) -> p k n", n=512) for _ in range(2)]
        p16_b = [P.buf("p16_%d" % i) for i in range(2)]
        hh = [ar.bf(4096).rearrange("p (k n) -> p k n", n=512) for _ in range(2)]
        hh_b = [P.buf("hC%d" % i) for i in range(2)]
        sqo_t = ar.bf(4096).rearrange("p (k n) -> p k n", n=512); sqo_b = P.buf("sqo")
        sqx_t = ar.bf(4096).rearrange("p (k n) -> p k n", n=512); sqx_b = P.buf("sqx")
        mg_t = ar.bf(4096).rearrange("p (k n) -> p k n", n=512); mg_b = P.buf("mgC")
        o_t = ar.f32(4096).rearrange("p (k n) -> p k n", n=512); o_b = P.buf("oC")
        ff_t = ar.bf(NFF * 512).rearrange("p (k n) -> p k n", n=512); ff_b = P.buf("ffC")
        rstd_t = ar.f32(512); rstd_b = P.buf("rstd")
        tmp_t = ar.f32(512); tmp_b = P.buf("tmp")
        gs = [ar.f32(512) for _ in range(3)]; gs_b = [P.buf("gs%d" % i) for i in range(3)]
        ma = [ar.f32(512) for _ in range(3)]; ma_b = [P.buf("ma%d" % i) for i in range(3)]
        tt = [ar.f32(512) for _ in range(2)]; tt_b = [P.buf("tt%d" % i) for i in range(2)]
        ring = Ring(6, 1024, "ringC")
        prc = {"i": 0}

        def nps():
            k = prc["i"] % 7; prc["i"] += 1
            return k

        def loadXY(t):
            k = t % 2; xk = t % 4
            P.dma("sp", [(xt[xk][:, 0:4, :], xtile_ap(xsrc(l), t)[:, 0:4, :]),
                         (xt[xk][:, 4:8, :], xtile_ap(xsrc(l), t)[:, 4:8, :])], xt_b[xk], R=[Xb], W=[xt_b[xk]])
            P.dma("sp", [(yt[k], YT16[:, t * 512:(t + 1) * 512].rearrange("(k p) n -> p k n", p=128))], yt_b[k], R=[YTb], W=[yt_b[k]])

        def loadP(t):
            k = t % 2
            P.dma("sp", [(pp[k], pT[l * PLE:(l + 1) * PLE, t * 512:(t + 1) * 512].rearrange("(k p) n -> p k n", p=128))], pp_b[k], W=[pp_b[k]])

        def stats(sq_t, sq_b):
            rms_stats(sq_t, sq_b, rstd_t, rstd_b, tmp_t, tmp_b)

        XK = {0: 0, 1: 1}

        def mk_h(k, gcol):
            for kc in range(8):
                P.op("dve", "scalar_tensor_tensor", [xt_b[XK[k]], rstd_b, gains_b], [hh_b[k]], out=hh[k][:, kc, :], in0=xt[XK[k]][:, kc, :],
                     scalar=gains_t[:, gcol + kc:gcol + kc + 1], in1=rstd_t, op0=ALU.mult, op1=ALU.mult)

        def pre1(k):
            for kc in range(8):
                ACT(sqx_t[:, kc, :], xt[XK[k]][:, kc, :], AF.Square, [xt_b[XK[k]]], [sqx_b])
            stats(sqx_t, sqx_b)
            mk_h(k, g0 + 0)

        def residual_steps(k, gcol, want_sq, want_h=False):
            xk = XK[k]
            steps = [lambda: stats(sqo_t, sqo_b)]

            def mk(m):
                def step():
                    q = m % 2
                    eng = "pool" if m % 2 == 0 else "dve"
                    P.op("dve", "scalar_tensor_tensor", [o_b, rstd_b, gains_b], [tt_b[q]], out=tt[q], in0=o_t[:, m, :],
                         scalar=gains_t[:, gcol + m:gcol + m + 1], in1=rstd_t, op0=ALU.mult, op1=ALU.mult)
                    TT(eng, xt[xk][:, m, :], xt[xk][:, m, :], tt[q], ALU.add, [xt_b[xk], tt_b[q]], [xt_b[xk]])
                    if want_sq:
                        TT(eng, sqx_t[:, m, :], xt[xk][:, m, :], xt[xk][:, m, :], ALU.mult, [xt_b[xk]], [sqx_b])
                    if want_h:
                        P.op(eng, "tensor_copy", [xt_b[xk]], [hh_b[k]], out=hh[k][:, m, :], in_=xt[xk][:, m, :])
                return step
            return steps + [mk(m) for m in range(8)]

        def run_bg(bg, n=1):
            for _ in range(n):
                if bg:
                    bg.pop(0)()

        def flush_bg(bg):
            while bg:
                bg.pop(0)()

        def evac_o(ps, m):
            ACT(sqo_t[:, m, :], psb[ps][:, :], AF.Square, [PSB[ps]], [sqo_b])
            ACT(o_t[:, m, :], psb[ps][:, :], AF.Copy, [PSB[ps]], [o_b])

        def S1(k, inject, bg):
            ychunks = [(0, 2), (2, 1), (3, 3)]
            for m in range(8):
                if m == 3:
                    flush_bg(bg)
                    if inject is not None:
                        inject()
                wb3, wb_b = ring.load(wslab("wBR", l, m), 768, [WB[l]])
                for b in range(3):
                    run_bg(bg)
                    wg3, wg_b = ring.load(wslab("wG", l, b * 8 + m), 1024, [WB[l]])
                    pg = nps()
                    mm_group(psb[pg][:, :], [(wg3[:, kc, :], hh[k][:, kc, :]) for kc in range(8)], [wg_b, hh_b[k]], [PSB[pg]])
                    ACT(gs[b], psb[pg][:, :], AF.Sigmoid, [PSB[pg]], [gs_b[b]])
                    c0, ncc = ychunks[b]
                    pq = nps()
                    mm_group(psb[pq][:, :], [(wb3[:, c0 + c, :], yt[k][:, c0 + c, :]) for c in range(ncc)], [wb_b, yt_b[k]], [PSB[pq]])
                    TT("dve", ma[b], psb[pq][:, :], gs[b], ALU.mult, [PSB[pq], gs_b[b]], [ma_b[b]])
                TT("pool", ma[0], ma[0], ma[1], ALU.add, [ma_b[0], ma_b[1]], [ma_b[0]])
                TT("pool", mg_t[:, m, :], ma[0], ma[2], ALU.add, [ma_b[0], ma_b[2]], [mg_b])
            for m in range(8):
                w3, w_b = ring.load(wslab("wO", l, m), 1024, [WB[l]])
                ps = nps()
                mm_group(psb[ps][:, :], [(w3[:, kc, :], mg_t[:, kc, :]) for kc in range(8)], [w_b, mg_b], [PSB[ps]])
                evac_o(ps, m)

        def S2(k, inject, bg):
            for j in range(NFF):
                run_bg(bg)
                if j == 8:
                    flush_bg(bg)
                    if inject is not None:
                        inject()
                wg3, wg_b = ring.load(wslab("wFG", l, j), 1024, [WB[l]])
                pg = nps()
                mm_group(psb[pg][:, :], [(wg3[:, kc, :], hh[k][:, kc, :]) for kc in range(8)], [wg_b, hh_b[k]], [PSB[pg]])
                kk = j % 3
                ACT(gs[kk], psb[pg][:, :], AF.Silu, [PSB[pg]], [gs_b[kk]])
                wu3, wu_b = ring.load(wslab("wFU", l, j), 1024, [WB[l]])
                pu = nps()
                mm_group(psb[pu][:, :], [(wu3[:, kc, :], hh[k][:, kc, :]) for kc in range(8)], [wu_b, hh_b[k]], [PSB[pu]])
                TT("dve", ff_t[:, j, :], psb[pu][:, :], gs[kk], ALU.mult, [PSB[pu], gs_b[kk]], [ff_b])
            for m in range(8):
                ps = nps()
                pieces = [(0, 8), (8, 16), (16, NFF)]
                first = True
                for (j0, j1) in pieces:
                    w3, w_b = ring.load(wslab("wFD", l, m)[:, j0 * 128:j1 * 128], (j1 - j0) * 128, [WB[l]])
                    for j in range(j0, j1):
                        MM(psb[ps][:, :], w3[:, j - j0, :], ff_t[:, j, :], first, j == NFF - 1, [w_b, ff_b], [PSB[ps]], inc=(j == j1 - 1))
                        first = False
                evac_o(ps, m)

        def d2(k):
            P.op("dve", "tensor_copy", [pp_b[k]], [p16_b[k]], out=p16[k], in_=pp[k])

        def S3(k, inject, bg):
            for m in range(8):
                run_bg(bg, 3)
                if m == 3:
                    flush_bg(bg)
                    if inject is not None:
                        inject()
                wg3, wg_b = ring.load(wslab("wPG", l, m), 1024, [WB[l]])
                pg = nps()
                mm_group(psb[pg][:, :], [(wg3[:, kc, :], hh[k][:, kc, :]) for kc in range(8)], [wg_b, hh_b[k]], [PSB[pg]])
                kk = m % 3
                ACT(gs[kk], psb[pg][:, :], AF.Sigmoid, [PSB[pg]], [gs_b[kk]])
                wp3, wp_b = ring.load(wslab("wPL", l, m), 256, [WB[l]])
                pu = nps()
                mm_group(psb[pu][:, :], [(wp3[:, kc, :], p16[k][:, kc, :]) for kc in range(2)], [wp_b, p16_b[k]], [PSB[pu]])
                TT("dve", o_t[:, m, :], psb[pu][:, :], gs[kk], ALU.mult, [PSB[pu], gs_b[kk]], [o_b])
                ACT(sqo_t[:, m, :], o_t[:, m, :], AF.Square, [o_b], [sqo_b])

        xs_b = [P.buf("xs%d" % i, True) for i in range(4)]

        def storeC(t):
            xk = t % 4
            P.dma("pool", [(xtile_ap(xdst(l), t)[:, 0:4, :], xt[xk][:, 0:4, :]), (xtile_ap(xdst(l), t)[:, 4:8, :], xt[xk][:, 4:8, :])],
                  xs_b[xk], R=[xt_b[xk]], W=[Xb])

        def c2(k):
            stats(sqx_t, sqx_b)
            mk_h(k, g0 + 16)

        loadXY(0); loadP(0); loadXY(1); loadP(1)
        XK[0] = 0
        pre1(0)
        bgq = []
        for tp in range(0, NT, 2):
            tA, tB = tp, tp + 1
            nxt = tp + 2 < NT
            XK[0] = tA % 4; XK[1] = tB % 4

            def inj_pre1B():
                pre1(1)
            S1(0, inj_pre1B, bgq)
            if nxt:
                loadXY(tA + 2)
            bgq = residual_steps(0, g0 + 8, True)
            S1(1, lambda: c2(0), bgq)
            if nxt:
                loadXY(tB + 2)
            bgq = residual_steps(1, g0 + 8, True)
            S2(0, lambda: c2(1), bgq)
            bgq = residual_steps(0, g0 + 24, False, True)
            S2(1, lambda: d2(0), bgq)
            if nxt:
                loadP(tA + 2)
            bgq = residual_steps(1, g0 + 24, False, True)
            S3(0, lambda: d2(1), bgq)
            if nxt:
                loadP(tB + 2)
            bgq = residual_steps(0, g0 + 32, False) + [(lambda t=tA: storeC(t))]

            def inj_next(tA=tA):
                XK[0] = (tA + 2) % 4
                pre1(0)
                XK[0] = tA % 4
            S3(1, inj_next if nxt else None, bgq)
            bgq = residual_steps(1, g0 + 32, False) + [(lambda t=tB: storeC(t))]
        flush_bg(bgq)
    P.barrier()
    block = es.enter_context(nc.Block())
    P.replay(block)
    es.close()
    return nc


def _slabs(w, cols_list, kc):
    out = np.empty((len(cols_list), 128, kc, 128), np.float32)
    wk = w.reshape(kc, 128, w.shape[1])
    for m, cols in enumerate(cols_list):
        out[m] = wk[:, :, cols].transpose(1, 0, 2)
    return out.reshape(len(cols_list) * 128, kc * 128)


def _host_weights(inp, L):
    r = {}
    ar_ = np.arange
    lists = {n: [] for n in ("wA", "wV", "wF", "wG", "wBR", "wO", "wFG", "wFU", "wFD", "wPL", "wPG")}
    for l in range(L):
        w_in = np.asarray(inp["w_in"][l], np.float32)
        colsA = []
        for base in (0, 1024):
            for pt in range(8):
                c = base + pt * 128 + ar_(128)
                colsA.append(c)
                if pt >= 2:
                    sw = base + pt * 128 + (ar_(128) // 64) * 64 + (ar_(128) % 64 + 32) % 64
                    colsA.append(sw)
        lists["wA"].append(_slabs(w_in, colsA, 8))
        wv = w_in[:, 2048:3072].reshape(8, 128, 1024).transpose(1, 0, 2).reshape(128, 8192)
        lists["wV"].append(wv)
        wf = w_in[:, 3072:3076].reshape(8, 128, 4).transpose(1, 0, 2).reshape(128, 32)
        lists["wF"].append(wf)
        lists["wG"].append(_slabs(w_in, [3076 + b * 1024 + m * 128 + ar_(128) for b in range(3) for m in range(8)], 8))
        wbr = np.concatenate([np.asarray(inp["w_br_a"][l]), np.asarray(inp["w_br_b"][l]), np.asarray(inp["w_br_c"][l])], axis=0)
        lists["wBR"].append(_slabs(wbr.astype(np.float32), [m * 128 + ar_(128) for m in range(8)], 6))
        lists["wO"].append(_slabs(np.asarray(inp["w_out"][l], np.float32), [m * 128 + ar_(128) for m in range(8)], 8))
        lists["wFG"].append(_slabs(np.asarray(inp["w_ffn_gate"][l], np.float32), [m * 128 + ar_(128) for m in range(NFF)], 8))
        lists["wFU"].append(_slabs(np.asarray(inp["w_ffn_up"][l], np.float32), [m * 128 + ar_(128) for m in range(NFF)], 8))
        lists["wFD"].append(_slabs(np.asarray(inp["w_ffn_down"][l], np.float32), [m * 128 + ar_(128) for m in range(8)], NFF))
        lists["wPL"].append(_slabs(np.asarray(inp["w_ple"][l], np.float32), [m * 128 + ar_(128) for m in range(8)], 2))
        lists["wPG"].append(_slabs(np.asarray(inp["w_ple_gate"][l], np.float32), [m * 128 + ar_(128) for m in range(8)], 8))
    for n, v in lists.items():
        r[n] = np.ascontiguousarray(np.concatenate(v, axis=0), dtype=np.float32)
    gl = []
    for l in range(L):
        for n in ("g_mix_pre", "g_mix_post", "g_ffn_pre", "g_ffn_post", "g_ple_post"):
            gl.append(np.asarray(inp[n][l], np.float32).reshape(8, 128).T)
    r["gains"] = np.ascontiguousarray(np.concatenate(gl, axis=1), dtype=np.float32)
    r["bfb"] = np.ascontiguousarray(np.broadcast_to(np.tile(np.asarray(inp["b_f"], np.float32).reshape(L, 1, 4), (1, 4, 1)).reshape(1, L * 16), (128, L * 16)))
    return r


def _consts(S):
    c = {}
    inv = (1.0 / (np.float32(10000.0) ** (np.arange(0, 64, 2, dtype=np.float32) / np.float32(64)))).astype(np.float32)
    ang = (np.arange(S, dtype=np.float32)[:, None] * inv[None, :]).astype(np.float32)
    cos = np.cos(ang.astype(np.float64)).astype(np.float32).T
    sin = np.sin(ang.astype(np.float64)).astype(np.float32).T
    c["cosT"] = np.ascontiguousarray(np.concatenate([cos, cos, cos, cos], axis=0))
    c["sinT"] = np.ascontiguousarray(np.concatenate([-sin, sin, -sin, sin], axis=0))
    p = np.arange(128)
    c["cmask"] = np.ascontiguousarray(np.concatenate([(p[:, None] >= p[None, :]), (p[:, None] <= p[None, :])], axis=1).astype(np.float32))
    c["trif"] = np.ascontiguousarray((p[:, None] <= p[None, :]).astype(np.float32))
    c["identf"] = np.eye(128, dtype=np.float32)
    c["blkind"] = np.ascontiguousarray(np.concatenate([(np.arange(S)[None, :] // 256 == np.arange(32)[:, None]), np.ones((1, S), bool)], axis=0).astype(np.float32))
    return c


_NC_CACHE = {}


def kernel(**inputs):
    x = np.asarray(inputs["x"], np.float32)
    p = np.asarray(inputs["p"], np.float32)
    B, S, _ = x.shape
    L = p.shape[0]
    key = (S, L)
    if key not in _NC_CACHE:
        _NC_CACHE[key] = build(S, L)
    nc = _NC_CACHE[key]
    shared = _host_weights(inputs, L)
    shared.update(_consts(S))
    in_maps = []
    for b in range(B):
        m = dict(shared)
        m["xT"] = np.ascontiguousarray(x[b].T)
        m["pT"] = np.ascontiguousarray(p[:, b].transpose(0, 2, 1).reshape(L * PLE, S))
        in_maps.append(m)
    res = run_bass_kernel_spmd(nc, in_maps, core_ids=list(range(B)))
    out = np.stack([np.ascontiguousarray(res.results[b]["outT"].T) for b in range(B)], axis=0)
    return out.astype(np.float32)
```
